# Optimizing a Trainium2 kernel written in Bass

```python
import math
import jax, jax.numpy as jnp
from jax import lax
import numpy as np

D_MODEL = 1024
BATCH = 8
SEQ = 4096
DEPTH = 2

HEAD_DIM = 64
A_GROUPS = ((128, 1), (512, 4), (2048, 16))
A_HEADS = 4
A_WIDTH = len(A_GROUPS) * A_HEADS * HEAD_DIM
A_OUT = A_HEADS * HEAD_DIM
B_HEADS = 16
B_KV_HEADS = 4
B_Q = B_HEADS * HEAD_DIM
B_KV = B_KV_HEADS * HEAD_DIM
Q_BLOCK = 128
GRID_W = 64
ROPE_THETA = 10000.0
N_BRANCH = 2
N_IN = 3 * A_WIDTH + B_Q + 2 * B_KV + N_BRANCH * D_MODEL
REL_BUCKETS = 32
REL_MAX_DIST = 1024
N_EXPERTS = 32
TOP_K = 4
D_EXPERT = D_MODEL
SWIGLU_LIMIT = 7.0
SWIGLU_ALPHA = 1.702
MOE_BLOCK = 512
N_MOD = 6
EPS = 1e-6
NEG_INF = -1e30

kernel_name = "hybrid_dilated_gqa_moe_encoder"


def _rmsnorm(x, g):
    xf = x.astype(jnp.float32)
    y = xf * lax.rsqrt(jnp.mean(xf * xf, axis=-1, keepdims=True) + EPS)
    return (y * g.astype(jnp.float32)).astype(x.dtype)


def _modulate(h, shift, scale):
    return h * (1.0 + scale[:, None, :]) + shift[:, None, :]


def _t5_bucket(rel):
    nb = REL_BUCKETS // 2
    max_exact = nb // 2
    ret = jnp.where(rel > 0, nb, 0)
    n = jnp.abs(rel)
    nf = jnp.maximum(n, 1).astype(jnp.float32)
    large = max_exact + (jnp.log(nf / max_exact) / math.log(REL_MAX_DIST / max_exact)
                         * (nb - max_exact)).astype(jnp.int32)
    large = jnp.minimum(large, nb - 1)
    return ret + jnp.where(n < max_exact, n, large)


def _dilated_window_attention(q, k, v, bias_tab, dilation, n_side):
    bsz, s, h, dh = q.shape
    L = s // dilation
    nb = n_side
    nblk = -(-L // nb)
    lp = nblk * nb

    def fold(t):
        return t.reshape(bsz, L, dilation, h, dh).swapaxes(1, 2).reshape(bsz * dilation, L, h, dh)

    qf, kf, vf = fold(q), fold(k), fold(v)
    qb = jnp.pad(qf, ((0, 0), (0, lp - L), (0, 0), (0, 0))).reshape(-1, nblk, nb, h, dh)

    def band(t):
        tp = jnp.pad(t, ((0, 0), (nb, lp - L + nb), (0, 0), (0, 0))).reshape(-1, nblk + 2, nb, h, dh)
        return jnp.concatenate([tp[:, :-2], tp[:, 1:-1], tp[:, 2:]], axis=2)

    kb, vb = band(kf), band(vf)
    rel = jnp.arange(3 * nb)[None, :] - nb - jnp.arange(nb)[:, None]
    key_idx = jnp.arange(nblk)[:, None] * nb + jnp.arange(3 * nb)[None, :] - nb
    mask = (jnp.abs(rel) <= n_side)[None] & ((key_idx >= 0) & (key_idx < L))[:, None, :]
    bias = jnp.transpose(bias_tab[_t5_bucket(rel * dilation)], (2, 0, 1)).astype(jnp.float32)

    scores = jnp.einsum('bnqhd,bnkhd->bnhqk', qb, kb).astype(jnp.float32) * (dh ** -0.5) + bias
    scores = jnp.where(mask[None, :, None], scores, NEG_INF)
    m = jnp.max(scores, axis=-1, keepdims=True)
    p = jnp.exp(scores - m)
    den = jnp.sum(p, axis=-1)
    o = jnp.einsum('bnhqk,bnkhd->bnqhd', p.astype(v.dtype), vb)
    o = o / jnp.transpose(den, (0, 1, 3, 2))[..., None].astype(o.dtype)
    lse = jnp.transpose(m[..., 0] + jnp.log(den), (0, 1, 3, 2))
    o = o.reshape(-1, lp, h, dh)[:, :L]
    lse = lse.reshape(-1, lp, h)[:, :L]
    o = o.reshape(bsz, dilation, L, h, dh).swapaxes(1, 2).reshape(bsz, s, h, dh)
    lse = lse.reshape(bsz, dilation, L, h).swapaxes(1, 2).reshape(bsz, s, h)
    return o, lse


def _mixer_dilated(za, rel_bias):
    bsz, s, _ = za.shape
    za = za.reshape(bsz, s, len(A_GROUPS), 3, A_HEADS, HEAD_DIM)
    outs, lses = [], []
    for g, (window, dilation) in enumerate(A_GROUPS):
        n_side = window // (2 * dilation)
        o, lse = _dilated_window_attention(za[:, :, g, 0], za[:, :, g, 1], za[:, :, g, 2],
                                           rel_bias[:, g * A_HEADS:(g + 1) * A_HEADS],
                                           dilation, n_side)
        outs.append(o)
        lses.append(lse)
    alpha = jax.nn.softmax(jnp.stack(lses, axis=0), axis=0)
    o = jnp.einsum('gbsh,gbshd->bshd', alpha.astype(outs[0].dtype), jnp.stack(outs, axis=0))
    return o.reshape(bsz, s, A_OUT)


def _axial_rope_tables(seq):
    n_rows = seq // GRID_W
    row = jnp.repeat(jnp.arange(n_rows), GRID_W).astype(jnp.float32)
    col = (jnp.arange(seq) % GRID_W).astype(jnp.float32)
    half = HEAD_DIM // 2
    inv = ROPE_THETA ** (-jnp.arange(0, half, 2, dtype=jnp.float32) / half)
    ang_r = row[:, None] * inv
    ang_c = col[:, None] * inv
    return jnp.cos(ang_r), jnp.sin(ang_r), jnp.cos(ang_c), jnp.sin(ang_c)


def _rotate(u, cos, sin):
    u1, u2 = jnp.split(u, 2, axis=-1)
    cos = cos[None, :, None, :]
    sin = sin[None, :, None, :]
    return jnp.concatenate([u1 * cos - u2 * sin, u2 * cos + u1 * sin], axis=-1)


def _axial_rope(x, tabs):
    cr, sr, cc, sc = tabs
    half = HEAD_DIM // 2
    xf = x.astype(jnp.float32)
    y = jnp.concatenate([_rotate(xf[..., :half], cr, sr), _rotate(xf[..., half:], cc, sc)], axis=-1)
    return y.astype(x.dtype)


def _mixer_gqa(zq, zk, zv, q_g, k_g, tabs):
    bsz, s, _ = zq.shape
    grp = B_HEADS // B_KV_HEADS
    q = _axial_rope(_rmsnorm(zq.reshape(bsz, s, B_HEADS, HEAD_DIM), q_g), tabs)
    k = _axial_rope(_rmsnorm(zk.reshape(bsz, s, B_KV_HEADS, HEAD_DIM), k_g), tabs)
    v = zv.reshape(bsz, s, B_KV_HEADS, HEAD_DIM)
    qb = q.reshape(bsz, s // Q_BLOCK, Q_BLOCK, B_KV_HEADS, grp, HEAD_DIM).transpose(1, 0, 2, 3, 4, 5)
    scale = HEAD_DIM ** -0.5

    def block(qblk):
        sc = jnp.einsum('bqhgd,bkhd->bhgqk', qblk, k).astype(jnp.float32) * scale
        p = jax.nn.softmax(sc, axis=-1)
        return jnp.einsum('bhgqk,bkhd->bqhgd', p.astype(v.dtype), v)

    o = lax.map(block, qb)
    return o.transpose(1, 0, 2, 3, 4, 5).reshape(bsz, s, B_Q)


def _moe(h, w_router, b_router, w_gu, b_gu, w_dn, b_dn):
    bsz, s, d = h.shape
    t = bsz * s
    ht = h.reshape(t, d)
    logits = (ht @ w_router + b_router).astype(jnp.float32)
    top_v, top_e = lax.top_k(logits, TOP_K)
    gates = jax.nn.softmax(top_v, axis=-1)
    n_assign = t * TOP_K
    e_flat = top_e.reshape(n_assign)
    tok_flat = jnp.repeat(jnp.arange(t, dtype=jnp.int32), TOP_K)
    w_flat = gates.reshape(n_assign)
    order = jnp.argsort(e_flat)
    e_s, tok_s, w_s = e_flat[order], tok_flat[order], w_flat[order]
    sizes = jax.ops.segment_sum(jnp.ones((n_assign,), jnp.int32), e_flat, num_segments=N_EXPERTS)
    starts = jnp.cumsum(sizes) - sizes
    padded = (sizes + MOE_BLOCK - 1) // MOE_BLOCK * MOE_BLOCK
    pad_end = jnp.cumsum(padded)
    pad_start = pad_end - padded
    dest = pad_start[e_s] + (jnp.arange(n_assign, dtype=jnp.int32) - starts[e_s])
    n_blocks = -(-n_assign // MOE_BLOCK) + N_EXPERTS
    cap = n_blocks * MOE_BLOCK
    buf_tok = jnp.zeros((cap,), jnp.int32).at[dest].set(tok_s)
    buf_w = jnp.zeros((cap,), jnp.float32).at[dest].set(w_s)
    blk_e = jnp.minimum(jnp.searchsorted(pad_end, jnp.arange(n_blocks) * MOE_BLOCK, side='right'),
                        N_EXPERTS - 1).astype(jnp.int32)

    def expert_block(args):
        tok_idx, e = args
        xb = ht[tok_idx]
        gu = xb @ w_gu[e] + b_gu[e]
        x_glu, x_lin = jnp.split(gu, 2, axis=-1)
        x_glu = jnp.minimum(x_glu, SWIGLU_LIMIT)
        x_lin = jnp.clip(x_lin, -SWIGLU_LIMIT, SWIGLU_LIMIT)
        act = x_glu * jax.nn.sigmoid(SWIGLU_ALPHA * x_glu) * (x_lin + 1.0)
        return act @ w_dn[e] + b_dn[e]

    out = lax.map(expert_block, (buf_tok.reshape(n_blocks, MOE_BLOCK), blk_e)).reshape(cap, d)
    y = jnp.zeros((t, d), h.dtype).at[buf_tok].add(out * buf_w[:, None].astype(out.dtype))
    return y.reshape(bsz, s, d)


def setup_inputs(seed: int = 0) -> dict:
    key = jax.random.key(seed)
    ks = jax.random.split(key, 22)
    f32 = jnp.float32

    def nrm(k, shape, scale):
        return jax.random.normal(k, shape, f32) * scale

    return {
        "x": nrm(ks[0], (BATCH, SEQ, D_MODEL), 1.0),
        "c": nrm(ks[1], (BATCH, D_MODEL), 1.0),
        "w_ada": nrm(ks[2], (DEPTH, D_MODEL, N_MOD * D_MODEL), 0.5 * D_MODEL ** -0.5),
        "b_ada": nrm(ks[3], (DEPTH, N_MOD * D_MODEL), 0.02),
        "norm1_g": 1.0 + nrm(ks[4], (DEPTH, D_MODEL), 0.01),
        "w_in": nrm(ks[5], (DEPTH, D_MODEL, N_IN), D_MODEL ** -0.5),
        "q_norm_g": 1.0 + nrm(ks[6], (DEPTH, HEAD_DIM), 0.01),
        "k_norm_g": 1.0 + nrm(ks[7], (DEPTH, HEAD_DIM), 0.01),
        "rel_bias": nrm(ks[8], (REL_BUCKETS, len(A_GROUPS) * A_HEADS), 0.5),
        "w_br_a": nrm(ks[9], (DEPTH, A_OUT, D_MODEL), A_OUT ** -0.5),
        "w_br_b": nrm(ks[10], (DEPTH, B_Q, D_MODEL), B_Q ** -0.5),
        "w_out": nrm(ks[11], (DEPTH, D_MODEL, D_MODEL), D_MODEL ** -0.5),
        "norm2_g": 1.0 + nrm(ks[12], (DEPTH, D_MODEL), 0.01),
        "w_router": nrm(ks[13], (DEPTH, D_MODEL, N_EXPERTS), D_MODEL ** -0.5),
        "b_router": nrm(ks[14], (DEPTH, N_EXPERTS), 0.01),
        "w_gate_up": nrm(ks[15], (DEPTH, N_EXPERTS, D_MODEL, 2 * D_EXPERT), D_MODEL ** -0.5),
        "b_gate_up": nrm(ks[16], (DEPTH, N_EXPERTS, 2 * D_EXPERT), 0.01),
        "w_down": nrm(ks[17], (DEPTH, N_EXPERTS, D_EXPERT, D_MODEL), D_EXPERT ** -0.5),
        "b_down": nrm(ks[18], (DEPTH, N_EXPERTS, D_MODEL), 0.01),
        "final_norm_g": 1.0 + nrm(ks[19], (D_MODEL,), 0.01),
    }


def reference(x, c, w_ada, b_ada, norm1_g, w_in, q_norm_g, k_norm_g, rel_bias, w_br_a, w_br_b,
              w_out, norm2_g, w_router, b_router, w_gate_up, b_gate_up, w_down, b_down,
              final_norm_g):
    bsz, s, d = x.shape
    tabs = _axial_rope_tables(s)
    c_act = jax.nn.silu(c)
    cuts = np.cumsum([3 * A_WIDTH, B_Q, B_KV, B_KV]).tolist()
    for l in range(DEPTH):
        mod = c_act @ w_ada[l] + b_ada[l]
        sh1, sc1, g1, sh2, sc2, g2 = jnp.split(mod, N_MOD, axis=-1)
        h = _modulate(_rmsnorm(x, norm1_g[l]), sh1, sc1)
        z = h @ w_in[l]
        za, zq, zk, zv, zg = jnp.split(z, cuts, axis=-1)
        o_a = _mixer_dilated(za, rel_bias)
        o_b = _mixer_gqa(zq, zk, zv, q_norm_g[l], k_norm_g[l], tabs)
        gate = jax.nn.sigmoid(zg.astype(jnp.float32)).astype(x.dtype).reshape(bsz, s, N_BRANCH, d)
        merged = gate[:, :, 0] * (o_a @ w_br_a[l]) + gate[:, :, 1] * (o_b @ w_br_b[l])
        x = x + g1[:, None, :] * (merged @ w_out[l])
        h2 = _modulate(_rmsnorm(x, norm2_g[l]), sh2, sc2)
        x = x + g2[:, None, :] * _moe(h2, w_router[l], b_router[l], w_gate_up[l], b_gate_up[l],
                                      w_down[l], b_down[l])
    return _rmsnorm(x, final_norm_g)
```

```python
import math
from contextlib import ExitStack

import numpy as np
import ml_dtypes
import concourse.bass as bass
import concourse.mybir as mybir
from concourse.bass_utils import run_bass_kernel_spmd

F32 = mybir.dt.float32
BF16 = mybir.dt.bfloat16
I32 = mybir.dt.int32
ALU = mybir.AluOpType
AF = mybir.ActivationFunctionType
AX = mybir.AxisListType

S = 4096
D = 1024
NT = 32
NE = 32
NBLK = 160
EPS = 1e-6
A_DIL = (1, 4, 16)
BIGIDX = 1.0e6


def sl(start, n, step):
    return slice(start, start + (n - 1) * step + 1, step)

ENGS = ["pe", "act", "dve", "pool", "sp"]


class Tok:
    __slots__ = ("w", "r")

    def __init__(self):
        self.w = None
        self.r = []


class Prog:
    N_DMA_SEMS = {"sp": 24, "pool": 16, "act": 8}

    def __init__(self, nc):
        self.nc = nc
        self.q = {e: [] for e in ENGS}
        self.cnt = {e: 0 for e in ENGS}
        self.seen = {e: {} for e in ENGS}
        self.dma_val = {}
        self.dma_rr = {e: 0 for e in ENGS}
        self.prologue = {}

    def _collect(self, eng, reads, writes):
        deps = {}

        def add(d):
            if d is not None and deps.get(d[0], 0) < d[1]:
                deps[d[0]] = d[1]

        for t in reads:
            add(t.w)
        for t in writes:
            add(t.w)
            for d in t.r:
                add(d)
        out = []
        own = "E:" + eng
        seen = self.seen[eng]
        for k, v in deps.items():
            if k == own and eng == "pe":
                continue
            if seen.get(k, 0) >= v:
                continue
            seen[k] = v
            out.append((k, v))
        return out

    def _note(self, my, reads, writes):
        for t in reads:
            t.r.append(my)
            if len(t.r) > 48:
                d = {}
                for k, v in t.r:
                    if d.get(k, 0) < v:
                        d[k] = v
                t.r = list(d.items())
        for t in writes:
            t.w = my
            t.r = []

    def op(self, eng, fn, reads=(), writes=()):
        waits = self._collect(eng, reads, writes)
        self.cnt[eng] += 1
        my = ("E:" + eng, self.cnt[eng])
        self._note(my, reads, writes)
        self.q[eng].append((waits, fn, my, 1))

    def dma(self, eng, fn, reads=(), writes=()):
        waits = self._collect(eng, reads, writes)
        n = self.N_DMA_SEMS[eng]
        slot = self.dma_rr[eng] % n
        self.dma_rr[eng] += 1
        key = "D:%s:%d" % (eng, slot)
        prev = self.dma_val.get(key, 0)
        if prev > 0 and self.seen[eng].get(key, 0) < prev:
            self.seen[eng][key] = prev
            waits.append((key, prev))
        self.dma_val[key] = prev + 16
        my = (key, prev + 16)
        self._note(my, reads, writes)
        self.q[eng].append((waits, fn, my, 16))

    def barrier(self):
        for eng in ENGS:
            waits = []
            for key, v in self.dma_val.items():
                if self.seen[eng].get(key, 0) < v:
                    waits.append((key, v))
                    self.seen[eng][key] = v
            for e in ENGS:
                if e == eng or self.cnt[e] == 0:
                    continue
                k = "E:" + e
                if self.seen[eng].get(k, 0) < self.cnt[e]:
                    waits.append((k, self.cnt[e]))
                    self.seen[eng][k] = self.cnt[e]
            if waits:
                self.q[eng].append((waits, None, None, 0))

    def emit(self):
        nc = self.nc
        keys = set()
        for e in ENGS:
            for waits, fn, my, inc in self.q[e]:
                for k, v in waits:
                    keys.add(k)
                if my is not None:
                    keys.add(my[0])
        keys = sorted(keys)
        with ExitStack() as st:
            sems = {k: st.enter_context(nc.semaphore(k.replace(":", "_"))) for k in keys}
            block = st.enter_context(nc.Block())
            hmap = {"pe": block.tensor, "act": block.scalar, "dve": block.vector,
                    "pool": block.gpsimd, "sp": block.sync}

            def make(e):
                def body(engh):
                    if e in self.prologue:
                        self.prologue[e](engh)
                    for waits, fn, my, inc in self.q[e]:
                        for k, v in waits:
                            engh.wait_ge(sems[k], v)
                        if fn is None:
                            continue
                        fn(engh).then_inc(sems[my[0]], inc)
                return body

            for e in ENGS:
                if self.q[e]:
                    hmap[e](make(e))


class Ring:
    def __init__(self, items):
        self.items = [(it, Tok()) for it in items]
        self.i = 0

    def next(self):
        it = self.items[self.i % len(self.items)]
        self.i += 1
        return it


class Builder:
    def __init__(self, nlayers=2, stop=None, dbg=(), small_moe=False):
        self.nlayers = nlayers
        self.stop = stop
        self.dbg = dbg
        NEW = 1 if small_moe else NE
        nc = self.nc = bass.Bass("TRN2", target_bir_lowering=False)
        self.P = Prog(nc)
        self.outs = ["out"]
        di = lambda n, s, d=F32: nc.dram_tensor(n, s, d, kind="ExternalInput").ap()
        self.x_in = di("x", [S, D])
        self.ccol = di("ccol", [128, 8])
        self.w_ada = di("w_ada", [2, D, 6 * D])
        self.b_ada = di("b_ada", [2, 6 * D])
        self.norm1_g = di("norm1_g", [2, D])
        self.w_in = di("w_in", [2, D, 5888])
        self.q_norm_g = di("q_norm_g", [2, 64])
        self.k_norm_g = di("k_norm_g", [2, 64])
        self.wbias = di("wbias", [128, 12, 384])
        self.w_br_a = di("w_br_a", [2, 256, D])
        self.w_br_b = di("w_br_b", [2, D, D])
        self.w_out = di("w_out", [2, D, D])
        self.norm2_g = di("norm2_g", [2, D])
        self.w_router = di("w_router", [2, D, NE])
        self.b_router = di("b_router", [2, NE])
        self.w_gu = di("w_gate_up", [2, NEW, D, 2 * D])
        self.b_gu = di("b_gate_up", [2, NE, 2 * D])
        self.w_dn = di("w_down", [2, NEW, D, D])
        self.b_dn = di("b_down", [2, NE, D])
        self.fng = di("final_norm_g", [D])
        self.c_identb = di("identb", [128, 128], BF16)
        self.c_identf = di("identf", [128, 128])
        self.c_cos = di("cosT", [128, NT, 64])
        self.c_sin = di("sinT", [128, NT, 64])
        self.c_tris = di("tri_s", [128, 128], BF16)
        self.c_tri32s = di("tri32s", [32, 32], BF16)
        self.c_tri32i = di("tri32i", [32, 32], BF16)
        self.c_iota160 = di("iota160", [32, NBLK])
        self.c_iotap = di("iotap", [128, 1])
        self.c_iotap32 = di("iotap32", [128, 1])
        self.out = nc.dram_tensor("out", [S, D], F32, kind="ExternalOutput").ap()
        self.xs_d = self.scratch("xs_d", [S, D], F32)
        self.obT_d = self.scratch("obT_d", [D, S], BF16)
        self.gT_d = self.scratch("gT_d", [2 * D, S], BF16)
        self.oaw_d = self.scratch("oaw_d", [3, S, 264], F32)
        self.h2_d = self.scratch("h2_d", [S, D], BF16)
        self.Xs_d = self.scratch("Xs_d", [NBLK * 128, D], BF16)
        self.Out_d = self.scratch("Out_d", [NBLK * 128, D], F32)
        self.modr_d = self.scratch("modr_d", [128, 6, D], F32)
        self.t_modrd = Tok()
        self.t_xs, self.t_obT, self.t_gT, self.t_oaw, self.t_h2d, self.t_Xs, self.t_Outd = [Tok() for _ in range(7)]
        self.t_out = Tok()

    def scratch(self, name, shape, dt):
        if name in self.dbg:
            self.outs.append(name)
            return self.nc.dram_tensor(name, shape, dt, kind="ExternalOutput").ap()
        return self.nc.dram_tensor(name, shape, dt, kind="Internal").ap()

    def sb(self, st, name, shape, dt):
        self._nsb = getattr(self, "_nsb", 0) + 1
        return st.enter_context(self.nc.sbuf_tensor("sb%d_%s" % (self._nsb, name), shape, dt))

    def tt(self, eng, out, in0, in1, op, r, w):
        self.P.op(eng, lambda e: e.tensor_tensor(out=out, in0=in0, in1=in1, op=op), r, w)

    def ts(self, eng, out, in0, s1, op0, r, w, s2=None, op1=None):
        if op1 is None:
            self.P.op(eng, lambda e: e.tensor_scalar(out=out, in0=in0, scalar1=s1, scalar2=None, op0=op0), r, w)
        else:
            self.P.op(eng, lambda e: e.tensor_scalar(out=out, in0=in0, scalar1=s1, scalar2=s2, op0=op0, op1=op1), r, w)

    def stt(self, out, in0, scalar, in1, op0, op1, r, w, accum=None):
        if accum is None:
            self.P.op("dve", lambda e: e.scalar_tensor_tensor(out=out, in0=in0, scalar=scalar, in1=in1, op0=op0, op1=op1), r, w)
        else:
            self.P.op("dve", lambda e: e.scalar_tensor_tensor(out=out, in0=in0, scalar=scalar, in1=in1, op0=op0, op1=op1,
                                                              accum_out=accum), r, w)

    def act(self, out, in_, func, r, w, bias=None, scale=None, accum=None):
        kw = {}
        if bias is not None:
            kw["bias"] = bias
        if scale is not None:
            kw["scale"] = scale
        if accum is not None:
            kw["accum_out"] = accum
        self.P.op("act", lambda e: e.activation(out=out, in_=in_, func=func, **kw), r, w)

    def cp(self, eng, out, in_, r, w):
        if eng == "act":
            self.P.op("act", lambda e: e.copy(out=out, in_=in_), r, w)
        else:
            self.P.op(eng, lambda e: e.tensor_copy(out=out, in_=in_), r, w)

    def mm(self, out, lhsT, rhs, start, stop, r, w):
        self.P.op("pe", lambda e: e.matmul(out, lhsT=lhsT, rhs=rhs, start=start, stop=stop), r, w)

    def tr(self, out, in_, ident, r, w):
        self.P.op("pe", lambda e: e.transpose(out=out, in_=in_, identity=ident), r, w)

    def dma(self, q, out, in_, r, w):
        self.P.dma(q, lambda e: e.dma_start(out=out, in_=in_), r, w)

    def red(self, out, in_, op, r, w):
        self.P.op("dve", lambda e: e.tensor_reduce(out=out, in_=in_, axis=AX.X, op=op), r, w)

    def recip(self, out, in_, r, w):
        self.P.op("dve", lambda e: e.reciprocal(out=out, in_=in_), r, w)

    def memset(self, eng, ap, val, w):
        self.P.op(eng, lambda e: e.memset(ap, val), (), w)

    def gather(self, out, in_, idx, r, w, bound):
        if bound is None:
            self.P.dma("pool", lambda e: e.indirect_dma_start(out=out, out_offset=None, in_=in_,
                                                              in_offset=bass.IndirectOffsetOnAxis(ap=idx, axis=0)), r, w)
            return
        if "pool" not in self.P.prologue:
            def pro(e):
                self.breg = e.alloc_register("bound_reg")
                e.reg_mov(self.breg, bound)
            self.P.prologue["pool"] = pro
            self.bound_val = bound
        assert bound == self.bound_val
        self.P.dma("pool", lambda e: e.indirect_dma_start(out=out, out_offset=None, in_=in_,
                                                          in_offset=bass.IndirectOffsetOnAxis(ap=idx, axis=0),
                                                          bounds_check=self.breg, oob_is_err=False), r, w)

    def scatter(self, out, in_, idx, r, w, bound):
        self.P.dma("pool", lambda e: e.indirect_dma_start(out=out, out_offset=bass.IndirectOffsetOnAxis(ap=idx, axis=0),
                                                          in_=in_, in_offset=None), r, w)

    def build(self):
        nc = self.nc
        with ExitStack() as gst:
            self.gst = gst
            self.ps = [gst.enter_context(nc.psum_tensor("ps%d" % i, [128, 512], F32)) for i in range(8)]
            self.tp = [Tok() for _ in range(8)]
            sb = self.sb
            self.identb = sb(gst, "identb", [128, 128], BF16)
            self.identf = sb(gst, "identf", [128, 128], F32)
            self.onesb = sb(gst, "onesb", [128, 128], BF16)
            self.onesf = sb(gst, "onesf", [128, 128], F32)
            self.epsc = sb(gst, "epsc", [128, 1], F32)
            self.t_const = Tok()
            tc = [self.t_const]
            self.dma("sp", self.identb[:], self.c_identb, [], tc)
            self.dma("sp", self.identf[:], self.c_identf, [], tc)
            self.memset("dve", self.onesb[:], 1.0, tc)
            self.memset("dve", self.onesf[:], 1.0, tc)
            self.memset("dve", self.epsc[:], EPS, tc)
            self.neghalf = sb(gst, "neghalf", [128, 16], F32)
            self.memset("dve", self.neghalf[:], -0.5, tc)
            self.P.barrier()
            xsrc = self.x_in
            for l in range(self.nlayers):
                last = (l == self.nlayers - 1)
                if self.layer(l, xsrc, last):
                    break
                xsrc = self.xs_d
            self.P.barrier()
            self.P.emit()
        return nc

    def layer(self, l, xsrc, last):
        P = self.P
        stop = self.stop if l == self.nlayers - 1 else None
        with ExitStack() as lst:
            self.oaT = self.sb(lst, "oaT", [128, 2, S], BF16)
            self.t_oaT = Tok()
            self.phase_mod(l)
            P.barrier()
            if stop == "mod":
                return True
            with ExitStack() as ast:
                self.hT = self.sb(ast, "hT", [128, 8, S], BF16)
                self.t_hT = Tok()
                self.phase_norm1(l, xsrc)
                P.barrier()
                if stop == "norm1":
                    return True
                self.phase_gates(l)
                P.barrier()
                if stop == "gates":
                    return True
                self.phase_window(l)
                P.barrier()
                if stop == "window":
                    return True
                self.phase_gqa(l)
                P.barrier()
                if stop == "gqa":
                    return True
            with ExitStack() as mst:
                self.logits_all = self.sb(mst, "logits_all", [128, NT, NE], F32)
                self.max8_all = self.sb(mst, "max8_all", [128, NT, 8], F32)
                self.g4_all = self.sb(mst, "g4_all", [128, NT, 4], F32)
                self.M_all = self.sb(mst, "M_all", [128, NT, NE], BF16)
                self.pos4_all = self.sb(mst, "pos4_all", [128, NT, 4], I32)
                self.widx = self.sb(mst, "widx", [128, NBLK], I32)
                self.OHall = self.sb(mst, "OHall", [64, NBLK], BF16)
                self.t_widx = Tok()
                self.t_route = Tok()
                self.t_pos4 = Tok()
                self.phase_merge(l, xsrc)
                P.barrier()
                if stop == "merge":
                    return True
                self.phase_slots(l)
                P.barrier()
                if stop == "slots":
                    return True
                self.phase_experts(l)
                P.barrier()
                if stop == "experts":
                    return True
                self.phase_combine(l, last)
                P.barrier()
        return False

    def phase_mod(self, l):
        with ExitStack() as st:
            sb = self.sb
            cc = sb(st, "cc", [128, 8], F32)
            cs = sb(st, "cs", [128, 8], F32)
            crep = sb(st, "crep", [128, 8, 128], F32)
            brep = sb(st, "brep", [128, 6 * D], F32)
            ng = sb(st, "ng", [128, 2, D], F32)
            modr = sb(st, "modr", [128, 6, D], F32)
            t_modr = Tok()
            wa = Ring([sb(st, "wa%d" % i, [128, 8, 512], F32) for i in range(2)])
            t_cc, t_cs, t_crep, t_brep, t_ng = Tok(), Tok(), Tok(), Tok(), Tok()
            self.dma("sp", cc[:], self.ccol, [], [t_cc])
            self.dma("sp", brep[:], self.b_ada[l:l + 1, :].partition_broadcast(128), [], [t_brep])
            self.dma("sp", ng[:, 0, :], self.norm1_g[l:l + 1, :].partition_broadcast(128), [], [t_ng])
            self.dma("sp", ng[:, 1, :], self.norm2_g[l:l + 1, :].partition_broadcast(128), [], [t_ng])
            self.act(cs[:], cc[:], AF.Silu, [t_cc], [t_cs])
            for kc in range(8):
                self.cp("dve", crep[:, kc, :], cs[:, kc:kc + 1].to_broadcast([128, 128]), [t_cs], [t_crep])
            modf = modr[:].rearrange("p a d -> p (a d)")
            psr = Ring(self.ps[0:2])
            psr.items = [(self.ps[0], self.tp[0]), (self.ps[1], self.tp[1])]
            for ch in range(12):
                w, t_w = wa.next()
                self.dma("sp", w[:], self.w_ada[l][:, ch * 512:(ch + 1) * 512].rearrange("(kc p) n -> p kc n", p=128), [], [t_w])
                ps, t_ps = psr.next()
                for kc in range(8):
                    self.mm(ps[:], crep[:, kc, :], w[:, kc, :], kc == 0, kc == 7, [t_crep, t_w], [t_ps])
                self.tt("dve", modf[:, ch * 512:(ch + 1) * 512], ps[:], brep[:, ch * 512:(ch + 1) * 512], ALU.add,
                        [t_ps, t_brep], [t_modr])
            for i, j in ((1, 0), (4, 1)):
                self.stt(modr[:, i, :], modr[:, i, :], 1.0, ng[:, j, :], ALU.add, ALU.mult, [t_modr, t_ng], [t_modr])
            self.dma("sp", self.modr_d, modr[:], [t_modr], [self.t_modrd])

    def load_mod(self, st, idxs):
        tl = self.sb(st, "modl", [128, len(idxs), D], F32)
        self.t_modr = Tok()
        self.modr = {}
        for n, i in enumerate(idxs):
            self.dma("sp", tl[:, n, :], self.modr_d[:, i, :], [self.t_modrd], [self.t_modr])
            self.modr[i] = tl[:, n, :]

    def rstd_from_ss(self, rstd, ss, n, t_in, t_out):
        self.act(rstd, ss, AF.Ln, [t_in, self.t_const], [t_out], bias=self.epsc[0:rstd.shape[0], 0:1], scale=1.0 / n)
        self.act(rstd, rstd, AF.Exp, [t_out], [t_out], scale=-0.5)

    def norm_mod(self, xt, t_x, i_g, i_sh, work, hb, t_hb, hf=None):
        junk, ss, rstd, tmp, t_w = work
        self.stt(junk[:], xt[:], 1.0, xt[:], ALU.mult, ALU.mult, [t_x], [t_w], accum=ss[:, 0:1])
        self.rstd_from_ss(rstd[:, 0:1], ss[:, 0:1], D, t_w, t_w)
        self.stt(tmp[:], xt[:], rstd[:, 0:1], self.modr[i_g], ALU.mult, ALU.mult, [t_x, t_w, self.t_modr], [t_w])
        if hf is not None:
            self.tt("dve", hf[:], tmp[:], self.modr[i_sh], ALU.add, [t_w, self.t_modr], [t_hb])
            self.cp("pool", hb[:], hf[:], [t_hb], [t_hb])
        else:
            self.tt("dve", hb[:], tmp[:], self.modr[i_sh], ALU.add, [t_w, self.t_modr], [t_hb])

    def phase_norm1(self, l, xsrc):
        with ExitStack() as st:
            sb = self.sb
            self.load_mod(st, [0, 1])
            xr = Ring([sb(st, "xt%d" % i, [128, D], F32) for i in range(2)])
            hr = Ring([sb(st, "hb%d" % i, [128, D], BF16) for i in range(2)])
            work = (sb(st, "junk", [128, D], F32), sb(st, "ss", [128, 1], F32), sb(st, "rstd", [128, 1], F32),
                    sb(st, "tmp", [128, D], F32), Tok())
            psb = [self.ps[i][:].bitcast(BF16).rearrange("p (a b) -> p a b", a=8) for i in range(2)]
            for t in range(NT):
                xt, t_x = xr.next()
                hb, t_hb = hr.next()
                self.dma("sp", xt[:], xsrc[t * 128:(t + 1) * 128, :], [self.t_xs], [t_x])
                self.norm_mod(xt, t_x, 1, 0, work, hb, t_hb)
                pT, t_p = psb[t % 2], self.tp[t % 2]
                for kc in range(8):
                    self.tr(pT[:, kc, :], hb[:, kc * 128:(kc + 1) * 128], self.identb[:], [t_hb, self.t_const], [t_p])
                self.cp("act", self.hT[:, :, t * 128:(t + 1) * 128], pT, [t_p], [self.t_hT])

    def load_w(self, w, src, t_w, kc=8):
        self.dma("pool", w, src.rearrange("(kc p) n -> p kc n", p=128), [], [t_w])

    def phase_gates(self, l):
        with ExitStack() as st:
            sb = self.sb
            wg = sb(st, "wg", [128, 8, 2 * D], BF16)
            t_wg = Tok()
            for q in range(4):
                self.load_w(wg[:, :, q * 512:(q + 1) * 512], self.w_in[l][:, 3840 + q * 512:3840 + (q + 1) * 512], t_wg)
            gr = Ring([sb(st, "gsb%d" % i, [128, 512], BF16) for i in range(3)])
            n = 0
            for c in range(8):
                for m in range(16):
                    ps, t_ps = self.ps[n % 4], self.tp[n % 4]
                    n += 1
                    for kc in range(8):
                        self.mm(ps[:], wg[:, kc, m * 128:(m + 1) * 128], self.hT[:, kc, c * 512:(c + 1) * 512], kc == 0, kc == 7,
                                [t_wg, self.t_hT], [t_ps])
                    g, t_g = gr.next()
                    self.act(g[:], ps[:], AF.Sigmoid, [t_ps], [t_g])
                    self.dma("sp", self.gT_d[m * 128:(m + 1) * 128, c * 512:(c + 1) * 512], g[:], [t_g], [self.t_gT])

    def qknorm_rope(self, src, H, grep, tile, wk, dst, t_src, t_dst):
        sq, ss, rstd, qn, t1, t2, t_w = wk
        W = H * 64
        v3 = lambda ap: ap[:, 0:W].rearrange("p (h d) -> p h d", h=H)
        v5 = lambda ap: ap[:, 0:W].rearrange("p (h a b c) -> p h a b c", h=H, a=2, b=2)
        self.tt("pool", sq[:, 0:W], src, src, ALU.mult, [t_src], [t_w])
        self.red(ss[:, 0:H], v3(sq), ALU.add, [t_w], [t_w])
        self.ts("pool", rstd[:, 0:H], ss[:, 0:H], 1.0 / 64, ALU.mult, [t_w], [t_w], s2=EPS, op1=ALU.add)
        self.tt("pool", rstd[:, 0:H], rstd[:, 0:H], self.neghalf[:, 0:H], ALU.pow, [t_w, self.t_const], [t_w])
        self.tt("dve", v3(qn), src.rearrange("p (h d) -> p h d", h=H), rstd[:, 0:H].unsqueeze(2).to_broadcast([128, H, 64]), ALU.mult,
                [t_src, t_w], [t_w])
        self.tt("pool", v3(qn), v3(qn), grep.unsqueeze(1).to_broadcast([128, H, 64]), ALU.mult, [t_w, self.t_const], [t_w])
        cosb = self.cos[:, tile, :].unsqueeze(1).to_broadcast([128, H, 64])
        sinv = self.sin[:, tile, :].rearrange("p (a b c) -> p a b c", a=2, b=2)
        self.tt("dve", v3(t1), v3(qn), cosb, ALU.mult, [t_w, self.t_const], [t_w])
        for b in range(2):
            sb_ = sinv[:, :, b, :].unsqueeze(1).to_broadcast([128, H, 2, 16])
            self.tt("pool", v5(t2)[:, :, :, b, :], v5(qn)[:, :, :, 1 - b, :], sb_, ALU.mult, [t_w, self.t_const], [t_w])
        self.tt("dve", dst, t1[:, 0:W], t2[:, 0:W], ALU.add, [t_w], [t_dst])

    def phase_gqa(self, l):
        with ExitStack() as st:
            sb = self.sb
            self.cos = sb(st, "cos", [128, NT, 64], F32)
            self.sin = sb(st, "sin", [128, NT, 64], F32)
            self.dma("sp", self.cos[:], self.c_cos, [], [self.t_const])
            self.dma("sp", self.sin[:], self.c_sin, [], [self.t_const])
            KT = sb(st, "KT", [128, 2, S], BF16)
            V = sb(st, "V", [128, NT, 4, 128], BF16)
            wkv = sb(st, "wkv", [128, 8, 512], BF16)
            gq = sb(st, "gq", [128, 64], F32)
            gk = sb(st, "gk", [128, 64], F32)
            t_KT, t_V, t_wkv = Tok(), Tok(), Tok()
            self.dma("sp", gq[:], self.q_norm_g[l:l + 1, :].partition_broadcast(128), [], [self.t_const])
            self.dma("sp", gk[:], self.k_norm_g[l:l + 1, :].partition_broadcast(128), [], [self.t_const])
            self.load_w(wkv[:], self.w_in[l][:, 3328:3840], t_wkv)
            self.memset("dve", V[:, :, :, 64:128], 1.0, [t_V])
            wk = (sb(st, "q_sq", [128, 256], F32), sb(st, "q_ss", [128, 4], F32), sb(st, "q_rstd", [128, 4], F32),
                  sb(st, "q_qn", [128, 256], F32), sb(st, "q_t1", [128, 256], F32), sb(st, "q_t2", [128, 256], F32), Tok())
            ksr = Ring([sb(st, "ksb%d" % i, [128, 256], F32) for i in range(2)])
            krr = Ring([sb(st, "kr%d" % i, [128, 256], BF16) for i in range(2)])
            psT = self.ps[7][:].bitcast(BF16).rearrange("p (a b) -> p a b", a=8)
            t_psT = self.tp[7]
            for t in range(NT):
                ps, t_ps = self.ps[6], self.tp[6]
                for kc in range(8):
                    self.mm(ps[:], self.hT[:, kc, t * 128:(t + 1) * 128], wkv[:, kc, :], kc == 0, kc == 7, [self.t_hT, t_wkv], [t_ps])
                self.cp("act", V[:, t, :, 0:64], ps[:, 256:512].rearrange("p (g d) -> p g d", g=4), [t_ps], [t_V])
                ksb, t_ksb = ksr.next()
                self.cp("act", ksb[:], ps[:, 0:256], [t_ps], [t_ksb])
                kr, t_kr = krr.next()
                self.qknorm_rope(ksb[:], 4, gk[:], t, wk, kr[:], t_ksb, t_kr)
                for m in range(2):
                    self.tr(psT[:, m, :], kr[:, m * 128:(m + 1) * 128], self.identb[:], [t_kr, self.t_const], [t_psT])
                self.cp("dve", KT[:, :, t * 128:(t + 1) * 128], psT[:, 0:2, :], [t_psT], [t_KT])
            wq = sb(st, "wq", [128, 8, 256], BF16)
            t_wq = Tok()
            QTb = [(sb(st, "QT%d" % i, [128, 4, 512], BF16), Tok()) for i in range(2)]
            PTr = Ring([sb(st, "PT%d" % i, [128, 512], BF16) for i in range(4)])
            rdr = Ring([sb(st, "rd%d" % i, [64, 512], F32) for i in range(2)])
            obr = Ring([sb(st, "ob%d" % i, [64, 512], BF16) for i in range(2)])
            Sr = Ring([None] * 3)
            Sr.items = [(self.ps[i], self.tp[i]) for i in range(3)]
            Or = Ring([None] * 2)
            Or.items = [(self.ps[i], self.tp[i]) for i in (3, 4)]
            units = [(g, c) for g in range(4) for c in range(8)]

            qst = {}

            def qproj1(u, t4):
                g, c = units[u]
                if c == 0 and t4 == 0:
                    self.load_w(wq[:], self.w_in[l][:, 2304 + g * 256:2304 + (g + 1) * 256], t_wq)
                t = c * 4 + t4
                ps, t_ps = self.ps[6], self.tp[6]
                for kc in range(8):
                    self.mm(ps[:, 0:256], self.hT[:, kc, t * 128:(t + 1) * 128], wq[:, kc, :], kc == 0, kc == 7,
                            [self.t_hT, t_wq], [t_ps])
                qsb, t_qsb = ksr.next()
                self.cp("dve", qsb[:], ps[:, 0:256], [t_ps], [t_qsb])
                qr, t_qr = krr.next()
                self.qknorm_rope(qsb[:], 4, gq[:], t, wk, qr[:], t_qsb, t_qr)
                qst[(u, t4)] = (qr, t_qr)

            def qproj2(u, t4):
                g, c = units[u]
                kb = (g % 2) * 64
                ko = 64 - kb
                QT, t_QT = QTb[u % 2]
                qr, t_qr = qst.pop((u, t4))
                for h in range(4):
                    self.tr(psT[0:64, h, :], qr[:, h * 64:(h + 1) * 64], self.identb[:], [t_qr, self.t_const], [t_psT])
                self.cp("dve", QT[kb:kb + 64, :, t4 * 128:(t4 + 1) * 128], psT[0:64, 0:4, :], [t_psT], [t_QT])
                self.memset("pool", QT[ko:ko + 64, :, t4 * 128:(t4 + 1) * 128], 0.0, [t_QT])

            def attend(u, h):
                g, c = units[u]
                QT, t_QT = QTb[u % 2]
                hq = g * 4 + h
                OT, t_OT = Or.next()
                pend = []

                def score(kt):
                    Sb, t_S = Sr.next()
                    self.mm(Sb[:], KT[:, g // 2, kt * 128:(kt + 1) * 128], QT[:, h, :], True, True, [t_KT, t_QT], [t_S])
                    PT, t_PT = PTr.next()
                    self.act(PT[:], Sb[:], AF.Exp, [t_S], [t_PT], scale=0.125)
                    pend.append((kt, PT, t_PT))

                def pv():
                    kt, PT, t_PT = pend.pop(0)
                    self.mm(OT[:], V[:, kt, g, :], PT[:], kt == 0, kt == NT - 1, [t_V, t_PT], [t_OT])

                score(0)
                score(1)
                for kt in range(NT):
                    if kt + 2 < NT:
                        score(kt + 2)
                    pv()
                def fin():
                    rd, t_rd = rdr.next()
                    self.recip(rd[0:64, :], OT[64:128, :], [t_OT], [t_rd])
                    ob, t_ob = obr.next()
                    self.tt("dve", ob[:], OT[0:64, :], rd[0:64, :], ALU.mult, [t_OT, t_rd], [t_ob])
                    self.dma("sp", self.obT_d[hq * 64:(hq + 1) * 64, c * 512:(c + 1) * 512], ob[:], [t_ob], [self.t_obT])
                return fin

            for t4 in range(4):
                qproj1(0, t4)
                qproj2(0, t4)
            for u in range(len(units)):
                nxt = u + 1 < len(units)
                f = attend(u, 0)
                f()
                if nxt:
                    qproj1(u + 1, 0)
                    qproj1(u + 1, 1)
                f = attend(u, 1)
                if nxt:
                    qproj2(u + 1, 0)
                    qproj2(u + 1, 1)
                f()
                if nxt:
                    qproj1(u + 1, 2)
                f = attend(u, 2)
                if nxt:
                    qproj2(u + 1, 2)
                f()
                if nxt:
                    qproj1(u + 1, 3)
                f = attend(u, 3)
                if nxt:
                    qproj2(u + 1, 3)
                f()

    def phase_window(self, l):
        with ExitStack() as st:
            sb = self.sb
            bias = sb(st, "wb", [128, 12, 384], F32)
            t_bias = Tok()
            self.dma("sp", bias[:], self.wbias, [], [t_bias])
            QT = sb(st, "wQT", [128, 2, S], BF16)
            KT = sb(st, "wKT", [128, 2, S], BF16)
            Vf = sb(st, "wVf", [128, NT, 256], BF16)
            wq = sb(st, "wwq", [128, 8, 768], BF16)
            t_QT, t_KT, t_Vf, t_wq = Tok(), Tok(), Tok(), Tok()
            sr = Ring([sb(st, "ws%d" % i, [128, 384], F32) for i in range(3)])
            pr = Ring([sb(st, "wp%d" % i, [128, 384], BF16) for i in range(4)])
            ptr = Ring([sb(st, "wpt%d" % i, [128, 3, 128], BF16) for i in range(2)])
            mr = Ring([sb(st, "wm%d" % i, [128, 2], F32) for i in range(6)])
            stg = Ring([sb(st, "wstg%d" % i, [128, 264], F32) for i in range(2)])
            Sr = Ring([None] * 3)
            Sr.items = [(self.ps[i], self.tp[i]) for i in (0, 1, 7)]
            Tr = Ring([None] * 2)
            Tr.items = [(self.ps[i][:].bitcast(BF16)[:, 0:384].rearrange("p (a b) -> p a b", a=3), self.tp[i]) for i in (2, 3)]
            Or = Ring([None] * 2)
            Or.items = [(self.ps[i], self.tp[i]) for i in (4, 5)]
            import os
            wstop = int(os.environ.get("WSTOP", "99"))
            for a, Dl in enumerate(A_DIL):
                L = S // Dl
                nj = L // 128
                if str(a) not in os.environ.get("WGRPS", "012"):
                    continue
                self.load_w(wq[:], self.w_in[l][:, a * 768:(a + 1) * 768], t_wq)
                n = 0
                for which, dst, t_dst in ((0, QT, t_QT), (1, KT, t_KT)):
                    for m in range(2):
                        for c in range(8):
                            ps, t_ps = self.ps[6], self.tp[6]
                            n += 1
                            col = which * 256 + m * 128
                            for kc in range(8):
                                self.mm(ps[:], wq[:, kc, col:col + 128], self.hT[:, kc, c * 512:(c + 1) * 512], kc == 0, kc == 7,
                                        [t_wq, self.t_hT], [t_ps])
                            self.cp("act" if n % 2 else "dve", dst[:, m, c * 512:(c + 1) * 512], ps[:], [t_ps], [t_dst])
                if wstop <= 1:
                    continue
                for r in range(Dl):
                    for j in range(nj):
                        bi = r * nj + j
                        tok0 = j * 128 * Dl + r
                        ps, t_ps = self.ps[6], self.tp[6]
                        for kc in range(8):
                            self.mm(ps[:, 0:256], self.hT[:, kc, sl(tok0, 128, Dl)], wq[:, kc, 512:768], kc == 0, kc == 7,
                                    [self.t_hT, t_wq], [t_ps])
                        self.cp("act" if bi % 2 else "dve", Vf[:, bi, :], ps[:, 0:256], [t_ps], [t_Vf])
                if wstop <= 2:
                    continue
                units = []
                for r in range(Dl):
                    for j in range(nj):
                        for h in range(4):
                            units.append((r, j, h))
                ust = {}
                blk = {}

                def W1(n):
                    r, j, h = units[n]
                    tok0 = j * 128 * Dl + r
                    jt0 = max(j - 1, 0)
                    jt1 = min(j + 1, nj - 1)
                    ntl = jt1 - jt0 + 1
                    c0 = (jt0 - (j - 1)) * 128
                    w = ntl * 128
                    k0 = jt0 * 128 * Dl + r
                    if h == 0:
                        blk[(r, j)] = (Or.next(), stg.next())
                    (O4, t_O4), (sg, t_sg) = blk[(r, j)]
                    Sb, t_S = Sr.next()
                    hb_ = (h % 2) * 64
                    self.mm(Sb[:, 0:w], QT[hb_:hb_ + 64, h // 2, sl(tok0, 128, Dl)], KT[hb_:hb_ + 64, h // 2, sl(k0, w, Dl)],
                            True, True, [t_QT, t_KT], [t_S])
                    s_, t_s = sr.next()
                    self.stt(s_[:, 0:w], Sb[:, 0:w], 0.125, bias[:, a * 4 + h, c0:c0 + w], ALU.mult, ALU.add, [t_S, t_bias], [t_s])
                    m, t_m = mr.next()
                    self.P.op("dve", lambda e, m=m, s_=s_, w=w: e.reduce_max(out=m[:, 0:1], in_=s_[:, 0:w], axis=AX.X), [t_s], [t_m])
                    self.ts("dve", m[:, 1:2], m[:, 0:1], -1.0, ALU.mult, [t_m], [t_m])
                    self.cp("dve", sg[:, 256 + h:257 + h], m[:, 0:1], [t_m], [t_sg])
                    ust[n] = (s_, t_s, m, t_m, sg, t_sg, w, h, ntl, jt0)

                def W1b(n):
                    s_, t_s, m, t_m, sg, t_sg, w, h, ntl, jt0 = ust[n]
                    p, t_p = pr.next()
                    self.act(p[:, 0:w], s_[:, 0:w], AF.Exp, [t_s, t_m], [t_p, t_sg], bias=m[:, 1:2], scale=1.0,
                             accum=sg[:, 260 + h:261 + h])
                    ust[n] = (p, t_p, ntl, jt0)

                def W2(n):
                    r, j, h = units[n]
                    tok0 = j * 128 * Dl + r
                    p, t_p, ntl, jt0 = ust.pop(n)
                    (O4, t_O4), (sg, t_sg) = blk[(r, j)]
                    pT, t_pT = Tr.next()
                    for ti in range(ntl):
                        self.tr(pT[:, ti, :], p[:, ti * 128:(ti + 1) * 128], self.identb[:], [t_p, self.t_const], [t_pT])
                    pts, t_pts = ptr.next()
                    self.cp("act", pts[:, 0:ntl, :], pT[:, 0:ntl, :], [t_pT], [t_pts])
                    for ti in range(ntl):
                        self.mm(O4[:, h * 64:(h + 1) * 64], pts[:, ti, :], Vf[:, r * nj + jt0 + ti, h * 64:(h + 1) * 64],
                                ti == 0, ti == ntl - 1, [t_pts, t_Vf], [t_O4])
                    if h == 3:
                        self.cp("act", sg[:, 0:256], O4[:, 0:256], [t_O4], [t_sg])
                        self.dma("sp", self.oaw_d[a, sl(tok0, 128, Dl), :], sg[:], [t_sg], [self.t_oaw])
                        del blk[(r, j)]

                NU = len(units)
                W1(0)
                W1(1)
                W1b(0)
                for n in range(NU):
                    if n + 2 < NU:
                        W1(n + 2)
                    W2(n)
                    if n + 1 < NU:
                        W1b(n + 1)
            if wstop <= 4:
                return
            self.P.barrier()
            cr = Ring([sb(st, "wc%d" % i, [128, 3, 264], F32) for i in range(2)])
            ms = sb(st, "wms", [128, 4], F32)
            wgt = sb(st, "wwgt", [128, 3, 4], F32)
            dt_ = sb(st, "wdt", [128, 4], F32)
            acc = sb(st, "wacc", [128, 256], F32)
            tmp = sb(st, "wtmp", [128, 256], F32)
            obr = Ring([sb(st, "wob%d" % i, [128, 256], BF16) for i in range(2)])
            t_k = Tok()
            for t in range(NT):
                ct, t_ct = cr.next()
                self.dma("sp", ct[:], self.oaw_d[:, t * 128:(t + 1) * 128, :].rearrange("a p c -> p a c"), [self.t_oaw], [t_ct])
                mv = ct[:, :, 256:260]
                dv = ct[:, :, 260:264]
                self.tt("dve", ms[:], mv[:, 0, :], mv[:, 1, :], ALU.max, [t_ct], [t_k])
                self.tt("dve", ms[:], ms[:], mv[:, 2, :], ALU.max, [t_ct, t_k], [t_k])
                self.tt("dve", wgt[:], mv, ms[:].unsqueeze(1).to_broadcast([128, 3, 4]), ALU.subtract, [t_ct, t_k], [t_k])
                self.act(wgt[:], wgt[:], AF.Exp, [t_k], [t_k])
                self.tt("dve", dv, dv, wgt[:], ALU.mult, [t_ct, t_k], [t_ct])
                self.tt("dve", dt_[:], dv[:, 0, :], dv[:, 1, :], ALU.add, [t_ct], [t_k])
                self.tt("dve", dt_[:], dt_[:], dv[:, 2, :], ALU.add, [t_ct, t_k], [t_k])
                self.recip(dt_[:], dt_[:], [t_k], [t_k])
                self.tt("dve", wgt[:], wgt[:], dt_[:].unsqueeze(1).to_broadcast([128, 3, 4]), ALU.mult, [t_k], [t_k])
                ob, t_ob = obr.next()
                v3 = lambda ap: ap.rearrange("p (h d) -> p h d", h=4)
                for a in range(3):
                    cb = wgt[:, a, :].unsqueeze(2).to_broadcast([128, 4, 64])
                    if a == 0:
                        self.tt("dve", v3(acc[:]), v3(ct[:, 0, 0:256]), cb, ALU.mult, [t_ct, t_k], [t_k])
                    else:
                        self.tt("dve", v3(tmp[:]), v3(ct[:, a, 0:256]), cb, ALU.mult, [t_ct, t_k], [t_k])
                        if a == 1:
                            self.tt("dve", acc[:], acc[:], tmp[:], ALU.add, [t_k], [t_k])
                        else:
                            self.tt("dve", ob[:], acc[:], tmp[:], ALU.add, [t_k], [t_ob])
                pT, t_pT = Tr.next()
                for kc in range(2):
                    self.tr(pT[:, kc, :], ob[:, kc * 128:(kc + 1) * 128], self.identb[:], [t_ob, self.t_const], [t_pT])
                self.cp("act", self.oaT[:, :, t * 128:(t + 1) * 128], pT[:, 0:2, :], [t_pT], [self.t_oaT])

    def phase_merge(self, l, xsrc):
        with ExitStack() as st:
            sb = self.sb
            self.load_mod(st, [2, 3, 4])
            wa = sb(st, "m_wa", [128, 2, D], BF16)
            wb = sb(st, "m_wb", [128, 8, D], BF16)
            wo = sb(st, "m_wo", [128, 8, D], BF16)
            wr = sb(st, "m_wr", [128, 8, NE], F32)
            brr = sb(st, "m_brr", [128, NE], F32)
            t_w = Tok()
            self.load_w(wa[:], self.w_br_a[l], t_w)
            self.load_w(wb[:], self.w_br_b[l], t_w)
            self.load_w(wo[:], self.w_out[l], t_w)
            self.dma("sp", wr[:], self.w_router[l].rearrange("(kc p) n -> p kc n", p=128), [], [t_w])
            self.dma("sp", brr[:], self.b_router[l:l + 1, :].partition_broadcast(128), [], [t_w])
            gtr = Ring([sb(st, "m_gt%d" % i, [128, 16, 512], BF16) for i in range(1)])
            obr = Ring([sb(st, "m_ob%d" % i, [128, 8, 512], BF16) for i in range(1)])
            mgr = Ring([sb(st, "m_mg%d" % i, [128, 8, 512], BF16) for i in range(1)])
            tA = sb(st, "m_tA", [128, 512], F32)
            tB = sb(st, "m_tB", [128, 512], F32)
            t_tA, t_tB = Tok(), Tok()
            xr = Ring([sb(st, "m_x%d" % i, [128, D], F32) for i in range(2)])
            x1r = Ring([sb(st, "m_x1%d" % i, [128, D], F32) for i in range(2)])
            h2fr = Ring([sb(st, "m_h2f%d" % i, [128, D], F32) for i in range(2)])
            h2br = Ring([sb(st, "m_h2b%d" % i, [128, D], BF16) for i in range(2)])
            h2Tr = Ring([sb(st, "m_h2T%d" % i, [128, 8, 128], F32) for i in range(1)])
            work = (sb(st, "m_junk", [128, D], F32), sb(st, "m_ss", [128, 1], F32), sb(st, "m_rstd", [128, 1], F32),
                    sb(st, "m_tmp", [128, D], F32), Tok())
            e4 = sb(st, "m_e4", [128, 4], F32)
            ssum = sb(st, "m_ssum", [128, 2], F32)
            t_e4 = Tok()
            Ar = Ring([None] * 2)
            Ar.items = [(self.ps[i], self.tp[i]) for i in (0, 1)]
            Br = Ring([None] * 2)
            Br.items = [(self.ps[i], self.tp[i]) for i in (2, 3)]
            Or = Ring([None] * 2)
            Or.items = [(self.ps[i], self.tp[i]) for i in (4, 5)]
            psT2 = [self.ps[i][:].rearrange("p (a b) -> p a b", a=4) for i in (6, 7)]
            for c in range(8):
                gt, t_gt = gtr.next()
                ob, t_ob = obr.next()
                mg, t_mg = mgr.next()
                self.dma("sp", gt[:], self.gT_d[:, c * 512:(c + 1) * 512].rearrange("(m p) n -> p m n", p=128), [self.t_gT], [t_gt])
                self.dma("sp", ob[:], self.obT_d[:, c * 512:(c + 1) * 512].rearrange("(m p) n -> p m n", p=128), [self.t_obT], [t_ob])
                for m in range(8):
                    pA, t_pA = Ar.next()
                    pB, t_pB = Br.next()
                    for kc in range(2):
                        self.mm(pA[:], wa[:, kc, m * 128:(m + 1) * 128], self.oaT[:, kc, c * 512:(c + 1) * 512], kc == 0, kc == 1,
                                [t_w, self.t_oaT], [t_pA])
                    for kc in range(8):
                        self.mm(pB[:], wb[:, kc, m * 128:(m + 1) * 128], ob[:, kc, :], kc == 0, kc == 7, [t_w, t_ob], [t_pB])
                    self.tt("dve", tA[:], pA[:], gt[:, m, :], ALU.mult, [t_pA, t_gt], [t_tA])
                    self.tt("dve", tB[:], pB[:], gt[:, 8 + m, :], ALU.mult, [t_pB, t_gt], [t_tB])
                    self.tt("pool", mg[:, m, :], tA[:], tB[:], ALU.add, [t_tA, t_tB], [t_mg])
                for t4 in range(4):
                    t = c * 4 + t4
                    xt, t_x = xr.next()
                    x1, t_x1 = x1r.next()
                    self.dma("sp", xt[:], xsrc[t * 128:(t + 1) * 128, :], [self.t_xs], [t_x])
                    for hf in range(2):
                        pO, t_pO = Or.next()
                        for kc in range(8):
                            self.mm(pO[:], mg[:, kc, t4 * 128:(t4 + 1) * 128], wo[:, kc, hf * 512:(hf + 1) * 512], kc == 0, kc == 7,
                                    [t_mg, t_w], [t_pO])
                        self.tt("dve", x1[:, hf * 512:(hf + 1) * 512], pO[:], self.modr[2][:, hf * 512:(hf + 1) * 512], ALU.mult,
                                [t_pO, self.t_modr], [t_x1])
                    self.tt("pool", x1[:], x1[:], xt[:], ALU.add, [t_x1, t_x], [t_x1])
                    self.dma("sp", self.xs_d[t * 128:(t + 1) * 128, :], x1[:], [t_x1], [self.t_xs])
                    h2f, t_h2f = h2fr.next()
                    h2b, t_h2b = h2br.next()
                    self.norm_mod(x1, t_x1, 4, 3, work, h2b, t_h2f, hf=h2f)
                    self.dma("sp", self.h2_d[t * 128:(t + 1) * 128, :], h2b[:], [t_h2f], [self.t_h2d])
                    h2T, t_h2T = h2Tr.next()
                    for hh in range(2):
                        for k4 in range(4):
                            kc = hh * 4 + k4
                            self.tr(psT2[hh][:, k4, :], h2f[:, kc * 128:(kc + 1) * 128], self.identf[:], [t_h2f, self.t_const], [self.tp[6 + hh]])
                        self.cp("act", h2T[:, hh * 4:(hh + 1) * 4, :], psT2[hh], [self.tp[6 + hh]], [t_h2T])
                    pL, t_pL = Or.next()
                    for kc in range(8):
                        self.mm(pL[:, 0:NE], h2T[:, kc, :], wr[:, kc, :], kc == 0, kc == 7, [t_h2T, t_w], [t_pL])
                    lg = self.logits_all[:, t, :]
                    m8 = self.max8_all[:, t, :]
                    self.tt("dve", lg, pL[:, 0:NE], brr[:], ALU.add, [t_pL, t_w], [self.t_route])
                    self.P.op("dve", lambda e, m8=m8, lg=lg: e.max(out=m8, in_=lg), [self.t_route], [self.t_route])
                    self.ts("dve", self.M_all[:, t, :], lg, m8[:, 3:4], ALU.is_ge, [self.t_route], [self.t_route])
                    self.ts("dve", ssum[:, 1:2], m8[:, 0:1], -1.0, ALU.mult, [self.t_route], [t_e4])
                    self.act(e4[:], m8[:, 0:4], AF.Exp, [self.t_route, t_e4], [t_e4], bias=ssum[:, 1:2], scale=1.0, accum=ssum[:, 0:1])
                    self.recip(ssum[:, 0:1], ssum[:, 0:1], [t_e4], [t_e4])
                    self.ts("dve", self.g4_all[:, t, :], e4[:], ssum[:, 0:1], ALU.mult, [t_e4], [self.t_route])

    def phase_slots(self, l):
        sb = self.sb
        with ExitStack() as s2:
            tris = sb(s2, "s_tris", [128, 128], BF16)
            tri32s = sb(s2, "s_t32s", [32, 32], BF16)
            tri32i = sb(s2, "s_t32i", [32, 32], BF16)
            iota160 = sb(s2, "s_iota", [32, NBLK], F32)
            iotap = sb(s2, "s_iotap", [128, 1], F32)
            iotap32 = sb(s2, "s_iotap32", [128, 1], F32)
            t_c = Tok()
            for dst, src in ((tris, self.c_tris), (tri32s, self.c_tri32s), (tri32i, self.c_tri32i), (iota160, self.c_iota160),
                             (iotap, self.c_iotap), (iotap32, self.c_iotap32)):
                self.dma("sp", dst[:], src, [], [t_c])
            cnt = sb(s2, "s_cnt", [32, 1], F32)
            cnti = sb(s2, "s_cnti", [32, 1], I32)
            nblk = sb(s2, "s_nblk", [32, 1], F32)
            nblkb = sb(s2, "s_nblkb", [32, 128], BF16)
            nblkc = sb(s2, "s_nblkc", [32, 1], BF16)
            bstart = sb(s2, "s_bstart", [128, NE], F32)
            bend = sb(s2, "s_bend", [32, 1], F32)
            cmp = sb(s2, "s_cmp", [32, NBLK], BF16)
            erep = sb(s2, "s_erep", [128, NBLK], F32)
            chg = sb(s2, "s_chg", [128, NBLK], F32)
            wf = sb(s2, "s_wf", [128, NBLK], F32)
            pos = sb(s2, "s_pos", [128, NE], F32)
            junk = sb(s2, "s_junk", [128, NE], F32)
            p4f = sb(s2, "s_p4f", [128, NT, 4], F32)
            t_s = Tok()
            pc, t_pc = self.ps[0], self.tp[0]
            for t in range(NT):
                self.mm(pc[0:32, 0:1], self.M_all[:, t, :], self.onesb[:, 0:1], t == 0, t == NT - 1, [self.t_route, self.t_const], [t_pc])
            self.ts("dve", cnt[:], pc[0:32, 0:1], 127.0, ALU.add, [t_pc], [t_s], s2=1.0 / 128.0, op1=ALU.mult)
            self.ts("dve", nblk[:], cnt[:], -0.49609375, ALU.add, [t_s], [t_s])
            self.cp("dve", cnti[:], nblk[:], [t_s], [t_s])
            self.cp("dve", nblk[:], cnti[:], [t_s], [t_s])
            self.tt("dve", cnt[:], cnt[:], nblk[:], ALU.subtract, [t_s], [t_s])
            self.ts("dve", cnt[:], cnt[:], 1.0, ALU.is_ge, [t_s], [t_s])
            self.tt("dve", nblk[:], nblk[:], cnt[:], ALU.add, [t_s], [t_s])
            self.ts("dve", nblk[:], nblk[:], 1.0, ALU.max, [t_s], [t_s])
            self.cp("dve", nblkb[:], nblk[:, 0:1].to_broadcast([32, 128]), [t_s], [t_s])
            self.cp("dve", nblkc[:], nblk[:], [t_s], [t_s])
            p1, t_p1 = self.ps[1], self.tp[1]
            self.mm(p1[:, 0:NE], nblkb[:], tri32s[:], True, True, [t_s, t_c], [t_p1])
            self.cp("dve", bstart[:], p1[:, 0:NE], [t_p1], [t_s])
            p2, t_p2 = self.ps[2], self.tp[2]
            self.mm(p2[0:32, 0:1], tri32i[:], nblkc[:], True, True, [t_s, t_c], [t_p2])
            self.cp("dve", bend[:], p2[0:32, 0:1], [t_p2], [t_s])
            self.ts("dve", cmp[:], iota160[:], bend[:, 0:1], ALU.is_ge, [t_s, t_c], [t_s])
            p3, t_p3 = self.ps[3], self.tp[3]
            self.mm(p3[:, 0:NBLK], self.onesb[0:32, :], cmp[:], True, True, [t_s, self.t_const], [t_p3])
            self.ts("dve", erep[:], p3[:, 0:NBLK], float(NE - 1), ALU.min, [t_p3], [t_s])
            self.memset("dve", chg[:, 0:1], 1.0, [t_s])
            self.tt("dve", chg[:, 1:NBLK], erep[:, 1:NBLK], erep[:, 0:NBLK - 1], ALU.not_equal, [t_s], [t_s])
            self.ts("dve", wf[:], erep[:], 128.0, ALU.mult, [t_s, t_c], [t_s], s2=iotap[:, 0:1], op1=ALU.add)
            self.ts("dve", chg[:], chg[:], -BIGIDX, ALU.mult, [t_s], [t_s], s2=BIGIDX, op1=ALU.add)
            self.tt("dve", wf[:], wf[:], chg[:], ALU.add, [t_s], [t_s])
            if l > 0:
                self.ts("dve", wf[:], wf[:], float(l * NE * 128), ALU.add, [t_s], [t_s])
            self.cp("dve", self.widx[:], wf[:], [t_s], [self.t_widx])
            self.ts("dve", self.OHall[:], erep[0:64, :], iotap32[0:64, 0:1], ALU.is_equal, [t_s, t_c], [self.t_widx])
            for t in range(NT):
                pr_, t_pr = self.ps[4 + t % 2], self.tp[4 + t % 2]
                self.mm(pr_[:, 0:NE], tris[:], self.M_all[:, t, :], True, t == 0, [t_c, self.t_route], [t_pr])
                for t2 in range(t):
                    self.mm(pr_[:, 0:NE], self.onesb[:], self.M_all[:, t2, :], False, t2 == t - 1, [self.t_const, self.t_route], [t_pr])
                self.stt(pos[:], bstart[:], 128.0, pr_[:, 0:NE], ALU.mult, ALU.add, [t_s, t_pr], [t_s])
                for k in range(4):
                    self.stt(junk[:], self.logits_all[:, t, :], self.max8_all[:, t, k:k + 1], pos[:], ALU.is_equal, ALU.mult,
                             [self.t_route, t_s], [t_s], accum=p4f[:, t, k:k + 1])
            self.cp("dve", self.pos4_all[:], p4f[:], [t_s], [self.t_pos4])
            self.P.barrier()
        with ExitStack() as s3:
            hr = Ring([sb(s3, "s_h2%d" % i, [128, D], BF16) for i in range(3)])
            for t in range(NT):
                hb, t_hb = hr.next()
                self.dma("sp", hb[:], self.h2_d[t * 128:(t + 1) * 128, :], [self.t_h2d], [t_hb])
                for k in range(4):
                    self.scatter(self.Xs_d, hb[:], self.pos4_all[:, t, k:k + 1], [t_hb, self.t_pos4], [self.t_Xs], NBLK * 128 - 1)
            self.P.barrier()

    def phase_experts(self, l):
        with ExitStack() as st:
            sb = self.sb
            wgu = sb(st, "e_wgu", [128, 8 * 2 * D], BF16)
            wdn = sb(st, "e_wdn", [128, 8 * D], BF16)
            bgu = sb(st, "e_bgu", [64, 2 * D], BF16)
            bdn = sb(st, "e_bdn", [64, D], BF16)
            bf = sb(st, "e_bf", [64, 3 * D], F32)
            bt = sb(st, "e_bt", [64, 3 * D], F32)
            t_wgu, t_wdn, t_b = Tok(), Tok(), Tok()
            for half in range(2):
                self.dma("sp", bf[half * 32:(half + 1) * 32, 0:2 * D], self.b_gu[l], [], [t_b])
                self.dma("sp", bf[half * 32:(half + 1) * 32, 2 * D:3 * D], self.b_dn[l], [], [t_b])
            self.cp("dve", bgu[0:32, :], bf[0:32, 0:2 * D], [t_b], [t_b])
            self.cp("dve", bdn[0:32, :], bf[0:32, 2 * D:3 * D], [t_b], [t_b])
            self.cp("dve", bgu[32:64, :], bf[32:64, 0:2 * D], [t_b], [t_b])
            self.cp("dve", bdn[32:64, :], bf[32:64, 2 * D:3 * D], [t_b], [t_b])
            self.cp("dve", bt[32:64, 0:2 * D], bgu[32:64, :], [t_b], [t_b])
            self.cp("dve", bt[32:64, 2 * D:3 * D], bdn[32:64, :], [t_b], [t_b])
            self.tt("dve", bt[32:64, :], bf[32:64, :], bt[32:64, :], ALU.subtract, [t_b], [t_b])
            self.cp("dve", bgu[32:64, :], bt[32:64, 0:2 * D], [t_b], [t_b])
            self.cp("dve", bdn[32:64, :], bt[32:64, 2 * D:3 * D], [t_b], [t_b])
            wgu_v = self.w_gu.rearrange("l e (p kc) n -> (l e p) (kc n)", kc=8)
            wdn_v = self.w_dn.rearrange("l e (p kc) n -> (l e p) (kc n)", kc=8)
            R = 3
            xb_ = [(sb(st, "e_x%d" % i, [128, D], BF16), Tok()) for i in range(R)]
            xT_ = [(sb(st, "e_xT%d" % i, [128, 8, 128], BF16), Tok()) for i in range(R)]
            oh_ = [(sb(st, "e_oh%d" % i, [64, 128], BF16), Tok()) for i in range(R)]
            gc_ = [(sb(st, "e_gc%d" % i, [128, 512], F32), Tok()) for i in range(2)]
            sg_ = [(sb(st, "e_sg%d" % i, [128, 512], F32), Tok()) for i in range(2)]
            lc_ = [(sb(st, "e_lc%d" % i, [128, 512], F32), Tok()) for i in range(2)]
            ac_ = [(sb(st, "e_ac%d" % i, [128, D], BF16), Tok()) for i in range(2)]
            aT_ = [(sb(st, "e_aT%d" % i, [128, 8, 128], BF16), Tok()) for i in range(2)]
            out_ = [(sb(st, "e_out%d" % i, [128, D], F32), Tok()) for i in range(2)]
            pX = self.ps[6][:].bitcast(BF16).rearrange("p (a b) -> p a b", a=8)
            pA = self.ps[7][:].bitcast(BF16).rearrange("p (a b) -> p a b", a=8)
            N = NBLK
            bound = 2 * NE * 128 - 1

            def Wgu(j):
                self.gather(wgu[:], wgu_v, self.widx[:, j:j + 1], [self.t_widx], [t_wgu], bound)

            def Wdn(j):
                self.gather(wdn[:], wdn_v, self.widx[:, j:j + 1], [self.t_widx], [t_wdn], bound)

            def Ax(j):
                xb, t_xb = xb_[j % R]
                self.dma("sp", xb[:], self.Xs_d[j * 128:(j + 1) * 128, :], [self.t_Xs], [t_xb])
                oh, t_oh = oh_[j % R]
                self.cp("dve", oh[:], self.OHall[:, j:j + 1].to_broadcast([64, 128]), [self.t_widx], [t_oh])
                xv = xb[:].rearrange("p (a kc) -> p kc a", kc=8)
                for kc in range(8):
                    self.tr(pX[:, kc, :], xv[:, kc, :], self.identb[:], [t_xb, self.t_const], [self.tp[6]])
                xT, t_xT = xT_[j % R]
                self.cp("act", xT[:], pX, [self.tp[6]], [t_xT])

            def Bh(j, hf):
                xT, t_xT = xT_[j % R]
                oh, t_oh = oh_[j % R]
                for nb in (hf, 2 + hf):
                    ps, t_ps = self.ps[nb], self.tp[nb]
                    for kc in range(8):
                        self.mm(ps[:], xT[:, kc, :], wgu[:, kc * 2048 + nb * 512:kc * 2048 + (nb + 1) * 512], kc == 0, False,
                                [t_xT, t_wgu], [t_ps])
                    self.mm(ps[:], oh[:], bgu[:, nb * 512:(nb + 1) * 512], False, True, [t_oh, t_b], [t_ps])

            def E(j, hf):
                gc, t_gc = gc_[hf]
                sg, t_sg = sg_[hf]
                lc, t_lc = lc_[hf]
                ac, t_ac = ac_[j % 2]
                self.ts("dve", gc[:], self.ps[hf][:], 7.0, ALU.min, [self.tp[hf]], [t_gc])
                self.act(sg[:], gc[:], AF.Sigmoid, [t_gc], [t_sg], scale=1.702)
                self.ts("dve", lc[:], self.ps[2 + hf][:], 7.0, ALU.min, [self.tp[2 + hf]], [t_lc], s2=-7.0, op1=ALU.max)
                self.stt(lc[:], lc[:], 1.0, gc[:], ALU.add, ALU.mult, [t_lc, t_gc], [t_lc])
                self.tt("dve", ac[:, hf * 512:(hf + 1) * 512], lc[:], sg[:], ALU.mult, [t_lc, t_sg], [t_ac])

            def Ca(j):
                ac, t_ac = ac_[j % 2]
                av = ac[:].rearrange("p (a kc) -> p kc a", kc=8)
                for kc in range(8):
                    self.tr(pA[:, kc, :], av[:, kc, :], self.identb[:], [t_ac, self.t_const], [self.tp[7]])
                aT, t_aT = aT_[j % 2]
                self.cp("act", aT[:], pA, [self.tp[7]], [t_aT])

            def Cb(j):
                oh, t_oh = oh_[j % R]
                aT, t_aT = aT_[j % 2]
                ot, t_ot = out_[j % 2]
                for hf in range(2):
                    ps, t_ps = self.ps[4 + hf], self.tp[4 + hf]
                    for kc in range(8):
                        self.mm(ps[:], aT[:, kc, :], wdn[:, kc * 1024 + hf * 512:kc * 1024 + (hf + 1) * 512], kc == 0, False,
                                [t_aT, t_wdn], [t_ps])
                    self.mm(ps[:], oh[:], bdn[:, hf * 512:(hf + 1) * 512], False, True, [t_oh, t_b], [t_ps])
                    self.cp("act" if hf else "dve", ot[:, hf * 512:(hf + 1) * 512], ps[:], [t_ps], [t_ot])
                self.dma("sp", self.Out_d[j * 128:(j + 1) * 128, :], ot[:], [t_ot], [self.t_Outd])

            Wgu(0)
            Wdn(0)
            Ax(0)
            Ax(1)
            Bh(0, 0)
            E(0, 0)
            Bh(0, 1)
            E(0, 1)
            Wgu(1)
            for j in range(N):
                if j + 1 < N:
                    Bh(j + 1, 0)
                if j + 2 < N:
                    Ax(j + 2)
                if j + 1 < N:
                    E(j + 1, 0)
                Ca(j)
                if j + 1 < N:
                    Bh(j + 1, 1)
                if j + 2 < N:
                    Wgu(j + 2)
                if j + 1 < N:
                    E(j + 1, 1)
                Cb(j)
                if j + 1 < N:
                    Wdn(j + 1)

    def phase_combine(self, l, last):
        with ExitStack() as st:
            sb = self.sb
            self.load_mod(st, [5])
            gr = Ring([sb(st, "c_g%d" % i, [128, D], F32) for i in range(4)])
            xr = Ring([sb(st, "c_x%d" % i, [128, D], F32) for i in range(2)])
            yr = Ring([sb(st, "c_y%d" % i, [128, D], F32) for i in range(2)])
            fg = sb(st, "c_fg", [128, D], F32)
            junk = sb(st, "c_junk", [128, D], F32)
            ss = sb(st, "c_ss", [128, 2], F32)
            t_fg, t_w = Tok(), Tok()
            if last:
                self.dma("sp", fg[:], self.fng.rearrange("(o d) -> o d", o=1).partition_broadcast(128), [], [t_fg])
            for t in range(NT):
                xt, t_x = xr.next()
                y, t_y = yr.next()
                self.dma("sp", xt[:], self.xs_d[t * 128:(t + 1) * 128, :], [self.t_xs], [t_x])
                for k in range(4):
                    g, t_g = gr.next()
                    self.gather(g[:], self.Out_d, self.pos4_all[:, t, k:k + 1], [self.t_Outd, self.t_pos4], [t_g], None)
                    if k == 0:
                        self.ts("dve", y[:], g[:], self.g4_all[:, t, 0:1], ALU.mult, [t_g, self.t_route], [t_y])
                    else:
                        self.stt(y[:], g[:], self.g4_all[:, t, k:k + 1], y[:], ALU.mult, ALU.add, [t_g, self.t_route, t_y], [t_y])
                self.tt("dve", y[:], y[:], self.modr[5], ALU.mult, [t_y, self.t_modr], [t_y])
                self.tt("pool", y[:], y[:], xt[:], ALU.add, [t_y, t_x], [t_y])
                if not last:
                    self.dma("sp", self.xs_d[t * 128:(t + 1) * 128, :], y[:], [t_y], [self.t_xs])
                else:
                    self.stt(junk[:], y[:], 1.0, y[:], ALU.mult, ALU.mult, [t_y], [t_w], accum=ss[:, 0:1])
                    self.rstd_from_ss(ss[:, 1:2], ss[:, 0:1], D, t_w, t_w)
                    self.stt(y[:], y[:], ss[:, 1:2], fg[:], ALU.mult, ALU.mult, [t_y, t_w, t_fg], [t_y])
                    self.dma("sp", self.out[t * 128:(t + 1) * 128, :], y[:], [t_y], [self.t_out])


def _t5_bucket(rel):
    nb = 16
    max_exact = 8
    ret = np.where(rel > 0, nb, 0)
    n = np.abs(rel)
    nf = np.maximum(n, 1).astype(np.float32)
    large = max_exact + (np.log(nf / max_exact) / math.log(1024 / max_exact) * (nb - max_exact)).astype(np.int32)
    large = np.minimum(large, nb - 1)
    return ret + np.where(n < max_exact, n, large)


def host_consts(rel_bias):
    bf = ml_dtypes.bfloat16
    c = {}
    c["identb"] = np.eye(128, dtype=np.float32).astype(bf)
    c["identf"] = np.eye(128, dtype=np.float32)
    tok = np.arange(S)
    row = (tok // 64).astype(np.float32)
    col = (tok % 64).astype(np.float32)
    inv = (10000.0 ** (-np.arange(0, 32, 2, dtype=np.float32) / 32)).astype(np.float32)
    ar = row[:, None] * inv
    ac = col[:, None] * inv
    cosT = np.concatenate([np.cos(ar), np.cos(ar), np.cos(ac), np.cos(ac)], 1).astype(np.float32)
    sinT = np.concatenate([-np.sin(ar), np.sin(ar), -np.sin(ac), np.sin(ac)], 1).astype(np.float32)
    c["cosT"] = np.ascontiguousarray(cosT.reshape(NT, 128, 64).transpose(1, 0, 2))
    c["sinT"] = np.ascontiguousarray(sinT.reshape(NT, 128, 64).transpose(1, 0, 2))
    k = np.arange(128)
    c["tri_s"] = (k[:, None] < k[None, :]).astype(np.float32).astype(bf)
    k32 = np.arange(32)
    c["tri32s"] = (k32[:, None] < k32[None, :]).astype(np.float32).astype(bf)
    c["tri32i"] = (k32[:, None] <= k32[None, :]).astype(np.float32).astype(bf)
    c["iota160"] = np.broadcast_to(np.arange(NBLK, dtype=np.float32), (32, NBLK)).copy()
    c["iotap"] = np.arange(128, dtype=np.float32).reshape(128, 1)
    c["iotap32"] = (np.arange(128) % 32).astype(np.float32).reshape(128, 1)
    q = np.arange(128)[:, None]
    cc = np.arange(384)[None, :]
    rel = cc - 128 - q
    wb = np.full((128, 12, 384), -30000.0, np.float32)
    valid = np.abs(rel) <= 64
    for a, dl in enumerate(A_DIL):
        bkt = _t5_bucket(rel * dl)
        for h in range(4):
            vals = rel_bias[bkt, a * 4 + h]
            wb[:, a * 4 + h, :] = np.where(valid, vals, np.float32(-30000.0))
    c["wbias"] = wb
    return c


_CACHE = {}


def kernel(**inputs):
    inp = {k: np.ascontiguousarray(np.asarray(v, dtype=np.float32)) for k, v in inputs.items()}
    if "nc" not in _CACHE:
        _CACHE["nc"] = Builder(nlayers=2).build()
    nc = _CACHE["nc"]
    consts = host_consts(inp["rel_bias"])
    shared = {k: inp[k] for k in ("w_ada", "b_ada", "norm1_g", "w_in", "q_norm_g", "k_norm_g", "w_br_a", "w_br_b", "w_out",
                                   "norm2_g", "w_router", "b_router", "w_gate_up", "b_gate_up", "w_down", "b_down", "final_norm_g")}
    in_maps = []
    for b in range(8):
        m = dict(shared)
        m.update(consts)
        m["x"] = inp["x"][b]
        m["ccol"] = np.ascontiguousarray(inp["c"][b].reshape(8, 128).T)
        in_maps.append(m)
    res = run_bass_kernel_spmd(nc, in_maps, core_ids=list(range(8)))
    return np.stack([np.asarray(res.results[b]["out"], dtype=np.float32) for b in range(8)], 0)
```

```python
import math
from contextlib import ExitStack

import numpy as np
import ml_dtypes
import concourse.bass as bass
import concourse.mybir as mybir
from concourse.bass_utils import run_bass_kernel_spmd

F32 = mybir.dt.float32
BF16 = mybir.dt.bfloat16
I32 = mybir.dt.int32
ALU = mybir.AluOpType
AF = mybir.ActivationFunctionType
AX = mybir.AxisListType

S = 4096
D = 1024
NT = 32
NE = 32
NBLK = 160
EPS = 1e-6
A_DIL = (1, 4, 16)
BIGIDX = 1.0e6


def sl(start, n, step):
    return slice(start, start + (n - 1) * step + 1, step)

ENGS = ["pe", "act", "dve", "pool", "sp"]


class Tok:
    __slots__ = ("w", "r")

    def __init__(self):
        self.w = None
        self.r = []


class Prog:
    N_DMA_SEMS = {"sp": 24, "pool": 16, "act": 8}

    def __init__(self, nc):
        self.nc = nc
        self.q = {e: [] for e in ENGS}
        self.cnt = {e: 0 for e in ENGS}
        self.seen = {e: {} for e in ENGS}
        self.dma_val = {}
        self.dma_rr = {e: 0 for e in ENGS}
        self.prologue = {}

    def _collect(self, eng, reads, writes):
        deps = {}

        def add(d):
            if d is not None and deps.get(d[0], 0) < d[1]:
                deps[d[0]] = d[1]

        for t in reads:
            add(t.w)
        for t in writes:
            add(t.w)
            for d in t.r:
                add(d)
        out = []
        own = "E:" + eng
        seen = self.seen[eng]
        for k, v in deps.items():
            if k == own and eng == "pe":
                continue
            if seen.get(k, 0) >= v:
                continue
            seen[k] = v
            out.append((k, v))
        return out

    def _note(self, my, reads, writes):
        for t in reads:
            t.r.append(my)
            if len(t.r) > 48:
                d = {}
                for k, v in t.r:
                    if d.get(k, 0) < v:
                        d[k] = v
                t.r = list(d.items())
        for t in writes:
            t.w = my
            t.r = []

    def op(self, eng, fn, reads=(), writes=()):
        waits = self._collect(eng, reads, writes)
        self.cnt[eng] += 1
        my = ("E:" + eng, self.cnt[eng])
        self._note(my, reads, writes)
        self.q[eng].append((waits, fn, my, 1))

    def dma(self, eng, fn, reads=(), writes=()):
        waits = self._collect(eng, reads, writes)
        n = self.N_DMA_SEMS[eng]
        slot = self.dma_rr[eng] % n
        self.dma_rr[eng] += 1
        key = "D:%s:%d" % (eng, slot)
        prev = self.dma_val.get(key, 0)
        if prev > 0 and self.seen[eng].get(key, 0) < prev:
            self.seen[eng][key] = prev
            waits.append((key, prev))
        self.dma_val[key] = prev + 16
        my = (key, prev + 16)
        self._note(my, reads, writes)
        self.q[eng].append((waits, fn, my, 16))

    def barrier(self):
        for eng in ENGS:
            waits = []
            for key, v in self.dma_val.items():
                if self.seen[eng].get(key, 0) < v:
                    waits.append((key, v))
                    self.seen[eng][key] = v
            for e in ENGS:
                if e == eng or self.cnt[e] == 0:
                    continue
                k = "E:" + e
                if self.seen[eng].get(k, 0) < self.cnt[e]:
                    waits.append((k, self.cnt[e]))
                    self.seen[eng][k] = self.cnt[e]
            if waits:
                self.q[eng].append((waits, None, None, 0))

    def emit(self):
        nc = self.nc
        keys = set()
        for e in ENGS:
            for waits, fn, my, inc in self.q[e]:
                for k, v in waits:
                    keys.add(k)
                if my is not None:
                    keys.add(my[0])
        keys = sorted(keys)
        with ExitStack() as st:
            sems = {k: st.enter_context(nc.semaphore(k.replace(":", "_"))) for k in keys}
            block = st.enter_context(nc.Block())
            hmap = {"pe": block.tensor, "act": block.scalar, "dve": block.vector,
                    "pool": block.gpsimd, "sp": block.sync}

            def make(e):
                def body(engh):
                    if e in self.prologue:
                        self.prologue[e](engh)
                    for waits, fn, my, inc in self.q[e]:
                        for k, v in waits:
                            engh.wait_ge(sems[k], v)
                        if fn is None:
                            continue
                        fn(engh).then_inc(sems[my[0]], inc)
                return body

            for e in ENGS:
                if self.q[e]:
                    hmap[e](make(e))


class Ring:
    def __init__(self, items):
        self.items = [(it, Tok()) for it in items]
        self.i = 0

    def next(self):
        it = self.items[self.i % len(self.items)]
        self.i += 1
        return it


class Builder:
    def __init__(self, nlayers=2, stop=None, dbg=(), small_moe=False):
        self.nlayers = nlayers
        self.stop = stop
        self.dbg = dbg
        NEW = 1 if small_moe else NE
        nc = self.nc = bass.Bass("TRN2", target_bir_lowering=False)
        self.P = Prog(nc)
        self.outs = ["out"]
        di = lambda n, s, d=F32: nc.dram_tensor(n, s, d, kind="ExternalInput").ap()
        self.x_in = di("x", [S, D])
        self.ccol = di("ccol", [128, 8])
        self.w_ada = di("w_ada", [2, D, 6 * D])
        self.b_ada = di("b_ada", [2, 6 * D])
        self.norm1_g = di("norm1_g", [2, D])
        self.w_in = di("w_in", [2, D, 5888])
        self.q_norm_g = di("q_norm_g", [2, 64])
        self.k_norm_g = di("k_norm_g", [2, 64])
        self.wbias = di("wbias", [128, 12, 384])
        self.w_br_a = di("w_br_a", [2, 256, D])
        self.w_br_b = di("w_br_b", [2, D, D])
        self.w_out = di("w_out", [2, D, D])
        self.norm2_g = di("norm2_g", [2, D])
        self.w_router = di("w_router", [2, D, NE])
        self.b_router = di("b_router", [2, NE])
        self.w_gu = di("w_gate_up", [2, NEW, D, 2 * D])
        self.b_gu = di("b_gate_up", [2, NE, 2 * D])
        self.w_dn = di("w_down", [2, NEW, D, D])
        self.b_dn = di("b_down", [2, NE, D])
        self.fng = di("final_norm_g", [D])
        self.c_identb = di("identb", [128, 128], BF16)
        self.c_identf = di("identf", [128, 128])
        self.c_cos = di("cosT", [128, NT, 64])
        self.c_sin = di("sinT", [128, NT, 64])
        self.c_tris = di("tri_s", [128, 128], BF16)
        self.c_tri32s = di("tri32s", [32, 32], BF16)
        self.c_tri32i = di("tri32i", [32, 32], BF16)
        self.c_iota160 = di("iota160", [32, NBLK])
        self.c_iotap = di("iotap", [128, 1])
        self.c_iotap32 = di("iotap32", [128, 1])
        self.out = nc.dram_tensor("out", [S, D], F32, kind="ExternalOutput").ap()
        self.xs_d = self.scratch("xs_d", [S, D], F32)
        self.obT_d = self.scratch("obT_d", [D, S], BF16)
        self.gT_d = self.scratch("gT_d", [2 * D, S], BF16)
        self.oaw_d = self.scratch("oaw_d", [3, S, 264], F32)
        self.h2_d = self.scratch("h2_d", [S, D], BF16)
        self.Xs_d = self.scratch("Xs_d", [NBLK * 128, D], BF16)
        self.Out_d = self.scratch("Out_d", [NBLK * 128, D], F32)
        self.modr_d = self.scratch("modr_d", [128, 6, D], F32)
        self.t_modrd = Tok()
        self.t_xs, self.t_obT, self.t_gT, self.t_oaw, self.t_h2d, self.t_Xs, self.t_Outd = [Tok() for _ in range(7)]
        self.t_out = Tok()

    def scratch(self, name, shape, dt):
        if name in self.dbg:
            self.outs.append(name)
            return self.nc.dram_tensor(name, shape, dt, kind="ExternalOutput").ap()
        return self.nc.dram_tensor(name, shape, dt, kind="Internal").ap()

    def sb(self, st, name, shape, dt):
        self._nsb = getattr(self, "_nsb", 0) + 1
        return st.enter_context(self.nc.sbuf_tensor("sb%d_%s" % (self._nsb, name), shape, dt))

    def tt(self, eng, out, in0, in1, op, r, w):
        self.P.op(eng, lambda e: e.tensor_tensor(out=out, in0=in0, in1=in1, op=op), r, w)

    def ts(self, eng, out, in0, s1, op0, r, w, s2=None, op1=None):
        if op1 is None:
            self.P.op(eng, lambda e: e.tensor_scalar(out=out, in0=in0, scalar1=s1, scalar2=None, op0=op0), r, w)
        else:
            self.P.op(eng, lambda e: e.tensor_scalar(out=out, in0=in0, scalar1=s1, scalar2=s2, op0=op0, op1=op1), r, w)

    def stt(self, out, in0, scalar, in1, op0, op1, r, w, accum=None):
        if accum is None:
            self.P.op("dve", lambda e: e.scalar_tensor_tensor(out=out, in0=in0, scalar=scalar, in1=in1, op0=op0, op1=op1), r, w)
        else:
            self.P.op("dve", lambda e: e.scalar_tensor_tensor(out=out, in0=in0, scalar=scalar, in1=in1, op0=op0, op1=op1,
                                                              accum_out=accum), r, w)

    def act(self, out, in_, func, r, w, bias=None, scale=None, accum=None):
        kw = {}
        if bias is not None:
            kw["bias"] = bias
        if scale is not None:
            kw["scale"] = scale
        if accum is not None:
            kw["accum_out"] = accum
        self.P.op("act", lambda e: e.activation(out=out, in_=in_, func=func, **kw), r, w)

    def cp(self, eng, out, in_, r, w):
        if eng == "act":
            self.P.op("act", lambda e: e.copy(out=out, in_=in_), r, w)
        else:
            self.P.op(eng, lambda e: e.tensor_copy(out=out, in_=in_), r, w)

    def mm(self, out, lhsT, rhs, start, stop, r, w):
        self.P.op("pe", lambda e: e.matmul(out, lhsT=lhsT, rhs=rhs, start=start, stop=stop), r, w)

    def tr(self, out, in_, ident, r, w):
        self.P.op("pe", lambda e: e.transpose(out=out, in_=in_, identity=ident), r, w)

    def dma(self, q, out, in_, r, w):
        self.P.dma(q, lambda e: e.dma_start(out=out, in_=in_), r, w)

    def red(self, out, in_, op, r, w):
        self.P.op("dve", lambda e: e.tensor_reduce(out=out, in_=in_, axis=AX.X, op=op), r, w)

    def recip(self, out, in_, r, w):
        self.P.op("dve", lambda e: e.reciprocal(out=out, in_=in_), r, w)

    def memset(self, eng, ap, val, w):
        self.P.op(eng, lambda e: e.memset(ap, val), (), w)

    def gather(self, out, in_, idx, r, w, bound):
        if bound is None:
            self.P.dma("pool", lambda e: e.indirect_dma_start(out=out, out_offset=None, in_=in_,
                                                              in_offset=bass.IndirectOffsetOnAxis(ap=idx, axis=0)), r, w)
            return
        if "pool" not in self.P.prologue:
            def pro(e):
                self.breg = e.alloc_register("bound_reg")
                e.reg_mov(self.breg, bound)
            self.P.prologue["pool"] = pro
            self.bound_val = bound
        assert bound == self.bound_val
        self.P.dma("pool", lambda e: e.indirect_dma_start(out=out, out_offset=None, in_=in_,
                                                          in_offset=bass.IndirectOffsetOnAxis(ap=idx, axis=0),
                                                          bounds_check=self.breg, oob_is_err=False), r, w)

    def scatter(self, out, in_, idx, r, w, bound):
        self.P.dma("pool", lambda e: e.indirect_dma_start(out=out, out_offset=bass.IndirectOffsetOnAxis(ap=idx, axis=0),
                                                          in_=in_, in_offset=None), r, w)

    def build(self):
        nc = self.nc
        with ExitStack() as gst:
            self.gst = gst
            self.ps = [gst.enter_context(nc.psum_tensor("ps%d" % i, [128, 512], F32)) for i in range(8)]
            self.tp = [Tok() for _ in range(8)]
            sb = self.sb
            self.identb = sb(gst, "identb", [128, 128], BF16)
            self.identf = sb(gst, "identf", [128, 128], F32)
            self.onesb = sb(gst, "onesb", [128, 128], BF16)
            self.onesf = sb(gst, "onesf", [128, 128], F32)
            self.epsc = sb(gst, "epsc", [128, 1], F32)
            self.t_const = Tok()
            tc = [self.t_const]
            self.dma("sp", self.identb[:], self.c_identb, [], tc)
            self.dma("sp", self.identf[:], self.c_identf, [], tc)
            self.memset("dve", self.onesb[:], 1.0, tc)
            self.memset("dve", self.onesf[:], 1.0, tc)
            self.memset("dve", self.epsc[:], EPS, tc)
            self.neghalf = sb(gst, "neghalf", [128, 16], F32)
            self.memset("dve", self.neghalf[:], -0.5, tc)
            self.P.barrier()
            xsrc = self.x_in
            for l in range(self.nlayers):
                last = (l == self.nlayers - 1)
                if self.layer(l, xsrc, last):
                    break
                xsrc = self.xs_d
            self.P.barrier()
            self.P.emit()
        return nc

    def layer(self, l, xsrc, last):
        P = self.P
        stop = self.stop if l == self.nlayers - 1 else None
        with ExitStack() as lst:
            self.oaT = self.sb(lst, "oaT", [128, 2, S], BF16)
            self.t_oaT = Tok()
            self.phase_mod(l)
            P.barrier()
            if stop == "mod":
                return True
            with ExitStack() as ast:
                self.hT = self.sb(ast, "hT", [128, 8, S], BF16)
                self.t_hT = Tok()
                self.phase_norm1(l, xsrc)
                P.barrier()
                if stop == "norm1":
                    return True
                self.phase_gates(l)
                P.barrier()
                if stop == "gates":
                    return True
                self.phase_window(l)
                P.barrier()
                if stop == "window":
                    return True
                self.phase_gqa(l)
                P.barrier()
                if stop == "gqa":
                    return True
            with ExitStack() as mst:
                self.logits_all = self.sb(mst, "logits_all", [128, NT, NE], F32)
                self.max8_all = self.sb(mst, "max8_all", [128, NT, 8], F32)
                self.g4_all = self.sb(mst, "g4_all", [128, NT, 4], F32)
                self.M_all = self.sb(mst, "M_all", [128, NT, NE], BF16)
                self.pos4_all = self.sb(mst, "pos4_all", [128, NT, 4], I32)
                self.widx = self.sb(mst, "widx", [128, NBLK], I32)
                self.OHall = self.sb(mst, "OHall", [64, NBLK], BF16)
                self.t_widx = Tok()
                self.t_route = Tok()
                self.t_pos4 = Tok()
                self.phase_merge(l, xsrc)
                P.barrier()
                if stop == "merge":
                    return True
                self.phase_slots(l)
                P.barrier()
                if stop == "slots":
                    return True
                self.phase_experts(l)
                P.barrier()
                if stop == "experts":
                    return True
                self.phase_combine(l, last)
                P.barrier()
        return False

    def phase_mod(self, l):
        with ExitStack() as st:
            sb = self.sb
            cc = sb(st, "cc", [128, 8], F32)
            cs = sb(st, "cs", [128, 8], F32)
            crep = sb(st, "crep", [128, 8, 128], F32)
            brep = sb(st, "brep", [128, 6 * D], F32)
            ng = sb(st, "ng", [128, 2, D], F32)
            modr = sb(st, "modr", [128, 6, D], F32)
            t_modr = Tok()
            wa = Ring([sb(st, "wa%d" % i, [128, 8, 512], F32) for i in range(2)])
            t_cc, t_cs, t_crep, t_brep, t_ng = Tok(), Tok(), Tok(), Tok(), Tok()
            self.dma("sp", cc[:], self.ccol, [], [t_cc])
            self.dma("sp", brep[:], self.b_ada[l:l + 1, :].partition_broadcast(128), [], [t_brep])
            self.dma("sp", ng[:, 0, :], self.norm1_g[l:l + 1, :].partition_broadcast(128), [], [t_ng])
            self.dma("sp", ng[:, 1, :], self.norm2_g[l:l + 1, :].partition_broadcast(128), [], [t_ng])
            self.act(cs[:], cc[:], AF.Silu, [t_cc], [t_cs])
            for kc in range(8):
                self.cp("dve", crep[:, kc, :], cs[:, kc:kc + 1].to_broadcast([128, 128]), [t_cs], [t_crep])
            modf = modr[:].rearrange("p a d -> p (a d)")
            psr = Ring(self.ps[0:2])
            psr.items = [(self.ps[0], self.tp[0]), (self.ps[1], self.tp[1])]
            for ch in range(12):
                w, t_w = wa.next()
                self.dma("sp", w[:], self.w_ada[l][:, ch * 512:(ch + 1) * 512].rearrange("(kc p) n -> p kc n", p=128), [], [t_w])
                ps, t_ps = psr.next()
                for kc in range(8):
                    self.mm(ps[:], crep[:, kc, :], w[:, kc, :], kc == 0, kc == 7, [t_crep, t_w], [t_ps])
                self.tt("dve", modf[:, ch * 512:(ch + 1) * 512], ps[:], brep[:, ch * 512:(ch + 1) * 512], ALU.add,
                        [t_ps, t_brep], [t_modr])
            for i, j in ((1, 0), (4, 1)):
                self.stt(modr[:, i, :], modr[:, i, :], 1.0, ng[:, j, :], ALU.add, ALU.mult, [t_modr, t_ng], [t_modr])
            self.dma("sp", self.modr_d, modr[:], [t_modr], [self.t_modrd])

    def load_mod(self, st, idxs):
        tl = self.sb(st, "modl", [128, len(idxs), D], F32)
        self.t_modr = Tok()
        self.modr = {}
        for n, i in enumerate(idxs):
            self.dma("sp", tl[:, n, :], self.modr_d[:, i, :], [self.t_modrd], [self.t_modr])
            self.modr[i] = tl[:, n, :]

    def rstd_from_ss(self, rstd, ss, n, t_in, t_out):
        self.act(rstd, ss, AF.Ln, [t_in, self.t_const], [t_out], bias=self.epsc[0:rstd.shape[0], 0:1], scale=1.0 / n)
        self.act(rstd, rstd, AF.Exp, [t_out], [t_out], scale=-0.5)

    def norm_mod(self, xt, t_x, i_g, i_sh, work, hb, t_hb, hf=None):
        junk, ss, rstd, tmp, t_w = work
        self.stt(junk[:], xt[:], 1.0, xt[:], ALU.mult, ALU.mult, [t_x], [t_w], accum=ss[:, 0:1])
        self.rstd_from_ss(rstd[:, 0:1], ss[:, 0:1], D, t_w, t_w)
        self.stt(tmp[:], xt[:], rstd[:, 0:1], self.modr[i_g], ALU.mult, ALU.mult, [t_x, t_w, self.t_modr], [t_w])
        if hf is not None:
            self.tt("dve", hf[:], tmp[:], self.modr[i_sh], ALU.add, [t_w, self.t_modr], [t_hb])
            self.cp("pool", hb[:], hf[:], [t_hb], [t_hb])
        else:
            self.tt("dve", hb[:], tmp[:], self.modr[i_sh], ALU.add, [t_w, self.t_modr], [t_hb])

    def phase_norm1(self, l, xsrc):
        with ExitStack() as st:
            sb = self.sb
            self.load_mod(st, [0, 1])
            xr = Ring([sb(st, "xt%d" % i, [128, D], F32) for i in range(2)])
            hr = Ring([sb(st, "hb%d" % i, [128, D], BF16) for i in range(2)])
            work = (sb(st, "junk", [128, D], F32), sb(st, "ss", [128, 1], F32), sb(st, "rstd", [128, 1], F32),
                    sb(st, "tmp", [128, D], F32), Tok())
            psb = [self.ps[i][:].bitcast(BF16).rearrange("p (a b) -> p a b", a=8) for i in range(2)]
            for t in range(NT):
                xt, t_x = xr.next()
                hb, t_hb = hr.next()
                self.dma("sp", xt[:], xsrc[t * 128:(t + 1) * 128, :], [self.t_xs], [t_x])
                self.norm_mod(xt, t_x, 1, 0, work, hb, t_hb)
                pT, t_p = psb[t % 2], self.tp[t % 2]
                for kc in range(8):
                    self.tr(pT[:, kc, :], hb[:, kc * 128:(kc + 1) * 128], self.identb[:], [t_hb, self.t_const], [t_p])
                self.cp("act", self.hT[:, :, t * 128:(t + 1) * 128], pT, [t_p], [self.t_hT])

    def load_w(self, w, src, t_w, kc=8):
        self.dma("pool", w, src.rearrange("(kc p) n -> p kc n", p=128), [], [t_w])

    def phase_gates(self, l):
        with ExitStack() as st:
            sb = self.sb
            wg = sb(st, "wg", [128, 8, 2 * D], BF16)
            t_wg = Tok()
            for q in range(4):
                self.load_w(wg[:, :, q * 512:(q + 1) * 512], self.w_in[l][:, 3840 + q * 512:3840 + (q + 1) * 512], t_wg)
            gr = Ring([sb(st, "gsb%d" % i, [128, 512], BF16) for i in range(3)])
            n = 0
            for c in range(8):
                for m in range(16):
                    ps, t_ps = self.ps[n % 4], self.tp[n % 4]
                    n += 1
                    for kc in range(8):
                        self.mm(ps[:], wg[:, kc, m * 128:(m + 1) * 128], self.hT[:, kc, c * 512:(c + 1) * 512], kc == 0, kc == 7,
                                [t_wg, self.t_hT], [t_ps])
                    g, t_g = gr.next()
                    self.act(g[:], ps[:], AF.Sigmoid, [t_ps], [t_g])
                    self.dma("sp", self.gT_d[m * 128:(m + 1) * 128, c * 512:(c + 1) * 512], g[:], [t_g, self.t_gT], [])

    def qknorm_rope(self, src, H, grep, tile, wk, dst, t_src, t_dst):
        sq, ss, rstd, qn, t1, t2, t_w = wk
        W = H * 64
        v3 = lambda ap: ap[:, 0:W].rearrange("p (h d) -> p h d", h=H)
        v5 = lambda ap: ap[:, 0:W].rearrange("p (h a b c) -> p h a b c", h=H, a=2, b=2)
        self.tt("pool", sq[:, 0:W], src, src, ALU.mult, [t_src], [t_w])
        self.red(ss[:, 0:H], v3(sq), ALU.add, [t_w], [t_w])
        self.ts("pool", rstd[:, 0:H], ss[:, 0:H], 1.0 / 64, ALU.mult, [t_w], [t_w], s2=EPS, op1=ALU.add)
        self.tt("pool", rstd[:, 0:H], rstd[:, 0:H], self.neghalf[:, 0:H], ALU.pow, [t_w, self.t_const], [t_w])
        self.tt("dve", v3(qn), src.rearrange("p (h d) -> p h d", h=H), rstd[:, 0:H].unsqueeze(2).to_broadcast([128, H, 64]), ALU.mult,
                [t_src, t_w], [t_w])
        self.tt("pool", v3(qn), v3(qn), grep.unsqueeze(1).to_broadcast([128, H, 64]), ALU.mult, [t_w, self.t_const], [t_w])
        cosb = self.cos[:, tile, :].unsqueeze(1).to_broadcast([128, H, 64])
        sinv = self.sin[:, tile, :].rearrange("p (a b c) -> p a b c", a=2, b=2)
        self.tt("dve", v3(t1), v3(qn), cosb, ALU.mult, [t_w, self.t_const], [t_w])
        for b in range(2):
            sb_ = sinv[:, :, b, :].unsqueeze(1).to_broadcast([128, H, 2, 16])
            self.tt("pool", v5(t2)[:, :, :, b, :], v5(qn)[:, :, :, 1 - b, :], sb_, ALU.mult, [t_w, self.t_const], [t_w])
        self.tt("dve", dst, t1[:, 0:W], t2[:, 0:W], ALU.add, [t_w], [t_dst])

    def phase_gqa(self, l):
        with ExitStack() as st:
            sb = self.sb
            self.cos = sb(st, "cos", [128, NT, 64], F32)
            self.sin = sb(st, "sin", [128, NT, 64], F32)
            self.dma("sp", self.cos[:], self.c_cos, [], [self.t_const])
            self.dma("sp", self.sin[:], self.c_sin, [], [self.t_const])
            KT = sb(st, "KT", [128, 2, S], BF16)
            V = sb(st, "V", [128, NT, 4, 128], BF16)
            wkv = sb(st, "wkv", [128, 8, 512], BF16)
            gq = sb(st, "gq", [128, 64], F32)
            gk = sb(st, "gk", [128, 64], F32)
            t_KT, t_V, t_wkv = Tok(), Tok(), Tok()
            self.dma("sp", gq[:], self.q_norm_g[l:l + 1, :].partition_broadcast(128), [], [self.t_const])
            self.dma("sp", gk[:], self.k_norm_g[l:l + 1, :].partition_broadcast(128), [], [self.t_const])
            self.load_w(wkv[:], self.w_in[l][:, 3328:3840], t_wkv)
            self.memset("dve", V[:, :, :, 64:128], 1.0, [t_V])
            wk = (sb(st, "q_sq", [128, 256], F32), sb(st, "q_ss", [128, 4], F32), sb(st, "q_rstd", [128, 4], F32),
                  sb(st, "q_qn", [128, 256], F32), sb(st, "q_t1", [128, 256], F32), sb(st, "q_t2", [128, 256], F32), Tok())
            psT = self.ps[7][:].bitcast(BF16).rearrange("p (a b) -> p a b", a=8)
            t_psT = self.tp[7]
            ksr = Ring([sb(st, "ksb3_%d" % i, [128, 256], F32) for i in range(3)])
            krr = Ring([sb(st, "kr3_%d" % i, [128, 256], BF16) for i in range(3)])
            kst = {}

            def kv1(t):
                ps, t_ps = self.ps[6], self.tp[6]
                for kc in range(8):
                    self.mm(ps[:], self.hT[:, kc, t * 128:(t + 1) * 128], wkv[:, kc, :], kc == 0, kc == 7, [self.t_hT, t_wkv], [t_ps])
                self.cp("act", V[:, t, :, 0:64], ps[:, 256:512].rearrange("p (g d) -> p g d", g=4), [t_ps], [t_V])
                ksb, t_ksb = ksr.next()
                self.cp("act", ksb[:], ps[:, 0:256], [t_ps], [t_ksb])
                kr, t_kr = krr.next()
                self.qknorm_rope(ksb[:], 4, gk[:], t, wk, kr[:], t_ksb, t_kr)
                kst[t] = (kr, t_kr)

            def kv2(t):
                kr, t_kr = kst.pop(t)
                for m in range(2):
                    self.tr(psT[:, m, :], kr[:, m * 128:(m + 1) * 128], self.identb[:], [t_kr, self.t_const], [t_psT])
                self.cp("dve", KT[:, :, t * 128:(t + 1) * 128], psT[:, 0:2, :], [t_psT], [t_KT])

            kv1(0)
            kv1(1)
            for t in range(NT):
                kv2(t)
                if t + 2 < NT:
                    kv1(t + 2)
            wq = sb(st, "wq", [128, 8, 256], BF16)
            t_wq = Tok()
            QTb = [(sb(st, "QT%d" % i, [128, 4, 512], BF16), Tok()) for i in range(2)]
            PTr = Ring([sb(st, "PT%d" % i, [128, 512], BF16) for i in range(4)])
            rdr = Ring([sb(st, "rd%d" % i, [64, 512], F32) for i in range(2)])
            obr = Ring([sb(st, "ob%d" % i, [64, 512], BF16) for i in range(2)])
            Sr = Ring([None] * 3)
            Sr.items = [(self.ps[i], self.tp[i]) for i in range(3)]
            Or = Ring([None] * 2)
            Or.items = [(self.ps[i], self.tp[i]) for i in (3, 4)]
            units = [(g, c) for g in range(4) for c in range(8)]

            qst = {}

            def qproj1(u, t4):
                g, c = units[u]
                if c == 0 and t4 == 0:
                    self.load_w(wq[:], self.w_in[l][:, 2304 + g * 256:2304 + (g + 1) * 256], t_wq)
                t = c * 4 + t4
                ps, t_ps = self.ps[6], self.tp[6]
                for kc in range(8):
                    self.mm(ps[:, 0:256], self.hT[:, kc, t * 128:(t + 1) * 128], wq[:, kc, :], kc == 0, kc == 7,
                            [self.t_hT, t_wq], [t_ps])
                qsb, t_qsb = ksr.next()
                self.cp("dve", qsb[:], ps[:, 0:256], [t_ps], [t_qsb])
                qr, t_qr = krr.next()
                self.qknorm_rope(qsb[:], 4, gq[:], t, wk, qr[:], t_qsb, t_qr)
                qst[(u, t4)] = (qr, t_qr)

            def qproj2(u, t4):
                g, c = units[u]
                kb = (g % 2) * 64
                ko = 64 - kb
                QT, t_QT = QTb[u % 2]
                qr, t_qr = qst.pop((u, t4))
                for h in range(4):
                    self.tr(psT[0:64, h, :], qr[:, h * 64:(h + 1) * 64], self.identb[:], [t_qr, self.t_const], [t_psT])
                self.cp("dve", QT[kb:kb + 64, :, t4 * 128:(t4 + 1) * 128], psT[0:64, 0:4, :], [t_psT], [t_QT])
                self.memset("pool", QT[ko:ko + 64, :, t4 * 128:(t4 + 1) * 128], 0.0, [t_QT])

            def attend(u, h):
                g, c = units[u]
                QT, t_QT = QTb[u % 2]
                hq = g * 4 + h
                OT, t_OT = Or.next()
                pend = []

                def score(kt):
                    Sb, t_S = Sr.next()
                    self.mm(Sb[:], KT[:, g // 2, kt * 128:(kt + 1) * 128], QT[:, h, :], True, True, [t_KT, t_QT], [t_S])
                    PT, t_PT = PTr.next()
                    self.act(PT[:], Sb[:], AF.Exp, [t_S], [t_PT], scale=0.125)
                    pend.append((kt, PT, t_PT))

                def pv():
                    kt, PT, t_PT = pend.pop(0)
                    self.mm(OT[:], V[:, kt, g, :], PT[:], kt == 0, kt == NT - 1, [t_V, t_PT], [t_OT])

                score(0)
                score(1)
                for kt in range(NT):
                    if kt + 2 < NT:
                        score(kt + 2)
                    pv()
                def fin():
                    rd, t_rd = rdr.next()
                    self.recip(rd[0:64, :], OT[64:128, :], [t_OT], [t_rd])
                    ob, t_ob = obr.next()
                    self.tt("dve", ob[:], OT[0:64, :], rd[0:64, :], ALU.mult, [t_OT, t_rd], [t_ob])
                    self.dma("sp", self.obT_d[hq * 64:(hq + 1) * 64, c * 512:(c + 1) * 512], ob[:], [t_ob], [self.t_obT])
                return fin

            for t4 in range(4):
                qproj1(0, t4)
                qproj2(0, t4)
            for u in range(len(units)):
                nxt = u + 1 < len(units)
                f = attend(u, 0)
                f()
                if nxt:
                    qproj1(u + 1, 0)
                    qproj1(u + 1, 1)
                f = attend(u, 1)
                if nxt:
                    qproj2(u + 1, 0)
                    qproj2(u + 1, 1)
                f()
                if nxt:
                    qproj1(u + 1, 2)
                f = attend(u, 2)
                if nxt:
                    qproj2(u + 1, 2)
                f()
                if nxt:
                    qproj1(u + 1, 3)
                f = attend(u, 3)
                if nxt:
                    qproj2(u + 1, 3)
                f()

    def phase_window(self, l):
        with ExitStack() as st:
            sb = self.sb
            bias = sb(st, "wb", [128, 12, 384], F32)
            t_bias = Tok()
            self.dma("sp", bias[:], self.wbias, [], [t_bias])
            QT = sb(st, "wQT", [128, 2, S], BF16)
            KT = sb(st, "wKT", [128, 2, S], BF16)
            Vf = sb(st, "wVf", [128, NT, 256], BF16)
            wq = sb(st, "wwq", [128, 8, 768], BF16)
            t_QT, t_KT, t_Vf, t_wq = Tok(), Tok(), Tok(), Tok()
            sr = Ring([sb(st, "ws%d" % i, [128, 384], F32) for i in range(3)])
            pr = Ring([sb(st, "wp%d" % i, [128, 384], BF16) for i in range(4)])
            ptr = Ring([sb(st, "wpt%d" % i, [128, 3, 128], BF16) for i in range(2)])
            mr = Ring([sb(st, "wm%d" % i, [128, 2], F32) for i in range(6)])
            stg = Ring([sb(st, "wstg%d" % i, [128, 264], F32) for i in range(2)])
            Sr = Ring([None] * 3)
            Sr.items = [(self.ps[i], self.tp[i]) for i in (0, 1, 7)]
            Tr = Ring([None] * 2)
            Tr.items = [(self.ps[i][:].bitcast(BF16)[:, 0:384].rearrange("p (a b) -> p a b", a=3), self.tp[i]) for i in (2, 3)]
            Or = Ring([None] * 2)
            Or.items = [(self.ps[i], self.tp[i]) for i in (4, 5)]
            import os
            wstop = int(os.environ.get("WSTOP", "99"))
            for a, Dl in enumerate(A_DIL):
                L = S // Dl
                nj = L // 128
                if str(a) not in os.environ.get("WGRPS", "012"):
                    continue
                self.load_w(wq[:], self.w_in[l][:, a * 768:(a + 1) * 768], t_wq)
                n = 0
                for which, dst, t_dst in ((0, QT, t_QT), (1, KT, t_KT)):
                    for m in range(2):
                        for c in range(8):
                            ps, t_ps = self.ps[6], self.tp[6]
                            n += 1
                            col = which * 256 + m * 128
                            for kc in range(8):
                                self.mm(ps[:], wq[:, kc, col:col + 128], self.hT[:, kc, c * 512:(c + 1) * 512], kc == 0, kc == 7,
                                        [t_wq, self.t_hT], [t_ps])
                            self.cp("act" if n % 2 else "dve", dst[:, m, c * 512:(c + 1) * 512], ps[:], [t_ps], [t_dst])
                if wstop <= 1:
                    continue
                for r in range(Dl):
                    for j in range(nj):
                        bi = r * nj + j
                        tok0 = j * 128 * Dl + r
                        ps, t_ps = self.ps[6], self.tp[6]
                        for kc in range(8):
                            self.mm(ps[:, 0:256], self.hT[:, kc, sl(tok0, 128, Dl)], wq[:, kc, 512:768], kc == 0, kc == 7,
                                    [self.t_hT, t_wq], [t_ps])
                        self.cp("act" if bi % 2 else "dve", Vf[:, bi, :], ps[:, 0:256], [t_ps], [t_Vf])
                if wstop <= 2:
                    continue
                units = []
                for r in range(Dl):
                    for j in range(nj):
                        for h in range(4):
                            units.append((r, j, h))
                ust = {}
                blk = {}

                def W1(n):
                    r, j, h = units[n]
                    tok0 = j * 128 * Dl + r
                    jt0 = max(j - 1, 0)
                    jt1 = min(j + 1, nj - 1)
                    ntl = jt1 - jt0 + 1
                    c0 = (jt0 - (j - 1)) * 128
                    w = ntl * 128
                    k0 = jt0 * 128 * Dl + r
                    if h == 0:
                        blk[(r, j)] = (Or.next(), stg.next())
                    (O4, t_O4), (sg, t_sg) = blk[(r, j)]
                    Sb, t_S = Sr.next()
                    hb_ = (h % 2) * 64
                    self.mm(Sb[:, 0:w], QT[hb_:hb_ + 64, h // 2, sl(tok0, 128, Dl)], KT[hb_:hb_ + 64, h // 2, sl(k0, w, Dl)],
                            True, True, [t_QT, t_KT], [t_S])
                    s_, t_s = sr.next()
                    self.stt(s_[:, 0:w], Sb[:, 0:w], 0.125, bias[:, a * 4 + h, c0:c0 + w], ALU.mult, ALU.add, [t_S, t_bias], [t_s])
                    m, t_m = mr.next()
                    self.P.op("dve", lambda e, m=m, s_=s_, w=w: e.reduce_max(out=m[:, 0:1], in_=s_[:, 0:w], axis=AX.X), [t_s], [t_m])
                    self.ts("dve", m[:, 1:2], m[:, 0:1], -1.0, ALU.mult, [t_m], [t_m])
                    self.cp("dve", sg[:, 256 + h:257 + h], m[:, 0:1], [t_m], [t_sg])
                    ust[n] = (s_, t_s, m, t_m, sg, t_sg, w, h, ntl, jt0)

                def W1b(n):
                    s_, t_s, m, t_m, sg, t_sg, w, h, ntl, jt0 = ust[n]
                    p, t_p = pr.next()
                    self.act(p[:, 0:w], s_[:, 0:w], AF.Exp, [t_s, t_m], [t_p, t_sg], bias=m[:, 1:2], scale=1.0,
                             accum=sg[:, 260 + h:261 + h])
                    ust[n] = (p, t_p, ntl, jt0)

                def W2(n):
                    r, j, h = units[n]
                    tok0 = j * 128 * Dl + r
                    p, t_p, ntl, jt0 = ust.pop(n)
                    (O4, t_O4), (sg, t_sg) = blk[(r, j)]
                    pT, t_pT = Tr.next()
                    for ti in range(ntl):
                        self.tr(pT[:, ti, :], p[:, ti * 128:(ti + 1) * 128], self.identb[:], [t_p, self.t_const], [t_pT])
                    pts, t_pts = ptr.next()
                    self.cp("act", pts[:, 0:ntl, :], pT[:, 0:ntl, :], [t_pT], [t_pts])
                    for ti in range(ntl):
                        self.mm(O4[:, h * 64:(h + 1) * 64], pts[:, ti, :], Vf[:, r * nj + jt0 + ti, h * 64:(h + 1) * 64],
                                ti == 0, ti == ntl - 1, [t_pts, t_Vf], [t_O4])
                    if h == 3:
                        self.cp("act", sg[:, 0:256], O4[:, 0:256], [t_O4], [t_sg])
                        self.dma("sp", self.oaw_d[a, sl(tok0, 128, Dl), :], sg[:], [t_sg], [self.t_oaw])
                        del blk[(r, j)]

                NU = len(units)
                W1(0)
                W1(1)
                W1b(0)
                for n in range(NU):
                    if n + 2 < NU:
                        W1(n + 2)
                    W2(n)
                    if n + 1 < NU:
                        W1b(n + 1)
            if wstop <= 4:
                return
            self.P.barrier()
            cr = Ring([sb(st, "wc%d" % i, [128, 3, 264], F32) for i in range(2)])
            ms = sb(st, "wms", [128, 4], F32)
            wgt = sb(st, "wwgt", [128, 3, 4], F32)
            dt_ = sb(st, "wdt", [128, 4], F32)
            acc = sb(st, "wacc", [128, 256], F32)
            tmp = sb(st, "wtmp", [128, 256], F32)
            obr = Ring([sb(st, "wob%d" % i, [128, 256], BF16) for i in range(2)])
            t_k = Tok()
            for t in range(NT):
                ct, t_ct = cr.next()
                self.dma("sp", ct[:], self.oaw_d[:, t * 128:(t + 1) * 128, :].rearrange("a p c -> p a c"), [self.t_oaw], [t_ct])
                mv = ct[:, :, 256:260]
                dv = ct[:, :, 260:264]
                self.tt("dve", ms[:], mv[:, 0, :], mv[:, 1, :], ALU.max, [t_ct], [t_k])
                self.tt("dve", ms[:], ms[:], mv[:, 2, :], ALU.max, [t_ct, t_k], [t_k])
                self.tt("dve", wgt[:], mv, ms[:].unsqueeze(1).to_broadcast([128, 3, 4]), ALU.subtract, [t_ct, t_k], [t_k])
                self.act(wgt[:], wgt[:], AF.Exp, [t_k], [t_k])
                self.tt("dve", dv, dv, wgt[:], ALU.mult, [t_ct, t_k], [t_ct])
                self.tt("dve", dt_[:], dv[:, 0, :], dv[:, 1, :], ALU.add, [t_ct], [t_k])
                self.tt("dve", dt_[:], dt_[:], dv[:, 2, :], ALU.add, [t_ct, t_k], [t_k])
                self.recip(dt_[:], dt_[:], [t_k], [t_k])
                self.tt("dve", wgt[:], wgt[:], dt_[:].unsqueeze(1).to_broadcast([128, 3, 4]), ALU.mult, [t_k], [t_k])
                ob, t_ob = obr.next()
                v3 = lambda ap: ap.rearrange("p (h d) -> p h d", h=4)
                for a in range(3):
                    cb = wgt[:, a, :].unsqueeze(2).to_broadcast([128, 4, 64])
                    if a == 0:
                        self.tt("dve", v3(acc[:]), v3(ct[:, 0, 0:256]), cb, ALU.mult, [t_ct, t_k], [t_k])
                    else:
                        self.tt("dve", v3(tmp[:]), v3(ct[:, a, 0:256]), cb, ALU.mult, [t_ct, t_k], [t_k])
                        if a == 1:
                            self.tt("dve", acc[:], acc[:], tmp[:], ALU.add, [t_k], [t_k])
                        else:
                            self.tt("dve", ob[:], acc[:], tmp[:], ALU.add, [t_k], [t_ob])
                pT, t_pT = Tr.next()
                for kc in range(2):
                    self.tr(pT[:, kc, :], ob[:, kc * 128:(kc + 1) * 128], self.identb[:], [t_ob, self.t_const], [t_pT])
                self.cp("act", self.oaT[:, :, t * 128:(t + 1) * 128], pT[:, 0:2, :], [t_pT], [self.t_oaT])

    def phase_merge(self, l, xsrc):
        with ExitStack() as st:
            sb = self.sb
            self.load_mod(st, [2, 3, 4])
            wa = sb(st, "m_wa", [128, 2, D], BF16)
            wb = sb(st, "m_wb", [128, 8, D], BF16)
            wo = sb(st, "m_wo", [128, 8, D], BF16)
            wr = sb(st, "m_wr", [128, 8, NE], F32)
            brr = sb(st, "m_brr", [128, NE], F32)
            t_w = Tok()
            self.load_w(wa[:], self.w_br_a[l], t_w)
            self.load_w(wb[:], self.w_br_b[l], t_w)
            self.load_w(wo[:], self.w_out[l], t_w)
            self.dma("sp", wr[:], self.w_router[l].rearrange("(kc p) n -> p kc n", p=128), [], [t_w])
            self.dma("sp", brr[:], self.b_router[l:l + 1, :].partition_broadcast(128), [], [t_w])
            gtr = Ring([sb(st, "m_gt%d" % i, [128, 16, 512], BF16) for i in range(1)])
            obr = Ring([sb(st, "m_ob%d" % i, [128, 8, 512], BF16) for i in range(1)])
            mgr = Ring([sb(st, "m_mg%d" % i, [128, 8, 512], BF16) for i in range(1)])
            tA = sb(st, "m_tA", [128, 512], F32)
            tB = sb(st, "m_tB", [128, 512], F32)
            t_tA, t_tB = Tok(), Tok()
            xr = Ring([sb(st, "m_x%d" % i, [128, D], F32) for i in range(2)])
            x1r = Ring([sb(st, "m_x1%d" % i, [128, D], F32) for i in range(2)])
            h2fr = Ring([sb(st, "m_h2f%d" % i, [128, D], F32) for i in range(2)])
            h2br = Ring([sb(st, "m_h2b%d" % i, [128, D], BF16) for i in range(2)])
            h2Tr = Ring([sb(st, "m_h2T%d" % i, [128, 8, 128], F32) for i in range(1)])
            work = (sb(st, "m_junk", [128, D], F32), sb(st, "m_ss", [128, 1], F32), sb(st, "m_rstd", [128, 1], F32),
                    sb(st, "m_tmp", [128, D], F32), Tok())
            e4 = sb(st, "m_e4", [128, 4], F32)
            ssum = sb(st, "m_ssum", [128, 2], F32)
            t_e4 = Tok()
            Ar = Ring([None] * 2)
            Ar.items = [(self.ps[i], self.tp[i]) for i in (0, 1)]
            Br = Ring([None] * 2)
            Br.items = [(self.ps[i], self.tp[i]) for i in (2, 3)]
            Or = Ring([None] * 2)
            Or.items = [(self.ps[i], self.tp[i]) for i in (4, 5)]
            psT2 = [self.ps[i][:].rearrange("p (a b) -> p a b", a=4) for i in (6, 7)]
            for c in range(8):
                gt, t_gt = gtr.next()
                ob, t_ob = obr.next()
                mg, t_mg = mgr.next()
                self.dma("sp", gt[:], self.gT_d[:, c * 512:(c + 1) * 512].rearrange("(m p) n -> p m n", p=128), [self.t_gT], [t_gt])
                self.dma("sp", ob[:], self.obT_d[:, c * 512:(c + 1) * 512].rearrange("(m p) n -> p m n", p=128), [self.t_obT], [t_ob])
                for m in range(8):
                    pA, t_pA = Ar.next()
                    pB, t_pB = Br.next()
                    for kc in range(2):
                        self.mm(pA[:], wa[:, kc, m * 128:(m + 1) * 128], self.oaT[:, kc, c * 512:(c + 1) * 512], kc == 0, kc == 1,
                                [t_w, self.t_oaT], [t_pA])
                    for kc in range(8):
                        self.mm(pB[:], wb[:, kc, m * 128:(m + 1) * 128], ob[:, kc, :], kc == 0, kc == 7, [t_w, t_ob], [t_pB])
                    self.tt("dve", tA[:], pA[:], gt[:, m, :], ALU.mult, [t_pA, t_gt], [t_tA])
                    self.tt("dve", tB[:], pB[:], gt[:, 8 + m, :], ALU.mult, [t_pB, t_gt], [t_tB])
                    self.tt("pool", mg[:, m, :], tA[:], tB[:], ALU.add, [t_tA, t_tB], [t_mg])
                for t4 in range(4):
                    t = c * 4 + t4
                    xt, t_x = xr.next()
                    x1, t_x1 = x1r.next()
                    self.dma("sp", xt[:], xsrc[t * 128:(t + 1) * 128, :], [self.t_xs], [t_x])
                    for hf in range(2):
                        pO, t_pO = Or.next()
                        for kc in range(8):
                            self.mm(pO[:], mg[:, kc, t4 * 128:(t4 + 1) * 128], wo[:, kc, hf * 512:(hf + 1) * 512], kc == 0, kc == 7,
                                    [t_mg, t_w], [t_pO])
                        self.tt("dve", x1[:, hf * 512:(hf + 1) * 512], pO[:], self.modr[2][:, hf * 512:(hf + 1) * 512], ALU.mult,
                                [t_pO, self.t_modr], [t_x1])
                    self.tt("pool", x1[:], x1[:], xt[:], ALU.add, [t_x1, t_x], [t_x1])
                    self.dma("sp", self.xs_d[t * 128:(t + 1) * 128, :], x1[:], [t_x1], [self.t_xs])
                    h2f, t_h2f = h2fr.next()
                    h2b, t_h2b = h2br.next()
                    self.norm_mod(x1, t_x1, 4, 3, work, h2b, t_h2f, hf=h2f)
                    self.dma("sp", self.h2_d[t * 128:(t + 1) * 128, :], h2b[:], [t_h2f], [self.t_h2d])
                    h2T, t_h2T = h2Tr.next()
                    for hh in range(2):
                        for k4 in range(4):
                            kc = hh * 4 + k4
                            self.tr(psT2[hh][:, k4, :], h2f[:, kc * 128:(kc + 1) * 128], self.identf[:], [t_h2f, self.t_const], [self.tp[6 + hh]])
                        self.cp("act", h2T[:, hh * 4:(hh + 1) * 4, :], psT2[hh], [self.tp[6 + hh]], [t_h2T])
                    pL, t_pL = Or.next()
                    for kc in range(8):
                        self.mm(pL[:, 0:NE], h2T[:, kc, :], wr[:, kc, :], kc == 0, kc == 7, [t_h2T, t_w], [t_pL])
                    lg = self.logits_all[:, t, :]
                    m8 = self.max8_all[:, t, :]
                    self.tt("dve", lg, pL[:, 0:NE], brr[:], ALU.add, [t_pL, t_w], [self.t_route])
                    self.P.op("dve", lambda e, m8=m8, lg=lg: e.max(out=m8, in_=lg), [self.t_route], [self.t_route])
                    self.ts("dve", self.M_all[:, t, :], lg, m8[:, 3:4], ALU.is_ge, [self.t_route], [self.t_route])
                    self.ts("dve", ssum[:, 1:2], m8[:, 0:1], -1.0, ALU.mult, [self.t_route], [t_e4])
                    self.act(e4[:], m8[:, 0:4], AF.Exp, [self.t_route, t_e4], [t_e4], bias=ssum[:, 1:2], scale=1.0, accum=ssum[:, 0:1])
                    self.recip(ssum[:, 0:1], ssum[:, 0:1], [t_e4], [t_e4])
                    self.ts("dve", self.g4_all[:, t, :], e4[:], ssum[:, 0:1], ALU.mult, [t_e4], [self.t_route])

    def phase_slots(self, l):
        sb = self.sb
        with ExitStack() as s2:
            tris = sb(s2, "s_tris", [128, 128], BF16)
            tri32s = sb(s2, "s_t32s", [32, 32], BF16)
            tri32i = sb(s2, "s_t32i", [32, 32], BF16)
            iota160 = sb(s2, "s_iota", [32, NBLK], F32)
            iotap = sb(s2, "s_iotap", [128, 1], F32)
            iotap32 = sb(s2, "s_iotap32", [128, 1], F32)
            t_c = Tok()
            for dst, src in ((tris, self.c_tris), (tri32s, self.c_tri32s), (tri32i, self.c_tri32i), (iota160, self.c_iota160),
                             (iotap, self.c_iotap), (iotap32, self.c_iotap32)):
                self.dma("sp", dst[:], src, [], [t_c])
            cnt = sb(s2, "s_cnt", [32, 1], F32)
            cnti = sb(s2, "s_cnti", [32, 1], I32)
            nblk = sb(s2, "s_nblk", [32, 1], F32)
            nblkb = sb(s2, "s_nblkb", [32, 128], BF16)
            nblkc = sb(s2, "s_nblkc", [32, 1], BF16)
            bstart = sb(s2, "s_bstart", [128, NE], F32)
            bend = sb(s2, "s_bend", [32, 1], F32)
            cmp = sb(s2, "s_cmp", [32, NBLK], BF16)
            erep = sb(s2, "s_erep", [128, NBLK], F32)
            chg = sb(s2, "s_chg", [128, NBLK], F32)
            wf = sb(s2, "s_wf", [128, NBLK], F32)
            pos = sb(s2, "s_pos", [128, NE], F32)
            junk = sb(s2, "s_junk", [128, NE], F32)
            p4f = sb(s2, "s_p4f", [128, NT, 4], F32)
            t_s = Tok()
            pc, t_pc = self.ps[0], self.tp[0]
            for t in range(NT):
                self.mm(pc[0:32, 0:1], self.M_all[:, t, :], self.onesb[:, 0:1], t == 0, t == NT - 1, [self.t_route, self.t_const], [t_pc])
            self.ts("dve", cnt[:], pc[0:32, 0:1], 127.0, ALU.add, [t_pc], [t_s], s2=1.0 / 128.0, op1=ALU.mult)
            self.ts("dve", nblk[:], cnt[:], -0.49609375, ALU.add, [t_s], [t_s])
            self.cp("dve", cnti[:], nblk[:], [t_s], [t_s])
            self.cp("dve", nblk[:], cnti[:], [t_s], [t_s])
            self.tt("dve", cnt[:], cnt[:], nblk[:], ALU.subtract, [t_s], [t_s])
            self.ts("dve", cnt[:], cnt[:], 1.0, ALU.is_ge, [t_s], [t_s])
            self.tt("dve", nblk[:], nblk[:], cnt[:], ALU.add, [t_s], [t_s])
            self.ts("dve", nblk[:], nblk[:], 1.0, ALU.max, [t_s], [t_s])
            self.cp("dve", nblkb[:], nblk[:, 0:1].to_broadcast([32, 128]), [t_s], [t_s])
            self.cp("dve", nblkc[:], nblk[:], [t_s], [t_s])
            p1, t_p1 = self.ps[1], self.tp[1]
            self.mm(p1[:, 0:NE], nblkb[:], tri32s[:], True, True, [t_s, t_c], [t_p1])
            self.cp("dve", bstart[:], p1[:, 0:NE], [t_p1], [t_s])
            p2, t_p2 = self.ps[2], self.tp[2]
            self.mm(p2[0:32, 0:1], tri32i[:], nblkc[:], True, True, [t_s, t_c], [t_p2])
            self.cp("dve", bend[:], p2[0:32, 0:1], [t_p2], [t_s])
            self.ts("dve", cmp[:], iota160[:], bend[:, 0:1], ALU.is_ge, [t_s, t_c], [t_s])
            p3, t_p3 = self.ps[3], self.tp[3]
            self.mm(p3[:, 0:NBLK], self.onesb[0:32, :], cmp[:], True, True, [t_s, self.t_const], [t_p3])
            self.ts("dve", erep[:], p3[:, 0:NBLK], float(NE - 1), ALU.min, [t_p3], [t_s])
            self.memset("dve", chg[:, 0:1], 1.0, [t_s])
            self.tt("dve", chg[:, 1:NBLK], erep[:, 1:NBLK], erep[:, 0:NBLK - 1], ALU.not_equal, [t_s], [t_s])
            self.ts("dve", wf[:], erep[:], 128.0, ALU.mult, [t_s, t_c], [t_s], s2=iotap[:, 0:1], op1=ALU.add)
            self.ts("dve", chg[:], chg[:], -BIGIDX, ALU.mult, [t_s], [t_s], s2=BIGIDX, op1=ALU.add)
            self.tt("dve", wf[:], wf[:], chg[:], ALU.add, [t_s], [t_s])
            if l > 0:
                self.ts("dve", wf[:], wf[:], float(l * NE * 128), ALU.add, [t_s], [t_s])
            self.cp("dve", self.widx[:], wf[:], [t_s], [self.t_widx])
            self.ts("dve", self.OHall[:], erep[0:64, :], iotap32[0:64, 0:1], ALU.is_equal, [t_s, t_c], [self.t_widx])
            for t in range(NT):
                pr_, t_pr = self.ps[4 + t % 2], self.tp[4 + t % 2]
                self.mm(pr_[:, 0:NE], tris[:], self.M_all[:, t, :], True, t == 0, [t_c, self.t_route], [t_pr])
                for t2 in range(t):
                    self.mm(pr_[:, 0:NE], self.onesb[:], self.M_all[:, t2, :], False, t2 == t - 1, [self.t_const, self.t_route], [t_pr])
                self.stt(pos[:], bstart[:], 128.0, pr_[:, 0:NE], ALU.mult, ALU.add, [t_s, t_pr], [t_s])
                for k in range(4):
                    self.stt(junk[:], self.logits_all[:, t, :], self.max8_all[:, t, k:k + 1], pos[:], ALU.is_equal, ALU.mult,
                             [self.t_route, t_s], [t_s], accum=p4f[:, t, k:k + 1])
            self.cp("dve", self.pos4_all[:], p4f[:], [t_s], [self.t_pos4])
            self.P.barrier()
        with ExitStack() as s3:
            hr = Ring([sb(s3, "s_h2%d" % i, [128, D], BF16) for i in range(3)])
            for t in range(NT):
                hb, t_hb = hr.next()
                self.dma("sp", hb[:], self.h2_d[t * 128:(t + 1) * 128, :], [self.t_h2d], [t_hb])
                for k in range(4):
                    self.scatter(self.Xs_d, hb[:], self.pos4_all[:, t, k:k + 1], [t_hb, self.t_pos4, self.t_Xs], [], NBLK * 128 - 1)
            self.P.barrier()

    def phase_experts(self, l):
        with ExitStack() as st:
            sb = self.sb
            wgu = sb(st, "e_wgu", [128, 8 * 2 * D], BF16)
            wdn = sb(st, "e_wdn", [128, 8 * D], BF16)
            bgu = sb(st, "e_bgu", [64, 2 * D], BF16)
            bdn = sb(st, "e_bdn", [64, D], BF16)
            bf = sb(st, "e_bf", [64, 3 * D], F32)
            bt = sb(st, "e_bt", [64, 3 * D], F32)
            t_wgu, t_wdn, t_b = Tok(), Tok(), Tok()
            for half in range(2):
                self.dma("sp", bf[half * 32:(half + 1) * 32, 0:2 * D], self.b_gu[l], [], [t_b])
                self.dma("sp", bf[half * 32:(half + 1) * 32, 2 * D:3 * D], self.b_dn[l], [], [t_b])
            self.cp("dve", bgu[0:32, :], bf[0:32, 0:2 * D], [t_b], [t_b])
            self.cp("dve", bdn[0:32, :], bf[0:32, 2 * D:3 * D], [t_b], [t_b])
            self.cp("dve", bgu[32:64, :], bf[32:64, 0:2 * D], [t_b], [t_b])
            self.cp("dve", bdn[32:64, :], bf[32:64, 2 * D:3 * D], [t_b], [t_b])
            self.cp("dve", bt[32:64, 0:2 * D], bgu[32:64, :], [t_b], [t_b])
            self.cp("dve", bt[32:64, 2 * D:3 * D], bdn[32:64, :], [t_b], [t_b])
            self.tt("dve", bt[32:64, :], bf[32:64, :], bt[32:64, :], ALU.subtract, [t_b], [t_b])
            self.cp("dve", bgu[32:64, :], bt[32:64, 0:2 * D], [t_b], [t_b])
            self.cp("dve", bdn[32:64, :], bt[32:64, 2 * D:3 * D], [t_b], [t_b])
            wgu_v = self.w_gu.rearrange("l e (p kc) n -> (l e p) (kc n)", kc=8)
            wdn_v = self.w_dn.rearrange("l e (p kc) n -> (l e p) (kc n)", kc=8)
            R = 3
            xb_ = [(sb(st, "e_x%d" % i, [128, D], BF16), Tok()) for i in range(R)]
            xT_ = [(sb(st, "e_xT%d" % i, [128, 8, 128], BF16), Tok()) for i in range(R)]
            oh_ = [(sb(st, "e_oh%d" % i, [64, 128], BF16), Tok()) for i in range(R)]
            gc_ = [(sb(st, "e_gc%d" % i, [128, 512], F32), Tok()) for i in range(2)]
            sg_ = [(sb(st, "e_sg%d" % i, [128, 512], F32), Tok()) for i in range(2)]
            lc_ = [(sb(st, "e_lc%d" % i, [128, 512], F32), Tok()) for i in range(2)]
            ac_ = [(sb(st, "e_ac%d" % i, [128, D], BF16), Tok()) for i in range(2)]
            aT_ = [(sb(st, "e_aT%d" % i, [128, 8, 128], BF16), Tok()) for i in range(2)]
            out_ = [(sb(st, "e_out%d" % i, [128, D], F32), Tok()) for i in range(2)]
            pX = self.ps[6][:].bitcast(BF16).rearrange("p (a b) -> p a b", a=8)
            pA = self.ps[7][:].bitcast(BF16).rearrange("p (a b) -> p a b", a=8)
            N = NBLK
            bound = 2 * NE * 128 - 1

            def Wgu(j):
                self.gather(wgu[:], wgu_v, self.widx[:, j:j + 1], [self.t_widx], [t_wgu], bound)

            def Wdn(j):
                self.gather(wdn[:], wdn_v, self.widx[:, j:j + 1], [self.t_widx], [t_wdn], bound)

            def Ax(j):
                xb, t_xb = xb_[j % R]
                self.dma("sp", xb[:], self.Xs_d[j * 128:(j + 1) * 128, :], [self.t_Xs], [t_xb])
                oh, t_oh = oh_[j % R]
                self.cp("dve", oh[:], self.OHall[:, j:j + 1].to_broadcast([64, 128]), [self.t_widx], [t_oh])
                xv = xb[:].rearrange("p (a kc) -> p kc a", kc=8)
                for kc in range(8):
                    self.tr(pX[:, kc, :], xv[:, kc, :], self.identb[:], [t_xb, self.t_const], [self.tp[6]])
                xT, t_xT = xT_[j % R]
                self.cp("act", xT[:], pX, [self.tp[6]], [t_xT])

            def Bh(j, hf):
                xT, t_xT = xT_[j % R]
                oh, t_oh = oh_[j % R]
                for nb in (hf, 2 + hf):
                    ps, t_ps = self.ps[nb], self.tp[nb]
                    for kc in range(8):
                        self.mm(ps[:], xT[:, kc, :], wgu[:, kc * 2048 + nb * 512:kc * 2048 + (nb + 1) * 512], kc == 0, False,
                                [t_xT, t_wgu], [t_ps])
                    self.mm(ps[:], oh[:], bgu[:, nb * 512:(nb + 1) * 512], False, True, [t_oh, t_b], [t_ps])

            def E(j, hf):
                gc, t_gc = gc_[hf]
                sg, t_sg = sg_[hf]
                lc, t_lc = lc_[hf]
                ac, t_ac = ac_[j % 2]
                self.ts("dve", gc[:], self.ps[hf][:], 7.0, ALU.min, [self.tp[hf]], [t_gc])
                self.act(sg[:], gc[:], AF.Sigmoid, [t_gc], [t_sg], scale=1.702)
                self.ts("dve", lc[:], self.ps[2 + hf][:], 7.0, ALU.min, [self.tp[2 + hf]], [t_lc], s2=-7.0, op1=ALU.max)
                self.stt(lc[:], lc[:], 1.0, gc[:], ALU.add, ALU.mult, [t_lc, t_gc], [t_lc])
                self.tt("dve", ac[:, hf * 512:(hf + 1) * 512], lc[:], sg[:], ALU.mult, [t_lc, t_sg], [t_ac])

            def Ca(j):
                ac, t_ac = ac_[j % 2]
                av = ac[:].rearrange("p (a kc) -> p kc a", kc=8)
                for kc in range(8):
                    self.tr(pA[:, kc, :], av[:, kc, :], self.identb[:], [t_ac, self.t_const], [self.tp[7]])
                aT, t_aT = aT_[j % 2]
                self.cp("act", aT[:], pA, [self.tp[7]], [t_aT])

            def Cb(j):
                oh, t_oh = oh_[j % R]
                aT, t_aT = aT_[j % 2]
                ot, t_ot = out_[j % 2]
                for hf in range(2):
                    ps, t_ps = self.ps[4 + hf], self.tp[4 + hf]
                    for kc in range(8):
                        self.mm(ps[:], aT[:, kc, :], wdn[:, kc * 1024 + hf * 512:kc * 1024 + (hf + 1) * 512], kc == 0, False,
                                [t_aT, t_wdn], [t_ps])
                    self.mm(ps[:], oh[:], bdn[:, hf * 512:(hf + 1) * 512], False, True, [t_oh, t_b], [t_ps])
                    self.cp("act" if hf else "dve", ot[:, hf * 512:(hf + 1) * 512], ps[:], [t_ps], [t_ot])
                self.dma("sp", self.Out_d[j * 128:(j + 1) * 128, :], ot[:], [t_ot], [self.t_Outd])

            Wgu(0)
            Wdn(0)
            Ax(0)
            Ax(1)
            Bh(0, 0)
            E(0, 0)
            Bh(0, 1)
            E(0, 1)
            Wgu(1)
            for j in range(N):
                if j + 1 < N:
                    Bh(j + 1, 0)
                if j + 2 < N:
                    Ax(j + 2)
                if j + 1 < N:
                    E(j + 1, 0)
                Ca(j)
                if j + 1 < N:
                    Bh(j + 1, 1)
                if j + 2 < N:
                    Wgu(j + 2)
                if j + 1 < N:
                    E(j + 1, 1)
                Cb(j)
                if j + 1 < N:
                    Wdn(j + 1)

    def phase_combine(self, l, last):
        with ExitStack() as st:
            sb = self.sb
            self.load_mod(st, [5])
            gr = Ring([sb(st, "c_g%d" % i, [128, D], F32) for i in range(4)])
            xr = Ring([sb(st, "c_x%d" % i, [128, D], F32) for i in range(2)])
            yr = Ring([sb(st, "c_y%d" % i, [128, D], F32) for i in range(2)])
            fg = sb(st, "c_fg", [128, D], F32)
            junk = sb(st, "c_junk", [128, D], F32)
            ss = sb(st, "c_ss", [128, 2], F32)
            t_fg, t_w = Tok(), Tok()
            if last:
                self.dma("sp", fg[:], self.fng.rearrange("(o d) -> o d", o=1).partition_broadcast(128), [], [t_fg])
            for t in range(NT):
                xt, t_x = xr.next()
                y, t_y = yr.next()
                self.dma("sp", xt[:], self.xs_d[t * 128:(t + 1) * 128, :], [self.t_xs], [t_x])
                for k in range(4):
                    g, t_g = gr.next()
                    self.gather(g[:], self.Out_d, self.pos4_all[:, t, k:k + 1], [self.t_Outd, self.t_pos4], [t_g], None)
                    if k == 0:
                        self.ts("dve", y[:], g[:], self.g4_all[:, t, 0:1], ALU.mult, [t_g, self.t_route], [t_y])
                    else:
                        self.stt(y[:], g[:], self.g4_all[:, t, k:k + 1], y[:], ALU.mult, ALU.add, [t_g, self.t_route, t_y], [t_y])
                self.tt("dve", y[:], y[:], self.modr[5], ALU.mult, [t_y, self.t_modr], [t_y])
                self.tt("pool", y[:], y[:], xt[:], ALU.add, [t_y, t_x], [t_y])
                if not last:
                    self.dma("sp", self.xs_d[t * 128:(t + 1) * 128, :], y[:], [t_y], [self.t_xs])
                else:
                    self.stt(junk[:], y[:], 1.0, y[:], ALU.mult, ALU.mult, [t_y], [t_w], accum=ss[:, 0:1])
                    self.rstd_from_ss(ss[:, 1:2], ss[:, 0:1], D, t_w, t_w)
                    self.stt(y[:], y[:], ss[:, 1:2], fg[:], ALU.mult, ALU.mult, [t_y, t_w, t_fg], [t_y])
                    self.dma("sp", self.out[t * 128:(t + 1) * 128, :], y[:], [t_y], [self.t_out])


def _t5_bucket(rel):
    nb = 16
    max_exact = 8
    ret = np.where(rel > 0, nb, 0)
    n = np.abs(rel)
    nf = np.maximum(n, 1).astype(np.float32)
    large = max_exact + (np.log(nf / max_exact) / math.log(1024 / max_exact) * (nb - max_exact)).astype(np.int32)
    large = np.minimum(large, nb - 1)
    return ret + np.where(n < max_exact, n, large)


def host_consts(rel_bias):
    bf = ml_dtypes.bfloat16
    c = {}
    c["identb"] = np.eye(128, dtype=np.float32).astype(bf)
    c["identf"] = np.eye(128, dtype=np.float32)
    tok = np.arange(S)
    row = (tok // 64).astype(np.float32)
    col = (tok % 64).astype(np.float32)
    inv = (10000.0 ** (-np.arange(0, 32, 2, dtype=np.float32) / 32)).astype(np.float32)
    ar = row[:, None] * inv
    ac = col[:, None] * inv
    cosT = np.concatenate([np.cos(ar), np.cos(ar), np.cos(ac), np.cos(ac)], 1).astype(np.float32)
    sinT = np.concatenate([-np.sin(ar), np.sin(ar), -np.sin(ac), np.sin(ac)], 1).astype(np.float32)
    c["cosT"] = np.ascontiguousarray(cosT.reshape(NT, 128, 64).transpose(1, 0, 2))
    c["sinT"] = np.ascontiguousarray(sinT.reshape(NT, 128, 64).transpose(1, 0, 2))
    k = np.arange(128)
    c["tri_s"] = (k[:, None] < k[None, :]).astype(np.float32).astype(bf)
    k32 = np.arange(32)
    c["tri32s"] = (k32[:, None] < k32[None, :]).astype(np.float32).astype(bf)
    c["tri32i"] = (k32[:, None] <= k32[None, :]).astype(np.float32).astype(bf)
    c["iota160"] = np.broadcast_to(np.arange(NBLK, dtype=np.float32), (32, NBLK)).copy()
    c["iotap"] = np.arange(128, dtype=np.float32).reshape(128, 1)
    c["iotap32"] = (np.arange(128) % 32).astype(np.float32).reshape(128, 1)
    q = np.arange(128)[:, None]
    cc = np.arange(384)[None, :]
    rel = cc - 128 - q
    wb = np.full((128, 12, 384), -30000.0, np.float32)
    valid = np.abs(rel) <= 64
    for a, dl in enumerate(A_DIL):
        bkt = _t5_bucket(rel * dl)
        for h in range(4):
            vals = rel_bias[bkt, a * 4 + h]
            wb[:, a * 4 + h, :] = np.where(valid, vals, np.float32(-30000.0))
    c["wbias"] = wb
    return c


_CACHE = {}


def kernel(**inputs):
    inp = {k: np.ascontiguousarray(np.asarray(v, dtype=np.float32)) for k, v in inputs.items()}
    if "nc" not in _CACHE:
        _CACHE["nc"] = Builder(nlayers=2).build()
    nc = _CACHE["nc"]
    consts = host_consts(inp["rel_bias"])
    shared = {k: inp[k] for k in ("w_ada", "b_ada", "norm1_g", "w_in", "q_norm_g", "k_norm_g", "w_br_a", "w_br_b", "w_out",
                                   "norm2_g", "w_router", "b_router", "w_gate_up", "b_gate_up", "w_down", "b_down", "final_norm_g")}
    in_maps = []
    for b in range(8):
        m = dict(shared)
        m.update(consts)
        m["x"] = inp["x"][b]
        m["ccol"] = np.ascontiguousarray(inp["c"][b].reshape(8, 128).T)
        in_maps.append(m)
    res = run_bass_kernel_spmd(nc, in_maps, core_ids=list(range(8)))
    return np.stack([np.asarray(res.results[b]["out"], dtype=np.float32) for b in range(8)], 0)
```

```python
import math
from contextlib import ExitStack

import numpy as np
import ml_dtypes
import concourse.bass as bass
import concourse.mybir as mybir
from concourse.bass_utils import run_bass_kernel_spmd

F32 = mybir.dt.float32
BF16 = mybir.dt.bfloat16
I32 = mybir.dt.int32
ALU = mybir.AluOpType
AF = mybir.ActivationFunctionType
AX = mybir.AxisListType

S = 4096
D = 1024
NT = 32
NE = 32
NBLK = 160
EPS = 1e-6
A_DIL = (1, 4, 16)
BIGIDX = 1.0e6


def sl(start, n, step):
    return slice(start, start + (n - 1) * step + 1, step)

ENGS = ["pe", "act", "dve", "pool", "sp"]


class Tok:
    __slots__ = ("w", "r")

    def __init__(self):
        self.w = None
        self.r = []


class Prog:
    N_DMA_SEMS = {"sp": 24, "pool": 16, "act": 8}

    def __init__(self, nc):
        self.nc = nc
        self.q = {e: [] for e in ENGS}
        self.cnt = {e: 0 for e in ENGS}
        self.seen = {e: {} for e in ENGS}
        self.dma_val = {}
        self.dma_rr = {e: 0 for e in ENGS}
        self.prologue = {}

    def _collect(self, eng, reads, writes):
        deps = {}

        def add(d):
            if d is not None and deps.get(d[0], 0) < d[1]:
                deps[d[0]] = d[1]

        for t in reads:
            add(t.w)
        for t in writes:
            add(t.w)
            for d in t.r:
                add(d)
        out = []
        own = "E:" + eng
        seen = self.seen[eng]
        for k, v in deps.items():
            if k == own and eng == "pe":
                continue
            if seen.get(k, 0) >= v:
                continue
            seen[k] = v
            out.append((k, v))
        return out

    def _note(self, my, reads, writes):
        for t in reads:
            t.r.append(my)
            if len(t.r) > 48:
                d = {}
                for k, v in t.r:
                    if d.get(k, 0) < v:
                        d[k] = v
                t.r = list(d.items())
        for t in writes:
            t.w = my
            t.r = []

    def op(self, eng, fn, reads=(), writes=()):
        waits = self._collect(eng, reads, writes)
        self.cnt[eng] += 1
        my = ("E:" + eng, self.cnt[eng])
        self._note(my, reads, writes)
        self.q[eng].append((waits, fn, my, 1))

    def dma(self, eng, fn, reads=(), writes=()):
        waits = self._collect(eng, reads, writes)
        n = self.N_DMA_SEMS[eng]
        slot = self.dma_rr[eng] % n
        self.dma_rr[eng] += 1
        key = "D:%s:%d" % (eng, slot)
        prev = self.dma_val.get(key, 0)
        if prev > 0 and self.seen[eng].get(key, 0) < prev:
            self.seen[eng][key] = prev
            waits.append((key, prev))
        self.dma_val[key] = prev + 16
        my = (key, prev + 16)
        self._note(my, reads, writes)
        self.q[eng].append((waits, fn, my, 16))

    def barrier(self):
        for eng in ENGS:
            waits = []
            for key, v in self.dma_val.items():
                if self.seen[eng].get(key, 0) < v:
                    waits.append((key, v))
                    self.seen[eng][key] = v
            for e in ENGS:
                if e == eng or self.cnt[e] == 0:
                    continue
                k = "E:" + e
                if self.seen[eng].get(k, 0) < self.cnt[e]:
                    waits.append((k, self.cnt[e]))
                    self.seen[eng][k] = self.cnt[e]
            if waits:
                self.q[eng].append((waits, None, None, 0))

    def emit(self):
        nc = self.nc
        keys = set()
        for e in ENGS:
            for waits, fn, my, inc in self.q[e]:
                for k, v in waits:
                    keys.add(k)
                if my is not None:
                    keys.add(my[0])
        keys = sorted(keys)
        with ExitStack() as st:
            sems = {k: st.enter_context(nc.semaphore(k.replace(":", "_"))) for k in keys}
            block = st.enter_context(nc.Block())
            hmap = {"pe": block.tensor, "act": block.scalar, "dve": block.vector,
                    "pool": block.gpsimd, "sp": block.sync}

            def make(e):
                def body(engh):
                    if e in self.prologue:
                        self.prologue[e](engh)
                    for waits, fn, my, inc in self.q[e]:
                        for k, v in waits:
                            engh.wait_ge(sems[k], v)
                        if fn is None:
                            continue
                        fn(engh).then_inc(sems[my[0]], inc)
                return body

            for e in ENGS:
                if self.q[e]:
                    hmap[e](make(e))


class Ring:
    def __init__(self, items):
        self.items = [(it, Tok()) for it in items]
        self.i = 0

    def next(self):
        it = self.items[self.i % len(self.items)]
        self.i += 1
        return it


class Builder:
    def __init__(self, nlayers=2, stop=None, dbg=(), small_moe=False):
        self.nlayers = nlayers
        self.stop = stop
        self.dbg = dbg
        NEW = 1 if small_moe else NE
        nc = self.nc = bass.Bass("TRN2", target_bir_lowering=False)
        self.P = Prog(nc)
        self.outs = ["out"]
        di = lambda n, s, d=F32: nc.dram_tensor(n, s, d, kind="ExternalInput").ap()
        self.x_in = di("x", [S, D])
        self.ccol = di("ccol", [128, 8])
        self.w_ada = di("w_ada", [2, D, 6 * D])
        self.b_ada = di("b_ada", [2, 6 * D])
        self.norm1_g = di("norm1_g", [2, D])
        self.w_in = di("w_in", [2, D, 5888])
        self.q_norm_g = di("q_norm_g", [2, 64])
        self.k_norm_g = di("k_norm_g", [2, 64])
        self.wbias = di("wbias", [128, 12, 384])
        self.w_br_a = di("w_br_a", [2, 256, D])
        self.w_br_b = di("w_br_b", [2, D, D])
        self.w_out = di("w_out", [2, D, D])
        self.norm2_g = di("norm2_g", [2, D])
        self.w_router = di("w_router", [2, D, NE])
        self.b_router = di("b_router", [2, NE])
        self.w_gu = di("w_gate_up", [2, NEW, D, 2 * D])
        self.b_gu = di("b_gate_up", [2, NE, 2 * D])
        self.w_dn = di("w_down", [2, NEW, D, D])
        self.b_dn = di("b_down", [2, NE, D])
        self.fng = di("final_norm_g", [D])
        self.c_identb = di("identb", [128, 128], BF16)
        self.c_identf = di("identf", [128, 128])
        self.c_cos = di("cosT", [128, NT, 64])
        self.c_sin = di("sinT", [128, NT, 64])
        self.c_tris = di("tri_s", [128, 128], BF16)
        self.c_tri32s = di("tri32s", [32, 32], BF16)
        self.c_tri32i = di("tri32i", [32, 32], BF16)
        self.c_iota160 = di("iota160", [32, NBLK])
        self.c_iotap = di("iotap", [128, 1])
        self.c_iotap32 = di("iotap32", [128, 1])
        self.out = nc.dram_tensor("out", [S, D], F32, kind="ExternalOutput").ap()
        self.xs_d = self.scratch("xs_d", [S, D], F32)
        self.obT_d = self.scratch("obT_d", [D, S], BF16)
        self.gT_d = self.scratch("gT_d", [2 * D, S], BF16)
        self.oaw_d = self.scratch("oaw_d", [3, S, 264], F32)
        self.h2_d = self.scratch("h2_d", [S, D], BF16)
        self.Xs_d = self.scratch("Xs_d", [NBLK * 128, D], BF16)
        self.Out_d = self.scratch("Out_d", [NBLK * 128, D], F32)
        self.modr_d = self.scratch("modr_d", [128, 6, D], F32)
        self.t_modrd = Tok()
        self.t_xs, self.t_obT, self.t_gT, self.t_oaw, self.t_h2d, self.t_Xs, self.t_Outd = [Tok() for _ in range(7)]
        self.t_out = Tok()

    def scratch(self, name, shape, dt):
        if name in self.dbg:
            self.outs.append(name)
            return self.nc.dram_tensor(name, shape, dt, kind="ExternalOutput").ap()
        return self.nc.dram_tensor(name, shape, dt, kind="Internal").ap()

    def sb(self, st, name, shape, dt):
        self._nsb = getattr(self, "_nsb", 0) + 1
        return st.enter_context(self.nc.sbuf_tensor("sb%d_%s" % (self._nsb, name), shape, dt))

    def tt(self, eng, out, in0, in1, op, r, w):
        self.P.op(eng, lambda e: e.tensor_tensor(out=out, in0=in0, in1=in1, op=op), r, w)

    def ts(self, eng, out, in0, s1, op0, r, w, s2=None, op1=None):
        if op1 is None:
            self.P.op(eng, lambda e: e.tensor_scalar(out=out, in0=in0, scalar1=s1, scalar2=None, op0=op0), r, w)
        else:
            self.P.op(eng, lambda e: e.tensor_scalar(out=out, in0=in0, scalar1=s1, scalar2=s2, op0=op0, op1=op1), r, w)

    def stt(self, out, in0, scalar, in1, op0, op1, r, w, accum=None):
        if accum is None:
            self.P.op("dve", lambda e: e.scalar_tensor_tensor(out=out, in0=in0, scalar=scalar, in1=in1, op0=op0, op1=op1), r, w)
        else:
            self.P.op("dve", lambda e: e.scalar_tensor_tensor(out=out, in0=in0, scalar=scalar, in1=in1, op0=op0, op1=op1,
                                                              accum_out=accum), r, w)

    def act(self, out, in_, func, r, w, bias=None, scale=None, accum=None):
        kw = {}
        if bias is not None:
            kw["bias"] = bias
        if scale is not None:
            kw["scale"] = scale
        if accum is not None:
            kw["accum_out"] = accum
        self.P.op("act", lambda e: e.activation(out=out, in_=in_, func=func, **kw), r, w)

    def cp(self, eng, out, in_, r, w):
        if eng == "act":
            self.P.op("act", lambda e: e.copy(out=out, in_=in_), r, w)
        else:
            self.P.op(eng, lambda e: e.tensor_copy(out=out, in_=in_), r, w)

    def mm(self, out, lhsT, rhs, start, stop, r, w):
        self.P.op("pe", lambda e: e.matmul(out, lhsT=lhsT, rhs=rhs, start=start, stop=stop), r, w)

    def tr(self, out, in_, ident, r, w):
        self.P.op("pe", lambda e: e.transpose(out=out, in_=in_, identity=ident), r, w)

    def dma(self, q, out, in_, r, w):
        self.P.dma(q, lambda e: e.dma_start(out=out, in_=in_), r, w)

    def red(self, out, in_, op, r, w):
        self.P.op("dve", lambda e: e.tensor_reduce(out=out, in_=in_, axis=AX.X, op=op), r, w)

    def recip(self, out, in_, r, w):
        self.P.op("dve", lambda e: e.reciprocal(out=out, in_=in_), r, w)

    def memset(self, eng, ap, val, w):
        self.P.op(eng, lambda e: e.memset(ap, val), (), w)

    def gather(self, out, in_, idx, r, w, bound):
        if bound is None:
            self.P.dma("pool", lambda e: e.indirect_dma_start(out=out, out_offset=None, in_=in_,
                                                              in_offset=bass.IndirectOffsetOnAxis(ap=idx, axis=0)), r, w)
            return
        if "pool" not in self.P.prologue:
            self.bregs = {}
            self.bounds = []

            def pro(e):
                for bv in self.bounds:
                    self.bregs[bv] = e.alloc_register("bound_reg_%d" % bv)
                    e.reg_mov(self.bregs[bv], bv)
            self.P.prologue["pool"] = pro
        if bound not in self.bounds:
            self.bounds.append(bound)
        self.P.dma("pool", lambda e: e.indirect_dma_start(out=out, out_offset=None, in_=in_,
                                                          in_offset=bass.IndirectOffsetOnAxis(ap=idx, axis=0),
                                                          bounds_check=self.bregs[bound], oob_is_err=False), r, w)

    def scatter(self, out, in_, idx, r, w, bound):
        self.P.dma("pool", lambda e: e.indirect_dma_start(out=out, out_offset=bass.IndirectOffsetOnAxis(ap=idx, axis=0),
                                                          in_=in_, in_offset=None), r, w)

    def build(self):
        nc = self.nc
        with ExitStack() as gst:
            self.gst = gst
            self.ps = [gst.enter_context(nc.psum_tensor("ps%d" % i, [128, 512], F32)) for i in range(8)]
            self.tp = [Tok() for _ in range(8)]
            sb = self.sb
            self.identb = sb(gst, "identb", [128, 128], BF16)
            self.identf = sb(gst, "identf", [128, 128], F32)
            self.onesb = sb(gst, "onesb", [128, 128], BF16)
            self.onesf = sb(gst, "onesf", [128, 128], F32)
            self.epsc = sb(gst, "epsc", [128, 1], F32)
            self.t_const = Tok()
            tc = [self.t_const]
            self.dma("sp", self.identb[:], self.c_identb, [], tc)
            self.dma("sp", self.identf[:], self.c_identf, [], tc)
            self.memset("dve", self.onesb[:], 1.0, tc)
            self.memset("dve", self.onesf[:], 1.0, tc)
            self.memset("dve", self.epsc[:], EPS, tc)
            self.neghalf = sb(gst, "neghalf", [128, 16], F32)
            self.memset("dve", self.neghalf[:], -0.5, tc)
            self.P.barrier()
            xsrc = self.x_in
            for l in range(self.nlayers):
                last = (l == self.nlayers - 1)
                if self.layer(l, xsrc, last):
                    break
                xsrc = self.xs_d
            self.P.barrier()
            self.P.emit()
        return nc

    def layer(self, l, xsrc, last):
        P = self.P
        stop = self.stop if l == self.nlayers - 1 else None
        with ExitStack() as lst:
            self.oaT = self.sb(lst, "oaT", [128, 2, S], BF16)
            self.t_oaT = Tok()
            self.phase_mod(l)
            P.barrier()
            if stop == "mod":
                return True
            with ExitStack() as ast:
                self.hT = self.sb(ast, "hT", [128, 8, S], BF16)
                self.t_hT = Tok()
                self.phase_norm1(l, xsrc)
                P.barrier()
                if stop == "norm1":
                    return True
                self.phase_gates(l)
                P.barrier()
                if stop == "gates":
                    return True
                self.phase_window(l)
                P.barrier()
                if stop == "window":
                    return True
                self.phase_gqa(l)
                P.barrier()
                if stop == "gqa":
                    return True
            with ExitStack() as mst:
                self.logits_all = self.sb(mst, "logits_all", [128, NT, NE], F32)
                self.max8_all = self.sb(mst, "max8_all", [128, NT, 8], F32)
                self.g4_all = self.sb(mst, "g4_all", [128, NT, 4], F32)
                self.M_all = self.sb(mst, "M_all", [128, NT, NE], BF16)
                self.pos4_all = self.sb(mst, "pos4_all", [128, NT, 4], I32)
                self.widx = self.sb(mst, "widx", [128, NBLK], I32)
                self.widxA = self.sb(mst, "widxA", [128, NBLK], I32)
                self.widxB = self.sb(mst, "widxB", [128, NBLK], I32)
                self.OHall = self.sb(mst, "OHall", [64, NBLK], BF16)
                self.t_widx = Tok()
                self.t_route = Tok()
                self.t_pos4 = Tok()
                self.phase_merge(l, xsrc)
                P.barrier()
                if stop == "merge":
                    return True
                self.phase_slots(l)
                P.barrier()
                if stop == "slots":
                    return True
                self.phase_experts(l)
                P.barrier()
                if stop == "experts":
                    return True
                self.phase_combine(l, last)
                P.barrier()
        return False

    def phase_mod(self, l):
        with ExitStack() as st:
            sb = self.sb
            cc = sb(st, "cc", [128, 8], F32)
            cs = sb(st, "cs", [128, 8], F32)
            crep = sb(st, "crep", [128, 8, 128], F32)
            brep = sb(st, "brep", [128, 6 * D], F32)
            ng = sb(st, "ng", [128, 2, D], F32)
            modr = sb(st, "modr", [128, 6, D], F32)
            t_modr = Tok()
            wa = Ring([sb(st, "wa%d" % i, [128, 8, 512], F32) for i in range(2)])
            t_cc, t_cs, t_crep, t_brep, t_ng = Tok(), Tok(), Tok(), Tok(), Tok()
            self.dma("sp", cc[:], self.ccol, [], [t_cc])
            self.dma("sp", brep[:], self.b_ada[l:l + 1, :].partition_broadcast(128), [], [t_brep])
            self.dma("sp", ng[:, 0, :], self.norm1_g[l:l + 1, :].partition_broadcast(128), [], [t_ng])
            self.dma("sp", ng[:, 1, :], self.norm2_g[l:l + 1, :].partition_broadcast(128), [], [t_ng])
            self.act(cs[:], cc[:], AF.Silu, [t_cc], [t_cs])
            for kc in range(8):
                self.cp("dve", crep[:, kc, :], cs[:, kc:kc + 1].to_broadcast([128, 128]), [t_cs], [t_crep])
            modf = modr[:].rearrange("p a d -> p (a d)")
            psr = Ring(self.ps[0:2])
            psr.items = [(self.ps[0], self.tp[0]), (self.ps[1], self.tp[1])]
            for ch in range(12):
                w, t_w = wa.next()
                self.dma("sp", w[:], self.w_ada[l][:, ch * 512:(ch + 1) * 512].rearrange("(kc p) n -> p kc n", p=128), [], [t_w])
                ps, t_ps = psr.next()
                for kc in range(8):
                    self.mm(ps[:], crep[:, kc, :], w[:, kc, :], kc == 0, kc == 7, [t_crep, t_w], [t_ps])
                self.tt("dve", modf[:, ch * 512:(ch + 1) * 512], ps[:], brep[:, ch * 512:(ch + 1) * 512], ALU.add,
                        [t_ps, t_brep], [t_modr])
            for i, j in ((1, 0), (4, 1)):
                self.stt(modr[:, i, :], modr[:, i, :], 1.0, ng[:, j, :], ALU.add, ALU.mult, [t_modr, t_ng], [t_modr])
            self.dma("sp", self.modr_d, modr[:], [t_modr], [self.t_modrd])

    def load_mod(self, st, idxs):
        tl = self.sb(st, "modl", [128, len(idxs), D], F32)
        self.t_modr = Tok()
        self.modr = {}
        for n, i in enumerate(idxs):
            self.dma("sp", tl[:, n, :], self.modr_d[:, i, :], [self.t_modrd], [self.t_modr])
            self.modr[i] = tl[:, n, :]

    def rstd_from_ss(self, rstd, ss, n, t_in, t_out):
        self.act(rstd, ss, AF.Ln, [t_in, self.t_const], [t_out], bias=self.epsc[0:rstd.shape[0], 0:1], scale=1.0 / n)
        self.act(rstd, rstd, AF.Exp, [t_out], [t_out], scale=-0.5)

    def norm_mod(self, xt, t_x, i_g, i_sh, work, hb, t_hb, hf=None):
        junk, ss, rstd, tmp, t_w = work
        self.stt(junk[:], xt[:], 1.0, xt[:], ALU.mult, ALU.mult, [t_x], [t_w], accum=ss[:, 0:1])
        self.rstd_from_ss(rstd[:, 0:1], ss[:, 0:1], D, t_w, t_w)
        self.stt(tmp[:], xt[:], rstd[:, 0:1], self.modr[i_g], ALU.mult, ALU.mult, [t_x, t_w, self.t_modr], [t_w])
        if hf is not None:
            self.tt("dve", hf[:], tmp[:], self.modr[i_sh], ALU.add, [t_w, self.t_modr], [t_hb])
            self.cp("pool", hb[:], hf[:], [t_hb], [t_hb])
        else:
            self.tt("dve", hb[:], tmp[:], self.modr[i_sh], ALU.add, [t_w, self.t_modr], [t_hb])

    def phase_norm1(self, l, xsrc):
        with ExitStack() as st:
            sb = self.sb
            self.load_mod(st, [0, 1])
            xr = Ring([sb(st, "xt%d" % i, [128, D], F32) for i in range(2)])
            hr = Ring([sb(st, "hb%d" % i, [128, D], BF16) for i in range(2)])
            work = (sb(st, "junk", [128, D], F32), sb(st, "ss", [128, 1], F32), sb(st, "rstd", [128, 1], F32),
                    sb(st, "tmp", [128, D], F32), Tok())
            psb = [self.ps[i][:].bitcast(BF16).rearrange("p (a b) -> p a b", a=8) for i in range(2)]
            for t in range(NT):
                xt, t_x = xr.next()
                hb, t_hb = hr.next()
                self.dma("sp", xt[:], xsrc[t * 128:(t + 1) * 128, :], [self.t_xs], [t_x])
                self.norm_mod(xt, t_x, 1, 0, work, hb, t_hb)
                pT, t_p = psb[t % 2], self.tp[t % 2]
                for kc in range(8):
                    self.tr(pT[:, kc, :], hb[:, kc * 128:(kc + 1) * 128], self.identb[:], [t_hb, self.t_const], [t_p])
                self.cp("act", self.hT[:, :, t * 128:(t + 1) * 128], pT, [t_p], [self.t_hT])

    def load_w(self, w, src, t_w, kc=8):
        self.dma("pool", w, src.rearrange("(kc p) n -> p kc n", p=128), [], [t_w])

    def phase_gates(self, l):
        with ExitStack() as st:
            sb = self.sb
            wg = sb(st, "wg", [128, 8, 2 * D], BF16)
            t_wg = Tok()
            for q in range(4):
                self.load_w(wg[:, :, q * 512:(q + 1) * 512], self.w_in[l][:, 3840 + q * 512:3840 + (q + 1) * 512], t_wg)
            gr = Ring([sb(st, "gsb%d" % i, [128, 512], BF16) for i in range(3)])
            n = 0
            for c in range(8):
                for m in range(16):
                    ps, t_ps = self.ps[n % 4], self.tp[n % 4]
                    n += 1
                    for kc in range(8):
                        self.mm(ps[:], wg[:, kc, m * 128:(m + 1) * 128], self.hT[:, kc, c * 512:(c + 1) * 512], kc == 0, kc == 7,
                                [t_wg, self.t_hT], [t_ps])
                    g, t_g = gr.next()
                    self.act(g[:], ps[:], AF.Sigmoid, [t_ps], [t_g])
                    self.dma("sp", self.gT_d[m * 128:(m + 1) * 128, c * 512:(c + 1) * 512], g[:], [t_g, self.t_gT], [])

    def qknorm_rope(self, src, H, grep, tile, wk, dst, t_src, t_dst):
        sq, ss, rstd, qn, t1, t2, t_w = wk
        W = H * 64
        v3 = lambda ap: ap[:, 0:W].rearrange("p (h d) -> p h d", h=H)
        v5 = lambda ap: ap[:, 0:W].rearrange("p (h a b c) -> p h a b c", h=H, a=2, b=2)
        self.tt("pool", sq[:, 0:W], src, src, ALU.mult, [t_src], [t_w])
        self.red(ss[:, 0:H], v3(sq), ALU.add, [t_w], [t_w])
        self.ts("pool", rstd[:, 0:H], ss[:, 0:H], 1.0 / 64, ALU.mult, [t_w], [t_w], s2=EPS, op1=ALU.add)
        self.tt("pool", rstd[:, 0:H], rstd[:, 0:H], self.neghalf[:, 0:H], ALU.pow, [t_w, self.t_const], [t_w])
        self.tt("dve", v3(qn), src.rearrange("p (h d) -> p h d", h=H), rstd[:, 0:H].unsqueeze(2).to_broadcast([128, H, 64]), ALU.mult,
                [t_src, t_w], [t_w])
        self.tt("pool", v3(qn), v3(qn), grep.unsqueeze(1).to_broadcast([128, H, 64]), ALU.mult, [t_w, self.t_const], [t_w])
        cosb = self.cos[:, tile, :].unsqueeze(1).to_broadcast([128, H, 64])
        sinv = self.sin[:, tile, :].rearrange("p (a b c) -> p a b c", a=2, b=2)
        self.tt("dve", v3(t1), v3(qn), cosb, ALU.mult, [t_w, self.t_const], [t_w])
        for b in range(2):
            sb_ = sinv[:, :, b, :].unsqueeze(1).to_broadcast([128, H, 2, 16])
            self.tt("pool", v5(t2)[:, :, :, b, :], v5(qn)[:, :, :, 1 - b, :], sb_, ALU.mult, [t_w, self.t_const], [t_w])
        self.tt("dve", dst, t1[:, 0:W], t2[:, 0:W], ALU.add, [t_w], [t_dst])

    def phase_gqa(self, l):
        with ExitStack() as st:
            sb = self.sb
            self.cos = sb(st, "cos", [128, NT, 64], F32)
            self.sin = sb(st, "sin", [128, NT, 64], F32)
            self.dma("sp", self.cos[:], self.c_cos, [], [self.t_const])
            self.dma("sp", self.sin[:], self.c_sin, [], [self.t_const])
            KT = sb(st, "KT", [128, 2, S], BF16)
            V = sb(st, "V", [128, NT, 4, 128], BF16)
            wkv = sb(st, "wkv", [128, 8, 512], BF16)
            gq = sb(st, "gq", [128, 64], F32)
            gk = sb(st, "gk", [128, 64], F32)
            t_KT, t_V, t_wkv = Tok(), Tok(), Tok()
            self.dma("sp", gq[:], self.q_norm_g[l:l + 1, :].partition_broadcast(128), [], [self.t_const])
            self.dma("sp", gk[:], self.k_norm_g[l:l + 1, :].partition_broadcast(128), [], [self.t_const])
            self.load_w(wkv[:], self.w_in[l][:, 3328:3840], t_wkv)
            self.memset("dve", V[:, :, :, 64:128], 1.0, [t_V])
            wk = (sb(st, "q_sq", [128, 256], F32), sb(st, "q_ss", [128, 4], F32), sb(st, "q_rstd", [128, 4], F32),
                  sb(st, "q_qn", [128, 256], F32), sb(st, "q_t1", [128, 256], F32), sb(st, "q_t2", [128, 256], F32), Tok())
            psT = self.ps[7][:].bitcast(BF16).rearrange("p (a b) -> p a b", a=8)
            t_psT = self.tp[7]
            ksr = Ring([sb(st, "ksb3_%d" % i, [128, 256], F32) for i in range(3)])
            krr = Ring([sb(st, "kr3_%d" % i, [128, 256], BF16) for i in range(3)])
            kst = {}

            def kv1(t):
                ps, t_ps = self.ps[6], self.tp[6]
                for kc in range(8):
                    self.mm(ps[:], self.hT[:, kc, t * 128:(t + 1) * 128], wkv[:, kc, :], kc == 0, kc == 7, [self.t_hT, t_wkv], [t_ps])
                self.cp("act", V[:, t, :, 0:64], ps[:, 256:512].rearrange("p (g d) -> p g d", g=4), [t_ps], [t_V])
                ksb, t_ksb = ksr.next()
                self.cp("act", ksb[:], ps[:, 0:256], [t_ps], [t_ksb])
                kr, t_kr = krr.next()
                self.qknorm_rope(ksb[:], 4, gk[:], t, wk, kr[:], t_ksb, t_kr)
                kst[t] = (kr, t_kr)

            def kv2(t):
                kr, t_kr = kst.pop(t)
                for m in range(2):
                    self.tr(psT[:, m, :], kr[:, m * 128:(m + 1) * 128], self.identb[:], [t_kr, self.t_const], [t_psT])
                self.cp("dve", KT[:, :, t * 128:(t + 1) * 128], psT[:, 0:2, :], [t_psT], [t_KT])

            kv1(0)
            kv1(1)
            for t in range(NT):
                kv2(t)
                if t + 2 < NT:
                    kv1(t + 2)
            wq = sb(st, "wq", [128, 8, 256], BF16)
            t_wq = Tok()
            QTb = [(sb(st, "QT%d" % i, [128, 4, 512], BF16), Tok()) for i in range(2)]
            PTr = Ring([sb(st, "PT%d" % i, [128, 512], BF16) for i in range(4)])
            rdr = Ring([sb(st, "rd%d" % i, [64, 512], F32) for i in range(2)])
            obr = Ring([sb(st, "ob%d" % i, [64, 512], BF16) for i in range(2)])
            Sr = Ring([None] * 3)
            Sr.items = [(self.ps[i], self.tp[i]) for i in range(3)]
            Or = Ring([None] * 2)
            Or.items = [(self.ps[i], self.tp[i]) for i in (3, 4)]
            units = [(g, c) for g in range(4) for c in range(8)]

            qst = {}

            def qproj1(u, t4):
                g, c = units[u]
                if c == 0 and t4 == 0:
                    self.load_w(wq[:], self.w_in[l][:, 2304 + g * 256:2304 + (g + 1) * 256], t_wq)
                t = c * 4 + t4
                ps, t_ps = self.ps[6], self.tp[6]
                for kc in range(8):
                    self.mm(ps[:, 0:256], self.hT[:, kc, t * 128:(t + 1) * 128], wq[:, kc, :], kc == 0, kc == 7,
                            [self.t_hT, t_wq], [t_ps])
                qsb, t_qsb = ksr.next()
                self.cp("dve", qsb[:], ps[:, 0:256], [t_ps], [t_qsb])
                qr, t_qr = krr.next()
                self.qknorm_rope(qsb[:], 4, gq[:], t, wk, qr[:], t_qsb, t_qr)
                qst[(u, t4)] = (qr, t_qr)

            def qproj2(u, t4):
                g, c = units[u]
                kb = (g % 2) * 64
                ko = 64 - kb
                QT, t_QT = QTb[u % 2]
                qr, t_qr = qst.pop((u, t4))
                for h in range(4):
                    self.tr(psT[0:64, h, :], qr[:, h * 64:(h + 1) * 64], self.identb[:], [t_qr, self.t_const], [t_psT])
                self.cp("dve", QT[kb:kb + 64, :, t4 * 128:(t4 + 1) * 128], psT[0:64, 0:4, :], [t_psT], [t_QT])
                self.memset("pool", QT[ko:ko + 64, :, t4 * 128:(t4 + 1) * 128], 0.0, [t_QT])

            def attend(u, h):
                g, c = units[u]
                QT, t_QT = QTb[u % 2]
                hq = g * 4 + h
                OT, t_OT = Or.next()
                pend = []

                def score(kt):
                    Sb, t_S = Sr.next()
                    self.mm(Sb[:], KT[:, g // 2, kt * 128:(kt + 1) * 128], QT[:, h, :], True, True, [t_KT, t_QT], [t_S])
                    PT, t_PT = PTr.next()
                    self.act(PT[:], Sb[:], AF.Exp, [t_S], [t_PT], scale=0.125)
                    pend.append((kt, PT, t_PT))

                def pv():
                    kt, PT, t_PT = pend.pop(0)
                    self.mm(OT[:], V[:, kt, g, :], PT[:], kt == 0, kt == NT - 1, [t_V, t_PT], [t_OT])

                score(0)
                score(1)
                for kt in range(NT):
                    if kt + 2 < NT:
                        score(kt + 2)
                    pv()
                def fin():
                    rd, t_rd = rdr.next()
                    self.recip(rd[0:64, :], OT[64:128, :], [t_OT], [t_rd])
                    ob, t_ob = obr.next()
                    self.tt("dve", ob[:], OT[0:64, :], rd[0:64, :], ALU.mult, [t_OT, t_rd], [t_ob])
                    self.dma("sp", self.obT_d[hq * 64:(hq + 1) * 64, c * 512:(c + 1) * 512], ob[:], [t_ob], [self.t_obT])
                return fin

            for t4 in range(4):
                qproj1(0, t4)
                qproj2(0, t4)
            for u in range(len(units)):
                nxt = u + 1 < len(units)
                f = attend(u, 0)
                f()
                if nxt:
                    qproj1(u + 1, 0)
                    qproj1(u + 1, 1)
                f = attend(u, 1)
                if nxt:
                    qproj2(u + 1, 0)
                    qproj2(u + 1, 1)
                f()
                if nxt:
                    qproj1(u + 1, 2)
                f = attend(u, 2)
                if nxt:
                    qproj2(u + 1, 2)
                f()
                if nxt:
                    qproj1(u + 1, 3)
                f = attend(u, 3)
                if nxt:
                    qproj2(u + 1, 3)
                f()

    def phase_window(self, l):
        with ExitStack() as st:
            sb = self.sb
            bias = sb(st, "wb", [128, 12, 384], F32)
            t_bias = Tok()
            self.dma("sp", bias[:], self.wbias, [], [t_bias])
            QT = sb(st, "wQT", [128, 2, S], BF16)
            KT = sb(st, "wKT", [128, 2, S], BF16)
            Vf = sb(st, "wVf", [128, NT, 256], BF16)
            wq = sb(st, "wwq", [128, 8, 768], BF16)
            t_QT, t_KT, t_Vf, t_wq = Tok(), Tok(), Tok(), Tok()
            sr = Ring([sb(st, "ws%d" % i, [128, 384], F32) for i in range(3)])
            pr = Ring([sb(st, "wp%d" % i, [128, 384], BF16) for i in range(4)])
            ptr = Ring([sb(st, "wpt%d" % i, [128, 3, 128], BF16) for i in range(2)])
            mr = Ring([sb(st, "wm%d" % i, [128, 2], F32) for i in range(6)])
            stg = Ring([sb(st, "wstg%d" % i, [128, 264], F32) for i in range(2)])
            Sr = Ring([None] * 3)
            Sr.items = [(self.ps[i], self.tp[i]) for i in (0, 1, 7)]
            Tr = Ring([None] * 2)
            Tr.items = [(self.ps[i][:].bitcast(BF16)[:, 0:384].rearrange("p (a b) -> p a b", a=3), self.tp[i]) for i in (2, 3)]
            Or = Ring([None] * 2)
            Or.items = [(self.ps[i], self.tp[i]) for i in (4, 5)]
            import os
            wstop = int(os.environ.get("WSTOP", "99"))
            for a, Dl in enumerate(A_DIL):
                L = S // Dl
                nj = L // 128
                if str(a) not in os.environ.get("WGRPS", "012"):
                    continue
                self.load_w(wq[:], self.w_in[l][:, a * 768:(a + 1) * 768], t_wq)
                n = 0
                for which, dst, t_dst in ((0, QT, t_QT), (1, KT, t_KT)):
                    for m in range(2):
                        for c in range(8):
                            ps, t_ps = self.ps[6], self.tp[6]
                            n += 1
                            col = which * 256 + m * 128
                            for kc in range(8):
                                self.mm(ps[:], wq[:, kc, col:col + 128], self.hT[:, kc, c * 512:(c + 1) * 512], kc == 0, kc == 7,
                                        [t_wq, self.t_hT], [t_ps])
                            self.cp("act" if n % 2 else "dve", dst[:, m, c * 512:(c + 1) * 512], ps[:], [t_ps], [t_dst])
                if wstop <= 1:
                    continue
                for r in range(Dl):
                    for j in range(nj):
                        bi = r * nj + j
                        tok0 = j * 128 * Dl + r
                        ps, t_ps = self.ps[6], self.tp[6]
                        for kc in range(8):
                            self.mm(ps[:, 0:256], self.hT[:, kc, sl(tok0, 128, Dl)], wq[:, kc, 512:768], kc == 0, kc == 7,
                                    [self.t_hT, t_wq], [t_ps])
                        self.cp("act" if bi % 2 else "dve", Vf[:, bi, :], ps[:, 0:256], [t_ps], [t_Vf])
                if wstop <= 2:
                    continue
                units = []
                for r in range(Dl):
                    for j in range(nj):
                        for h in range(4):
                            units.append((r, j, h))
                ust = {}
                blk = {}

                def W1(n):
                    r, j, h = units[n]
                    tok0 = j * 128 * Dl + r
                    jt0 = max(j - 1, 0)
                    jt1 = min(j + 1, nj - 1)
                    ntl = jt1 - jt0 + 1
                    c0 = (jt0 - (j - 1)) * 128
                    w = ntl * 128
                    k0 = jt0 * 128 * Dl + r
                    if h == 0:
                        blk[(r, j)] = (Or.next(), stg.next())
                    (O4, t_O4), (sg, t_sg) = blk[(r, j)]
                    Sb, t_S = Sr.next()
                    hb_ = (h % 2) * 64
                    self.mm(Sb[:, 0:w], QT[hb_:hb_ + 64, h // 2, sl(tok0, 128, Dl)], KT[hb_:hb_ + 64, h // 2, sl(k0, w, Dl)],
                            True, True, [t_QT, t_KT], [t_S])
                    s_, t_s = sr.next()
                    self.stt(s_[:, 0:w], Sb[:, 0:w], 0.125, bias[:, a * 4 + h, c0:c0 + w], ALU.mult, ALU.add, [t_S, t_bias], [t_s])
                    m, t_m = mr.next()
                    self.P.op("dve", lambda e, m=m, s_=s_, w=w: e.reduce_max(out=m[:, 0:1], in_=s_[:, 0:w], axis=AX.X), [t_s], [t_m])
                    self.ts("dve", m[:, 1:2], m[:, 0:1], -1.0, ALU.mult, [t_m], [t_m])
                    self.cp("dve", sg[:, 256 + h:257 + h], m[:, 0:1], [t_m], [t_sg])
                    ust[n] = (s_, t_s, m, t_m, sg, t_sg, w, h, ntl, jt0)

                def W1b(n):
                    s_, t_s, m, t_m, sg, t_sg, w, h, ntl, jt0 = ust[n]
                    p, t_p = pr.next()
                    self.act(p[:, 0:w], s_[:, 0:w], AF.Exp, [t_s, t_m], [t_p, t_sg], bias=m[:, 1:2], scale=1.0,
                             accum=sg[:, 260 + h:261 + h])
                    ust[n] = (p, t_p, ntl, jt0)

                def W2(n):
                    r, j, h = units[n]
                    tok0 = j * 128 * Dl + r
                    p, t_p, ntl, jt0 = ust.pop(n)
                    (O4, t_O4), (sg, t_sg) = blk[(r, j)]
                    pT, t_pT = Tr.next()
                    for ti in range(ntl):
                        self.tr(pT[:, ti, :], p[:, ti * 128:(ti + 1) * 128], self.identb[:], [t_p, self.t_const], [t_pT])
                    pts, t_pts = ptr.next()
                    self.cp("act", pts[:, 0:ntl, :], pT[:, 0:ntl, :], [t_pT], [t_pts])
                    for ti in range(ntl):
                        self.mm(O4[:, h * 64:(h + 1) * 64], pts[:, ti, :], Vf[:, r * nj + jt0 + ti, h * 64:(h + 1) * 64],
                                ti == 0, ti == ntl - 1, [t_pts, t_Vf], [t_O4])
                    if h == 3:
                        self.cp("act", sg[:, 0:256], O4[:, 0:256], [t_O4], [t_sg])
                        self.dma("sp", self.oaw_d[a, sl(tok0, 128, Dl), :], sg[:], [t_sg], [self.t_oaw])
                        del blk[(r, j)]

                NU = len(units)
                W1(0)
                W1(1)
                W1b(0)
                for n in range(NU):
                    if n + 2 < NU:
                        W1(n + 2)
                    W2(n)
                    if n + 1 < NU:
                        W1b(n + 1)
            if wstop <= 4:
                return
            self.P.barrier()
            cr = Ring([sb(st, "wc%d" % i, [128, 3, 264], F32) for i in range(2)])
            ms = sb(st, "wms", [128, 4], F32)
            wgt = sb(st, "wwgt", [128, 3, 4], F32)
            dt_ = sb(st, "wdt", [128, 4], F32)
            acc = sb(st, "wacc", [128, 256], F32)
            tmp = sb(st, "wtmp", [128, 256], F32)
            obr = Ring([sb(st, "wob%d" % i, [128, 256], BF16) for i in range(2)])
            t_k = Tok()
            for t in range(NT):
                ct, t_ct = cr.next()
                self.dma("sp", ct[:], self.oaw_d[:, t * 128:(t + 1) * 128, :].rearrange("a p c -> p a c"), [self.t_oaw], [t_ct])
                mv = ct[:, :, 256:260]
                dv = ct[:, :, 260:264]
                self.tt("dve", ms[:], mv[:, 0, :], mv[:, 1, :], ALU.max, [t_ct], [t_k])
                self.tt("dve", ms[:], ms[:], mv[:, 2, :], ALU.max, [t_ct, t_k], [t_k])
                self.tt("dve", wgt[:], mv, ms[:].unsqueeze(1).to_broadcast([128, 3, 4]), ALU.subtract, [t_ct, t_k], [t_k])
                self.act(wgt[:], wgt[:], AF.Exp, [t_k], [t_k])
                self.tt("dve", dv, dv, wgt[:], ALU.mult, [t_ct, t_k], [t_ct])
                self.tt("dve", dt_[:], dv[:, 0, :], dv[:, 1, :], ALU.add, [t_ct], [t_k])
                self.tt("dve", dt_[:], dt_[:], dv[:, 2, :], ALU.add, [t_ct, t_k], [t_k])
                self.recip(dt_[:], dt_[:], [t_k], [t_k])
                self.tt("dve", wgt[:], wgt[:], dt_[:].unsqueeze(1).to_broadcast([128, 3, 4]), ALU.mult, [t_k], [t_k])
                ob, t_ob = obr.next()
                v3 = lambda ap: ap.rearrange("p (h d) -> p h d", h=4)
                for a in range(3):
                    cb = wgt[:, a, :].unsqueeze(2).to_broadcast([128, 4, 64])
                    if a == 0:
                        self.tt("dve", v3(acc[:]), v3(ct[:, 0, 0:256]), cb, ALU.mult, [t_ct, t_k], [t_k])
                    else:
                        self.tt("dve", v3(tmp[:]), v3(ct[:, a, 0:256]), cb, ALU.mult, [t_ct, t_k], [t_k])
                        if a == 1:
                            self.tt("dve", acc[:], acc[:], tmp[:], ALU.add, [t_k], [t_k])
                        else:
                            self.tt("dve", ob[:], acc[:], tmp[:], ALU.add, [t_k], [t_ob])
                pT, t_pT = Tr.next()
                for kc in range(2):
                    self.tr(pT[:, kc, :], ob[:, kc * 128:(kc + 1) * 128], self.identb[:], [t_ob, self.t_const], [t_pT])
                self.cp("act", self.oaT[:, :, t * 128:(t + 1) * 128], pT[:, 0:2, :], [t_pT], [self.t_oaT])

    def phase_merge(self, l, xsrc):
        with ExitStack() as st:
            sb = self.sb
            self.load_mod(st, [2, 3, 4])
            wa = sb(st, "m_wa", [128, 2, D], BF16)
            wb = sb(st, "m_wb", [128, 8, D], BF16)
            wo = sb(st, "m_wo", [128, 8, D], BF16)
            wr = sb(st, "m_wr", [128, 8, NE], F32)
            brr = sb(st, "m_brr", [128, NE], F32)
            t_w = Tok()
            self.load_w(wa[:], self.w_br_a[l], t_w)
            self.load_w(wb[:], self.w_br_b[l], t_w)
            self.load_w(wo[:], self.w_out[l], t_w)
            self.dma("sp", wr[:], self.w_router[l].rearrange("(kc p) n -> p kc n", p=128), [], [t_w])
            self.dma("sp", brr[:], self.b_router[l:l + 1, :].partition_broadcast(128), [], [t_w])
            gtr = Ring([sb(st, "m_gt%d" % i, [128, 16, 512], BF16) for i in range(1)])
            obr = Ring([sb(st, "m_ob%d" % i, [128, 8, 512], BF16) for i in range(1)])
            mgr = Ring([sb(st, "m_mg%d" % i, [128, 8, 512], BF16) for i in range(1)])
            tA = sb(st, "m_tA", [128, 512], F32)
            tB = sb(st, "m_tB", [128, 512], F32)
            t_tA, t_tB = Tok(), Tok()
            xr = Ring([sb(st, "m_x%d" % i, [128, D], F32) for i in range(2)])
            x1r = Ring([sb(st, "m_x1%d" % i, [128, D], F32) for i in range(2)])
            h2fr = Ring([sb(st, "m_h2f%d" % i, [128, D], F32) for i in range(2)])
            h2br = Ring([sb(st, "m_h2b%d" % i, [128, D], BF16) for i in range(2)])
            h2Tr = Ring([sb(st, "m_h2T%d" % i, [128, 8, 128], F32) for i in range(1)])
            work = (sb(st, "m_junk", [128, D], F32), sb(st, "m_ss", [128, 1], F32), sb(st, "m_rstd", [128, 1], F32),
                    sb(st, "m_tmp", [128, D], F32), Tok())
            e4 = sb(st, "m_e4", [128, 4], F32)
            ssum = sb(st, "m_ssum", [128, 2], F32)
            t_e4 = Tok()
            Ar = Ring([None] * 2)
            Ar.items = [(self.ps[i], self.tp[i]) for i in (0, 1)]
            Br = Ring([None] * 2)
            Br.items = [(self.ps[i], self.tp[i]) for i in (2, 3)]
            Or = Ring([None] * 2)
            Or.items = [(self.ps[i], self.tp[i]) for i in (4, 5)]
            psT2 = [self.ps[i][:].rearrange("p (a b) -> p a b", a=4) for i in (6, 7)]
            for c in range(8):
                gt, t_gt = gtr.next()
                ob, t_ob = obr.next()
                mg, t_mg = mgr.next()
                self.dma("sp", gt[:], self.gT_d[:, c * 512:(c + 1) * 512].rearrange("(m p) n -> p m n", p=128), [self.t_gT], [t_gt])
                self.dma("sp", ob[:], self.obT_d[:, c * 512:(c + 1) * 512].rearrange("(m p) n -> p m n", p=128), [self.t_obT], [t_ob])
                for m in range(8):
                    pA, t_pA = Ar.next()
                    pB, t_pB = Br.next()
                    for kc in range(2):
                        self.mm(pA[:], wa[:, kc, m * 128:(m + 1) * 128], self.oaT[:, kc, c * 512:(c + 1) * 512], kc == 0, kc == 1,
                                [t_w, self.t_oaT], [t_pA])
                    for kc in range(8):
                        self.mm(pB[:], wb[:, kc, m * 128:(m + 1) * 128], ob[:, kc, :], kc == 0, kc == 7, [t_w, t_ob], [t_pB])
                    self.tt("dve", tA[:], pA[:], gt[:, m, :], ALU.mult, [t_pA, t_gt], [t_tA])
                    self.tt("dve", tB[:], pB[:], gt[:, 8 + m, :], ALU.mult, [t_pB, t_gt], [t_tB])
                    self.tt("pool", mg[:, m, :], tA[:], tB[:], ALU.add, [t_tA, t_tB], [t_mg])
                for t4 in range(4):
                    t = c * 4 + t4
                    xt, t_x = xr.next()
                    x1, t_x1 = x1r.next()
                    self.dma("sp", xt[:], xsrc[t * 128:(t + 1) * 128, :], [self.t_xs], [t_x])
                    for hf in range(2):
                        pO, t_pO = Or.next()
                        for kc in range(8):
                            self.mm(pO[:], mg[:, kc, t4 * 128:(t4 + 1) * 128], wo[:, kc, hf * 512:(hf + 1) * 512], kc == 0, kc == 7,
                                    [t_mg, t_w], [t_pO])
                        self.tt("dve", x1[:, hf * 512:(hf + 1) * 512], pO[:], self.modr[2][:, hf * 512:(hf + 1) * 512], ALU.mult,
                                [t_pO, self.t_modr], [t_x1])
                    self.tt("pool", x1[:], x1[:], xt[:], ALU.add, [t_x1, t_x], [t_x1])
                    self.dma("sp", self.xs_d[t * 128:(t + 1) * 128, :], x1[:], [t_x1], [self.t_xs])
                    h2f, t_h2f = h2fr.next()
                    h2b, t_h2b = h2br.next()
                    self.norm_mod(x1, t_x1, 4, 3, work, h2b, t_h2f, hf=h2f)
                    self.dma("sp", self.h2_d[t * 128:(t + 1) * 128, :], h2b[:], [t_h2f], [self.t_h2d])
                    h2T, t_h2T = h2Tr.next()
                    for hh in range(2):
                        for k4 in range(4):
                            kc = hh * 4 + k4
                            self.tr(psT2[hh][:, k4, :], h2f[:, kc * 128:(kc + 1) * 128], self.identf[:], [t_h2f, self.t_const], [self.tp[6 + hh]])
                        self.cp("act", h2T[:, hh * 4:(hh + 1) * 4, :], psT2[hh], [self.tp[6 + hh]], [t_h2T])
                    pL, t_pL = Or.next()
                    for kc in range(8):
                        self.mm(pL[:, 0:NE], h2T[:, kc, :], wr[:, kc, :], kc == 0, kc == 7, [t_h2T, t_w], [t_pL])
                    lg = self.logits_all[:, t, :]
                    m8 = self.max8_all[:, t, :]
                    self.tt("dve", lg, pL[:, 0:NE], brr[:], ALU.add, [t_pL, t_w], [self.t_route])
                    self.P.op("dve", lambda e, m8=m8, lg=lg: e.max(out=m8, in_=lg), [self.t_route], [self.t_route])
                    self.ts("dve", self.M_all[:, t, :], lg, m8[:, 3:4], ALU.is_ge, [self.t_route], [self.t_route])
                    self.ts("dve", ssum[:, 1:2], m8[:, 0:1], -1.0, ALU.mult, [self.t_route], [t_e4])
                    self.act(e4[:], m8[:, 0:4], AF.Exp, [self.t_route, t_e4], [t_e4], bias=ssum[:, 1:2], scale=1.0, accum=ssum[:, 0:1])
                    self.recip(ssum[:, 0:1], ssum[:, 0:1], [t_e4], [t_e4])
                    self.ts("dve", self.g4_all[:, t, :], e4[:], ssum[:, 0:1], ALU.mult, [t_e4], [self.t_route])

    def phase_slots(self, l):
        sb = self.sb
        with ExitStack() as s2:
            tris = sb(s2, "s_tris", [128, 128], BF16)
            tri32s = sb(s2, "s_t32s", [32, 32], BF16)
            tri32i = sb(s2, "s_t32i", [32, 32], BF16)
            iota160 = sb(s2, "s_iota", [32, NBLK], F32)
            iotap = sb(s2, "s_iotap", [128, 1], F32)
            iotap32 = sb(s2, "s_iotap32", [128, 1], F32)
            t_c = Tok()
            for dst, src in ((tris, self.c_tris), (tri32s, self.c_tri32s), (tri32i, self.c_tri32i), (iota160, self.c_iota160),
                             (iotap, self.c_iotap), (iotap32, self.c_iotap32)):
                self.dma("sp", dst[:], src, [], [t_c])
            cnt = sb(s2, "s_cnt", [32, 1], F32)
            cnti = sb(s2, "s_cnti", [32, 1], I32)
            nblk = sb(s2, "s_nblk", [32, 1], F32)
            nblkb = sb(s2, "s_nblkb", [32, 128], BF16)
            nblkc = sb(s2, "s_nblkc", [32, 1], BF16)
            bstart = sb(s2, "s_bstart", [128, NE], F32)
            bend = sb(s2, "s_bend", [32, 1], F32)
            cmp = sb(s2, "s_cmp", [32, NBLK], BF16)
            erep = sb(s2, "s_erep", [128, NBLK], F32)
            chg = sb(s2, "s_chg", [128, NBLK], F32)
            wf = sb(s2, "s_wf", [128, NBLK], F32)
            pos = sb(s2, "s_pos", [128, NE], F32)
            junk = sb(s2, "s_junk", [128, NE], F32)
            p4f = sb(s2, "s_p4f", [128, NT, 4], F32)
            t_s = Tok()
            pc, t_pc = self.ps[0], self.tp[0]
            for t in range(NT):
                self.mm(pc[0:32, 0:1], self.M_all[:, t, :], self.onesb[:, 0:1], t == 0, t == NT - 1, [self.t_route, self.t_const], [t_pc])
            self.ts("dve", cnt[:], pc[0:32, 0:1], 127.0, ALU.add, [t_pc], [t_s], s2=1.0 / 128.0, op1=ALU.mult)
            self.ts("dve", nblk[:], cnt[:], -0.49609375, ALU.add, [t_s], [t_s])
            self.cp("dve", cnti[:], nblk[:], [t_s], [t_s])
            self.cp("dve", nblk[:], cnti[:], [t_s], [t_s])
            self.tt("dve", cnt[:], cnt[:], nblk[:], ALU.subtract, [t_s], [t_s])
            self.ts("dve", cnt[:], cnt[:], 1.0, ALU.is_ge, [t_s], [t_s])
            self.tt("dve", nblk[:], nblk[:], cnt[:], ALU.add, [t_s], [t_s])
            self.ts("dve", nblk[:], nblk[:], 1.0, ALU.max, [t_s], [t_s])
            self.cp("dve", nblkb[:], nblk[:, 0:1].to_broadcast([32, 128]), [t_s], [t_s])
            self.cp("dve", nblkc[:], nblk[:], [t_s], [t_s])
            p1, t_p1 = self.ps[1], self.tp[1]
            self.mm(p1[:, 0:NE], nblkb[:], tri32s[:], True, True, [t_s, t_c], [t_p1])
            self.cp("dve", bstart[:], p1[:, 0:NE], [t_p1], [t_s])
            p2, t_p2 = self.ps[2], self.tp[2]
            self.mm(p2[0:32, 0:1], tri32i[:], nblkc[:], True, True, [t_s, t_c], [t_p2])
            self.cp("dve", bend[:], p2[0:32, 0:1], [t_p2], [t_s])
            self.ts("dve", cmp[:], iota160[:], bend[:, 0:1], ALU.is_ge, [t_s, t_c], [t_s])
            p3, t_p3 = self.ps[3], self.tp[3]
            self.mm(p3[:, 0:NBLK], self.onesb[0:32, :], cmp[:], True, True, [t_s, self.t_const], [t_p3])
            self.ts("dve", erep[:], p3[:, 0:NBLK], float(NE - 1), ALU.min, [t_p3], [t_s])
            self.memset("dve", chg[:, 0:1], 1.0, [t_s])
            self.tt("dve", chg[:, 1:NBLK], erep[:, 1:NBLK], erep[:, 0:NBLK - 1], ALU.not_equal, [t_s], [t_s])
            self.ts("dve", wf[:], erep[:], 128.0, ALU.mult, [t_s, t_c], [t_s], s2=iotap[:, 0:1], op1=ALU.add)
            self.ts("dve", chg[:], chg[:], -BIGIDX, ALU.mult, [t_s], [t_s], s2=BIGIDX, op1=ALU.add)
            self.tt("dve", wf[:], wf[:], chg[:], ALU.add, [t_s], [t_s])
            if l > 0:
                self.ts("dve", wf[:], wf[:], float(l * NE * 128), ALU.add, [t_s], [t_s])
            self.cp("dve", self.widx[:], wf[:], [t_s], [self.t_widx])
            self.ts("dve", wf[:], wf[:], 2.0, ALU.mult, [t_s], [t_s])
            self.cp("dve", self.widxA[:], wf[:], [t_s], [self.t_widx])
            self.ts("dve", wf[:], wf[:], 1.0, ALU.add, [t_s], [t_s])
            self.cp("dve", self.widxB[:], wf[:], [t_s], [self.t_widx])
            self.ts("dve", self.OHall[:], erep[0:64, :], iotap32[0:64, 0:1], ALU.is_equal, [t_s, t_c], [self.t_widx])
            for t in range(NT):
                pr_, t_pr = self.ps[4 + t % 2], self.tp[4 + t % 2]
                self.mm(pr_[:, 0:NE], tris[:], self.M_all[:, t, :], True, t == 0, [t_c, self.t_route], [t_pr])
                for t2 in range(t):
                    self.mm(pr_[:, 0:NE], self.onesb[:], self.M_all[:, t2, :], False, t2 == t - 1, [self.t_const, self.t_route], [t_pr])
                self.stt(pos[:], bstart[:], 128.0, pr_[:, 0:NE], ALU.mult, ALU.add, [t_s, t_pr], [t_s])
                for k in range(4):
                    self.stt(junk[:], self.logits_all[:, t, :], self.max8_all[:, t, k:k + 1], pos[:], ALU.is_equal, ALU.mult,
                             [self.t_route, t_s], [t_s], accum=p4f[:, t, k:k + 1])
            self.cp("dve", self.pos4_all[:], p4f[:], [t_s], [self.t_pos4])
            self.P.barrier()
        with ExitStack() as s3:
            hr = Ring([sb(s3, "s_h2%d" % i, [128, D], BF16) for i in range(3)])
            for t in range(NT):
                hb, t_hb = hr.next()
                self.dma("sp", hb[:], self.h2_d[t * 128:(t + 1) * 128, :], [self.t_h2d], [t_hb])
                for k in range(4):
                    self.scatter(self.Xs_d, hb[:], self.pos4_all[:, t, k:k + 1], [t_hb, self.t_pos4, self.t_Xs], [], NBLK * 128 - 1)
            self.P.barrier()

    def phase_experts(self, l):
        with ExitStack() as st:
            sb = self.sb
            wguA = sb(st, "e_wguA", [128, 4 * 2 * D], BF16)
            wguB = sb(st, "e_wguB", [128, 4 * 2 * D], BF16)
            t_wgA, t_wgB = Tok(), Tok()
            wdn = sb(st, "e_wdn", [128, 8 * D], BF16)
            bgu = sb(st, "e_bgu", [64, 2 * D], BF16)
            bdn = sb(st, "e_bdn", [64, D], BF16)
            bf = sb(st, "e_bf", [64, 3 * D], F32)
            bt = sb(st, "e_bt", [64, 3 * D], F32)
            t_wgu, t_wdn, t_b = Tok(), Tok(), Tok()
            for half in range(2):
                self.dma("sp", bf[half * 32:(half + 1) * 32, 0:2 * D], self.b_gu[l], [], [t_b])
                self.dma("sp", bf[half * 32:(half + 1) * 32, 2 * D:3 * D], self.b_dn[l], [], [t_b])
            self.cp("dve", bgu[0:32, :], bf[0:32, 0:2 * D], [t_b], [t_b])
            self.cp("dve", bdn[0:32, :], bf[0:32, 2 * D:3 * D], [t_b], [t_b])
            self.cp("dve", bgu[32:64, :], bf[32:64, 0:2 * D], [t_b], [t_b])
            self.cp("dve", bdn[32:64, :], bf[32:64, 2 * D:3 * D], [t_b], [t_b])
            self.cp("dve", bt[32:64, 0:2 * D], bgu[32:64, :], [t_b], [t_b])
            self.cp("dve", bt[32:64, 2 * D:3 * D], bdn[32:64, :], [t_b], [t_b])
            self.tt("dve", bt[32:64, :], bf[32:64, :], bt[32:64, :], ALU.subtract, [t_b], [t_b])
            self.cp("dve", bgu[32:64, :], bt[32:64, 0:2 * D], [t_b], [t_b])
            self.cp("dve", bdn[32:64, :], bt[32:64, 2 * D:3 * D], [t_b], [t_b])
            wgu_v = self.w_gu.rearrange("l e (p kh kl) n -> (l e p kh) (kl n)", kh=2, kl=4)
            wdn_v = self.w_dn.rearrange("l e (p kc) n -> (l e p) (kc n)", kc=8)
            R = 3
            xb_ = [(sb(st, "e_x%d" % i, [128, D], BF16), Tok()) for i in range(R)]
            xT_ = [(sb(st, "e_xT%d" % i, [128, 8, 128], BF16), Tok()) for i in range(R)]
            oh_ = [(sb(st, "e_oh%d" % i, [64, 128], BF16), Tok()) for i in range(R)]
            gc_ = [(sb(st, "e_gc%d" % i, [128, 512], F32), Tok()) for i in range(2)]
            sg_ = [(sb(st, "e_sg%d" % i, [128, 512], F32), Tok()) for i in range(2)]
            lc_ = [(sb(st, "e_lc%d" % i, [128, 512], F32), Tok()) for i in range(2)]
            ac_ = [(sb(st, "e_ac%d" % i, [128, D], BF16), Tok()) for i in range(2)]
            aT_ = [(sb(st, "e_aT%d" % i, [128, 8, 128], BF16), Tok()) for i in range(2)]
            out_ = [(sb(st, "e_out%d" % i, [128, D], F32), Tok()) for i in range(2)]
            pX = self.ps[6][:].bitcast(BF16).rearrange("p (a b) -> p a b", a=8)
            pA = self.ps[7][:].bitcast(BF16).rearrange("p (a b) -> p a b", a=8)
            N = NBLK
            bound = 2 * NE * 128 - 1

            def WguA(j):
                self.gather(wguA[:], wgu_v, self.widxA[:, j:j + 1], [self.t_widx], [t_wgA], 2 * bound + 1)

            def WguB(j):
                self.gather(wguB[:], wgu_v, self.widxB[:, j:j + 1], [self.t_widx], [t_wgB], 2 * bound + 1)

            def Wdn(j):
                self.gather(wdn[:], wdn_v, self.widx[:, j:j + 1], [self.t_widx], [t_wdn], bound)

            def Ax(j):
                xb, t_xb = xb_[j % R]
                self.dma("sp", xb[:], self.Xs_d[j * 128:(j + 1) * 128, :], [self.t_Xs], [t_xb])
                oh, t_oh = oh_[j % R]
                self.cp("dve", oh[:], self.OHall[:, j:j + 1].to_broadcast([64, 128]), [self.t_widx], [t_oh])
                xv = xb[:].rearrange("p (a kc) -> p kc a", kc=8)
                for kc in range(8):
                    self.tr(pX[:, kc, :], xv[:, kc, :], self.identb[:], [t_xb, self.t_const], [self.tp[6]])
                xT, t_xT = xT_[j % R]
                self.cp("act", xT[:], pX, [self.tp[6]], [t_xT])

            def Bk(j, half):
                xT, t_xT = xT_[j % R]
                oh, t_oh = oh_[j % R]
                wb, t_wb = (wguA, t_wgA) if half == 0 else (wguB, t_wgB)
                for k4 in range(4):
                    kc = half * 4 + k4
                    for nb in range(4):
                        self.mm(self.ps[nb][:], xT[:, kc, :], wb[:, k4 * 2048 + nb * 512:k4 * 2048 + (nb + 1) * 512], kc == 0, False,
                                [t_xT, t_wb], [self.tp[nb]])
                if half == 1:
                    for nb in range(4):
                        self.mm(self.ps[nb][:], oh[:], bgu[:, nb * 512:(nb + 1) * 512], False, True, [t_oh, t_b], [self.tp[nb]])

            def E(j, hf):
                gc, t_gc = gc_[hf]
                sg, t_sg = sg_[hf]
                lc, t_lc = lc_[hf]
                ac, t_ac = ac_[j % 2]
                self.ts("dve", gc[:], self.ps[hf][:], 7.0, ALU.min, [self.tp[hf]], [t_gc])
                self.act(sg[:], gc[:], AF.Sigmoid, [t_gc], [t_sg], scale=1.702)
                self.ts("dve", lc[:], self.ps[2 + hf][:], 7.0, ALU.min, [self.tp[2 + hf]], [t_lc], s2=-7.0, op1=ALU.max)
                self.stt(lc[:], lc[:], 1.0, gc[:], ALU.add, ALU.mult, [t_lc, t_gc], [t_lc])
                self.tt("dve", ac[:, hf * 512:(hf + 1) * 512], lc[:], sg[:], ALU.mult, [t_lc, t_sg], [t_ac])

            def Ca(j):
                ac, t_ac = ac_[j % 2]
                av = ac[:].rearrange("p (a kc) -> p kc a", kc=8)
                for kc in range(8):
                    self.tr(pA[:, kc, :], av[:, kc, :], self.identb[:], [t_ac, self.t_const], [self.tp[7]])
                aT, t_aT = aT_[j % 2]
                self.cp("act", aT[:], pA, [self.tp[7]], [t_aT])

            def Cb(j):
                oh, t_oh = oh_[j % R]
                aT, t_aT = aT_[j % 2]
                ot, t_ot = out_[j % 2]
                for hf in range(2):
                    ps, t_ps = self.ps[4 + hf], self.tp[4 + hf]
                    for kc in range(8):
                        self.mm(ps[:], aT[:, kc, :], wdn[:, kc * 1024 + hf * 512:kc * 1024 + (hf + 1) * 512], kc == 0, False,
                                [t_aT, t_wdn], [t_ps])
                    self.mm(ps[:], oh[:], bdn[:, hf * 512:(hf + 1) * 512], False, True, [t_oh, t_b], [t_ps])
                    self.cp("act" if hf else "dve", ot[:, hf * 512:(hf + 1) * 512], ps[:], [t_ps], [t_ot])
                self.dma("sp", self.Out_d[j * 128:(j + 1) * 128, :], ot[:], [t_ot], [self.t_Outd])

            WguA(0)
            WguB(0)
            Wdn(0)
            Ax(0)
            Ax(1)
            Bk(0, 0)
            WguA(1)
            Bk(0, 1)
            WguB(1)
            E(0, 0)
            E(0, 1)
            for j in range(N):
                if j + 1 < N:
                    Bk(j + 1, 0)
                if j + 2 < N:
                    WguA(j + 2)
                if j + 1 < N:
                    Bk(j + 1, 1)
                if j + 2 < N:
                    WguB(j + 2)
                Ca(j)
                if j + 2 < N:
                    Ax(j + 2)
                if j + 1 < N:
                    E(j + 1, 0)
                Cb(j)
                if j + 1 < N:
                    Wdn(j + 1)
                    E(j + 1, 1)

    def phase_combine(self, l, last):
        with ExitStack() as st:
            sb = self.sb
            self.load_mod(st, [5])
            gr = Ring([sb(st, "c_g%d" % i, [128, D], F32) for i in range(4)])
            xr = Ring([sb(st, "c_x%d" % i, [128, D], F32) for i in range(2)])
            yr = Ring([sb(st, "c_y%d" % i, [128, D], F32) for i in range(2)])
            fg = sb(st, "c_fg", [128, D], F32)
            junk = sb(st, "c_junk", [128, D], F32)
            ss = sb(st, "c_ss", [128, 2], F32)
            t_fg, t_w = Tok(), Tok()
            if last:
                self.dma("sp", fg[:], self.fng.rearrange("(o d) -> o d", o=1).partition_broadcast(128), [], [t_fg])
            for t in range(NT):
                xt, t_x = xr.next()
                y, t_y = yr.next()
                self.dma("sp", xt[:], self.xs_d[t * 128:(t + 1) * 128, :], [self.t_xs], [t_x])
                for k in range(4):
                    g, t_g = gr.next()
                    self.gather(g[:], self.Out_d, self.pos4_all[:, t, k:k + 1], [self.t_Outd, self.t_pos4], [t_g], None)
                    if k == 0:
                        self.ts("dve", y[:], g[:], self.g4_all[:, t, 0:1], ALU.mult, [t_g, self.t_route], [t_y])
                    else:
                        self.stt(y[:], g[:], self.g4_all[:, t, k:k + 1], y[:], ALU.mult, ALU.add, [t_g, self.t_route, t_y], [t_y])
                self.tt("dve", y[:], y[:], self.modr[5], ALU.mult, [t_y, self.t_modr], [t_y])
                self.tt("pool", y[:], y[:], xt[:], ALU.add, [t_y, t_x], [t_y])
                if not last:
                    self.dma("sp", self.xs_d[t * 128:(t + 1) * 128, :], y[:], [t_y], [self.t_xs])
                else:
                    self.stt(junk[:], y[:], 1.0, y[:], ALU.mult, ALU.mult, [t_y], [t_w], accum=ss[:, 0:1])
                    self.rstd_from_ss(ss[:, 1:2], ss[:, 0:1], D, t_w, t_w)
                    self.stt(y[:], y[:], ss[:, 1:2], fg[:], ALU.mult, ALU.mult, [t_y, t_w, t_fg], [t_y])
                    self.dma("sp", self.out[t * 128:(t + 1) * 128, :], y[:], [t_y], [self.t_out])


def _t5_bucket(rel):
    nb = 16
    max_exact = 8
    ret = np.where(rel > 0, nb, 0)
    n = np.abs(rel)
    nf = np.maximum(n, 1).astype(np.float32)
    large = max_exact + (np.log(nf / max_exact) / math.log(1024 / max_exact) * (nb - max_exact)).astype(np.int32)
    large = np.minimum(large, nb - 1)
    return ret + np.where(n < max_exact, n, large)


def host_consts(rel_bias):
    bf = ml_dtypes.bfloat16
    c = {}
    c["identb"] = np.eye(128, dtype=np.float32).astype(bf)
    c["identf"] = np.eye(128, dtype=np.float32)
    tok = np.arange(S)
    row = (tok // 64).astype(np.float32)
    col = (tok % 64).astype(np.float32)
    inv = (10000.0 ** (-np.arange(0, 32, 2, dtype=np.float32) / 32)).astype(np.float32)
    ar = row[:, None] * inv
    ac = col[:, None] * inv
    cosT = np.concatenate([np.cos(ar), np.cos(ar), np.cos(ac), np.cos(ac)], 1).astype(np.float32)
    sinT = np.concatenate([-np.sin(ar), np.sin(ar), -np.sin(ac), np.sin(ac)], 1).astype(np.float32)
    c["cosT"] = np.ascontiguousarray(cosT.reshape(NT, 128, 64).transpose(1, 0, 2))
    c["sinT"] = np.ascontiguousarray(sinT.reshape(NT, 128, 64).transpose(1, 0, 2))
    k = np.arange(128)
    c["tri_s"] = (k[:, None] < k[None, :]).astype(np.float32).astype(bf)
    k32 = np.arange(32)
    c["tri32s"] = (k32[:, None] < k32[None, :]).astype(np.float32).astype(bf)
    c["tri32i"] = (k32[:, None] <= k32[None, :]).astype(np.float32).astype(bf)
    c["iota160"] = np.broadcast_to(np.arange(NBLK, dtype=np.float32), (32, NBLK)).copy()
    c["iotap"] = np.arange(128, dtype=np.float32).reshape(128, 1)
    c["iotap32"] = (np.arange(128) % 32).astype(np.float32).reshape(128, 1)
    q = np.arange(128)[:, None]
    cc = np.arange(384)[None, :]
    rel = cc - 128 - q
    wb = np.full((128, 12, 384), -30000.0, np.float32)
    valid = np.abs(rel) <= 64
    for a, dl in enumerate(A_DIL):
        bkt = _t5_bucket(rel * dl)
        for h in range(4):
            vals = rel_bias[bkt, a * 4 + h]
            wb[:, a * 4 + h, :] = np.where(valid, vals, np.float32(-30000.0))
    c["wbias"] = wb
    return c


_CACHE = {}


def kernel(**inputs):
    inp = {k: np.ascontiguousarray(np.asarray(v, dtype=np.float32)) for k, v in inputs.items()}
    if "nc" not in _CACHE:
        _CACHE["nc"] = Builder(nlayers=2).build()
    nc = _CACHE["nc"]
    consts = host_consts(inp["rel_bias"])
    shared = {k: inp[k] for k in ("w_ada", "b_ada", "norm1_g", "w_in", "q_norm_g", "k_norm_g", "w_br_a", "w_br_b", "w_out",
                                   "norm2_g", "w_router", "b_router", "w_gate_up", "b_gate_up", "w_down", "b_down", "final_norm_g")}
    in_maps = []
    for b in range(8):
        m = dict(shared)
        m.update(consts)
        m["x"] = inp["x"][b]
        m["ccol"] = np.ascontiguousarray(inp["c"][b].reshape(8, 128).T)
        in_maps.append(m)
    res = run_bass_kernel_spmd(nc, in_maps, core_ids=list(range(8)))
    return np.stack([np.asarray(res.results[b]["out"], dtype=np.float32) for b in range(8)], 0)
```

```python
import math
from contextlib import ExitStack

import numpy as np
import ml_dtypes
import concourse.bass as bass
import concourse.mybir as mybir
from concourse.bass_utils import run_bass_kernel_spmd

F32 = mybir.dt.float32
BF16 = mybir.dt.bfloat16
I32 = mybir.dt.int32
ALU = mybir.AluOpType
AF = mybir.ActivationFunctionType
AX = mybir.AxisListType

S = 4096
D = 1024
NT = 32
NE = 32
NBLK = 160
EPS = 1e-6
A_DIL = (1, 4, 16)
BIGIDX = 1.0e6


def sl(start, n, step):
    return slice(start, start + (n - 1) * step + 1, step)

ENGS = ["pe", "act", "dve", "pool", "sp"]


class Tok:
    __slots__ = ("w", "r")

    def __init__(self):
        self.w = None
        self.r = []


class Prog:
    N_DMA_SEMS = {"sp": 24, "pool": 16, "act": 8}

    def __init__(self, nc):
        self.nc = nc
        self.q = {e: [] for e in ENGS}
        self.cnt = {e: 0 for e in ENGS}
        self.seen = {e: {} for e in ENGS}
        self.dma_val = {}
        self.dma_rr = {e: 0 for e in ENGS}
        self.prologue = {}

    def _collect(self, eng, reads, writes):
        deps = {}

        def add(d):
            if d is not None and deps.get(d[0], 0) < d[1]:
                deps[d[0]] = d[1]

        for t in reads:
            add(t.w)
        for t in writes:
            add(t.w)
            for d in t.r:
                add(d)
        out = []
        own = "E:" + eng
        seen = self.seen[eng]
        for k, v in deps.items():
            if k == own and eng == "pe":
                continue
            if seen.get(k, 0) >= v:
                continue
            seen[k] = v
            out.append((k, v))
        return out

    def _note(self, my, reads, writes):
        for t in reads:
            t.r.append(my)
            if len(t.r) > 48:
                d = {}
                for k, v in t.r:
                    if d.get(k, 0) < v:
                        d[k] = v
                t.r = list(d.items())
        for t in writes:
            t.w = my
            t.r = []

    def op(self, eng, fn, reads=(), writes=()):
        waits = self._collect(eng, reads, writes)
        self.cnt[eng] += 1
        my = ("E:" + eng, self.cnt[eng])
        self._note(my, reads, writes)
        self.q[eng].append((waits, fn, my, 1))

    def dma(self, eng, fn, reads=(), writes=()):
        waits = self._collect(eng, reads, writes)
        n = self.N_DMA_SEMS[eng]
        slot = self.dma_rr[eng] % n
        self.dma_rr[eng] += 1
        key = "D:%s:%d" % (eng, slot)
        prev = self.dma_val.get(key, 0)
        if prev > 0 and self.seen[eng].get(key, 0) < prev:
            self.seen[eng][key] = prev
            waits.append((key, prev))
        self.dma_val[key] = prev + 16
        my = (key, prev + 16)
        self._note(my, reads, writes)
        self.q[eng].append((waits, fn, my, 16))

    def barrier(self):
        for eng in ENGS:
            waits = []
            for key, v in self.dma_val.items():
                if self.seen[eng].get(key, 0) < v:
                    waits.append((key, v))
                    self.seen[eng][key] = v
            for e in ENGS:
                if e == eng or self.cnt[e] == 0:
                    continue
                k = "E:" + e
                if self.seen[eng].get(k, 0) < self.cnt[e]:
                    waits.append((k, self.cnt[e]))
                    self.seen[eng][k] = self.cnt[e]
            if waits:
                self.q[eng].append((waits, None, None, 0))

    def emit(self):
        nc = self.nc
        keys = set()
        for e in ENGS:
            for waits, fn, my, inc in self.q[e]:
                for k, v in waits:
                    keys.add(k)
                if my is not None:
                    keys.add(my[0])
        keys = sorted(keys)
        with ExitStack() as st:
            sems = {k: st.enter_context(nc.semaphore(k.replace(":", "_"))) for k in keys}
            block = st.enter_context(nc.Block())
            hmap = {"pe": block.tensor, "act": block.scalar, "dve": block.vector,
                    "pool": block.gpsimd, "sp": block.sync}

            def make(e):
                def body(engh):
                    if e in self.prologue:
                        self.prologue[e](engh)
                    for waits, fn, my, inc in self.q[e]:
                        for k, v in waits:
                            engh.wait_ge(sems[k], v)
                        if fn is None:
                            continue
                        fn(engh).then_inc(sems[my[0]], inc)
                return body

            for e in ENGS:
                if self.q[e]:
                    hmap[e](make(e))


class Ring:
    def __init__(self, items):
        self.items = [(it, Tok()) for it in items]
        self.i = 0

    def next(self):
        it = self.items[self.i % len(self.items)]
        self.i += 1
        return it


class Builder:
    def __init__(self, nlayers=2, stop=None, dbg=(), small_moe=False):
        self.nlayers = nlayers
        self.stop = stop
        self.dbg = dbg
        NEW = 1 if small_moe else NE
        nc = self.nc = bass.Bass("TRN2", target_bir_lowering=False)
        self.P = Prog(nc)
        self.outs = ["out"]
        di = lambda n, s, d=F32: nc.dram_tensor(n, s, d, kind="ExternalInput").ap()
        self.x_in = di("x", [S, D])
        self.ccol = di("ccol", [128, 8])
        self.w_ada = di("w_ada", [2, D, 6 * D])
        self.b_ada = di("b_ada", [2, 6 * D])
        self.norm1_g = di("norm1_g", [2, D])
        self.w_in = di("w_in", [2, D, 5888])
        self.q_norm_g = di("q_norm_g", [2, 64])
        self.k_norm_g = di("k_norm_g", [2, 64])
        self.wbias = di("wbias", [128, 12, 384])
        self.w_br_a = di("w_br_a", [2, 256, D])
        self.w_br_b = di("w_br_b", [2, D, D])
        self.w_out = di("w_out", [2, D, D])
        self.norm2_g = di("norm2_g", [2, D])
        self.w_router = di("w_router", [2, D, NE])
        self.b_router = di("b_router", [2, NE])
        self.w_gu = di("w_gate_up", [2, NEW, D, 2 * D])
        self.b_gu = di("b_gate_up", [2, NE, 2 * D])
        self.w_dn = di("w_down", [2, NEW, D, D])
        self.b_dn = di("b_down", [2, NE, D])
        self.fng = di("final_norm_g", [D])
        self.c_identb = di("identb", [128, 128], BF16)
        self.c_identf = di("identf", [128, 128])
        self.c_cos = di("cosT", [128, NT, 64])
        self.c_sin = di("sinT", [128, NT, 64])
        self.c_tris = di("tri_s", [128, 128], BF16)
        self.c_tri32s = di("tri32s", [32, 32], BF16)
        self.c_tri32i = di("tri32i", [32, 32], BF16)
        self.c_iota160 = di("iota160", [32, NBLK])
        self.c_iotap = di("iotap", [128, 1])
        self.c_iotap32 = di("iotap32", [128, 1])
        self.out = nc.dram_tensor("out", [S, D], F32, kind="ExternalOutput").ap()
        self.xs_d = self.scratch("xs_d", [S, D], F32)
        self.obT_d = self.scratch("obT_d", [D, S], BF16)
        self.gT_d = self.scratch("gT_d", [2 * D, S], BF16)
        self.oaw_d = self.scratch("oaw_d", [3, S, 264], F32)
        self.h2_d = self.scratch("h2_d", [S, D], BF16)
        self.Xs_d = self.scratch("Xs_d", [NBLK * 128, D], BF16)
        self.Out_d = self.scratch("Out_d", [NBLK * 128, D], F32)
        self.modr_d = self.scratch("modr_d", [128, 6, D], F32)
        self.t_modrd = Tok()
        self.t_xs, self.t_obT, self.t_gT, self.t_oaw, self.t_h2d, self.t_Xs, self.t_Outd = [Tok() for _ in range(7)]
        self.t_out = Tok()

    def scratch(self, name, shape, dt):
        if name in self.dbg:
            self.outs.append(name)
            return self.nc.dram_tensor(name, shape, dt, kind="ExternalOutput").ap()
        return self.nc.dram_tensor(name, shape, dt, kind="Internal").ap()

    def sb(self, st, name, shape, dt):
        self._nsb = getattr(self, "_nsb", 0) + 1
        return st.enter_context(self.nc.sbuf_tensor("sb%d_%s" % (self._nsb, name), shape, dt))

    def tt(self, eng, out, in0, in1, op, r, w):
        self.P.op(eng, lambda e: e.tensor_tensor(out=out, in0=in0, in1=in1, op=op), r, w)

    def ts(self, eng, out, in0, s1, op0, r, w, s2=None, op1=None):
        if op1 is None:
            self.P.op(eng, lambda e: e.tensor_scalar(out=out, in0=in0, scalar1=s1, scalar2=None, op0=op0), r, w)
        else:
            self.P.op(eng, lambda e: e.tensor_scalar(out=out, in0=in0, scalar1=s1, scalar2=s2, op0=op0, op1=op1), r, w)

    def stt(self, out, in0, scalar, in1, op0, op1, r, w, accum=None):
        if accum is None:
            self.P.op("dve", lambda e: e.scalar_tensor_tensor(out=out, in0=in0, scalar=scalar, in1=in1, op0=op0, op1=op1), r, w)
        else:
            self.P.op("dve", lambda e: e.scalar_tensor_tensor(out=out, in0=in0, scalar=scalar, in1=in1, op0=op0, op1=op1,
                                                              accum_out=accum), r, w)

    def act(self, out, in_, func, r, w, bias=None, scale=None, accum=None):
        kw = {}
        if bias is not None:
            kw["bias"] = bias
        if scale is not None:
            kw["scale"] = scale
        if accum is not None:
            kw["accum_out"] = accum
        self.P.op("act", lambda e: e.activation(out=out, in_=in_, func=func, **kw), r, w)

    def cp(self, eng, out, in_, r, w):
        if eng == "act":
            self.P.op("act", lambda e: e.copy(out=out, in_=in_), r, w)
        else:
            self.P.op(eng, lambda e: e.tensor_copy(out=out, in_=in_), r, w)

    def mm(self, out, lhsT, rhs, start, stop, r, w):
        self.P.op("pe", lambda e: e.matmul(out, lhsT=lhsT, rhs=rhs, start=start, stop=stop), r, w)

    def tr(self, out, in_, ident, r, w):
        self.P.op("pe", lambda e: e.transpose(out=out, in_=in_, identity=ident), r, w)

    def dma(self, q, out, in_, r, w):
        self.P.dma(q, lambda e: e.dma_start(out=out, in_=in_), r, w)

    def red(self, out, in_, op, r, w):
        self.P.op("dve", lambda e: e.tensor_reduce(out=out, in_=in_, axis=AX.X, op=op), r, w)

    def recip(self, out, in_, r, w):
        self.P.op("dve", lambda e: e.reciprocal(out=out, in_=in_), r, w)

    def memset(self, eng, ap, val, w):
        self.P.op(eng, lambda e: e.memset(ap, val), (), w)

    def gather(self, out, in_, idx, r, w, bound):
        if bound is None:
            self.P.dma("pool", lambda e: e.indirect_dma_start(out=out, out_offset=None, in_=in_,
                                                              in_offset=bass.IndirectOffsetOnAxis(ap=idx, axis=0)), r, w)
            return
        if "pool" not in self.P.prologue:
            self.bregs = {}
            self.bounds = []

            def pro(e):
                for bv in self.bounds:
                    self.bregs[bv] = e.alloc_register("bound_reg_%d" % bv)
                    e.reg_mov(self.bregs[bv], bv)
            self.P.prologue["pool"] = pro
        if bound not in self.bounds:
            self.bounds.append(bound)
        self.P.dma("pool", lambda e: e.indirect_dma_start(out=out, out_offset=None, in_=in_,
                                                          in_offset=bass.IndirectOffsetOnAxis(ap=idx, axis=0),
                                                          bounds_check=self.bregs[bound], oob_is_err=False), r, w)

    def scatter(self, out, in_, idx, r, w, bound):
        self.P.dma("pool", lambda e: e.indirect_dma_start(out=out, out_offset=bass.IndirectOffsetOnAxis(ap=idx, axis=0),
                                                          in_=in_, in_offset=None), r, w)

    def build(self):
        nc = self.nc
        with ExitStack() as gst:
            self.gst = gst
            self.ps = [gst.enter_context(nc.psum_tensor("ps%d" % i, [128, 512], F32)) for i in range(8)]
            self.tp = [Tok() for _ in range(8)]
            sb = self.sb
            self.identb = sb(gst, "identb", [128, 128], BF16)
            self.identf = sb(gst, "identf", [128, 128], F32)
            self.onesb = sb(gst, "onesb", [128, 128], BF16)
            self.onesf = sb(gst, "onesf", [128, 128], F32)
            self.epsc = sb(gst, "epsc", [128, 1], F32)
            self.t_const = Tok()
            tc = [self.t_const]
            self.dma("sp", self.identb[:], self.c_identb, [], tc)
            self.dma("sp", self.identf[:], self.c_identf, [], tc)
            self.memset("dve", self.onesb[:], 1.0, tc)
            self.memset("dve", self.onesf[:], 1.0, tc)
            self.memset("dve", self.epsc[:], EPS, tc)
            self.neghalf = sb(gst, "neghalf", [128, 16], F32)
            self.memset("dve", self.neghalf[:], -0.5, tc)
            self.P.barrier()
            xsrc = self.x_in
            for l in range(self.nlayers):
                last = (l == self.nlayers - 1)
                if self.layer(l, xsrc, last):
                    break
                xsrc = self.xs_d
            self.P.barrier()
            self.P.emit()
        return nc

    def layer(self, l, xsrc, last):
        P = self.P
        stop = self.stop if l == self.nlayers - 1 else None
        with ExitStack() as lst:
            self.oaT = self.sb(lst, "oaT", [128, 2, S], BF16)
            self.t_oaT = Tok()
            self.phase_mod(l)
            P.barrier()
            if stop == "mod":
                return True
            with ExitStack() as ast:
                self.hT = self.sb(ast, "hT", [128, 8, S], BF16)
                self.t_hT = Tok()
                self.phase_norm1(l, xsrc)
                P.barrier()
                if stop == "norm1":
                    return True
                self.phase_gates(l)
                P.barrier()
                if stop == "gates":
                    return True
                self.phase_window(l)
                P.barrier()
                if stop == "window":
                    return True
                self.phase_gqa(l)
                P.barrier()
                if stop == "gqa":
                    return True
            with ExitStack() as mst:
                self.logits_all = self.sb(mst, "logits_all", [128, NT, NE], F32)
                self.max8_all = self.sb(mst, "max8_all", [128, NT, 8], F32)
                self.g4_all = self.sb(mst, "g4_all", [128, NT, 4], F32)
                self.M_all = self.sb(mst, "M_all", [128, NT, NE], BF16)
                self.pos4_all = self.sb(mst, "pos4_all", [128, NT, 4], I32)
                self.widx = self.sb(mst, "widx", [128, NBLK], I32)
                self.widxA = self.sb(mst, "widxA", [128, NBLK], I32)
                self.widxB = self.sb(mst, "widxB", [128, NBLK], I32)
                self.OHall = self.sb(mst, "OHall", [64, NBLK], BF16)
                self.t_widx = Tok()
                self.t_route = Tok()
                self.t_pos4 = Tok()
                self.phase_merge(l, xsrc)
                P.barrier()
                if stop == "merge":
                    return True
                self.phase_slots(l)
                P.barrier()
                if stop == "slots":
                    return True
                self.phase_experts(l)
                P.barrier()
                if stop == "experts":
                    return True
                self.phase_combine(l, last)
                P.barrier()
        return False

    def phase_mod(self, l):
        with ExitStack() as st:
            sb = self.sb
            cc = sb(st, "cc", [128, 8], F32)
            cs = sb(st, "cs", [128, 8], F32)
            crep = sb(st, "crep", [128, 8, 128], F32)
            brep = sb(st, "brep", [128, 6 * D], F32)
            ng = sb(st, "ng", [128, 2, D], F32)
            modr = sb(st, "modr", [128, 6, D], F32)
            t_modr = Tok()
            wa = Ring([sb(st, "wa%d" % i, [128, 8, 512], F32) for i in range(2)])
            t_cc, t_cs, t_crep, t_brep, t_ng = Tok(), Tok(), Tok(), Tok(), Tok()
            self.dma("sp", cc[:], self.ccol, [], [t_cc])
            self.dma("sp", brep[:], self.b_ada[l:l + 1, :].partition_broadcast(128), [], [t_brep])
            self.dma("sp", ng[:, 0, :], self.norm1_g[l:l + 1, :].partition_broadcast(128), [], [t_ng])
            self.dma("sp", ng[:, 1, :], self.norm2_g[l:l + 1, :].partition_broadcast(128), [], [t_ng])
            self.act(cs[:], cc[:], AF.Silu, [t_cc], [t_cs])
            for kc in range(8):
                self.cp("dve", crep[:, kc, :], cs[:, kc:kc + 1].to_broadcast([128, 128]), [t_cs], [t_crep])
            modf = modr[:].rearrange("p a d -> p (a d)")
            psr = Ring(self.ps[0:2])
            psr.items = [(self.ps[0], self.tp[0]), (self.ps[1], self.tp[1])]
            for ch in range(12):
                w, t_w = wa.next()
                self.dma("sp", w[:], self.w_ada[l][:, ch * 512:(ch + 1) * 512].rearrange("(kc p) n -> p kc n", p=128), [], [t_w])
                ps, t_ps = psr.next()
                for kc in range(8):
                    self.mm(ps[:], crep[:, kc, :], w[:, kc, :], kc == 0, kc == 7, [t_crep, t_w], [t_ps])
                self.tt("dve", modf[:, ch * 512:(ch + 1) * 512], ps[:], brep[:, ch * 512:(ch + 1) * 512], ALU.add,
                        [t_ps, t_brep], [t_modr])
            for i, j in ((1, 0), (4, 1)):
                self.stt(modr[:, i, :], modr[:, i, :], 1.0, ng[:, j, :], ALU.add, ALU.mult, [t_modr, t_ng], [t_modr])
            self.dma("sp", self.modr_d, modr[:], [t_modr], [self.t_modrd])

    def load_mod(self, st, idxs):
        tl = self.sb(st, "modl", [128, len(idxs), D], F32)
        self.t_modr = Tok()
        self.modr = {}
        for n, i in enumerate(idxs):
            self.dma("sp", tl[:, n, :], self.modr_d[:, i, :], [self.t_modrd], [self.t_modr])
            self.modr[i] = tl[:, n, :]

    def rstd_from_ss(self, rstd, ss, n, t_in, t_out):
        self.act(rstd, ss, AF.Ln, [t_in, self.t_const], [t_out], bias=self.epsc[0:rstd.shape[0], 0:1], scale=1.0 / n)
        self.act(rstd, rstd, AF.Exp, [t_out], [t_out], scale=-0.5)

    def norm_mod(self, xt, t_x, i_g, i_sh, work, hb, t_hb, hf=None):
        junk, ss, rstd, tmp, t_w = work
        self.stt(junk[:], xt[:], 1.0, xt[:], ALU.mult, ALU.mult, [t_x], [t_w], accum=ss[:, 0:1])
        self.rstd_from_ss(rstd[:, 0:1], ss[:, 0:1], D, t_w, t_w)
        self.stt(tmp[:], xt[:], rstd[:, 0:1], self.modr[i_g], ALU.mult, ALU.mult, [t_x, t_w, self.t_modr], [t_w])
        if hf is not None:
            self.tt("dve", hf[:], tmp[:], self.modr[i_sh], ALU.add, [t_w, self.t_modr], [t_hb])
            self.cp("pool", hb[:], hf[:], [t_hb], [t_hb])
        else:
            self.tt("dve", hb[:], tmp[:], self.modr[i_sh], ALU.add, [t_w, self.t_modr], [t_hb])

    def phase_norm1(self, l, xsrc):
        with ExitStack() as st:
            sb = self.sb
            self.load_mod(st, [0, 1])
            xr = Ring([sb(st, "xt%d" % i, [128, D], F32) for i in range(2)])
            hr = Ring([sb(st, "hb%d" % i, [128, D], BF16) for i in range(2)])
            work = (sb(st, "junk", [128, D], F32), sb(st, "ss", [128, 1], F32), sb(st, "rstd", [128, 1], F32),
                    sb(st, "tmp", [128, D], F32), Tok())
            psb = [self.ps[i][:].bitcast(BF16).rearrange("p (a b) -> p a b", a=8) for i in range(2)]
            for t in range(NT):
                xt, t_x = xr.next()
                hb, t_hb = hr.next()
                self.dma("sp", xt[:], xsrc[t * 128:(t + 1) * 128, :], [self.t_xs], [t_x])
                self.norm_mod(xt, t_x, 1, 0, work, hb, t_hb)
                pT, t_p = psb[t % 2], self.tp[t % 2]
                for kc in range(8):
                    self.tr(pT[:, kc, :], hb[:, kc * 128:(kc + 1) * 128], self.identb[:], [t_hb, self.t_const], [t_p])
                self.cp("act", self.hT[:, :, t * 128:(t + 1) * 128], pT, [t_p], [self.t_hT])

    def load_w(self, w, src, t_w, kc=8):
        self.dma("pool", w, src.rearrange("(kc p) n -> p kc n", p=128), [], [t_w])

    def phase_gates(self, l):
        with ExitStack() as st:
            sb = self.sb
            wg = sb(st, "wg", [128, 8, 2 * D], BF16)
            t_wg = Tok()
            for q in range(4):
                self.load_w(wg[:, :, q * 512:(q + 1) * 512], self.w_in[l][:, 3840 + q * 512:3840 + (q + 1) * 512], t_wg)
            gr = Ring([sb(st, "gsb%d" % i, [128, 512], BF16) for i in range(3)])
            n = 0
            for c in range(8):
                for m in range(16):
                    ps, t_ps = self.ps[n % 4], self.tp[n % 4]
                    n += 1
                    for kc in range(8):
                        self.mm(ps[:], wg[:, kc, m * 128:(m + 1) * 128], self.hT[:, kc, c * 512:(c + 1) * 512], kc == 0, kc == 7,
                                [t_wg, self.t_hT], [t_ps])
                    g, t_g = gr.next()
                    self.act(g[:], ps[:], AF.Sigmoid, [t_ps], [t_g])
                    self.dma("sp", self.gT_d[m * 128:(m + 1) * 128, c * 512:(c + 1) * 512], g[:], [t_g, self.t_gT], [])

    def qknorm_rope(self, src, H, grep, tile, wk, dst, t_src, t_dst):
        sq, ss, rstd, qn, t1, t2, t_w = wk
        W = H * 64
        v3 = lambda ap: ap[:, 0:W].rearrange("p (h d) -> p h d", h=H)
        v5 = lambda ap: ap[:, 0:W].rearrange("p (h a b c) -> p h a b c", h=H, a=2, b=2)
        self.tt("pool", sq[:, 0:W], src, src, ALU.mult, [t_src], [t_w])
        self.red(ss[:, 0:H], v3(sq), ALU.add, [t_w], [t_w])
        self.ts("pool", rstd[:, 0:H], ss[:, 0:H], 1.0 / 64, ALU.mult, [t_w], [t_w], s2=EPS, op1=ALU.add)
        self.tt("pool", rstd[:, 0:H], rstd[:, 0:H], self.neghalf[:, 0:H], ALU.pow, [t_w, self.t_const], [t_w])
        self.tt("dve", v3(qn), src.rearrange("p (h d) -> p h d", h=H), rstd[:, 0:H].unsqueeze(2).to_broadcast([128, H, 64]), ALU.mult,
                [t_src, t_w], [t_w])
        self.tt("pool", v3(qn), v3(qn), grep.unsqueeze(1).to_broadcast([128, H, 64]), ALU.mult, [t_w, self.t_const], [t_w])
        cosb = self.cos[:, tile, :].unsqueeze(1).to_broadcast([128, H, 64])
        sinv = self.sin[:, tile, :].rearrange("p (a b c) -> p a b c", a=2, b=2)
        self.tt("dve", v3(t1), v3(qn), cosb, ALU.mult, [t_w, self.t_const], [t_w])
        for b in range(2):
            sb_ = sinv[:, :, b, :].unsqueeze(1).to_broadcast([128, H, 2, 16])
            self.tt("pool", v5(t2)[:, :, :, b, :], v5(qn)[:, :, :, 1 - b, :], sb_, ALU.mult, [t_w, self.t_const], [t_w])
        self.tt("dve", dst, t1[:, 0:W], t2[:, 0:W], ALU.add, [t_w], [t_dst])

    def phase_gqa(self, l):
        with ExitStack() as st:
            sb = self.sb
            self.cos = sb(st, "cos", [128, NT, 64], F32)
            self.sin = sb(st, "sin", [128, NT, 64], F32)
            self.dma("sp", self.cos[:], self.c_cos, [], [self.t_const])
            self.dma("sp", self.sin[:], self.c_sin, [], [self.t_const])
            KT = sb(st, "KT", [128, 2, S], BF16)
            V = sb(st, "V", [128, NT, 4, 128], BF16)
            wkv = sb(st, "wkv", [128, 8, 512], BF16)
            gq = sb(st, "gq", [128, 64], F32)
            gk = sb(st, "gk", [128, 64], F32)
            t_KT, t_V, t_wkv = Tok(), Tok(), Tok()
            self.dma("sp", gq[:], self.q_norm_g[l:l + 1, :].partition_broadcast(128), [], [self.t_const])
            self.dma("sp", gk[:], self.k_norm_g[l:l + 1, :].partition_broadcast(128), [], [self.t_const])
            self.load_w(wkv[:], self.w_in[l][:, 3328:3840], t_wkv)
            self.memset("dve", V[:, :, :, 64:128], 1.0, [t_V])
            wk = (sb(st, "q_sq", [128, 256], F32), sb(st, "q_ss", [128, 4], F32), sb(st, "q_rstd", [128, 4], F32),
                  sb(st, "q_qn", [128, 256], F32), sb(st, "q_t1", [128, 256], F32), sb(st, "q_t2", [128, 256], F32), Tok())
            psT = self.ps[7][:].bitcast(BF16).rearrange("p (a b) -> p a b", a=8)
            t_psT = self.tp[7]
            ksr = Ring([sb(st, "ksb3_%d" % i, [128, 256], F32) for i in range(3)])
            krr = Ring([sb(st, "kr3_%d" % i, [128, 256], BF16) for i in range(3)])
            kst = {}

            def kv1(t):
                ps, t_ps = self.ps[6], self.tp[6]
                for kc in range(8):
                    self.mm(ps[:], self.hT[:, kc, t * 128:(t + 1) * 128], wkv[:, kc, :], kc == 0, kc == 7, [self.t_hT, t_wkv], [t_ps])
                self.cp("act", V[:, t, :, 0:64], ps[:, 256:512].rearrange("p (g d) -> p g d", g=4), [t_ps], [t_V])
                ksb, t_ksb = ksr.next()
                self.cp("act", ksb[:], ps[:, 0:256], [t_ps], [t_ksb])
                kr, t_kr = krr.next()
                self.qknorm_rope(ksb[:], 4, gk[:], t, wk, kr[:], t_ksb, t_kr)
                kst[t] = (kr, t_kr)

            def kv2(t):
                kr, t_kr = kst.pop(t)
                for m in range(2):
                    self.tr(psT[:, m, :], kr[:, m * 128:(m + 1) * 128], self.identb[:], [t_kr, self.t_const], [t_psT])
                self.cp("dve", KT[:, :, t * 128:(t + 1) * 128], psT[:, 0:2, :], [t_psT], [t_KT])

            kv1(0)
            kv1(1)
            for t in range(NT):
                kv2(t)
                if t + 2 < NT:
                    kv1(t + 2)
            wq = sb(st, "wq", [128, 8, 256], BF16)
            t_wq = Tok()
            QTb = [(sb(st, "QT%d" % i, [128, 4, 512], BF16), Tok()) for i in range(2)]
            PTr = Ring([sb(st, "PT%d" % i, [128, 512], BF16) for i in range(4)])
            rdr = Ring([sb(st, "rd%d" % i, [64, 512], F32) for i in range(2)])
            obr = Ring([sb(st, "ob%d" % i, [64, 512], BF16) for i in range(2)])
            Sr = Ring([None] * 3)
            Sr.items = [(self.ps[i], self.tp[i]) for i in range(3)]
            Or = Ring([None] * 2)
            Or.items = [(self.ps[i], self.tp[i]) for i in (3, 4)]
            units = [(g, c) for g in range(4) for c in range(8)]

            qst = {}

            def qproj1(u, t4):
                g, c = units[u]
                if c == 0 and t4 == 0:
                    self.load_w(wq[:], self.w_in[l][:, 2304 + g * 256:2304 + (g + 1) * 256], t_wq)
                t = c * 4 + t4
                ps, t_ps = self.ps[6], self.tp[6]
                for kc in range(8):
                    self.mm(ps[:, 0:256], self.hT[:, kc, t * 128:(t + 1) * 128], wq[:, kc, :], kc == 0, kc == 7,
                            [self.t_hT, t_wq], [t_ps])
                qsb, t_qsb = ksr.next()
                self.cp("dve", qsb[:], ps[:, 0:256], [t_ps], [t_qsb])
                qr, t_qr = krr.next()
                self.qknorm_rope(qsb[:], 4, gq[:], t, wk, qr[:], t_qsb, t_qr)
                qst[(u, t4)] = (qr, t_qr)

            def qproj2(u, t4):
                g, c = units[u]
                kb = (g % 2) * 64
                ko = 64 - kb
                QT, t_QT = QTb[u % 2]
                qr, t_qr = qst.pop((u, t4))
                for h in range(4):
                    self.tr(psT[0:64, h, :], qr[:, h * 64:(h + 1) * 64], self.identb[:], [t_qr, self.t_const], [t_psT])
                self.cp("dve", QT[kb:kb + 64, :, t4 * 128:(t4 + 1) * 128], psT[0:64, 0:4, :], [t_psT], [t_QT])
                self.memset("pool", QT[ko:ko + 64, :, t4 * 128:(t4 + 1) * 128], 0.0, [t_QT])

            def attend(u, h):
                g, c = units[u]
                QT, t_QT = QTb[u % 2]
                hq = g * 4 + h
                OT, t_OT = Or.next()
                pend = []

                def score(kt):
                    Sb, t_S = Sr.next()
                    self.mm(Sb[:], KT[:, g // 2, kt * 128:(kt + 1) * 128], QT[:, h, :], True, True, [t_KT, t_QT], [t_S])
                    PT, t_PT = PTr.next()
                    self.act(PT[:], Sb[:], AF.Exp, [t_S], [t_PT], scale=0.125)
                    pend.append((kt, PT, t_PT))

                def pv():
                    kt, PT, t_PT = pend.pop(0)
                    self.mm(OT[:], V[:, kt, g, :], PT[:], kt == 0, kt == NT - 1, [t_V, t_PT], [t_OT])

                score(0)
                score(1)
                for kt in range(NT):
                    if kt + 2 < NT:
                        score(kt + 2)
                    pv()
                def fin():
                    rd, t_rd = rdr.next()
                    self.recip(rd[0:64, :], OT[64:128, :], [t_OT], [t_rd])
                    ob, t_ob = obr.next()
                    self.tt("dve", ob[:], OT[0:64, :], rd[0:64, :], ALU.mult, [t_OT, t_rd], [t_ob])
                    self.dma("sp", self.obT_d[hq * 64:(hq + 1) * 64, c * 512:(c + 1) * 512], ob[:], [t_ob], [self.t_obT])
                return fin

            for t4 in range(4):
                qproj1(0, t4)
                qproj2(0, t4)
            for u in range(len(units)):
                nxt = u + 1 < len(units)
                f = attend(u, 0)
                f()
                if nxt:
                    qproj1(u + 1, 0)
                    qproj1(u + 1, 1)
                f = attend(u, 1)
                if nxt:
                    qproj2(u + 1, 0)
                    qproj2(u + 1, 1)
                f()
                if nxt:
                    qproj1(u + 1, 2)
                f = attend(u, 2)
                if nxt:
                    qproj2(u + 1, 2)
                f()
                if nxt:
                    qproj1(u + 1, 3)
                f = attend(u, 3)
                if nxt:
                    qproj2(u + 1, 3)
                f()

    def phase_window(self, l):
        with ExitStack() as st:
            sb = self.sb
            bias = sb(st, "wb", [128, 12, 384], F32)
            t_bias = Tok()
            self.dma("sp", bias[:], self.wbias, [], [t_bias])
            QT = sb(st, "wQT", [128, 2, S], BF16)
            KT = sb(st, "wKT", [128, 2, S], BF16)
            Vf = sb(st, "wVf", [128, NT, 256], BF16)
            wq = sb(st, "wwq", [128, 8, 768], BF16)
            t_QT, t_KT, t_Vf, t_wq = Tok(), Tok(), Tok(), Tok()
            sr = Ring([sb(st, "ws%d" % i, [128, 384], F32) for i in range(3)])
            pr = Ring([sb(st, "wp%d" % i, [128, 384], BF16) for i in range(4)])
            ptr = Ring([sb(st, "wpt%d" % i, [128, 3, 128], BF16) for i in range(2)])
            mr = Ring([sb(st, "wm%d" % i, [128, 2], F32) for i in range(6)])
            stg = Ring([sb(st, "wstg%d" % i, [128, 264], F32) for i in range(2)])
            Sr = Ring([None] * 3)
            Sr.items = [(self.ps[i], self.tp[i]) for i in (0, 1, 7)]
            Tr = Ring([None] * 2)
            Tr.items = [(self.ps[i][:].bitcast(BF16)[:, 0:384].rearrange("p (a b) -> p a b", a=3), self.tp[i]) for i in (2, 3)]
            Or = Ring([None] * 2)
            Or.items = [(self.ps[i], self.tp[i]) for i in (4, 5)]
            import os
            wstop = int(os.environ.get("WSTOP", "99"))
            for a, Dl in enumerate(A_DIL):
                L = S // Dl
                nj = L // 128
                if str(a) not in os.environ.get("WGRPS", "012"):
                    continue
                self.load_w(wq[:], self.w_in[l][:, a * 768:(a + 1) * 768], t_wq)
                n = 0
                for which, dst, t_dst in ((0, QT, t_QT), (1, KT, t_KT)):
                    for m in range(2):
                        for c in range(8):
                            ps, t_ps = self.ps[6], self.tp[6]
                            n += 1
                            col = which * 256 + m * 128
                            for kc in range(8):
                                self.mm(ps[:], wq[:, kc, col:col + 128], self.hT[:, kc, c * 512:(c + 1) * 512], kc == 0, kc == 7,
                                        [t_wq, self.t_hT], [t_ps])
                            self.cp("act" if n % 2 else "dve", dst[:, m, c * 512:(c + 1) * 512], ps[:], [t_ps], [t_dst])
                if wstop <= 1:
                    continue
                for r in range(Dl):
                    for j in range(nj):
                        bi = r * nj + j
                        tok0 = j * 128 * Dl + r
                        ps, t_ps = self.ps[6], self.tp[6]
                        for kc in range(8):
                            self.mm(ps[:, 0:256], self.hT[:, kc, sl(tok0, 128, Dl)], wq[:, kc, 512:768], kc == 0, kc == 7,
                                    [self.t_hT, t_wq], [t_ps])
                        self.cp("act" if bi % 2 else "dve", Vf[:, bi, :], ps[:, 0:256], [t_ps], [t_Vf])
                if wstop <= 2:
                    continue
                units = []
                for r in range(Dl):
                    for j in range(nj):
                        for h in range(4):
                            units.append((r, j, h))
                ust = {}
                blk = {}

                def W1(n):
                    r, j, h = units[n]
                    tok0 = j * 128 * Dl + r
                    jt0 = max(j - 1, 0)
                    jt1 = min(j + 1, nj - 1)
                    ntl = jt1 - jt0 + 1
                    c0 = (jt0 - (j - 1)) * 128
                    w = ntl * 128
                    k0 = jt0 * 128 * Dl + r
                    if h == 0:
                        blk[(r, j)] = (Or.next(), stg.next())
                    (O4, t_O4), (sg, t_sg) = blk[(r, j)]
                    Sb, t_S = Sr.next()
                    hb_ = (h % 2) * 64
                    self.mm(Sb[:, 0:w], QT[hb_:hb_ + 64, h // 2, sl(tok0, 128, Dl)], KT[hb_:hb_ + 64, h // 2, sl(k0, w, Dl)],
                            True, True, [t_QT, t_KT], [t_S])
                    s_, t_s = sr.next()
                    self.stt(s_[:, 0:w], Sb[:, 0:w], 0.125, bias[:, a * 4 + h, c0:c0 + w], ALU.mult, ALU.add, [t_S, t_bias], [t_s])
                    m, t_m = mr.next()
                    self.P.op("dve", lambda e, m=m, s_=s_, w=w: e.reduce_max(out=m[:, 0:1], in_=s_[:, 0:w], axis=AX.X), [t_s], [t_m])
                    self.ts("dve", m[:, 1:2], m[:, 0:1], -1.0, ALU.mult, [t_m], [t_m])
                    self.cp("dve", sg[:, 256 + h:257 + h], m[:, 0:1], [t_m], [t_sg])
                    ust[n] = (s_, t_s, m, t_m, sg, t_sg, w, h, ntl, jt0)

                def W1b(n):
                    s_, t_s, m, t_m, sg, t_sg, w, h, ntl, jt0 = ust[n]
                    p, t_p = pr.next()
                    self.act(p[:, 0:w], s_[:, 0:w], AF.Exp, [t_s, t_m], [t_p, t_sg], bias=m[:, 1:2], scale=1.0,
                             accum=sg[:, 260 + h:261 + h])
                    ust[n] = (p, t_p, ntl, jt0)

                def W2(n):
                    r, j, h = units[n]
                    tok0 = j * 128 * Dl + r
                    p, t_p, ntl, jt0 = ust.pop(n)
                    (O4, t_O4), (sg, t_sg) = blk[(r, j)]
                    pT, t_pT = Tr.next()
                    for ti in range(ntl):
                        self.tr(pT[:, ti, :], p[:, ti * 128:(ti + 1) * 128], self.identb[:], [t_p, self.t_const], [t_pT])
                    pts, t_pts = ptr.next()
                    self.cp("act", pts[:, 0:ntl, :], pT[:, 0:ntl, :], [t_pT], [t_pts])
                    for ti in range(ntl):
                        self.mm(O4[:, h * 64:(h + 1) * 64], pts[:, ti, :], Vf[:, r * nj + jt0 + ti, h * 64:(h + 1) * 64],
                                ti == 0, ti == ntl - 1, [t_pts, t_Vf], [t_O4])
                    if h == 3:
                        self.cp("act", sg[:, 0:256], O4[:, 0:256], [t_O4], [t_sg])
                        self.dma("sp", self.oaw_d[a, sl(tok0, 128, Dl), :], sg[:], [t_sg], [self.t_oaw])
                        del blk[(r, j)]

                NU = len(units)
                W1(0)
                W1(1)
                W1b(0)
                for n in range(NU):
                    if n + 2 < NU:
                        W1(n + 2)
                    W2(n)
                    if n + 1 < NU:
                        W1b(n + 1)
            if wstop <= 4:
                return
            self.P.barrier()
            cr = Ring([sb(st, "wc%d" % i, [128, 3, 264], F32) for i in range(2)])
            ms = sb(st, "wms", [128, 4], F32)
            wgt = sb(st, "wwgt", [128, 3, 4], F32)
            dt_ = sb(st, "wdt", [128, 4], F32)
            acc = sb(st, "wacc", [128, 256], F32)
            tmp = sb(st, "wtmp", [128, 256], F32)
            obr = Ring([sb(st, "wob%d" % i, [128, 256], BF16) for i in range(2)])
            t_k = Tok()
            for t in range(NT):
                ct, t_ct = cr.next()
                self.dma("sp", ct[:], self.oaw_d[:, t * 128:(t + 1) * 128, :].rearrange("a p c -> p a c"), [self.t_oaw], [t_ct])
                mv = ct[:, :, 256:260]
                dv = ct[:, :, 260:264]
                self.tt("dve", ms[:], mv[:, 0, :], mv[:, 1, :], ALU.max, [t_ct], [t_k])
                self.tt("dve", ms[:], ms[:], mv[:, 2, :], ALU.max, [t_ct, t_k], [t_k])
                self.tt("dve", wgt[:], mv, ms[:].unsqueeze(1).to_broadcast([128, 3, 4]), ALU.subtract, [t_ct, t_k], [t_k])
                self.act(wgt[:], wgt[:], AF.Exp, [t_k], [t_k])
                self.tt("dve", dv, dv, wgt[:], ALU.mult, [t_ct, t_k], [t_ct])
                self.tt("dve", dt_[:], dv[:, 0, :], dv[:, 1, :], ALU.add, [t_ct], [t_k])
                self.tt("dve", dt_[:], dt_[:], dv[:, 2, :], ALU.add, [t_ct, t_k], [t_k])
                self.recip(dt_[:], dt_[:], [t_k], [t_k])
                self.tt("dve", wgt[:], wgt[:], dt_[:].unsqueeze(1).to_broadcast([128, 3, 4]), ALU.mult, [t_k], [t_k])
                ob, t_ob = obr.next()
                v3 = lambda ap: ap.rearrange("p (h d) -> p h d", h=4)
                for a in range(3):
                    cb = wgt[:, a, :].unsqueeze(2).to_broadcast([128, 4, 64])
                    if a == 0:
                        self.tt("dve", v3(acc[:]), v3(ct[:, 0, 0:256]), cb, ALU.mult, [t_ct, t_k], [t_k])
                    else:
                        self.tt("dve", v3(tmp[:]), v3(ct[:, a, 0:256]), cb, ALU.mult, [t_ct, t_k], [t_k])
                        if a == 1:
                            self.tt("dve", acc[:], acc[:], tmp[:], ALU.add, [t_k], [t_k])
                        else:
                            self.tt("dve", ob[:], acc[:], tmp[:], ALU.add, [t_k], [t_ob])
                pT, t_pT = Tr.next()
                for kc in range(2):
                    self.tr(pT[:, kc, :], ob[:, kc * 128:(kc + 1) * 128], self.identb[:], [t_ob, self.t_const], [t_pT])
                self.cp("act", self.oaT[:, :, t * 128:(t + 1) * 128], pT[:, 0:2, :], [t_pT], [self.t_oaT])

    def phase_merge(self, l, xsrc):
        with ExitStack() as st:
            sb = self.sb
            self.load_mod(st, [2, 3, 4])
            wa = sb(st, "m_wa", [128, 2, D], BF16)
            wb = sb(st, "m_wb", [128, 8, D], BF16)
            wo = sb(st, "m_wo", [128, 8, D], BF16)
            wr = sb(st, "m_wr", [128, 8, NE], F32)
            brr = sb(st, "m_brr", [128, NE], F32)
            t_w = Tok()
            self.load_w(wa[:], self.w_br_a[l], t_w)
            self.load_w(wb[:], self.w_br_b[l], t_w)
            self.load_w(wo[:], self.w_out[l], t_w)
            self.dma("sp", wr[:], self.w_router[l].rearrange("(kc p) n -> p kc n", p=128), [], [t_w])
            self.dma("sp", brr[:], self.b_router[l:l + 1, :].partition_broadcast(128), [], [t_w])
            gtr = Ring([sb(st, "m_gt%d" % i, [128, 16, 512], BF16) for i in range(2)])
            obr = Ring([sb(st, "m_ob%d" % i, [128, 8, 512], BF16) for i in range(2)])
            mgr = Ring([sb(st, "m_mg%d" % i, [128, 8, 512], BF16) for i in range(2)])
            tA = sb(st, "m_tA", [128, 512], F32)
            tB = sb(st, "m_tB", [128, 512], F32)
            t_tA, t_tB = Tok(), Tok()
            xr = Ring([sb(st, "m_x%d" % i, [128, D], F32) for i in range(2)])
            x1r = Ring([sb(st, "m_x1%d" % i, [128, D], F32) for i in range(2)])
            h2fr = Ring([sb(st, "m_h2f%d" % i, [128, D], F32) for i in range(2)])
            h2br = Ring([sb(st, "m_h2b%d" % i, [128, D], BF16) for i in range(2)])
            h2Tr = Ring([sb(st, "m_h2T%d" % i, [128, 8, 128], F32) for i in range(1)])
            work = (sb(st, "m_junk", [128, D], F32), sb(st, "m_ss", [128, 1], F32), sb(st, "m_rstd", [128, 1], F32),
                    sb(st, "m_tmp", [128, D], F32), Tok())
            e4 = sb(st, "m_e4", [128, 4], F32)
            ssum = sb(st, "m_ssum", [128, 2], F32)
            t_e4 = Tok()
            Ar = Ring([None] * 2)
            Ar.items = [(self.ps[i], self.tp[i]) for i in (0, 1)]
            Br = Ring([None] * 2)
            Br.items = [(self.ps[i], self.tp[i]) for i in (2, 3)]
            Or = Ring([None] * 2)
            Or.items = [(self.ps[i], self.tp[i]) for i in (4, 5)]
            psT2 = [self.ps[i][:].rearrange("p (a b) -> p a b", a=4) for i in (6, 7)]
            def router(t, h2f, t_h2f):
                h2T, t_h2T = h2Tr.next()
                for hh in range(2):
                    for k4 in range(4):
                        kc = hh * 4 + k4
                        self.tr(psT2[hh][:, k4, :], h2f[:, kc * 128:(kc + 1) * 128], self.identf[:], [t_h2f, self.t_const], [self.tp[6 + hh]])
                    self.cp("act", h2T[:, hh * 4:(hh + 1) * 4, :], psT2[hh], [self.tp[6 + hh]], [t_h2T])
                pL, t_pL = Or.next()
                for kc in range(8):
                    self.mm(pL[:, 0:NE], h2T[:, kc, :], wr[:, kc, :], kc == 0, kc == 7, [t_h2T, t_w], [t_pL])
                lg = self.logits_all[:, t, :]
                m8 = self.max8_all[:, t, :]
                self.tt("dve", lg, pL[:, 0:NE], brr[:], ALU.add, [t_pL, t_w], [self.t_route])
                self.P.op("dve", lambda e, m8=m8, lg=lg: e.max(out=m8, in_=lg), [self.t_route], [self.t_route])
                self.ts("dve", self.M_all[:, t, :], lg, m8[:, 3:4], ALU.is_ge, [self.t_route], [self.t_route])
                self.ts("dve", ssum[:, 1:2], m8[:, 0:1], -1.0, ALU.mult, [self.t_route], [t_e4])
                self.act(e4[:], m8[:, 0:4], AF.Exp, [self.t_route, t_e4], [t_e4], bias=ssum[:, 1:2], scale=1.0, accum=ssum[:, 0:1])
                self.recip(ssum[:, 0:1], ssum[:, 0:1], [t_e4], [t_e4])
                self.ts("dve", self.g4_all[:, t, :], e4[:], ssum[:, 0:1], ALU.mult, [t_e4], [self.t_route])

            pend_router = [None]
            for c in range(8):
                gt, t_gt = gtr.next()
                ob, t_ob = obr.next()
                mg, t_mg = mgr.next()
                self.dma("sp", gt[:], self.gT_d[:, c * 512:(c + 1) * 512].rearrange("(m p) n -> p m n", p=128), [self.t_gT], [t_gt])
                self.dma("sp", ob[:], self.obT_d[:, c * 512:(c + 1) * 512].rearrange("(m p) n -> p m n", p=128), [self.t_obT], [t_ob])
                for m in range(8):
                    pA, t_pA = Ar.next()
                    pB, t_pB = Br.next()
                    for kc in range(2):
                        self.mm(pA[:], wa[:, kc, m * 128:(m + 1) * 128], self.oaT[:, kc, c * 512:(c + 1) * 512], kc == 0, kc == 1,
                                [t_w, self.t_oaT], [t_pA])
                    for kc in range(8):
                        self.mm(pB[:], wb[:, kc, m * 128:(m + 1) * 128], ob[:, kc, :], kc == 0, kc == 7, [t_w, t_ob], [t_pB])
                    self.tt("dve", tA[:], pA[:], gt[:, m, :], ALU.mult, [t_pA, t_gt], [t_tA])
                    self.tt("dve", tB[:], pB[:], gt[:, 8 + m, :], ALU.mult, [t_pB, t_gt], [t_tB])
                    self.tt("pool", mg[:, m, :], tA[:], tB[:], ALU.add, [t_tA, t_tB], [t_mg])
                for t4 in range(4):
                    t = c * 4 + t4
                    xt, t_x = xr.next()
                    x1, t_x1 = x1r.next()
                    self.dma("sp", xt[:], xsrc[t * 128:(t + 1) * 128, :], [], [t_x])
                    for hf in range(2):
                        pO, t_pO = Or.next()
                        for kc in range(8):
                            self.mm(pO[:], mg[:, kc, t4 * 128:(t4 + 1) * 128], wo[:, kc, hf * 512:(hf + 1) * 512], kc == 0, kc == 7,
                                    [t_mg, t_w], [t_pO])
                        self.tt("dve", x1[:, hf * 512:(hf + 1) * 512], pO[:], self.modr[2][:, hf * 512:(hf + 1) * 512], ALU.mult,
                                [t_pO, self.t_modr], [t_x1])
                    self.tt("pool", x1[:], x1[:], xt[:], ALU.add, [t_x1, t_x], [t_x1])
                    self.dma("sp", self.xs_d[t * 128:(t + 1) * 128, :], x1[:], [t_x1, self.t_xs], [])
                    h2f, t_h2f = h2fr.next()
                    h2b, t_h2b = h2br.next()
                    self.norm_mod(x1, t_x1, 4, 3, work, h2b, t_h2f, hf=h2f)
                    self.dma("sp", self.h2_d[t * 128:(t + 1) * 128, :], h2b[:], [t_h2f, self.t_h2d], [])
                    if pend_router[0] is not None:
                        pend_router[0]()
                    pend_router[0] = (lambda t=t, h2f=h2f, t_h2f=t_h2f: router(t, h2f, t_h2f))
            pend_router[0]()

    def phase_slots(self, l):
        sb = self.sb
        with ExitStack() as s2:
            tris = sb(s2, "s_tris", [128, 128], BF16)
            tri32s = sb(s2, "s_t32s", [32, 32], BF16)
            tri32i = sb(s2, "s_t32i", [32, 32], BF16)
            iota160 = sb(s2, "s_iota", [32, NBLK], F32)
            iotap = sb(s2, "s_iotap", [128, 1], F32)
            iotap32 = sb(s2, "s_iotap32", [128, 1], F32)
            t_c = Tok()
            for dst, src in ((tris, self.c_tris), (tri32s, self.c_tri32s), (tri32i, self.c_tri32i), (iota160, self.c_iota160),
                             (iotap, self.c_iotap), (iotap32, self.c_iotap32)):
                self.dma("sp", dst[:], src, [], [t_c])
            cnt = sb(s2, "s_cnt", [32, 1], F32)
            cnti = sb(s2, "s_cnti", [32, 1], I32)
            nblk = sb(s2, "s_nblk", [32, 1], F32)
            nblkb = sb(s2, "s_nblkb", [32, 128], BF16)
            nblkc = sb(s2, "s_nblkc", [32, 1], BF16)
            bstart = sb(s2, "s_bstart", [128, NE], F32)
            bend = sb(s2, "s_bend", [32, 1], F32)
            cmp = sb(s2, "s_cmp", [32, NBLK], BF16)
            erep = sb(s2, "s_erep", [128, NBLK], F32)
            chg = sb(s2, "s_chg", [128, NBLK], F32)
            wf = sb(s2, "s_wf", [128, NBLK], F32)
            pos = sb(s2, "s_pos", [128, NE], F32)
            junk = sb(s2, "s_junk", [128, NE], F32)
            p4f = sb(s2, "s_p4f", [128, NT, 4], F32)
            t_s = Tok()
            pc, t_pc = self.ps[0], self.tp[0]
            for t in range(NT):
                self.mm(pc[0:32, 0:1], self.M_all[:, t, :], self.onesb[:, 0:1], t == 0, t == NT - 1, [self.t_route, self.t_const], [t_pc])
            self.ts("dve", cnt[:], pc[0:32, 0:1], 127.0, ALU.add, [t_pc], [t_s], s2=1.0 / 128.0, op1=ALU.mult)
            self.ts("dve", nblk[:], cnt[:], -0.49609375, ALU.add, [t_s], [t_s])
            self.cp("dve", cnti[:], nblk[:], [t_s], [t_s])
            self.cp("dve", nblk[:], cnti[:], [t_s], [t_s])
            self.tt("dve", cnt[:], cnt[:], nblk[:], ALU.subtract, [t_s], [t_s])
            self.ts("dve", cnt[:], cnt[:], 1.0, ALU.is_ge, [t_s], [t_s])
            self.tt("dve", nblk[:], nblk[:], cnt[:], ALU.add, [t_s], [t_s])
            self.ts("dve", nblk[:], nblk[:], 1.0, ALU.max, [t_s], [t_s])
            self.cp("dve", nblkb[:], nblk[:, 0:1].to_broadcast([32, 128]), [t_s], [t_s])
            self.cp("dve", nblkc[:], nblk[:], [t_s], [t_s])
            p1, t_p1 = self.ps[1], self.tp[1]
            self.mm(p1[:, 0:NE], nblkb[:], tri32s[:], True, True, [t_s, t_c], [t_p1])
            self.cp("dve", bstart[:], p1[:, 0:NE], [t_p1], [t_s])
            p2, t_p2 = self.ps[2], self.tp[2]
            self.mm(p2[0:32, 0:1], tri32i[:], nblkc[:], True, True, [t_s, t_c], [t_p2])
            self.cp("dve", bend[:], p2[0:32, 0:1], [t_p2], [t_s])
            self.ts("dve", cmp[:], iota160[:], bend[:, 0:1], ALU.is_ge, [t_s, t_c], [t_s])
            p3, t_p3 = self.ps[3], self.tp[3]
            self.mm(p3[:, 0:NBLK], self.onesb[0:32, :], cmp[:], True, True, [t_s, self.t_const], [t_p3])
            self.ts("dve", erep[:], p3[:, 0:NBLK], float(NE - 1), ALU.min, [t_p3], [t_s])
            self.memset("dve", chg[:, 0:1], 1.0, [t_s])
            self.tt("dve", chg[:, 1:NBLK], erep[:, 1:NBLK], erep[:, 0:NBLK - 1], ALU.not_equal, [t_s], [t_s])
            self.ts("dve", wf[:], erep[:], 128.0, ALU.mult, [t_s, t_c], [t_s], s2=iotap[:, 0:1], op1=ALU.add)
            self.ts("dve", chg[:], chg[:], -BIGIDX, ALU.mult, [t_s], [t_s], s2=BIGIDX, op1=ALU.add)
            self.tt("dve", wf[:], wf[:], chg[:], ALU.add, [t_s], [t_s])
            if l > 0:
                self.ts("dve", wf[:], wf[:], float(l * NE * 128), ALU.add, [t_s], [t_s])
            self.cp("dve", self.widx[:], wf[:], [t_s], [self.t_widx])
            self.ts("dve", wf[:], wf[:], 2.0, ALU.mult, [t_s], [t_s])
            self.cp("dve", self.widxA[:], wf[:], [t_s], [self.t_widx])
            self.ts("dve", wf[:], wf[:], 1.0, ALU.add, [t_s], [t_s])
            self.cp("dve", self.widxB[:], wf[:], [t_s], [self.t_widx])
            self.ts("dve", self.OHall[:], erep[0:64, :], iotap32[0:64, 0:1], ALU.is_equal, [t_s, t_c], [self.t_widx])
            for t in range(NT):
                pr_, t_pr = self.ps[4 + t % 2], self.tp[4 + t % 2]
                self.mm(pr_[:, 0:NE], tris[:], self.M_all[:, t, :], True, t == 0, [t_c, self.t_route], [t_pr])
                for t2 in range(t):
                    self.mm(pr_[:, 0:NE], self.onesb[:], self.M_all[:, t2, :], False, t2 == t - 1, [self.t_const, self.t_route], [t_pr])
                self.stt(pos[:], bstart[:], 128.0, pr_[:, 0:NE], ALU.mult, ALU.add, [t_s, t_pr], [t_s])
                for k in range(4):
                    self.stt(junk[:], self.logits_all[:, t, :], self.max8_all[:, t, k:k + 1], pos[:], ALU.is_equal, ALU.mult,
                             [self.t_route, t_s], [t_s], accum=p4f[:, t, k:k + 1])
            self.cp("dve", self.pos4_all[:], p4f[:], [t_s], [self.t_pos4])
            self.P.barrier()
        with ExitStack() as s3:
            hr = Ring([sb(s3, "s_h2%d" % i, [128, D], BF16) for i in range(3)])
            for t in range(NT):
                hb, t_hb = hr.next()
                self.dma("sp", hb[:], self.h2_d[t * 128:(t + 1) * 128, :], [self.t_h2d], [t_hb])
                for k in range(4):
                    self.scatter(self.Xs_d, hb[:], self.pos4_all[:, t, k:k + 1], [t_hb, self.t_pos4, self.t_Xs], [], NBLK * 128 - 1)
            self.P.barrier()

    def phase_experts(self, l):
        with ExitStack() as st:
            sb = self.sb
            wguA = sb(st, "e_wguA", [128, 4 * 2 * D], BF16)
            wguB = sb(st, "e_wguB", [128, 4 * 2 * D], BF16)
            t_wgA, t_wgB = Tok(), Tok()
            wdn = sb(st, "e_wdn", [128, 8 * D], BF16)
            bgu = sb(st, "e_bgu", [64, 2 * D], BF16)
            bdn = sb(st, "e_bdn", [64, D], BF16)
            bf = sb(st, "e_bf", [64, 3 * D], F32)
            bt = sb(st, "e_bt", [64, 3 * D], F32)
            t_wgu, t_wdn, t_b = Tok(), Tok(), Tok()
            for half in range(2):
                self.dma("sp", bf[half * 32:(half + 1) * 32, 0:2 * D], self.b_gu[l], [], [t_b])
                self.dma("sp", bf[half * 32:(half + 1) * 32, 2 * D:3 * D], self.b_dn[l], [], [t_b])
            self.cp("dve", bgu[0:32, :], bf[0:32, 0:2 * D], [t_b], [t_b])
            self.cp("dve", bdn[0:32, :], bf[0:32, 2 * D:3 * D], [t_b], [t_b])
            self.cp("dve", bgu[32:64, :], bf[32:64, 0:2 * D], [t_b], [t_b])
            self.cp("dve", bdn[32:64, :], bf[32:64, 2 * D:3 * D], [t_b], [t_b])
            self.cp("dve", bt[32:64, 0:2 * D], bgu[32:64, :], [t_b], [t_b])
            self.cp("dve", bt[32:64, 2 * D:3 * D], bdn[32:64, :], [t_b], [t_b])
            self.tt("dve", bt[32:64, :], bf[32:64, :], bt[32:64, :], ALU.subtract, [t_b], [t_b])
            self.cp("dve", bgu[32:64, :], bt[32:64, 0:2 * D], [t_b], [t_b])
            self.cp("dve", bdn[32:64, :], bt[32:64, 2 * D:3 * D], [t_b], [t_b])
            wgu_v = self.w_gu.rearrange("l e (p kh kl) n -> (l e p kh) (kl n)", kh=2, kl=4)
            wdn_v = self.w_dn.rearrange("l e (p kc) n -> (l e p) (kc n)", kc=8)
            R = 3
            xb_ = [(sb(st, "e_x%d" % i, [128, D], BF16), Tok()) for i in range(R)]
            xT_ = [(sb(st, "e_xT%d" % i, [128, 8, 128], BF16), Tok()) for i in range(R)]
            oh_ = [(sb(st, "e_oh%d" % i, [64, 128], BF16), Tok()) for i in range(R)]
            gc_ = [(sb(st, "e_gc%d" % i, [128, 512], F32), Tok()) for i in range(2)]
            sg_ = [(sb(st, "e_sg%d" % i, [128, 512], F32), Tok()) for i in range(2)]
            lc_ = [(sb(st, "e_lc%d" % i, [128, 512], F32), Tok()) for i in range(2)]
            ac_ = [(sb(st, "e_ac%d" % i, [128, D], BF16), Tok()) for i in range(2)]
            aT_ = [(sb(st, "e_aT%d" % i, [128, 8, 128], BF16), Tok()) for i in range(2)]
            out_ = [(sb(st, "e_out%d" % i, [128, D], F32), Tok()) for i in range(2)]
            pX = self.ps[6][:].bitcast(BF16).rearrange("p (a b) -> p a b", a=8)
            pA = self.ps[7][:].bitcast(BF16).rearrange("p (a b) -> p a b", a=8)
            N = NBLK
            bound = 2 * NE * 128 - 1

            def WguA(j):
                self.gather(wguA[:], wgu_v, self.widxA[:, j:j + 1], [self.t_widx], [t_wgA], 2 * bound + 1)

            def WguB(j):
                self.gather(wguB[:], wgu_v, self.widxB[:, j:j + 1], [self.t_widx], [t_wgB], 2 * bound + 1)

            def Wdn(j):
                self.gather(wdn[:], wdn_v, self.widx[:, j:j + 1], [self.t_widx], [t_wdn], bound)

            def Ax(j):
                xb, t_xb = xb_[j % R]
                self.dma("sp", xb[:], self.Xs_d[j * 128:(j + 1) * 128, :], [self.t_Xs], [t_xb])
                oh, t_oh = oh_[j % R]
                self.cp("dve", oh[:], self.OHall[:, j:j + 1].to_broadcast([64, 128]), [self.t_widx], [t_oh])
                xv = xb[:].rearrange("p (a kc) -> p kc a", kc=8)
                for kc in range(8):
                    self.tr(pX[:, kc, :], xv[:, kc, :], self.identb[:], [t_xb, self.t_const], [self.tp[6]])
                xT, t_xT = xT_[j % R]
                self.cp("act", xT[:], pX, [self.tp[6]], [t_xT])

            def Bk(j, half):
                xT, t_xT = xT_[j % R]
                oh, t_oh = oh_[j % R]
                wb, t_wb = (wguA, t_wgA) if half == 0 else (wguB, t_wgB)
                for k4 in range(4):
                    kc = half * 4 + k4
                    for nb in range(4):
                        self.mm(self.ps[nb][:], xT[:, kc, :], wb[:, k4 * 2048 + nb * 512:k4 * 2048 + (nb + 1) * 512], kc == 0, False,
                                [t_xT, t_wb], [self.tp[nb]])
                if half == 1:
                    for nb in range(4):
                        self.mm(self.ps[nb][:], oh[:], bgu[:, nb * 512:(nb + 1) * 512], False, True, [t_oh, t_b], [self.tp[nb]])

            def E(j, hf):
                gc, t_gc = gc_[hf]
                sg, t_sg = sg_[hf]
                lc, t_lc = lc_[hf]
                ac, t_ac = ac_[j % 2]
                self.ts("dve", gc[:], self.ps[hf][:], 7.0, ALU.min, [self.tp[hf]], [t_gc])
                self.act(sg[:], gc[:], AF.Sigmoid, [t_gc], [t_sg], scale=1.702)
                self.ts("dve", lc[:], self.ps[2 + hf][:], 7.0, ALU.min, [self.tp[2 + hf]], [t_lc], s2=-7.0, op1=ALU.max)
                self.stt(lc[:], lc[:], 1.0, gc[:], ALU.add, ALU.mult, [t_lc, t_gc], [t_lc])
                self.tt("dve", ac[:, hf * 512:(hf + 1) * 512], lc[:], sg[:], ALU.mult, [t_lc, t_sg], [t_ac])

            def Ca(j):
                ac, t_ac = ac_[j % 2]
                av = ac[:].rearrange("p (a kc) -> p kc a", kc=8)
                for kc in range(8):
                    self.tr(pA[:, kc, :], av[:, kc, :], self.identb[:], [t_ac, self.t_const], [self.tp[7]])
                aT, t_aT = aT_[j % 2]
                self.cp("act", aT[:], pA, [self.tp[7]], [t_aT])

            def Cb(j):
                oh, t_oh = oh_[j % R]
                aT, t_aT = aT_[j % 2]
                ot, t_ot = out_[j % 2]
                for hf in range(2):
                    ps, t_ps = self.ps[4 + hf], self.tp[4 + hf]
                    for kc in range(8):
                        self.mm(ps[:], aT[:, kc, :], wdn[:, kc * 1024 + hf * 512:kc * 1024 + (hf + 1) * 512], kc == 0, False,
                                [t_aT, t_wdn], [t_ps])
                    self.mm(ps[:], oh[:], bdn[:, hf * 512:(hf + 1) * 512], False, True, [t_oh, t_b], [t_ps])
                    self.cp("act" if hf else "dve", ot[:, hf * 512:(hf + 1) * 512], ps[:], [t_ps], [t_ot])
                self.dma("sp", self.Out_d[j * 128:(j + 1) * 128, :], ot[:], [t_ot, self.t_Outd], [])

            WguA(0)
            WguB(0)
            Wdn(0)
            Ax(0)
            Ax(1)
            Bk(0, 0)
            WguA(1)
            Bk(0, 1)
            WguB(1)
            E(0, 0)
            E(0, 1)
            for j in range(N):
                if j + 1 < N:
                    Bk(j + 1, 0)
                if j + 2 < N:
                    WguA(j + 2)
                if j + 1 < N:
                    Bk(j + 1, 1)
                if j + 2 < N:
                    WguB(j + 2)
                Ca(j)
                if j + 2 < N:
                    Ax(j + 2)
                if j + 1 < N:
                    E(j + 1, 0)
                Cb(j)
                if j + 1 < N:
                    Wdn(j + 1)
                    E(j + 1, 1)

    def phase_combine(self, l, last):
        with ExitStack() as st:
            sb = self.sb
            self.load_mod(st, [5])
            gr = Ring([sb(st, "c_g%d" % i, [128, D], F32) for i in range(12)])
            xr = Ring([sb(st, "c_x%d" % i, [128, D], F32) for i in range(3)])
            yr = Ring([sb(st, "c_y%d" % i, [128, D], F32) for i in range(3)])
            fg = sb(st, "c_fg", [128, D], F32)
            junk = sb(st, "c_junk", [128, D], F32)
            ss = sb(st, "c_ss", [128, 2], F32)
            t_fg, t_w = Tok(), Tok()
            if last:
                self.dma("sp", fg[:], self.fng.rearrange("(o d) -> o d", o=1).partition_broadcast(128), [], [t_fg])
            for t in range(NT):
                xt, t_x = xr.next()
                y, t_y = yr.next()
                self.dma("sp", xt[:], self.xs_d[t * 128:(t + 1) * 128, :], [], [t_x])
                for k in range(4):
                    g, t_g = gr.next()
                    self.gather(g[:], self.Out_d, self.pos4_all[:, t, k:k + 1], [self.t_Outd, self.t_pos4], [t_g], None)
                    if k == 0:
                        self.ts("dve", y[:], g[:], self.g4_all[:, t, 0:1], ALU.mult, [t_g, self.t_route], [t_y])
                    else:
                        self.stt(y[:], g[:], self.g4_all[:, t, k:k + 1], y[:], ALU.mult, ALU.add, [t_g, self.t_route, t_y], [t_y])
                self.tt("dve", y[:], y[:], self.modr[5], ALU.mult, [t_y, self.t_modr], [t_y])
                self.tt("pool", y[:], y[:], xt[:], ALU.add, [t_y, t_x], [t_y])
                if not last:
                    self.dma("sp", self.xs_d[t * 128:(t + 1) * 128, :], y[:], [t_y, self.t_xs], [])
                else:
                    self.stt(junk[:], y[:], 1.0, y[:], ALU.mult, ALU.mult, [t_y], [t_w], accum=ss[:, 0:1])
                    self.rstd_from_ss(ss[:, 1:2], ss[:, 0:1], D, t_w, t_w)
                    self.stt(y[:], y[:], ss[:, 1:2], fg[:], ALU.mult, ALU.mult, [t_y, t_w, t_fg], [t_y])
                    self.dma("sp", self.out[t * 128:(t + 1) * 128, :], y[:], [t_y], [])


def _t5_bucket(rel):
    nb = 16
    max_exact = 8
    ret = np.where(rel > 0, nb, 0)
    n = np.abs(rel)
    nf = np.maximum(n, 1).astype(np.float32)
    large = max_exact + (np.log(nf / max_exact) / math.log(1024 / max_exact) * (nb - max_exact)).astype(np.int32)
    large = np.minimum(large, nb - 1)
    return ret + np.where(n < max_exact, n, large)


def host_consts(rel_bias):
    bf = ml_dtypes.bfloat16
    c = {}
    c["identb"] = np.eye(128, dtype=np.float32).astype(bf)
    c["identf"] = np.eye(128, dtype=np.float32)
    tok = np.arange(S)
    row = (tok // 64).astype(np.float32)
    col = (tok % 64).astype(np.float32)
    inv = (10000.0 ** (-np.arange(0, 32, 2, dtype=np.float32) / 32)).astype(np.float32)
    ar = row[:, None] * inv
    ac = col[:, None] * inv
    cosT = np.concatenate([np.cos(ar), np.cos(ar), np.cos(ac), np.cos(ac)], 1).astype(np.float32)
    sinT = np.concatenate([-np.sin(ar), np.sin(ar), -np.sin(ac), np.sin(ac)], 1).astype(np.float32)
    c["cosT"] = np.ascontiguousarray(cosT.reshape(NT, 128, 64).transpose(1, 0, 2))
    c["sinT"] = np.ascontiguousarray(sinT.reshape(NT, 128, 64).transpose(1, 0, 2))
    k = np.arange(128)
    c["tri_s"] = (k[:, None] < k[None, :]).astype(np.float32).astype(bf)
    k32 = np.arange(32)
    c["tri32s"] = (k32[:, None] < k32[None, :]).astype(np.float32).astype(bf)
    c["tri32i"] = (k32[:, None] <= k32[None, :]).astype(np.float32).astype(bf)
    c["iota160"] = np.broadcast_to(np.arange(NBLK, dtype=np.float32), (32, NBLK)).copy()
    c["iotap"] = np.arange(128, dtype=np.float32).reshape(128, 1)
    c["iotap32"] = (np.arange(128) % 32).astype(np.float32).reshape(128, 1)
    q = np.arange(128)[:, None]
    cc = np.arange(384)[None, :]
    rel = cc - 128 - q
    wb = np.full((128, 12, 384), -30000.0, np.float32)
    valid = np.abs(rel) <= 64
    for a, dl in enumerate(A_DIL):
        bkt = _t5_bucket(rel * dl)
        for h in range(4):
            vals = rel_bias[bkt, a * 4 + h]
            wb[:, a * 4 + h, :] = np.where(valid, vals, np.float32(-30000.0))
    c["wbias"] = wb
    return c


_CACHE = {}


def kernel(**inputs):
    inp = {k: np.ascontiguousarray(np.asarray(v, dtype=np.float32)) for k, v in inputs.items()}
    if "nc" not in _CACHE:
        _CACHE["nc"] = Builder(nlayers=2).build()
    nc = _CACHE["nc"]
    consts = host_consts(inp["rel_bias"])
    shared = {k: inp[k] for k in ("w_ada", "b_ada", "norm1_g", "w_in", "q_norm_g", "k_norm_g", "w_br_a", "w_br_b", "w_out",
                                   "norm2_g", "w_router", "b_router", "w_gate_up", "b_gate_up", "w_down", "b_down", "final_norm_g")}
    in_maps = []
    for b in range(8):
        m = dict(shared)
        m.update(consts)
        m["x"] = inp["x"][b]
        m["ccol"] = np.ascontiguousarray(inp["c"][b].reshape(8, 128).T)
        in_maps.append(m)
    res = run_bass_kernel_spmd(nc, in_maps, core_ids=list(range(8)))
    return np.stack([np.asarray(res.results[b]["out"], dtype=np.float32) for b in range(8)], 0)
```

```python
import math
from contextlib import ExitStack

import numpy as np
import ml_dtypes
import concourse.bass as bass
import concourse.mybir as mybir
from concourse.bass_utils import run_bass_kernel_spmd

F32 = mybir.dt.float32
BF16 = mybir.dt.bfloat16
I32 = mybir.dt.int32
ALU = mybir.AluOpType
AF = mybir.ActivationFunctionType
AX = mybir.AxisListType

S = 4096
D = 1024
NT = 32
NE = 32
NBLK = 160
EPS = 1e-6
A_DIL = (1, 4, 16)
BIGIDX = 1.0e6


def sl(start, n, step):
    return slice(start, start + (n - 1) * step + 1, step)

ENGS = ["pe", "act", "dve", "pool", "sp"]


class Tok:
    __slots__ = ("w", "r")

    def __init__(self):
        self.w = None
        self.r = []


class Prog:
    N_DMA_SEMS = {"sp": 24, "pool": 16, "act": 8}

    def __init__(self, nc):
        self.nc = nc
        self.q = {e: [] for e in ENGS}
        self.cnt = {e: 0 for e in ENGS}
        self.seen = {e: {} for e in ENGS}
        self.dma_val = {}
        self.dma_rr = {e: 0 for e in ENGS}
        self.prologue = {}

    def _collect(self, eng, reads, writes):
        deps = {}

        def add(d):
            if d is not None and deps.get(d[0], 0) < d[1]:
                deps[d[0]] = d[1]

        for t in reads:
            add(t.w)
        for t in writes:
            add(t.w)
            for d in t.r:
                add(d)
        out = []
        own = "E:" + eng
        seen = self.seen[eng]
        for k, v in deps.items():
            if k == own and eng == "pe":
                continue
            if seen.get(k, 0) >= v:
                continue
            seen[k] = v
            out.append((k, v))
        return out

    def _note(self, my, reads, writes):
        for t in reads:
            t.r.append(my)
            if len(t.r) > 48:
                d = {}
                for k, v in t.r:
                    if d.get(k, 0) < v:
                        d[k] = v
                t.r = list(d.items())
        for t in writes:
            t.w = my
            t.r = []

    def op(self, eng, fn, reads=(), writes=()):
        waits = self._collect(eng, reads, writes)
        self.cnt[eng] += 1
        my = ("E:" + eng, self.cnt[eng])
        self._note(my, reads, writes)
        self.q[eng].append((waits, fn, my, 1))

    def dma(self, eng, fn, reads=(), writes=()):
        waits = self._collect(eng, reads, writes)
        n = self.N_DMA_SEMS[eng]
        slot = self.dma_rr[eng] % n
        self.dma_rr[eng] += 1
        key = "D:%s:%d" % (eng, slot)
        prev = self.dma_val.get(key, 0)
        if prev > 0 and self.seen[eng].get(key, 0) < prev:
            self.seen[eng][key] = prev
            waits.append((key, prev))
        self.dma_val[key] = prev + 16
        my = (key, prev + 16)
        self._note(my, reads, writes)
        self.q[eng].append((waits, fn, my, 16))

    def barrier(self):
        for eng in ENGS:
            waits = []
            for key, v in self.dma_val.items():
                if self.seen[eng].get(key, 0) < v:
                    waits.append((key, v))
                    self.seen[eng][key] = v
            for e in ENGS:
                if e == eng or self.cnt[e] == 0:
                    continue
                k = "E:" + e
                if self.seen[eng].get(k, 0) < self.cnt[e]:
                    waits.append((k, self.cnt[e]))
                    self.seen[eng][k] = self.cnt[e]
            if waits:
                self.q[eng].append((waits, None, None, 0))

    def emit(self):
        nc = self.nc
        keys = set()
        for e in ENGS:
            for waits, fn, my, inc in self.q[e]:
                for k, v in waits:
                    keys.add(k)
                if my is not None:
                    keys.add(my[0])
        keys = sorted(keys)
        with ExitStack() as st:
            sems = {k: st.enter_context(nc.semaphore(k.replace(":", "_"))) for k in keys}
            block = st.enter_context(nc.Block())
            hmap = {"pe": block.tensor, "act": block.scalar, "dve": block.vector,
                    "pool": block.gpsimd, "sp": block.sync}

            def make(e):
                def body(engh):
                    if e in self.prologue:
                        self.prologue[e](engh)
                    for waits, fn, my, inc in self.q[e]:
                        for k, v in waits:
                            engh.wait_ge(sems[k], v)
                        if fn is None:
                            continue
                        fn(engh).then_inc(sems[my[0]], inc)
                return body

            for e in ENGS:
                if self.q[e]:
                    hmap[e](make(e))


class Ring:
    def __init__(self, items):
        self.items = [(it, Tok()) for it in items]
        self.i = 0

    def next(self):
        it = self.items[self.i % len(self.items)]
        self.i += 1
        return it


class Builder:
    def __init__(self, nlayers=2, stop=None, dbg=(), small_moe=False):
        self.nlayers = nlayers
        self.stop = stop
        self.dbg = dbg
        NEW = 1 if small_moe else NE
        nc = self.nc = bass.Bass("TRN2", target_bir_lowering=False)
        self.P = Prog(nc)
        self.outs = ["out"]
        di = lambda n, s, d=F32: nc.dram_tensor(n, s, d, kind="ExternalInput").ap()
        self.x_in = di("x", [S, D])
        self.ccol = di("ccol", [128, 8])
        self.w_ada = di("w_ada", [2, D, 6 * D])
        self.b_ada = di("b_ada", [2, 6 * D])
        self.norm1_g = di("norm1_g", [2, D])
        self.w_in = di("w_in", [2, D, 5888])
        self.q_norm_g = di("q_norm_g", [2, 64])
        self.k_norm_g = di("k_norm_g", [2, 64])
        self.wbias = di("wbias", [128, 12, 384])
        self.w_br_a = di("w_br_a", [2, 256, D])
        self.w_br_b = di("w_br_b", [2, D, D])
        self.w_out = di("w_out", [2, D, D])
        self.norm2_g = di("norm2_g", [2, D])
        self.w_router = di("w_router", [2, D, NE])
        self.b_router = di("b_router", [2, NE])
        self.w_gu = di("w_gate_up", [2, NEW, D, 2 * D])
        self.b_gu = di("b_gate_up", [2, NE, 2 * D])
        self.w_dn = di("w_down", [2, NEW, D, D])
        self.b_dn = di("b_down", [2, NE, D])
        self.fng = di("final_norm_g", [D])
        self.c_identb = di("identb", [128, 128], BF16)
        self.c_identf = di("identf", [128, 128])
        self.c_cos = di("cosT", [128, NT, 64])
        self.c_sin = di("sinT", [128, NT, 64])
        self.c_tris = di("tri_s", [128, 128], BF16)
        self.c_tri32s = di("tri32s", [32, 32], BF16)
        self.c_tri32i = di("tri32i", [32, 32], BF16)
        self.c_iota160 = di("iota160", [32, NBLK])
        self.c_iotap = di("iotap", [128, 1])
        self.c_iotap32 = di("iotap32", [128, 1])
        self.out = nc.dram_tensor("out", [S, D], F32, kind="ExternalOutput").ap()
        self.xs_d = self.scratch("xs_d", [S, D], F32)
        self.obT_d = self.scratch("obT_d", [D, S], BF16)
        self.gT_d = self.scratch("gT_d", [2 * D, S], BF16)
        self.oaw_d = self.scratch("oaw_d", [3, S, 264], F32)
        self.h2_d = self.scratch("h2_d", [S, D], BF16)
        self.Xs_d = self.scratch("Xs_d", [NBLK * 128, D], BF16)
        self.Out_d = self.scratch("Out_d", [NBLK * 128, D], F32)
        self.modr_d = self.scratch("modr_d", [128, 6, D], F32)
        self.t_modrd = Tok()
        self.t_xs, self.t_obT, self.t_gT, self.t_oaw, self.t_h2d, self.t_Xs, self.t_Outd = [Tok() for _ in range(7)]
        self.t_out = Tok()

    def scratch(self, name, shape, dt):
        if name in self.dbg:
            self.outs.append(name)
            return self.nc.dram_tensor(name, shape, dt, kind="ExternalOutput").ap()
        return self.nc.dram_tensor(name, shape, dt, kind="Internal").ap()

    def sb(self, st, name, shape, dt):
        self._nsb = getattr(self, "_nsb", 0) + 1
        return st.enter_context(self.nc.sbuf_tensor("sb%d_%s" % (self._nsb, name), shape, dt))

    def tt(self, eng, out, in0, in1, op, r, w):
        self.P.op(eng, lambda e: e.tensor_tensor(out=out, in0=in0, in1=in1, op=op), r, w)

    def ts(self, eng, out, in0, s1, op0, r, w, s2=None, op1=None):
        if op1 is None:
            self.P.op(eng, lambda e: e.tensor_scalar(out=out, in0=in0, scalar1=s1, scalar2=None, op0=op0), r, w)
        else:
            self.P.op(eng, lambda e: e.tensor_scalar(out=out, in0=in0, scalar1=s1, scalar2=s2, op0=op0, op1=op1), r, w)

    def stt(self, out, in0, scalar, in1, op0, op1, r, w, accum=None):
        if accum is None:
            self.P.op("dve", lambda e: e.scalar_tensor_tensor(out=out, in0=in0, scalar=scalar, in1=in1, op0=op0, op1=op1), r, w)
        else:
            self.P.op("dve", lambda e: e.scalar_tensor_tensor(out=out, in0=in0, scalar=scalar, in1=in1, op0=op0, op1=op1,
                                                              accum_out=accum), r, w)

    def act(self, out, in_, func, r, w, bias=None, scale=None, accum=None):
        kw = {}
        if bias is not None:
            kw["bias"] = bias
        if scale is not None:
            kw["scale"] = scale
        if accum is not None:
            kw["accum_out"] = accum
        self.P.op("act", lambda e: e.activation(out=out, in_=in_, func=func, **kw), r, w)

    def cp(self, eng, out, in_, r, w):
        if eng == "act":
            self.P.op("act", lambda e: e.copy(out=out, in_=in_), r, w)
        else:
            self.P.op(eng, lambda e: e.tensor_copy(out=out, in_=in_), r, w)

    def mm(self, out, lhsT, rhs, start, stop, r, w):
        self.P.op("pe", lambda e: e.matmul(out, lhsT=lhsT, rhs=rhs, start=start, stop=stop), r, w)

    def tr(self, out, in_, ident, r, w):
        self.P.op("pe", lambda e: e.transpose(out=out, in_=in_, identity=ident), r, w)

    def dma(self, q, out, in_, r, w):
        self.P.dma(q, lambda e: e.dma_start(out=out, in_=in_), r, w)

    def red(self, out, in_, op, r, w):
        self.P.op("dve", lambda e: e.tensor_reduce(out=out, in_=in_, axis=AX.X, op=op), r, w)

    def recip(self, out, in_, r, w):
        self.P.op("dve", lambda e: e.reciprocal(out=out, in_=in_), r, w)

    def memset(self, eng, ap, val, w):
        self.P.op(eng, lambda e: e.memset(ap, val), (), w)

    def gather(self, out, in_, idx, r, w, bound):
        if bound is None:
            self.P.dma("pool", lambda e: e.indirect_dma_start(out=out, out_offset=None, in_=in_,
                                                              in_offset=bass.IndirectOffsetOnAxis(ap=idx, axis=0)), r, w)
            return
        if "pool" not in self.P.prologue:
            self.bregs = {}
            self.bounds = []

            def pro(e):
                for bv in self.bounds:
                    self.bregs[bv] = e.alloc_register("bound_reg_%d" % bv)
                    e.reg_mov(self.bregs[bv], bv)
            self.P.prologue["pool"] = pro
        if bound not in self.bounds:
            self.bounds.append(bound)
        self.P.dma("pool", lambda e: e.indirect_dma_start(out=out, out_offset=None, in_=in_,
                                                          in_offset=bass.IndirectOffsetOnAxis(ap=idx, axis=0),
                                                          bounds_check=self.bregs[bound], oob_is_err=False), r, w)

    def scatter(self, out, in_, idx, r, w, bound):
        self.P.dma("pool", lambda e: e.indirect_dma_start(out=out, out_offset=bass.IndirectOffsetOnAxis(ap=idx, axis=0),
                                                          in_=in_, in_offset=None), r, w)

    def build(self):
        nc = self.nc
        with ExitStack() as gst:
            self.gst = gst
            self.ps = [gst.enter_context(nc.psum_tensor("ps%d" % i, [128, 512], F32)) for i in range(8)]
            self.tp = [Tok() for _ in range(8)]
            sb = self.sb
            self.identb = sb(gst, "identb", [128, 128], BF16)
            self.identf = sb(gst, "identf", [128, 128], F32)
            self.onesb = sb(gst, "onesb", [128, 128], BF16)
            self.onesf = sb(gst, "onesf", [128, 128], F32)
            self.epsc = sb(gst, "epsc", [128, 1], F32)
            self.t_const = Tok()
            tc = [self.t_const]
            self.dma("sp", self.identb[:], self.c_identb, [], tc)
            self.dma("sp", self.identf[:], self.c_identf, [], tc)
            self.memset("dve", self.onesb[:], 1.0, tc)
            self.memset("dve", self.onesf[:], 1.0, tc)
            self.memset("dve", self.epsc[:], EPS, tc)
            self.neghalf = sb(gst, "neghalf", [128, 16], F32)
            self.memset("dve", self.neghalf[:], -0.5, tc)
            self.P.barrier()
            xsrc = self.x_in
            for l in range(self.nlayers):
                last = (l == self.nlayers - 1)
                if self.layer(l, xsrc, last):
                    break
                xsrc = self.xs_d
            self.P.barrier()
            self.P.emit()
        return nc

    def layer(self, l, xsrc, last):
        P = self.P
        stop = self.stop if l == self.nlayers - 1 else None
        with ExitStack() as lst:
            self.oaT = self.sb(lst, "oaT", [128, 2, S], BF16)
            self.t_oaT = Tok()
            self.phase_mod(l)
            P.barrier()
            if stop == "mod":
                return True
            with ExitStack() as ast:
                self.hT = self.sb(ast, "hT", [128, 8, S], BF16)
                self.t_hT = Tok()
                self.phase_norm1(l, xsrc)
                P.barrier()
                if stop == "norm1":
                    return True
                self.phase_gates(l)
                P.barrier()
                if stop == "gates":
                    return True
                self.phase_window(l)
                P.barrier()
                if stop == "window":
                    return True
                self.phase_gqa(l)
                P.barrier()
                if stop == "gqa":
                    return True
            with ExitStack() as mst:
                self.logits_all = self.sb(mst, "logits_all", [128, NT, NE], F32)
                self.max8_all = self.sb(mst, "max8_all", [128, NT, 8], F32)
                self.g4_all = self.sb(mst, "g4_all", [128, NT, 4], F32)
                self.M_all = self.sb(mst, "M_all", [128, NT, NE], BF16)
                self.pos4_all = self.sb(mst, "pos4_all", [128, NT, 4], I32)
                self.widx = self.sb(mst, "widx", [128, NBLK], I32)
                self.widxA = self.sb(mst, "widxA", [128, NBLK], I32)
                self.widxB = self.sb(mst, "widxB", [128, NBLK], I32)
                self.OHall = self.sb(mst, "OHall", [64, NBLK], BF16)
                self.t_widx = Tok()
                self.t_route = Tok()
                self.t_pos4 = Tok()
                self.phase_merge(l, xsrc)
                P.barrier()
                if stop == "merge":
                    return True
                self.phase_slots(l)
                P.barrier()
                if stop == "slots":
                    return True
                self.phase_experts(l)
                P.barrier()
                if stop == "experts":
                    return True
                self.phase_combine(l, last)
                P.barrier()
        return False

    def phase_mod(self, l):
        with ExitStack() as st:
            sb = self.sb
            cc = sb(st, "cc", [128, 8], F32)
            cs = sb(st, "cs", [128, 8], F32)
            crep = sb(st, "crep", [128, 8, 128], F32)
            brep = sb(st, "brep", [128, 6 * D], F32)
            ng = sb(st, "ng", [128, 2, D], F32)
            modr = sb(st, "modr", [128, 6, D], F32)
            t_modr = Tok()
            wa = Ring([sb(st, "wa%d" % i, [128, 8, 512], F32) for i in range(2)])
            t_cc, t_cs, t_crep, t_brep, t_ng = Tok(), Tok(), Tok(), Tok(), Tok()
            self.dma("sp", cc[:], self.ccol, [], [t_cc])
            self.dma("sp", brep[:], self.b_ada[l:l + 1, :].partition_broadcast(128), [], [t_brep])
            self.dma("sp", ng[:, 0, :], self.norm1_g[l:l + 1, :].partition_broadcast(128), [], [t_ng])
            self.dma("sp", ng[:, 1, :], self.norm2_g[l:l + 1, :].partition_broadcast(128), [], [t_ng])
            self.act(cs[:], cc[:], AF.Silu, [t_cc], [t_cs])
            for kc in range(8):
                self.cp("dve", crep[:, kc, :], cs[:, kc:kc + 1].to_broadcast([128, 128]), [t_cs], [t_crep])
            modf = modr[:].rearrange("p a d -> p (a d)")
            psr = Ring(self.ps[0:2])
            psr.items = [(self.ps[0], self.tp[0]), (self.ps[1], self.tp[1])]
            for ch in range(12):
                w, t_w = wa.next()
                self.dma("sp", w[:], self.w_ada[l][:, ch * 512:(ch + 1) * 512].rearrange("(kc p) n -> p kc n", p=128), [], [t_w])
                ps, t_ps = psr.next()
                for kc in range(8):
                    self.mm(ps[:], crep[:, kc, :], w[:, kc, :], kc == 0, kc == 7, [t_crep, t_w], [t_ps])
                self.tt("dve", modf[:, ch * 512:(ch + 1) * 512], ps[:], brep[:, ch * 512:(ch + 1) * 512], ALU.add,
                        [t_ps, t_brep], [t_modr])
            for i, j in ((1, 0), (4, 1)):
                self.stt(modr[:, i, :], modr[:, i, :], 1.0, ng[:, j, :], ALU.add, ALU.mult, [t_modr, t_ng], [t_modr])
            self.dma("sp", self.modr_d, modr[:], [t_modr], [self.t_modrd])

    def load_mod(self, st, idxs):
        tl = self.sb(st, "modl", [128, len(idxs), D], F32)
        self.t_modr = Tok()
        self.modr = {}
        for n, i in enumerate(idxs):
            self.dma("sp", tl[:, n, :], self.modr_d[:, i, :], [self.t_modrd], [self.t_modr])
            self.modr[i] = tl[:, n, :]

    def rstd_from_ss(self, rstd, ss, n, t_in, t_out):
        self.act(rstd, ss, AF.Ln, [t_in, self.t_const], [t_out], bias=self.epsc[0:rstd.shape[0], 0:1], scale=1.0 / n)
        self.act(rstd, rstd, AF.Exp, [t_out], [t_out], scale=-0.5)

    def norm_mod(self, xt, t_x, i_g, i_sh, work, hb, t_hb, hf=None):
        junk, ss, rstd, tmp, t_w = work
        self.stt(junk[:], xt[:], 1.0, xt[:], ALU.mult, ALU.mult, [t_x], [t_w], accum=ss[:, 0:1])
        self.rstd_from_ss(rstd[:, 0:1], ss[:, 0:1], D, t_w, t_w)
        self.stt(tmp[:], xt[:], rstd[:, 0:1], self.modr[i_g], ALU.mult, ALU.mult, [t_x, t_w, self.t_modr], [t_w])
        if hf is not None:
            self.tt("dve", hf[:], tmp[:], self.modr[i_sh], ALU.add, [t_w, self.t_modr], [t_hb])
            self.cp("pool", hb[:], hf[:], [t_hb], [t_hb])
        else:
            self.tt("dve", hb[:], tmp[:], self.modr[i_sh], ALU.add, [t_w, self.t_modr], [t_hb])

    def phase_norm1(self, l, xsrc):
        with ExitStack() as st:
            sb = self.sb
            self.load_mod(st, [0, 1])
            xr = Ring([sb(st, "xt%d" % i, [128, D], F32) for i in range(2)])
            hr = Ring([sb(st, "hb%d" % i, [128, D], BF16) for i in range(2)])
            works = [(sb(st, "junk%d" % i, [128, D], F32), sb(st, "ss%d" % i, [128, 1], F32), sb(st, "rstd%d" % i, [128, 1], F32),
                      sb(st, "tmp%d" % i, [128, D], F32), Tok()) for i in range(2)]
            psb = [self.ps[i][:].bitcast(BF16).rearrange("p (a b) -> p a b", a=8) for i in range(2)]
            for t in range(NT):
                xt, t_x = xr.next()
                hb, t_hb = hr.next()
                self.dma("sp", xt[:], xsrc[t * 128:(t + 1) * 128, :], [self.t_xs], [t_x])
                self.norm_mod(xt, t_x, 1, 0, works[t % 2], hb, t_hb)
                pT, t_p = psb[t % 2], self.tp[t % 2]
                for kc in range(8):
                    self.tr(pT[:, kc, :], hb[:, kc * 128:(kc + 1) * 128], self.identb[:], [t_hb, self.t_const], [t_p])
                self.cp("act", self.hT[:, :, t * 128:(t + 1) * 128], pT, [t_p], [self.t_hT])

    def load_w(self, w, src, t_w, kc=8):
        self.dma("pool", w, src.rearrange("(kc p) n -> p kc n", p=128), [], [t_w])

    def phase_gates(self, l):
        with ExitStack() as st:
            sb = self.sb
            wg = sb(st, "wg", [128, 8, 2 * D], BF16)
            t_wg = Tok()
            for q in range(4):
                self.load_w(wg[:, :, q * 512:(q + 1) * 512], self.w_in[l][:, 3840 + q * 512:3840 + (q + 1) * 512], t_wg)
            gr = Ring([sb(st, "gsb%d" % i, [128, 512], BF16) for i in range(3)])
            n = 0
            for c in range(8):
                for m in range(16):
                    ps, t_ps = self.ps[n % 4], self.tp[n % 4]
                    n += 1
                    for kc in range(8):
                        self.mm(ps[:], wg[:, kc, m * 128:(m + 1) * 128], self.hT[:, kc, c * 512:(c + 1) * 512], kc == 0, kc == 7,
                                [t_wg, self.t_hT], [t_ps])
                    g, t_g = gr.next()
                    self.act(g[:], ps[:], AF.Sigmoid, [t_ps], [t_g])
                    self.dma("sp", self.gT_d[m * 128:(m + 1) * 128, c * 512:(c + 1) * 512], g[:], [t_g, self.t_gT], [])

    def qknorm_rope(self, src, H, grep, tile, wk, dst, t_src, t_dst):
        sq, ss, rstd, qn, t1, t2, t_w = wk
        W = H * 64
        v3 = lambda ap: ap[:, 0:W].rearrange("p (h d) -> p h d", h=H)
        v5 = lambda ap: ap[:, 0:W].rearrange("p (h a b c) -> p h a b c", h=H, a=2, b=2)
        self.tt("pool", sq[:, 0:W], src, src, ALU.mult, [t_src], [t_w])
        self.red(ss[:, 0:H], v3(sq), ALU.add, [t_w], [t_w])
        self.ts("pool", rstd[:, 0:H], ss[:, 0:H], 1.0 / 64, ALU.mult, [t_w], [t_w], s2=EPS, op1=ALU.add)
        self.tt("pool", rstd[:, 0:H], rstd[:, 0:H], self.neghalf[:, 0:H], ALU.pow, [t_w, self.t_const], [t_w])
        self.tt("dve", v3(qn), src.rearrange("p (h d) -> p h d", h=H), rstd[:, 0:H].unsqueeze(2).to_broadcast([128, H, 64]), ALU.mult,
                [t_src, t_w], [t_w])
        self.tt("pool", v3(qn), v3(qn), grep.unsqueeze(1).to_broadcast([128, H, 64]), ALU.mult, [t_w, self.t_const], [t_w])
        cosb = self.cos[:, tile, :].unsqueeze(1).to_broadcast([128, H, 64])
        sinv = self.sin[:, tile, :].rearrange("p (a b c) -> p a b c", a=2, b=2)
        self.tt("dve", v3(t1), v3(qn), cosb, ALU.mult, [t_w, self.t_const], [t_w])
        for b in range(2):
            sb_ = sinv[:, :, b, :].unsqueeze(1).to_broadcast([128, H, 2, 16])
            self.tt("pool", v5(t2)[:, :, :, b, :], v5(qn)[:, :, :, 1 - b, :], sb_, ALU.mult, [t_w, self.t_const], [t_w])
        self.tt("dve", dst, t1[:, 0:W], t2[:, 0:W], ALU.add, [t_w], [t_dst])

    def phase_gqa(self, l):
        with ExitStack() as st:
            sb = self.sb
            self.cos = sb(st, "cos", [128, NT, 64], F32)
            self.sin = sb(st, "sin", [128, NT, 64], F32)
            self.dma("sp", self.cos[:], self.c_cos, [], [self.t_const])
            self.dma("sp", self.sin[:], self.c_sin, [], [self.t_const])
            KT = sb(st, "KT", [128, 2, S], BF16)
            V = sb(st, "V", [128, NT, 4, 128], BF16)
            wkv = sb(st, "wkv", [128, 8, 512], BF16)
            gq = sb(st, "gq", [128, 64], F32)
            gk = sb(st, "gk", [128, 64], F32)
            t_KT, t_V, t_wkv = Tok(), Tok(), Tok()
            self.dma("sp", gq[:], self.q_norm_g[l:l + 1, :].partition_broadcast(128), [], [self.t_const])
            self.dma("sp", gk[:], self.k_norm_g[l:l + 1, :].partition_broadcast(128), [], [self.t_const])
            self.load_w(wkv[:], self.w_in[l][:, 3328:3840], t_wkv)
            self.memset("dve", V[:, :, :, 64:128], 1.0, [t_V])
            wks = [(sb(st, "q_sq%d" % i, [128, 256], F32), sb(st, "q_ss%d" % i, [128, 4], F32), sb(st, "q_rstd%d" % i, [128, 4], F32),
                    sb(st, "q_qn%d" % i, [128, 256], F32), sb(st, "q_t1%d" % i, [128, 256], F32), sb(st, "q_t2%d" % i, [128, 256], F32), Tok())
                   for i in range(2)]
            wkn = [0]

            def next_wk():
                wkn[0] += 1
                return wks[wkn[0] % 2]
            psT = self.ps[7][:].bitcast(BF16).rearrange("p (a b) -> p a b", a=8)
            t_psT = self.tp[7]
            ksr = Ring([sb(st, "ksb3_%d" % i, [128, 256], F32) for i in range(3)])
            krr = Ring([sb(st, "kr3_%d" % i, [128, 256], BF16) for i in range(3)])
            kst = {}

            def kv1(t):
                ps, t_ps = self.ps[6 - t % 2], self.tp[6 - t % 2]
                for kc in range(8):
                    self.mm(ps[:], self.hT[:, kc, t * 128:(t + 1) * 128], wkv[:, kc, :], kc == 0, kc == 7, [self.t_hT, t_wkv], [t_ps])
                self.cp("act", V[:, t, :, 0:64], ps[:, 256:512].rearrange("p (g d) -> p g d", g=4), [t_ps], [t_V])
                ksb, t_ksb = ksr.next()
                self.cp("act", ksb[:], ps[:, 0:256], [t_ps], [t_ksb])
                kr, t_kr = krr.next()
                self.qknorm_rope(ksb[:], 4, gk[:], t, next_wk(), kr[:], t_ksb, t_kr)
                kst[t] = (kr, t_kr)

            def kv2(t):
                kr, t_kr = kst.pop(t)
                for m in range(2):
                    self.tr(psT[:, m, :], kr[:, m * 128:(m + 1) * 128], self.identb[:], [t_kr, self.t_const], [t_psT])
                self.cp("dve", KT[:, :, t * 128:(t + 1) * 128], psT[:, 0:2, :], [t_psT], [t_KT])

            kv1(0)
            kv1(1)
            for t in range(NT):
                kv2(t)
                if t + 2 < NT:
                    kv1(t + 2)
            wq = sb(st, "wq", [128, 8, 256], BF16)
            t_wq = Tok()
            QTb = [(sb(st, "QT%d" % i, [128, 4, 512], BF16), Tok()) for i in range(2)]
            PTr = Ring([sb(st, "PT%d" % i, [128, 512], BF16) for i in range(4)])
            rdr = Ring([sb(st, "rd%d" % i, [64, 512], F32) for i in range(2)])
            obr = Ring([sb(st, "ob%d" % i, [64, 512], BF16) for i in range(2)])
            Sr = Ring([None] * 3)
            Sr.items = [(self.ps[i], self.tp[i]) for i in range(3)]
            Or = Ring([None] * 2)
            Or.items = [(self.ps[i], self.tp[i]) for i in (3, 4)]
            units = [(g, c) for g in range(4) for c in range(8)]

            qst = {}

            def qproj1(u, t4):
                g, c = units[u]
                if c == 0 and t4 == 0:
                    self.load_w(wq[:], self.w_in[l][:, 2304 + g * 256:2304 + (g + 1) * 256], t_wq)
                t = c * 4 + t4
                ps, t_ps = self.ps[6 - t4 % 2], self.tp[6 - t4 % 2]
                for kc in range(8):
                    self.mm(ps[:, 0:256], self.hT[:, kc, t * 128:(t + 1) * 128], wq[:, kc, :], kc == 0, kc == 7,
                            [self.t_hT, t_wq], [t_ps])
                qsb, t_qsb = ksr.next()
                self.cp("dve", qsb[:], ps[:, 0:256], [t_ps], [t_qsb])
                qr, t_qr = krr.next()
                self.qknorm_rope(qsb[:], 4, gq[:], t, next_wk(), qr[:], t_qsb, t_qr)
                qst[(u, t4)] = (qr, t_qr)

            def qproj2(u, t4):
                g, c = units[u]
                kb = (g % 2) * 64
                ko = 64 - kb
                QT, t_QT = QTb[u % 2]
                qr, t_qr = qst.pop((u, t4))
                for h in range(4):
                    self.tr(psT[0:64, h, :], qr[:, h * 64:(h + 1) * 64], self.identb[:], [t_qr, self.t_const], [t_psT])
                self.cp("dve", QT[kb:kb + 64, :, t4 * 128:(t4 + 1) * 128], psT[0:64, 0:4, :], [t_psT], [t_QT])
                self.memset("pool", QT[ko:ko + 64, :, t4 * 128:(t4 + 1) * 128], 0.0, [t_QT])

            def attend(u, h):
                g, c = units[u]
                QT, t_QT = QTb[u % 2]
                hq = g * 4 + h
                OT, t_OT = Or.next()
                pend = []

                def score(kt):
                    Sb, t_S = Sr.next()
                    self.mm(Sb[:], KT[:, g // 2, kt * 128:(kt + 1) * 128], QT[:, h, :], True, True, [t_KT, t_QT], [t_S])
                    PT, t_PT = PTr.next()
                    self.act(PT[:], Sb[:], AF.Exp, [t_S], [t_PT], scale=0.125)
                    pend.append((kt, PT, t_PT))

                def pv():
                    kt, PT, t_PT = pend.pop(0)
                    self.mm(OT[:], V[:, kt, g, :], PT[:], kt == 0, kt == NT - 1, [t_V, t_PT], [t_OT])

                score(0)
                score(1)
                for kt in range(NT):
                    if kt + 2 < NT:
                        score(kt + 2)
                    pv()
                def fin():
                    rd, t_rd = rdr.next()
                    self.recip(rd[0:64, :], OT[64:128, :], [t_OT], [t_rd])
                    ob, t_ob = obr.next()
                    self.tt("dve", ob[:], OT[0:64, :], rd[0:64, :], ALU.mult, [t_OT, t_rd], [t_ob])
                    self.dma("sp", self.obT_d[hq * 64:(hq + 1) * 64, c * 512:(c + 1) * 512], ob[:], [t_ob], [self.t_obT])
                return fin

            for t4 in range(4):
                qproj1(0, t4)
                qproj2(0, t4)
            for u in range(len(units)):
                nxt = u + 1 < len(units)
                f = attend(u, 0)
                f()
                if nxt:
                    qproj1(u + 1, 0)
                    qproj1(u + 1, 1)
                f = attend(u, 1)
                if nxt:
                    qproj2(u + 1, 0)
                    qproj2(u + 1, 1)
                f()
                if nxt:
                    qproj1(u + 1, 2)
                f = attend(u, 2)
                if nxt:
                    qproj2(u + 1, 2)
                f()
                if nxt:
                    qproj1(u + 1, 3)
                f = attend(u, 3)
                if nxt:
                    qproj2(u + 1, 3)
                f()

    def phase_window(self, l):
        with ExitStack() as st:
            sb = self.sb
            bias = sb(st, "wb", [128, 12, 384], F32)
            t_bias = Tok()
            self.dma("sp", bias[:], self.wbias, [], [t_bias])
            QT = sb(st, "wQT", [128, 2, S], BF16)
            KT = sb(st, "wKT", [128, 2, S], BF16)
            Vf = sb(st, "wVf", [128, NT, 256], BF16)
            wq = sb(st, "wwq", [128, 8, 768], BF16)
            t_QT, t_KT, t_Vf, t_wq = Tok(), Tok(), Tok(), Tok()
            sr = Ring([sb(st, "ws%d" % i, [128, 384], F32) for i in range(3)])
            pr = Ring([sb(st, "wp%d" % i, [128, 384], BF16) for i in range(4)])
            ptr = Ring([sb(st, "wpt%d" % i, [128, 3, 128], BF16) for i in range(2)])
            mr = Ring([sb(st, "wm%d" % i, [128, 2], F32) for i in range(6)])
            stg = Ring([sb(st, "wstg%d" % i, [128, 264], F32) for i in range(2)])
            Sr = Ring([None] * 3)
            Sr.items = [(self.ps[i], self.tp[i]) for i in (0, 1, 7)]
            Tr = Ring([None] * 2)
            Tr.items = [(self.ps[i][:].bitcast(BF16)[:, 0:384].rearrange("p (a b) -> p a b", a=3), self.tp[i]) for i in (2, 3)]
            Or = Ring([None] * 2)
            Or.items = [(self.ps[i], self.tp[i]) for i in (4, 5)]
            import os
            wstop = int(os.environ.get("WSTOP", "99"))
            for a, Dl in enumerate(A_DIL):
                L = S // Dl
                nj = L // 128
                if str(a) not in os.environ.get("WGRPS", "012"):
                    continue
                self.load_w(wq[:], self.w_in[l][:, a * 768:(a + 1) * 768], t_wq)
                n = 0
                for which, dst, t_dst in ((0, QT, t_QT), (1, KT, t_KT)):
                    for m in range(2):
                        for c in range(8):
                            pbk = (6, 7, 0, 1)[n % 4]
                            ps, t_ps = self.ps[pbk], self.tp[pbk]
                            n += 1
                            col = which * 256 + m * 128
                            for kc in range(8):
                                self.mm(ps[:], wq[:, kc, col:col + 128], self.hT[:, kc, c * 512:(c + 1) * 512], kc == 0, kc == 7,
                                        [t_wq, self.t_hT], [t_ps])
                            self.cp("act" if n % 2 else "dve", dst[:, m, c * 512:(c + 1) * 512], ps[:], [t_ps], [t_dst])
                if wstop <= 1:
                    continue
                for r in range(Dl):
                    for j in range(nj):
                        bi = r * nj + j
                        tok0 = j * 128 * Dl + r
                        pbk = (6, 7, 0, 1)[bi % 4]
                        ps, t_ps = self.ps[pbk], self.tp[pbk]
                        for kc in range(8):
                            self.mm(ps[:, 0:256], self.hT[:, kc, sl(tok0, 128, Dl)], wq[:, kc, 512:768], kc == 0, kc == 7,
                                    [self.t_hT, t_wq], [t_ps])
                        self.cp("act" if bi % 2 else "dve", Vf[:, bi, :], ps[:, 0:256], [t_ps], [t_Vf])
                if wstop <= 2:
                    continue
                units = []
                for r in range(Dl):
                    for j in range(nj):
                        for h in range(4):
                            units.append((r, j, h))
                ust = {}
                blk = {}

                def W1(n):
                    r, j, h = units[n]
                    tok0 = j * 128 * Dl + r
                    jt0 = max(j - 1, 0)
                    jt1 = min(j + 1, nj - 1)
                    ntl = jt1 - jt0 + 1
                    c0 = (jt0 - (j - 1)) * 128
                    w = ntl * 128
                    k0 = jt0 * 128 * Dl + r
                    if h == 0:
                        blk[(r, j)] = (Or.next(), stg.next())
                    (O4, t_O4), (sg, t_sg) = blk[(r, j)]
                    Sb, t_S = Sr.next()
                    hb_ = (h % 2) * 64
                    self.mm(Sb[:, 0:w], QT[hb_:hb_ + 64, h // 2, sl(tok0, 128, Dl)], KT[hb_:hb_ + 64, h // 2, sl(k0, w, Dl)],
                            True, True, [t_QT, t_KT], [t_S])
                    s_, t_s = sr.next()
                    self.stt(s_[:, 0:w], Sb[:, 0:w], 0.125, bias[:, a * 4 + h, c0:c0 + w], ALU.mult, ALU.add, [t_S, t_bias], [t_s])
                    m, t_m = mr.next()
                    self.P.op("dve", lambda e, m=m, s_=s_, w=w: e.reduce_max(out=m[:, 0:1], in_=s_[:, 0:w], axis=AX.X), [t_s], [t_m])
                    self.ts("dve", m[:, 1:2], m[:, 0:1], -1.0, ALU.mult, [t_m], [t_m])
                    self.cp("dve", sg[:, 256 + h:257 + h], m[:, 0:1], [t_m], [t_sg])
                    ust[n] = (s_, t_s, m, t_m, sg, t_sg, w, h, ntl, jt0)

                def W1b(n):
                    s_, t_s, m, t_m, sg, t_sg, w, h, ntl, jt0 = ust[n]
                    p, t_p = pr.next()
                    self.act(p[:, 0:w], s_[:, 0:w], AF.Exp, [t_s, t_m], [t_p, t_sg], bias=m[:, 1:2], scale=1.0,
                             accum=sg[:, 260 + h:261 + h])
                    ust[n] = (p, t_p, ntl, jt0)

                def W2(n):
                    r, j, h = units[n]
                    tok0 = j * 128 * Dl + r
                    p, t_p, ntl, jt0 = ust.pop(n)
                    (O4, t_O4), (sg, t_sg) = blk[(r, j)]
                    pT, t_pT = Tr.next()
                    for ti in range(ntl):
                        self.tr(pT[:, ti, :], p[:, ti * 128:(ti + 1) * 128], self.identb[:], [t_p, self.t_const], [t_pT])
                    pts, t_pts = ptr.next()
                    self.cp("act", pts[:, 0:ntl, :], pT[:, 0:ntl, :], [t_pT], [t_pts])
                    for ti in range(ntl):
                        self.mm(O4[:, h * 64:(h + 1) * 64], pts[:, ti, :], Vf[:, r * nj + jt0 + ti, h * 64:(h + 1) * 64],
                                ti == 0, ti == ntl - 1, [t_pts, t_Vf], [t_O4])
                    if h == 3:
                        self.cp("act", sg[:, 0:256], O4[:, 0:256], [t_O4], [t_sg])
                        self.dma("sp", self.oaw_d[a, sl(tok0, 128, Dl), :], sg[:], [t_sg], [self.t_oaw])
                        del blk[(r, j)]

                NU = len(units)
                W1(0)
                W1(1)
                W1b(0)
                for n in range(NU):
                    if n + 2 < NU:
                        W1(n + 2)
                    W2(n)
                    if n + 1 < NU:
                        W1b(n + 1)
            if wstop <= 4:
                return
            self.P.barrier()
            cr = Ring([sb(st, "wc%d" % i, [128, 3, 264], F32) for i in range(2)])
            ms = sb(st, "wms", [128, 4], F32)
            wgt = sb(st, "wwgt", [128, 3, 4], F32)
            dt_ = sb(st, "wdt", [128, 4], F32)
            acc = sb(st, "wacc", [128, 256], F32)
            tmp = sb(st, "wtmp", [128, 256], F32)
            obr = Ring([sb(st, "wob%d" % i, [128, 256], BF16) for i in range(2)])
            t_k = Tok()
            for t in range(NT):
                ct, t_ct = cr.next()
                self.dma("sp", ct[:], self.oaw_d[:, t * 128:(t + 1) * 128, :].rearrange("a p c -> p a c"), [self.t_oaw], [t_ct])
                mv = ct[:, :, 256:260]
                dv = ct[:, :, 260:264]
                self.tt("dve", ms[:], mv[:, 0, :], mv[:, 1, :], ALU.max, [t_ct], [t_k])
                self.tt("dve", ms[:], ms[:], mv[:, 2, :], ALU.max, [t_ct, t_k], [t_k])
                self.tt("dve", wgt[:], mv, ms[:].unsqueeze(1).to_broadcast([128, 3, 4]), ALU.subtract, [t_ct, t_k], [t_k])
                self.act(wgt[:], wgt[:], AF.Exp, [t_k], [t_k])
                self.tt("dve", dv, dv, wgt[:], ALU.mult, [t_ct, t_k], [t_ct])
                self.tt("dve", dt_[:], dv[:, 0, :], dv[:, 1, :], ALU.add, [t_ct], [t_k])
                self.tt("dve", dt_[:], dt_[:], dv[:, 2, :], ALU.add, [t_ct, t_k], [t_k])
                self.recip(dt_[:], dt_[:], [t_k], [t_k])
                self.tt("dve", wgt[:], wgt[:], dt_[:].unsqueeze(1).to_broadcast([128, 3, 4]), ALU.mult, [t_k], [t_k])
                ob, t_ob = obr.next()
                v3 = lambda ap: ap.rearrange("p (h d) -> p h d", h=4)
                for a in range(3):
                    cb = wgt[:, a, :].unsqueeze(2).to_broadcast([128, 4, 64])
                    if a == 0:
                        self.tt("dve", v3(acc[:]), v3(ct[:, 0, 0:256]), cb, ALU.mult, [t_ct, t_k], [t_k])
                    else:
                        self.tt("dve", v3(tmp[:]), v3(ct[:, a, 0:256]), cb, ALU.mult, [t_ct, t_k], [t_k])
                        if a == 1:
                            self.tt("dve", acc[:], acc[:], tmp[:], ALU.add, [t_k], [t_k])
                        else:
                            self.tt("dve", ob[:], acc[:], tmp[:], ALU.add, [t_k], [t_ob])
                pT, t_pT = Tr.next()
                for kc in range(2):
                    self.tr(pT[:, kc, :], ob[:, kc * 128:(kc + 1) * 128], self.identb[:], [t_ob, self.t_const], [t_pT])
                self.cp("act", self.oaT[:, :, t * 128:(t + 1) * 128], pT[:, 0:2, :], [t_pT], [self.t_oaT])

    def phase_merge(self, l, xsrc):
        with ExitStack() as st:
            sb = self.sb
            self.load_mod(st, [2, 3, 4])
            wa = sb(st, "m_wa", [128, 2, D], BF16)
            wb = sb(st, "m_wb", [128, 8, D], BF16)
            wo = sb(st, "m_wo", [128, 8, D], BF16)
            wr = sb(st, "m_wr", [128, 8, NE], F32)
            brr = sb(st, "m_brr", [128, NE], F32)
            t_w = Tok()
            self.load_w(wa[:], self.w_br_a[l], t_w)
            self.load_w(wb[:], self.w_br_b[l], t_w)
            self.load_w(wo[:], self.w_out[l], t_w)
            self.dma("sp", wr[:], self.w_router[l].rearrange("(kc p) n -> p kc n", p=128), [], [t_w])
            self.dma("sp", brr[:], self.b_router[l:l + 1, :].partition_broadcast(128), [], [t_w])
            gtr = Ring([sb(st, "m_gt%d" % i, [128, 16, 512], BF16) for i in range(2)])
            obr = Ring([sb(st, "m_ob%d" % i, [128, 8, 512], BF16) for i in range(2)])
            mgr = Ring([sb(st, "m_mg%d" % i, [128, 8, 512], BF16) for i in range(2)])
            tA = sb(st, "m_tA", [128, 512], F32)
            tB = sb(st, "m_tB", [128, 512], F32)
            t_tA, t_tB = Tok(), Tok()
            xr = Ring([sb(st, "m_x%d" % i, [128, D], F32) for i in range(2)])
            x1r = Ring([sb(st, "m_x1%d" % i, [128, D], F32) for i in range(2)])
            h2fr = Ring([sb(st, "m_h2f%d" % i, [128, D], F32) for i in range(2)])
            h2br = Ring([sb(st, "m_h2b%d" % i, [128, D], BF16) for i in range(2)])
            h2Tr = Ring([sb(st, "m_h2T%d" % i, [128, 8, 128], F32) for i in range(1)])
            work = (sb(st, "m_junk", [128, D], F32), sb(st, "m_ss", [128, 1], F32), sb(st, "m_rstd", [128, 1], F32),
                    sb(st, "m_tmp", [128, D], F32), Tok())
            e4 = sb(st, "m_e4", [128, 4], F32)
            ssum = sb(st, "m_ssum", [128, 2], F32)
            t_e4 = Tok()
            Ar = Ring([None] * 2)
            Ar.items = [(self.ps[i], self.tp[i]) for i in (0, 1)]
            Br = Ring([None] * 2)
            Br.items = [(self.ps[i], self.tp[i]) for i in (2, 3)]
            Or = Ring([None] * 2)
            Or.items = [(self.ps[i], self.tp[i]) for i in (4, 5)]
            psT2 = [self.ps[i][:].rearrange("p (a b) -> p a b", a=4) for i in (6, 7)]
            def router(t, h2f, t_h2f):
                h2T, t_h2T = h2Tr.next()
                for hh in range(2):
                    for k4 in range(4):
                        kc = hh * 4 + k4
                        self.tr(psT2[hh][:, k4, :], h2f[:, kc * 128:(kc + 1) * 128], self.identf[:], [t_h2f, self.t_const], [self.tp[6 + hh]])
                    self.cp("act", h2T[:, hh * 4:(hh + 1) * 4, :], psT2[hh], [self.tp[6 + hh]], [t_h2T])
                pL, t_pL = Or.next()
                for kc in range(8):
                    self.mm(pL[:, 0:NE], h2T[:, kc, :], wr[:, kc, :], kc == 0, kc == 7, [t_h2T, t_w], [t_pL])
                lg = self.logits_all[:, t, :]
                m8 = self.max8_all[:, t, :]
                self.tt("dve", lg, pL[:, 0:NE], brr[:], ALU.add, [t_pL, t_w], [self.t_route])
                self.P.op("dve", lambda e, m8=m8, lg=lg: e.max(out=m8, in_=lg), [self.t_route], [self.t_route])
                self.ts("dve", self.M_all[:, t, :], lg, m8[:, 3:4], ALU.is_ge, [self.t_route], [self.t_route])
                self.ts("dve", ssum[:, 1:2], m8[:, 0:1], -1.0, ALU.mult, [self.t_route], [t_e4])
                self.act(e4[:], m8[:, 0:4], AF.Exp, [self.t_route, t_e4], [t_e4], bias=ssum[:, 1:2], scale=1.0, accum=ssum[:, 0:1])
                self.recip(ssum[:, 0:1], ssum[:, 0:1], [t_e4], [t_e4])
                self.ts("dve", self.g4_all[:, t, :], e4[:], ssum[:, 0:1], ALU.mult, [t_e4], [self.t_route])

            pend_router = [None]
            for c in range(8):
                gt, t_gt = gtr.next()
                ob, t_ob = obr.next()
                mg, t_mg = mgr.next()
                self.dma("sp", gt[:], self.gT_d[:, c * 512:(c + 1) * 512].rearrange("(m p) n -> p m n", p=128), [self.t_gT], [t_gt])
                self.dma("sp", ob[:], self.obT_d[:, c * 512:(c + 1) * 512].rearrange("(m p) n -> p m n", p=128), [self.t_obT], [t_ob])
                for m in range(8):
                    pA, t_pA = Ar.next()
                    pB, t_pB = Br.next()
                    for kc in range(2):
                        self.mm(pA[:], wa[:, kc, m * 128:(m + 1) * 128], self.oaT[:, kc, c * 512:(c + 1) * 512], kc == 0, kc == 1,
                                [t_w, self.t_oaT], [t_pA])
                    for kc in range(8):
                        self.mm(pB[:], wb[:, kc, m * 128:(m + 1) * 128], ob[:, kc, :], kc == 0, kc == 7, [t_w, t_ob], [t_pB])
                    self.tt("dve", tA[:], pA[:], gt[:, m, :], ALU.mult, [t_pA, t_gt], [t_tA])
                    self.tt("dve", tB[:], pB[:], gt[:, 8 + m, :], ALU.mult, [t_pB, t_gt], [t_tB])
                    self.tt("pool", mg[:, m, :], tA[:], tB[:], ALU.add, [t_tA, t_tB], [t_mg])
                for t4 in range(4):
                    t = c * 4 + t4
                    xt, t_x = xr.next()
                    x1, t_x1 = x1r.next()
                    self.dma("sp", xt[:], xsrc[t * 128:(t + 1) * 128, :], [], [t_x])
                    for hf in range(2):
                        pO, t_pO = Or.next()
                        for kc in range(8):
                            self.mm(pO[:], mg[:, kc, t4 * 128:(t4 + 1) * 128], wo[:, kc, hf * 512:(hf + 1) * 512], kc == 0, kc == 7,
                                    [t_mg, t_w], [t_pO])
                        self.tt("dve", x1[:, hf * 512:(hf + 1) * 512], pO[:], self.modr[2][:, hf * 512:(hf + 1) * 512], ALU.mult,
                                [t_pO, self.t_modr], [t_x1])
                    self.tt("pool", x1[:], x1[:], xt[:], ALU.add, [t_x1, t_x], [t_x1])
                    self.dma("sp", self.xs_d[t * 128:(t + 1) * 128, :], x1[:], [t_x1, self.t_xs], [])
                    h2f, t_h2f = h2fr.next()
                    h2b, t_h2b = h2br.next()
                    self.norm_mod(x1, t_x1, 4, 3, work, h2b, t_h2f, hf=h2f)
                    self.dma("sp", self.h2_d[t * 128:(t + 1) * 128, :], h2b[:], [t_h2f, self.t_h2d], [])
                    if pend_router[0] is not None:
                        pend_router[0]()
                    pend_router[0] = (lambda t=t, h2f=h2f, t_h2f=t_h2f: router(t, h2f, t_h2f))
            pend_router[0]()

    def phase_slots(self, l):
        sb = self.sb
        with ExitStack() as s2:
            tris = sb(s2, "s_tris", [128, 128], BF16)
            tri32s = sb(s2, "s_t32s", [32, 32], BF16)
            tri32i = sb(s2, "s_t32i", [32, 32], BF16)
            iota160 = sb(s2, "s_iota", [32, NBLK], F32)
            iotap = sb(s2, "s_iotap", [128, 1], F32)
            iotap32 = sb(s2, "s_iotap32", [128, 1], F32)
            t_c = Tok()
            for dst, src in ((tris, self.c_tris), (tri32s, self.c_tri32s), (tri32i, self.c_tri32i), (iota160, self.c_iota160),
                             (iotap, self.c_iotap), (iotap32, self.c_iotap32)):
                self.dma("sp", dst[:], src, [], [t_c])
            cnt = sb(s2, "s_cnt", [32, 1], F32)
            cnti = sb(s2, "s_cnti", [32, 1], I32)
            nblk = sb(s2, "s_nblk", [32, 1], F32)
            nblkb = sb(s2, "s_nblkb", [32, 128], BF16)
            nblkc = sb(s2, "s_nblkc", [32, 1], BF16)
            bstart = sb(s2, "s_bstart", [128, NE], F32)
            bend = sb(s2, "s_bend", [32, 1], F32)
            cmp = sb(s2, "s_cmp", [32, NBLK], BF16)
            erep = sb(s2, "s_erep", [128, NBLK], F32)
            chg = sb(s2, "s_chg", [128, NBLK], F32)
            wf = sb(s2, "s_wf", [128, NBLK], F32)
            pos = sb(s2, "s_pos", [128, NE], F32)
            junk = sb(s2, "s_junk", [128, NE], F32)
            p4f = sb(s2, "s_p4f", [128, NT, 4], F32)
            t_s = Tok()
            pc, t_pc = self.ps[0], self.tp[0]
            for t in range(NT):
                self.mm(pc[0:32, 0:1], self.M_all[:, t, :], self.onesb[:, 0:1], t == 0, t == NT - 1, [self.t_route, self.t_const], [t_pc])
            self.ts("dve", cnt[:], pc[0:32, 0:1], 127.0, ALU.add, [t_pc], [t_s], s2=1.0 / 128.0, op1=ALU.mult)
            self.ts("dve", nblk[:], cnt[:], -0.49609375, ALU.add, [t_s], [t_s])
            self.cp("dve", cnti[:], nblk[:], [t_s], [t_s])
            self.cp("dve", nblk[:], cnti[:], [t_s], [t_s])
            self.tt("dve", cnt[:], cnt[:], nblk[:], ALU.subtract, [t_s], [t_s])
            self.ts("dve", cnt[:], cnt[:], 1.0, ALU.is_ge, [t_s], [t_s])
            self.tt("dve", nblk[:], nblk[:], cnt[:], ALU.add, [t_s], [t_s])
            self.ts("dve", nblk[:], nblk[:], 1.0, ALU.max, [t_s], [t_s])
            self.cp("dve", nblkb[:], nblk[:, 0:1].to_broadcast([32, 128]), [t_s], [t_s])
            self.cp("dve", nblkc[:], nblk[:], [t_s], [t_s])
            p1, t_p1 = self.ps[1], self.tp[1]
            self.mm(p1[:, 0:NE], nblkb[:], tri32s[:], True, True, [t_s, t_c], [t_p1])
            self.cp("dve", bstart[:], p1[:, 0:NE], [t_p1], [t_s])
            p2, t_p2 = self.ps[2], self.tp[2]
            self.mm(p2[0:32, 0:1], tri32i[:], nblkc[:], True, True, [t_s, t_c], [t_p2])
            self.cp("dve", bend[:], p2[0:32, 0:1], [t_p2], [t_s])
            self.ts("dve", cmp[:], iota160[:], bend[:, 0:1], ALU.is_ge, [t_s, t_c], [t_s])
            p3, t_p3 = self.ps[3], self.tp[3]
            self.mm(p3[:, 0:NBLK], self.onesb[0:32, :], cmp[:], True, True, [t_s, self.t_const], [t_p3])
            self.ts("dve", erep[:], p3[:, 0:NBLK], float(NE - 1), ALU.min, [t_p3], [t_s])
            self.memset("dve", chg[:, 0:1], 1.0, [t_s])
            self.tt("dve", chg[:, 1:NBLK], erep[:, 1:NBLK], erep[:, 0:NBLK - 1], ALU.not_equal, [t_s], [t_s])
            self.ts("dve", wf[:], erep[:], 128.0, ALU.mult, [t_s, t_c], [t_s], s2=iotap[:, 0:1], op1=ALU.add)
            self.ts("dve", chg[:], chg[:], -BIGIDX, ALU.mult, [t_s], [t_s], s2=BIGIDX, op1=ALU.add)
            self.tt("dve", wf[:], wf[:], chg[:], ALU.add, [t_s], [t_s])
            if l > 0:
                self.ts("dve", wf[:], wf[:], float(l * NE * 128), ALU.add, [t_s], [t_s])
            self.cp("dve", self.widx[:], wf[:], [t_s], [self.t_widx])
            self.ts("dve", wf[:], wf[:], 2.0, ALU.mult, [t_s], [t_s])
            self.cp("dve", self.widxA[:], wf[:], [t_s], [self.t_widx])
            self.ts("dve", wf[:], wf[:], 1.0, ALU.add, [t_s], [t_s])
            self.cp("dve", self.widxB[:], wf[:], [t_s], [self.t_widx])
            self.ts("dve", self.OHall[:], erep[0:64, :], iotap32[0:64, 0:1], ALU.is_equal, [t_s, t_c], [self.t_widx])
            for t in range(NT):
                pr_, t_pr = self.ps[4 + t % 2], self.tp[4 + t % 2]
                self.mm(pr_[:, 0:NE], tris[:], self.M_all[:, t, :], True, t == 0, [t_c, self.t_route], [t_pr])
                for t2 in range(t):
                    self.mm(pr_[:, 0:NE], self.onesb[:], self.M_all[:, t2, :], False, t2 == t - 1, [self.t_const, self.t_route], [t_pr])
                self.stt(pos[:], bstart[:], 128.0, pr_[:, 0:NE], ALU.mult, ALU.add, [t_s, t_pr], [t_s])
                for k in range(4):
                    self.stt(junk[:], self.logits_all[:, t, :], self.max8_all[:, t, k:k + 1], pos[:], ALU.is_equal, ALU.mult,
                             [self.t_route, t_s], [t_s], accum=p4f[:, t, k:k + 1])
            self.cp("dve", self.pos4_all[:], p4f[:], [t_s], [self.t_pos4])
            self.P.barrier()
        with ExitStack() as s3:
            hr = Ring([sb(s3, "s_h2%d" % i, [128, D], BF16) for i in range(3)])
            for t in range(NT):
                hb, t_hb = hr.next()
                self.dma("sp", hb[:], self.h2_d[t * 128:(t + 1) * 128, :], [self.t_h2d], [t_hb])
                for k in range(4):
                    self.scatter(self.Xs_d, hb[:], self.pos4_all[:, t, k:k + 1], [t_hb, self.t_pos4, self.t_Xs], [], NBLK * 128 - 1)
            self.P.barrier()

    def phase_experts(self, l):
        with ExitStack() as st:
            sb = self.sb
            wguA = sb(st, "e_wguA", [128, 4 * 2 * D], BF16)
            wguB = sb(st, "e_wguB", [128, 4 * 2 * D], BF16)
            t_wgA, t_wgB = Tok(), Tok()
            wdn = sb(st, "e_wdn", [128, 8 * D], BF16)
            bgu = sb(st, "e_bgu", [64, 2 * D], BF16)
            bdn = sb(st, "e_bdn", [64, D], BF16)
            bf = sb(st, "e_bf", [64, 3 * D], F32)
            bt = sb(st, "e_bt", [64, 3 * D], F32)
            t_wgu, t_wdn, t_b = Tok(), Tok(), Tok()
            for half in range(2):
                self.dma("sp", bf[half * 32:(half + 1) * 32, 0:2 * D], self.b_gu[l], [], [t_b])
                self.dma("sp", bf[half * 32:(half + 1) * 32, 2 * D:3 * D], self.b_dn[l], [], [t_b])
            self.cp("dve", bgu[0:32, :], bf[0:32, 0:2 * D], [t_b], [t_b])
            self.cp("dve", bdn[0:32, :], bf[0:32, 2 * D:3 * D], [t_b], [t_b])
            self.cp("dve", bgu[32:64, :], bf[32:64, 0:2 * D], [t_b], [t_b])
            self.cp("dve", bdn[32:64, :], bf[32:64, 2 * D:3 * D], [t_b], [t_b])
            self.cp("dve", bt[32:64, 0:2 * D], bgu[32:64, :], [t_b], [t_b])
            self.cp("dve", bt[32:64, 2 * D:3 * D], bdn[32:64, :], [t_b], [t_b])
            self.tt("dve", bt[32:64, :], bf[32:64, :], bt[32:64, :], ALU.subtract, [t_b], [t_b])
            self.cp("dve", bgu[32:64, :], bt[32:64, 0:2 * D], [t_b], [t_b])
            self.cp("dve", bdn[32:64, :], bt[32:64, 2 * D:3 * D], [t_b], [t_b])
            wgu_v = self.w_gu.rearrange("l e (p kh kl) n -> (l e p kh) (kl n)", kh=2, kl=4)
            wdn_v = self.w_dn.rearrange("l e (p kc) n -> (l e p) (kc n)", kc=8)
            R = 3
            xb_ = [(sb(st, "e_x%d" % i, [128, D], BF16), Tok()) for i in range(R)]
            xT_ = [(sb(st, "e_xT%d" % i, [128, 8, 128], BF16), Tok()) for i in range(R)]
            oh_ = [(sb(st, "e_oh%d" % i, [64, 128], BF16), Tok()) for i in range(R)]
            gc_ = [(sb(st, "e_gc%d" % i, [128, 512], F32), Tok()) for i in range(2)]
            sg_ = [(sb(st, "e_sg%d" % i, [128, 512], F32), Tok()) for i in range(2)]
            lc_ = [(sb(st, "e_lc%d" % i, [128, 512], F32), Tok()) for i in range(2)]
            ac_ = [(sb(st, "e_ac%d" % i, [128, D], BF16), Tok()) for i in range(2)]
            aT_ = [(sb(st, "e_aT%d" % i, [128, 8, 128], BF16), Tok()) for i in range(2)]
            out_ = [(sb(st, "e_out%d" % i, [128, D], F32), Tok()) for i in range(2)]
            pX = self.ps[6][:].bitcast(BF16).rearrange("p (a b) -> p a b", a=8)
            pA = self.ps[7][:].bitcast(BF16).rearrange("p (a b) -> p a b", a=8)
            N = NBLK
            bound = 2 * NE * 128 - 1

            def WguA(j):
                self.gather(wguA[:], wgu_v, self.widxA[:, j:j + 1], [self.t_widx], [t_wgA], 2 * bound + 1)

            def WguB(j):
                self.gather(wguB[:], wgu_v, self.widxB[:, j:j + 1], [self.t_widx], [t_wgB], 2 * bound + 1)

            def Wdn(j):
                self.gather(wdn[:], wdn_v, self.widx[:, j:j + 1], [self.t_widx], [t_wdn], bound)

            def Ax(j):
                xb, t_xb = xb_[j % R]
                self.dma("sp", xb[:], self.Xs_d[j * 128:(j + 1) * 128, :], [self.t_Xs], [t_xb])
                oh, t_oh = oh_[j % R]
                self.cp("dve", oh[:], self.OHall[:, j:j + 1].to_broadcast([64, 128]), [self.t_widx], [t_oh])
                xv = xb[:].rearrange("p (a kc) -> p kc a", kc=8)
                for kc in range(8):
                    self.tr(pX[:, kc, :], xv[:, kc, :], self.identb[:], [t_xb, self.t_const], [self.tp[6]])
                xT, t_xT = xT_[j % R]
                self.cp("act", xT[:], pX, [self.tp[6]], [t_xT])

            def Bk(j, half):
                xT, t_xT = xT_[j % R]
                oh, t_oh = oh_[j % R]
                wb, t_wb = (wguA, t_wgA) if half == 0 else (wguB, t_wgB)
                for k4 in range(4):
                    kc = half * 4 + k4
                    for nb in range(4):
                        self.mm(self.ps[nb][:], xT[:, kc, :], wb[:, k4 * 2048 + nb * 512:k4 * 2048 + (nb + 1) * 512], kc == 0, False,
                                [t_xT, t_wb], [self.tp[nb]])
                if half == 1:
                    for nb in range(4):
                        self.mm(self.ps[nb][:], oh[:], bgu[:, nb * 512:(nb + 1) * 512], False, True, [t_oh, t_b], [self.tp[nb]])

            def E(j, hf):
                gc, t_gc = gc_[hf]
                sg, t_sg = sg_[hf]
                lc, t_lc = lc_[hf]
                ac, t_ac = ac_[j % 2]
                self.ts("dve", gc[:], self.ps[hf][:], 7.0, ALU.min, [self.tp[hf]], [t_gc])
                self.act(sg[:], gc[:], AF.Sigmoid, [t_gc], [t_sg], scale=1.702)
                self.ts("dve", lc[:], self.ps[2 + hf][:], 7.0, ALU.min, [self.tp[2 + hf]], [t_lc], s2=-7.0, op1=ALU.max)
                self.stt(lc[:], lc[:], 1.0, gc[:], ALU.add, ALU.mult, [t_lc, t_gc], [t_lc])
                self.tt("dve", ac[:, hf * 512:(hf + 1) * 512], lc[:], sg[:], ALU.mult, [t_lc, t_sg], [t_ac])

            def Ca(j):
                ac, t_ac = ac_[j % 2]
                av = ac[:].rearrange("p (a kc) -> p kc a", kc=8)
                for kc in range(8):
                    self.tr(pA[:, kc, :], av[:, kc, :], self.identb[:], [t_ac, self.t_const], [self.tp[7]])
                aT, t_aT = aT_[j % 2]
                self.cp("act", aT[:], pA, [self.tp[7]], [t_aT])

            def Cb(j):
                oh, t_oh = oh_[j % R]
                aT, t_aT = aT_[j % 2]
                ot, t_ot = out_[j % 2]
                for hf in range(2):
                    ps, t_ps = self.ps[4 + hf], self.tp[4 + hf]
                    for kc in range(8):
                        self.mm(ps[:], aT[:, kc, :], wdn[:, kc * 1024 + hf * 512:kc * 1024 + (hf + 1) * 512], kc == 0, False,
                                [t_aT, t_wdn], [t_ps])
                    self.mm(ps[:], oh[:], bdn[:, hf * 512:(hf + 1) * 512], False, True, [t_oh, t_b], [t_ps])
                    self.cp("act" if hf else "dve", ot[:, hf * 512:(hf + 1) * 512], ps[:], [t_ps], [t_ot])
                self.dma("sp", self.Out_d[j * 128:(j + 1) * 128, :], ot[:], [t_ot, self.t_Outd], [])

            WguA(0)
            WguB(0)
            Wdn(0)
            Ax(0)
            Ax(1)
            Bk(0, 0)
            WguA(1)
            Bk(0, 1)
            WguB(1)
            E(0, 0)
            E(0, 1)
            for j in range(N):
                if j + 1 < N:
                    Bk(j + 1, 0)
                if j + 2 < N:
                    WguA(j + 2)
                if j + 1 < N:
                    Bk(j + 1, 1)
                if j + 2 < N:
                    WguB(j + 2)
                Ca(j)
                if j + 2 < N:
                    Ax(j + 2)
                if j + 1 < N:
                    E(j + 1, 0)
                Cb(j)
                if j + 1 < N:
                    Wdn(j + 1)
                    E(j + 1, 1)

    def phase_combine(self, l, last):
        with ExitStack() as st:
            sb = self.sb
            self.load_mod(st, [5])
            gr = Ring([sb(st, "c_g%d" % i, [128, D], F32) for i in range(12)])
            xr = Ring([sb(st, "c_x%d" % i, [128, D], F32) for i in range(3)])
            yr = Ring([sb(st, "c_y%d" % i, [128, D], F32) for i in range(3)])
            fg = sb(st, "c_fg", [128, D], F32)
            junk = sb(st, "c_junk", [128, D], F32)
            ss = sb(st, "c_ss", [128, 2], F32)
            t_fg, t_w = Tok(), Tok()
            if last:
                self.dma("sp", fg[:], self.fng.rearrange("(o d) -> o d", o=1).partition_broadcast(128), [], [t_fg])
            for t in range(NT):
                xt, t_x = xr.next()
                y, t_y = yr.next()
                self.dma("sp", xt[:], self.xs_d[t * 128:(t + 1) * 128, :], [], [t_x])
                for k in range(4):
                    g, t_g = gr.next()
                    self.gather(g[:], self.Out_d, self.pos4_all[:, t, k:k + 1], [self.t_Outd, self.t_pos4], [t_g], None)
                    if k == 0:
                        self.ts("dve", y[:], g[:], self.g4_all[:, t, 0:1], ALU.mult, [t_g, self.t_route], [t_y])
                    else:
                        self.stt(y[:], g[:], self.g4_all[:, t, k:k + 1], y[:], ALU.mult, ALU.add, [t_g, self.t_route, t_y], [t_y])
                self.tt("dve", y[:], y[:], self.modr[5], ALU.mult, [t_y, self.t_modr], [t_y])
                self.tt("pool", y[:], y[:], xt[:], ALU.add, [t_y, t_x], [t_y])
                if not last:
                    self.dma("sp", self.xs_d[t * 128:(t + 1) * 128, :], y[:], [t_y, self.t_xs], [])
                else:
                    self.stt(junk[:], y[:], 1.0, y[:], ALU.mult, ALU.mult, [t_y], [t_w], accum=ss[:, 0:1])
                    self.rstd_from_ss(ss[:, 1:2], ss[:, 0:1], D, t_w, t_w)
                    self.stt(y[:], y[:], ss[:, 1:2], fg[:], ALU.mult, ALU.mult, [t_y, t_w, t_fg], [t_y])
                    self.dma("sp", self.out[t * 128:(t + 1) * 128, :], y[:], [t_y], [])


def _t5_bucket(rel):
    nb = 16
    max_exact = 8
    ret = np.where(rel > 0, nb, 0)
    n = np.abs(rel)
    nf = np.maximum(n, 1).astype(np.float32)
    large = max_exact + (np.log(nf / max_exact) / math.log(1024 / max_exact) * (nb - max_exact)).astype(np.int32)
    large = np.minimum(large, nb - 1)
    return ret + np.where(n < max_exact, n, large)


def host_consts(rel_bias):
    bf = ml_dtypes.bfloat16
    c = {}
    c["identb"] = np.eye(128, dtype=np.float32).astype(bf)
    c["identf"] = np.eye(128, dtype=np.float32)
    tok = np.arange(S)
    row = (tok // 64).astype(np.float32)
    col = (tok % 64).astype(np.float32)
    inv = (10000.0 ** (-np.arange(0, 32, 2, dtype=np.float32) / 32)).astype(np.float32)
    ar = row[:, None] * inv
    ac = col[:, None] * inv
    cosT = np.concatenate([np.cos(ar), np.cos(ar), np.cos(ac), np.cos(ac)], 1).astype(np.float32)
    sinT = np.concatenate([-np.sin(ar), np.sin(ar), -np.sin(ac), np.sin(ac)], 1).astype(np.float32)
    c["cosT"] = np.ascontiguousarray(cosT.reshape(NT, 128, 64).transpose(1, 0, 2))
    c["sinT"] = np.ascontiguousarray(sinT.reshape(NT, 128, 64).transpose(1, 0, 2))
    k = np.arange(128)
    c["tri_s"] = (k[:, None] < k[None, :]).astype(np.float32).astype(bf)
    k32 = np.arange(32)
    c["tri32s"] = (k32[:, None] < k32[None, :]).astype(np.float32).astype(bf)
    c["tri32i"] = (k32[:, None] <= k32[None, :]).astype(np.float32).astype(bf)
    c["iota160"] = np.broadcast_to(np.arange(NBLK, dtype=np.float32), (32, NBLK)).copy()
    c["iotap"] = np.arange(128, dtype=np.float32).reshape(128, 1)
    c["iotap32"] = (np.arange(128) % 32).astype(np.float32).reshape(128, 1)
    q = np.arange(128)[:, None]
    cc = np.arange(384)[None, :]
    rel = cc - 128 - q
    wb = np.full((128, 12, 384), -30000.0, np.float32)
    valid = np.abs(rel) <= 64
    for a, dl in enumerate(A_DIL):
        bkt = _t5_bucket(rel * dl)
        for h in range(4):
            vals = rel_bias[bkt, a * 4 + h]
            wb[:, a * 4 + h, :] = np.where(valid, vals, np.float32(-30000.0))
    c["wbias"] = wb
    return c


_CACHE = {}


def kernel(**inputs):
    inp = {k: np.ascontiguousarray(np.asarray(v, dtype=np.float32)) for k, v in inputs.items()}
    if "nc" not in _CACHE:
        _CACHE["nc"] = Builder(nlayers=2).build()
    nc = _CACHE["nc"]
    consts = host_consts(inp["rel_bias"])
    shared = {k: inp[k] for k in ("w_ada", "b_ada", "norm1_g", "w_in", "q_norm_g", "k_norm_g", "w_br_a", "w_br_b", "w_out",
                                   "norm2_g", "w_router", "b_router", "w_gate_up", "b_gate_up", "w_down", "b_down", "final_norm_g")}
    in_maps = []
    for b in range(8):
        m = dict(shared)
        m.update(consts)
        m["x"] = inp["x"][b]
        m["ccol"] = np.ascontiguousarray(inp["c"][b].reshape(8, 128).T)
        in_maps.append(m)
    res = run_bass_kernel_spmd(nc, in_maps, core_ids=list(range(8)))
    return np.stack([np.asarray(res.results[b]["out"], dtype=np.float32) for b in range(8)], 0)
```

```python
import math
from contextlib import ExitStack

import numpy as np
import ml_dtypes
import concourse.bass as bass
import concourse.mybir as mybir
from concourse.bass_utils import run_bass_kernel_spmd

F32 = mybir.dt.float32
BF16 = mybir.dt.bfloat16
I32 = mybir.dt.int32
ALU = mybir.AluOpType
AF = mybir.ActivationFunctionType
AX = mybir.AxisListType

S = 4096
D = 1024
NT = 32
NE = 32
NBLK = 160
EPS = 1e-6
A_DIL = (1, 4, 16)
BIGIDX = 1.0e6


def sl(start, n, step):
    return slice(start, start + (n - 1) * step + 1, step)

ENGS = ["pe", "act", "dve", "pool", "sp"]


class Tok:
    __slots__ = ("w", "r")

    def __init__(self):
        self.w = None
        self.r = []


class Prog:
    N_DMA_SEMS = {"sp": 24, "pool": 16, "act": 8}

    def __init__(self, nc):
        self.nc = nc
        self.q = {e: [] for e in ENGS}
        self.cnt = {e: 0 for e in ENGS}
        self.seen = {e: {} for e in ENGS}
        self.dma_val = {}
        self.dma_rr = {e: 0 for e in ENGS}
        self.prologue = {}

    def _collect(self, eng, reads, writes):
        deps = {}

        def add(d):
            if d is not None and deps.get(d[0], 0) < d[1]:
                deps[d[0]] = d[1]

        for t in reads:
            add(t.w)
        for t in writes:
            add(t.w)
            for d in t.r:
                add(d)
        out = []
        own = "E:" + eng
        seen = self.seen[eng]
        for k, v in deps.items():
            if k == own and eng == "pe":
                continue
            if seen.get(k, 0) >= v:
                continue
            seen[k] = v
            out.append((k, v))
        return out

    def _note(self, my, reads, writes):
        for t in reads:
            t.r.append(my)
            if len(t.r) > 48:
                d = {}
                for k, v in t.r:
                    if d.get(k, 0) < v:
                        d[k] = v
                t.r = list(d.items())
        for t in writes:
            t.w = my
            t.r = []

    def op(self, eng, fn, reads=(), writes=()):
        waits = self._collect(eng, reads, writes)
        self.cnt[eng] += 1
        my = ("E:" + eng, self.cnt[eng])
        self._note(my, reads, writes)
        self.q[eng].append((waits, fn, my, 1))

    def dma(self, eng, fn, reads=(), writes=()):
        waits = self._collect(eng, reads, writes)
        n = self.N_DMA_SEMS[eng]
        slot = self.dma_rr[eng] % n
        self.dma_rr[eng] += 1
        key = "D:%s:%d" % (eng, slot)
        prev = self.dma_val.get(key, 0)
        if prev > 0 and self.seen[eng].get(key, 0) < prev:
            self.seen[eng][key] = prev
            waits.append((key, prev))
        self.dma_val[key] = prev + 16
        my = (key, prev + 16)
        self._note(my, reads, writes)
        self.q[eng].append((waits, fn, my, 16))

    def barrier(self):
        for eng in ENGS:
            waits = []
            for key, v in self.dma_val.items():
                if self.seen[eng].get(key, 0) < v:
                    waits.append((key, v))
                    self.seen[eng][key] = v
            for e in ENGS:
                if e == eng or self.cnt[e] == 0:
                    continue
                k = "E:" + e
                if self.seen[eng].get(k, 0) < self.cnt[e]:
                    waits.append((k, self.cnt[e]))
                    self.seen[eng][k] = self.cnt[e]
            if waits:
                self.q[eng].append((waits, None, None, 0))

    def emit(self):
        nc = self.nc
        keys = set()
        for e in ENGS:
            for waits, fn, my, inc in self.q[e]:
                for k, v in waits:
                    keys.add(k)
                if my is not None:
                    keys.add(my[0])
        keys = sorted(keys)
        with ExitStack() as st:
            sems = {k: st.enter_context(nc.semaphore(k.replace(":", "_"))) for k in keys}
            block = st.enter_context(nc.Block())
            hmap = {"pe": block.tensor, "act": block.scalar, "dve": block.vector,
                    "pool": block.gpsimd, "sp": block.sync}

            def make(e):
                def body(engh):
                    if e in self.prologue:
                        self.prologue[e](engh)
                    for waits, fn, my, inc in self.q[e]:
                        for k, v in waits:
                            engh.wait_ge(sems[k], v)
                        if fn is None:
                            continue
                        fn(engh).then_inc(sems[my[0]], inc)
                return body

            for e in ENGS:
                if self.q[e]:
                    hmap[e](make(e))


class Ring:
    def __init__(self, items):
        self.items = [(it, Tok()) for it in items]
        self.i = 0

    def next(self):
        it = self.items[self.i % len(self.items)]
        self.i += 1
        return it


class Builder:
    def __init__(self, nlayers=2, stop=None, dbg=(), small_moe=False):
        self.nlayers = nlayers
        self.stop = stop
        self.dbg = dbg
        NEW = 1 if small_moe else NE
        nc = self.nc = bass.Bass("TRN2", target_bir_lowering=False)
        self.P = Prog(nc)
        self.outs = ["out"]
        di = lambda n, s, d=F32: nc.dram_tensor(n, s, d, kind="ExternalInput").ap()
        self.x_in = di("x", [S, D])
        self.ccol = di("ccol", [128, 8])
        self.w_ada = di("w_ada", [2, D, 6 * D])
        self.b_ada = di("b_ada", [2, 6 * D])
        self.norm1_g = di("norm1_g", [2, D])
        self.w_in = di("w_in", [2, D, 5888])
        self.q_norm_g = di("q_norm_g", [2, 64])
        self.k_norm_g = di("k_norm_g", [2, 64])
        self.wbias = di("wbias", [128, 12, 384])
        self.w_br_a = di("w_br_a", [2, 256, D])
        self.w_br_b = di("w_br_b", [2, D, D])
        self.w_out = di("w_out", [2, D, D])
        self.norm2_g = di("norm2_g", [2, D])
        self.w_router = di("w_router", [2, D, NE])
        self.b_router = di("b_router", [2, NE])
        self.w_gu = di("w_gate_up", [2, NEW, D, 2 * D])
        self.b_gu = di("b_gate_up", [2, NE, 2 * D])
        self.w_dn = di("w_down", [2, NEW, D, D])
        self.b_dn = di("b_down", [2, NE, D])
        self.fng = di("final_norm_g", [D])
        self.c_identb = di("identb", [128, 128], BF16)
        self.c_identf = di("identf", [128, 128])
        self.c_cos = di("cosT", [128, NT, 64])
        self.c_sin = di("sinT", [128, NT, 64])
        self.c_tris = di("tri_s", [128, 128], BF16)
        self.c_tri32s = di("tri32s", [32, 32], BF16)
        self.c_tri32i = di("tri32i", [32, 32], BF16)
        self.c_iota160 = di("iota160", [32, NBLK])
        self.c_iotap = di("iotap", [128, 1])
        self.c_iotap32 = di("iotap32", [128, 1])
        self.out = nc.dram_tensor("out", [S, D], F32, kind="ExternalOutput").ap()
        self.xs_d = self.scratch("xs_d", [S, D], F32)
        self.obT_d = self.scratch("obT_d", [D, S], BF16)
        self.gT_d = self.scratch("gT_d", [2 * D, S], BF16)
        self.oaw_d = self.scratch("oaw_d", [3, S, 264], F32)
        self.h2_d = self.scratch("h2_d", [S, D], BF16)
        self.Xs_d = self.scratch("Xs_d", [NBLK * 128, D], BF16)
        self.Out_d = self.scratch("Out_d", [NBLK * 128, D], F32)
        self.modr_d = self.scratch("modr_d", [128, 6, D], F32)
        self.t_modrd = Tok()
        self.t_xs, self.t_obT, self.t_gT, self.t_oaw, self.t_h2d, self.t_Xs, self.t_Outd = [Tok() for _ in range(7)]
        self.t_out = Tok()

    def scratch(self, name, shape, dt):
        if name in self.dbg:
            self.outs.append(name)
            return self.nc.dram_tensor(name, shape, dt, kind="ExternalOutput").ap()
        return self.nc.dram_tensor(name, shape, dt, kind="Internal").ap()

    def sb(self, st, name, shape, dt):
        self._nsb = getattr(self, "_nsb", 0) + 1
        return st.enter_context(self.nc.sbuf_tensor("sb%d_%s" % (self._nsb, name), shape, dt))

    def tt(self, eng, out, in0, in1, op, r, w):
        self.P.op(eng, lambda e: e.tensor_tensor(out=out, in0=in0, in1=in1, op=op), r, w)

    def ts(self, eng, out, in0, s1, op0, r, w, s2=None, op1=None):
        if op1 is None:
            self.P.op(eng, lambda e: e.tensor_scalar(out=out, in0=in0, scalar1=s1, scalar2=None, op0=op0), r, w)
        else:
            self.P.op(eng, lambda e: e.tensor_scalar(out=out, in0=in0, scalar1=s1, scalar2=s2, op0=op0, op1=op1), r, w)

    def stt(self, out, in0, scalar, in1, op0, op1, r, w, accum=None):
        if accum is None:
            self.P.op("dve", lambda e: e.scalar_tensor_tensor(out=out, in0=in0, scalar=scalar, in1=in1, op0=op0, op1=op1), r, w)
        else:
            self.P.op("dve", lambda e: e.scalar_tensor_tensor(out=out, in0=in0, scalar=scalar, in1=in1, op0=op0, op1=op1,
                                                              accum_out=accum), r, w)

    def act(self, out, in_, func, r, w, bias=None, scale=None, accum=None):
        kw = {}
        if bias is not None:
            kw["bias"] = bias
        if scale is not None:
            kw["scale"] = scale
        if accum is not None:
            kw["accum_out"] = accum
        self.P.op("act", lambda e: e.activation(out=out, in_=in_, func=func, **kw), r, w)

    def cp(self, eng, out, in_, r, w):
        if eng == "act":
            self.P.op("act", lambda e: e.copy(out=out, in_=in_), r, w)
        else:
            self.P.op(eng, lambda e: e.tensor_copy(out=out, in_=in_), r, w)

    def mm(self, out, lhsT, rhs, start, stop, r, w):
        self.P.op("pe", lambda e: e.matmul(out, lhsT=lhsT, rhs=rhs, start=start, stop=stop), r, w)

    def tr(self, out, in_, ident, r, w):
        self.P.op("pe", lambda e: e.transpose(out=out, in_=in_, identity=ident), r, w)

    def dma(self, q, out, in_, r, w):
        self.P.dma(q, lambda e: e.dma_start(out=out, in_=in_), r, w)

    def red(self, out, in_, op, r, w):
        self.P.op("dve", lambda e: e.tensor_reduce(out=out, in_=in_, axis=AX.X, op=op), r, w)

    def recip(self, out, in_, r, w):
        self.P.op("dve", lambda e: e.reciprocal(out=out, in_=in_), r, w)

    def memset(self, eng, ap, val, w):
        self.P.op(eng, lambda e: e.memset(ap, val), (), w)

    def gather(self, out, in_, idx, r, w, bound):
        if bound is None:
            self.P.dma("pool", lambda e: e.indirect_dma_start(out=out, out_offset=None, in_=in_,
                                                              in_offset=bass.IndirectOffsetOnAxis(ap=idx, axis=0)), r, w)
            return
        if "pool" not in self.P.prologue:
            self.bregs = {}
            self.bounds = []

            def pro(e):
                for bv in self.bounds:
                    self.bregs[bv] = e.alloc_register("bound_reg_%d" % bv)
                    e.reg_mov(self.bregs[bv], bv)
            self.P.prologue["pool"] = pro
        if bound not in self.bounds:
            self.bounds.append(bound)
        self.P.dma("pool", lambda e: e.indirect_dma_start(out=out, out_offset=None, in_=in_,
                                                          in_offset=bass.IndirectOffsetOnAxis(ap=idx, axis=0),
                                                          bounds_check=self.bregs[bound], oob_is_err=False), r, w)

    def scatter(self, out, in_, idx, r, w, bound):
        self.P.dma("pool", lambda e: e.indirect_dma_start(out=out, out_offset=bass.IndirectOffsetOnAxis(ap=idx, axis=0),
                                                          in_=in_, in_offset=None), r, w)

    def build(self):
        nc = self.nc
        with ExitStack() as gst:
            self.gst = gst
            self.ps = [gst.enter_context(nc.psum_tensor("ps%d" % i, [128, 512], F32)) for i in range(8)]
            self.tp = [Tok() for _ in range(8)]
            sb = self.sb
            self.identb = sb(gst, "identb", [128, 128], BF16)
            self.identf = sb(gst, "identf", [128, 128], F32)
            self.onesb = sb(gst, "onesb", [128, 128], BF16)
            self.onesf = sb(gst, "onesf", [128, 128], F32)
            self.epsc = sb(gst, "epsc", [128, 1], F32)
            self.t_const = Tok()
            tc = [self.t_const]
            self.dma("sp", self.identb[:], self.c_identb, [], tc)
            self.dma("sp", self.identf[:], self.c_identf, [], tc)
            self.memset("dve", self.onesb[:], 1.0, tc)
            self.memset("dve", self.onesf[:], 1.0, tc)
            self.memset("dve", self.epsc[:], EPS, tc)
            self.neghalf = sb(gst, "neghalf", [128, 16], F32)
            self.memset("dve", self.neghalf[:], -0.5, tc)
            self.P.barrier()
            xsrc = self.x_in
            for l in range(self.nlayers):
                last = (l == self.nlayers - 1)
                if self.layer(l, xsrc, last):
                    break
                xsrc = self.xs_d
            self.P.barrier()
            self.P.emit()
        return nc

    def layer(self, l, xsrc, last):
        P = self.P
        stop = self.stop if l == self.nlayers - 1 else None
        with ExitStack() as lst:
            self.oaT = self.sb(lst, "oaT", [128, 2, S], BF16)
            self.t_oaT = Tok()
            self.phase_mod(l)
            P.barrier()
            if stop == "mod":
                return True
            with ExitStack() as ast:
                self.hT = self.sb(ast, "hT", [128, 8, S], BF16)
                self.t_hT = Tok()
                self.phase_norm1(l, xsrc)
                P.barrier()
                if stop == "norm1":
                    return True
                self.phase_gates(l)
                P.barrier()
                if stop == "gates":
                    return True
                self.phase_window(l)
                P.barrier()
                if stop == "window":
                    return True
                self.phase_gqa(l)
                P.barrier()
                if stop == "gqa":
                    return True
            with ExitStack() as mst:
                self.logits_all = self.sb(mst, "logits_all", [128, NT, NE], F32)
                self.max8_all = self.sb(mst, "max8_all", [128, NT, 8], F32)
                self.g4_all = self.sb(mst, "g4_all", [128, NT, 4], F32)
                self.M_all = self.sb(mst, "M_all", [128, NT, NE], BF16)
                self.pos4_all = self.sb(mst, "pos4_all", [128, NT, 4], I32)
                self.widx = self.sb(mst, "widx", [128, NBLK], I32)
                self.widxA = self.sb(mst, "widxA", [128, NBLK], I32)
                self.widxB = self.sb(mst, "widxB", [128, NBLK], I32)
                self.OHall = self.sb(mst, "OHall", [64, NBLK], BF16)
                self.t_widx = Tok()
                self.t_route = Tok()
                self.t_pos4 = Tok()
                self.phase_merge(l, xsrc)
                P.barrier()
                if stop == "merge":
                    return True
                self.phase_slots(l)
                P.barrier()
                if stop == "slots":
                    return True
                self.phase_experts(l)
                P.barrier()
                if stop == "experts":
                    return True
                self.phase_combine(l, last)
                P.barrier()
        return False

    def phase_mod(self, l):
        with ExitStack() as st:
            sb = self.sb
            cc = sb(st, "cc", [128, 8], F32)
            cs = sb(st, "cs", [128, 8], F32)
            crep = sb(st, "crep", [128, 8, 128], F32)
            brep = sb(st, "brep", [128, 6 * D], F32)
            ng = sb(st, "ng", [128, 2, D], F32)
            modr = sb(st, "modr", [128, 6, D], F32)
            t_modr = Tok()
            wa = Ring([sb(st, "wa%d" % i, [128, 8, 512], F32) for i in range(2)])
            t_cc, t_cs, t_crep, t_brep, t_ng = Tok(), Tok(), Tok(), Tok(), Tok()
            self.dma("sp", cc[:], self.ccol, [], [t_cc])
            self.dma("sp", brep[:], self.b_ada[l:l + 1, :].partition_broadcast(128), [], [t_brep])
            self.dma("sp", ng[:, 0, :], self.norm1_g[l:l + 1, :].partition_broadcast(128), [], [t_ng])
            self.dma("sp", ng[:, 1, :], self.norm2_g[l:l + 1, :].partition_broadcast(128), [], [t_ng])
            self.act(cs[:], cc[:], AF.Silu, [t_cc], [t_cs])
            for kc in range(8):
                self.cp("dve", crep[:, kc, :], cs[:, kc:kc + 1].to_broadcast([128, 128]), [t_cs], [t_crep])
            modf = modr[:].rearrange("p a d -> p (a d)")
            psr = Ring(self.ps[0:2])
            psr.items = [(self.ps[0], self.tp[0]), (self.ps[1], self.tp[1])]
            for ch in range(12):
                w, t_w = wa.next()
                self.dma("sp", w[:], self.w_ada[l][:, ch * 512:(ch + 1) * 512].rearrange("(kc p) n -> p kc n", p=128), [], [t_w])
                ps, t_ps = psr.next()
                for kc in range(8):
                    self.mm(ps[:], crep[:, kc, :], w[:, kc, :], kc == 0, kc == 7, [t_crep, t_w], [t_ps])
                self.tt("dve", modf[:, ch * 512:(ch + 1) * 512], ps[:], brep[:, ch * 512:(ch + 1) * 512], ALU.add,
                        [t_ps, t_brep], [t_modr])
            for i, j in ((1, 0), (4, 1)):
                self.stt(modr[:, i, :], modr[:, i, :], 1.0, ng[:, j, :], ALU.add, ALU.mult, [t_modr, t_ng], [t_modr])
            self.dma("sp", self.modr_d, modr[:], [t_modr], [self.t_modrd])

    def load_mod(self, st, idxs):
        tl = self.sb(st, "modl", [128, len(idxs), D], F32)
        self.t_modr = Tok()
        self.modr = {}
        for n, i in enumerate(idxs):
            self.dma("sp", tl[:, n, :], self.modr_d[:, i, :], [self.t_modrd], [self.t_modr])
            self.modr[i] = tl[:, n, :]

    def rstd_from_ss(self, rstd, ss, n, t_in, t_out):
        self.act(rstd, ss, AF.Ln, [t_in, self.t_const], [t_out], bias=self.epsc[0:rstd.shape[0], 0:1], scale=1.0 / n)
        self.act(rstd, rstd, AF.Exp, [t_out], [t_out], scale=-0.5)

    def norm_mod(self, xt, t_x, i_g, i_sh, work, hb, t_hb, hf=None):
        junk, ss, rstd, tmp, t_w = work
        self.stt(junk[:], xt[:], 1.0, xt[:], ALU.mult, ALU.mult, [t_x], [t_w], accum=ss[:, 0:1])
        self.rstd_from_ss(rstd[:, 0:1], ss[:, 0:1], D, t_w, t_w)
        self.stt(tmp[:], xt[:], rstd[:, 0:1], self.modr[i_g], ALU.mult, ALU.mult, [t_x, t_w, self.t_modr], [t_w])
        if hf is not None:
            self.tt("dve", hf[:], tmp[:], self.modr[i_sh], ALU.add, [t_w, self.t_modr], [t_hb])
            self.cp("pool", hb[:], hf[:], [t_hb], [t_hb])
        else:
            self.tt("dve", hb[:], tmp[:], self.modr[i_sh], ALU.add, [t_w, self.t_modr], [t_hb])

    def phase_norm1(self, l, xsrc):
        with ExitStack() as st:
            sb = self.sb
            self.load_mod(st, [0, 1])
            xr = Ring([sb(st, "xt%d" % i, [128, D], F32) for i in range(2)])
            hr = Ring([sb(st, "hb%d" % i, [128, D], BF16) for i in range(2)])
            works = [(sb(st, "junk%d" % i, [128, D], F32), sb(st, "ss%d" % i, [128, 1], F32), sb(st, "rstd%d" % i, [128, 1], F32),
                      sb(st, "tmp%d" % i, [128, D], F32), Tok()) for i in range(2)]
            psb = [self.ps[i][:].bitcast(BF16).rearrange("p (a b) -> p a b", a=8) for i in range(2)]
            for t in range(NT):
                xt, t_x = xr.next()
                hb, t_hb = hr.next()
                self.dma("sp", xt[:], xsrc[t * 128:(t + 1) * 128, :], [self.t_xs], [t_x])
                self.norm_mod(xt, t_x, 1, 0, works[t % 2], hb, t_hb)
                pT, t_p = psb[t % 2], self.tp[t % 2]
                for kc in range(8):
                    self.tr(pT[:, kc, :], hb[:, kc * 128:(kc + 1) * 128], self.identb[:], [t_hb, self.t_const], [t_p])
                self.cp("act", self.hT[:, :, t * 128:(t + 1) * 128], pT, [t_p], [self.t_hT])

    def load_w(self, w, src, t_w, kc=8):
        self.dma("pool", w, src.rearrange("(kc p) n -> p kc n", p=128), [], [t_w])

    def phase_gates(self, l):
        with ExitStack() as st:
            sb = self.sb
            wg = sb(st, "wg", [128, 8, 2 * D], BF16)
            t_wg = Tok()
            for q in range(4):
                self.load_w(wg[:, :, q * 512:(q + 1) * 512], self.w_in[l][:, 3840 + q * 512:3840 + (q + 1) * 512], t_wg)
            gr = Ring([sb(st, "gsb%d" % i, [128, 512], BF16) for i in range(3)])
            n = 0
            for c in range(8):
                for m in range(16):
                    ps, t_ps = self.ps[n % 4], self.tp[n % 4]
                    n += 1
                    for kc in range(8):
                        self.mm(ps[:], wg[:, kc, m * 128:(m + 1) * 128], self.hT[:, kc, c * 512:(c + 1) * 512], kc == 0, kc == 7,
                                [t_wg, self.t_hT], [t_ps])
                    g, t_g = gr.next()
                    self.act(g[:], ps[:], AF.Sigmoid, [t_ps], [t_g])
                    self.dma("sp", self.gT_d[m * 128:(m + 1) * 128, c * 512:(c + 1) * 512], g[:], [t_g, self.t_gT], [])

    def qknorm_rope(self, src, H, grep, tile, wk, dst, t_src, t_dst):
        sq, ss, rstd, qn, t1, t2, t_w = wk
        W = H * 64
        v3 = lambda ap: ap[:, 0:W].rearrange("p (h d) -> p h d", h=H)
        v5 = lambda ap: ap[:, 0:W].rearrange("p (h a b c) -> p h a b c", h=H, a=2, b=2)
        self.tt("pool", sq[:, 0:W], src, src, ALU.mult, [t_src], [t_w])
        self.red(ss[:, 0:H], v3(sq), ALU.add, [t_w], [t_w])
        self.ts("pool", rstd[:, 0:H], ss[:, 0:H], 1.0 / 64, ALU.mult, [t_w], [t_w], s2=EPS, op1=ALU.add)
        self.tt("pool", rstd[:, 0:H], rstd[:, 0:H], self.neghalf[:, 0:H], ALU.pow, [t_w, self.t_const], [t_w])
        self.tt("dve", v3(qn), src.rearrange("p (h d) -> p h d", h=H), rstd[:, 0:H].unsqueeze(2).to_broadcast([128, H, 64]), ALU.mult,
                [t_src, t_w], [t_w])
        self.tt("pool", v3(qn), v3(qn), grep.unsqueeze(1).to_broadcast([128, H, 64]), ALU.mult, [t_w, self.t_const], [t_w])
        cosb = self.cos[:, tile, :].unsqueeze(1).to_broadcast([128, H, 64])
        sinv = self.sin[:, tile, :].rearrange("p (a b c) -> p a b c", a=2, b=2)
        self.tt("dve", v3(t1), v3(qn), cosb, ALU.mult, [t_w, self.t_const], [t_w])
        for b in range(2):
            sb_ = sinv[:, :, b, :].unsqueeze(1).to_broadcast([128, H, 2, 16])
            self.tt("pool", v5(t2)[:, :, :, b, :], v5(qn)[:, :, :, 1 - b, :], sb_, ALU.mult, [t_w, self.t_const], [t_w])
        self.tt("dve", dst, t1[:, 0:W], t2[:, 0:W], ALU.add, [t_w], [t_dst])

    def phase_gqa(self, l):
        with ExitStack() as st:
            sb = self.sb
            self.cos = sb(st, "cos", [128, NT, 64], F32)
            self.sin = sb(st, "sin", [128, NT, 64], F32)
            self.dma("sp", self.cos[:], self.c_cos, [], [self.t_const])
            self.dma("sp", self.sin[:], self.c_sin, [], [self.t_const])
            KT = sb(st, "KT", [128, 2, S], BF16)
            V = sb(st, "V", [128, NT, 4, 128], BF16)
            wkv = sb(st, "wkv", [128, 8, 512], BF16)
            gq = sb(st, "gq", [128, 64], F32)
            gk = sb(st, "gk", [128, 64], F32)
            t_KT, t_V, t_wkv = Tok(), Tok(), Tok()
            self.dma("sp", gq[:], self.q_norm_g[l:l + 1, :].partition_broadcast(128), [], [self.t_const])
            self.dma("sp", gk[:], self.k_norm_g[l:l + 1, :].partition_broadcast(128), [], [self.t_const])
            self.load_w(wkv[:], self.w_in[l][:, 3328:3840], t_wkv)
            self.memset("dve", V[:, :, :, 64:128], 1.0, [t_V])
            wks = [(sb(st, "q_sq%d" % i, [128, 256], F32), sb(st, "q_ss%d" % i, [128, 4], F32), sb(st, "q_rstd%d" % i, [128, 4], F32),
                    sb(st, "q_qn%d" % i, [128, 256], F32), sb(st, "q_t1%d" % i, [128, 256], F32), sb(st, "q_t2%d" % i, [128, 256], F32), Tok())
                   for i in range(2)]
            wkn = [0]

            def next_wk():
                wkn[0] += 1
                return wks[wkn[0] % 2]
            psT = self.ps[7][:].bitcast(BF16).rearrange("p (a b) -> p a b", a=8)
            t_psT = self.tp[7]
            ksr = Ring([sb(st, "ksb3_%d" % i, [128, 256], F32) for i in range(3)])
            krr = Ring([sb(st, "kr3_%d" % i, [128, 256], BF16) for i in range(3)])
            kst = {}

            def kv1(t):
                ps, t_ps = self.ps[6 - t % 2], self.tp[6 - t % 2]
                for kc in range(8):
                    self.mm(ps[:], self.hT[:, kc, t * 128:(t + 1) * 128], wkv[:, kc, :], kc == 0, kc == 7, [self.t_hT, t_wkv], [t_ps])
                self.cp("act", V[:, t, :, 0:64], ps[:, 256:512].rearrange("p (g d) -> p g d", g=4), [t_ps], [t_V])
                ksb, t_ksb = ksr.next()
                self.cp("act", ksb[:], ps[:, 0:256], [t_ps], [t_ksb])
                kr, t_kr = krr.next()
                self.qknorm_rope(ksb[:], 4, gk[:], t, next_wk(), kr[:], t_ksb, t_kr)
                kst[t] = (kr, t_kr)

            def kv2(t):
                kr, t_kr = kst.pop(t)
                for m in range(2):
                    self.tr(psT[:, m, :], kr[:, m * 128:(m + 1) * 128], self.identb[:], [t_kr, self.t_const], [t_psT])
                self.cp("dve", KT[:, :, t * 128:(t + 1) * 128], psT[:, 0:2, :], [t_psT], [t_KT])

            kv1(0)
            kv1(1)
            for t in range(NT):
                kv2(t)
                if t + 2 < NT:
                    kv1(t + 2)
            wq = sb(st, "wq", [128, 8, 256], BF16)
            t_wq = Tok()
            QTb = [(sb(st, "QT%d" % i, [128, 4, 512], BF16), Tok()) for i in range(2)]
            PTr = Ring([sb(st, "PT%d" % i, [128, 512], BF16) for i in range(4)])
            rdr = Ring([sb(st, "rd%d" % i, [64, 512], F32) for i in range(2)])
            obr = Ring([sb(st, "ob%d" % i, [64, 512], BF16) for i in range(2)])
            Sr = Ring([None] * 3)
            Sr.items = [(self.ps[i], self.tp[i]) for i in range(3)]
            Or = Ring([None] * 2)
            Or.items = [(self.ps[i], self.tp[i]) for i in (3, 4)]
            units = [(g, c) for g in range(4) for c in range(8)]

            qst = {}

            def qproj1(u, t4):
                g, c = units[u]
                if c == 0 and t4 == 0:
                    self.load_w(wq[:], self.w_in[l][:, 2304 + g * 256:2304 + (g + 1) * 256], t_wq)
                t = c * 4 + t4
                ps, t_ps = self.ps[6 - t4 % 2], self.tp[6 - t4 % 2]
                for kc in range(8):
                    self.mm(ps[:, 0:256], self.hT[:, kc, t * 128:(t + 1) * 128], wq[:, kc, :], kc == 0, kc == 7,
                            [self.t_hT, t_wq], [t_ps])
                qsb, t_qsb = ksr.next()
                self.cp("dve", qsb[:], ps[:, 0:256], [t_ps], [t_qsb])
                qr, t_qr = krr.next()
                self.qknorm_rope(qsb[:], 4, gq[:], t, next_wk(), qr[:], t_qsb, t_qr)
                qst[(u, t4)] = (qr, t_qr)

            def qproj2(u, t4):
                g, c = units[u]
                kb = (g % 2) * 64
                ko = 64 - kb
                QT, t_QT = QTb[u % 2]
                qr, t_qr = qst.pop((u, t4))
                for h in range(4):
                    self.tr(psT[0:64, h, :], qr[:, h * 64:(h + 1) * 64], self.identb[:], [t_qr, self.t_const], [t_psT])
                self.cp("dve", QT[kb:kb + 64, :, t4 * 128:(t4 + 1) * 128], psT[0:64, 0:4, :], [t_psT], [t_QT])
                self.memset("pool", QT[ko:ko + 64, :, t4 * 128:(t4 + 1) * 128], 0.0, [t_QT])

            def attend(u, h):
                g, c = units[u]
                QT, t_QT = QTb[u % 2]
                hq = g * 4 + h
                OT, t_OT = Or.next()
                pend = []

                def score(kt):
                    Sb, t_S = Sr.next()
                    self.mm(Sb[:], KT[:, g // 2, kt * 128:(kt + 1) * 128], QT[:, h, :], True, True, [t_KT, t_QT], [t_S])
                    PT, t_PT = PTr.next()
                    self.act(PT[:], Sb[:], AF.Exp, [t_S], [t_PT], scale=0.125)
                    pend.append((kt, PT, t_PT))

                def pv():
                    kt, PT, t_PT = pend.pop(0)
                    self.mm(OT[:], V[:, kt, g, :], PT[:], kt == 0, kt == NT - 1, [t_V, t_PT], [t_OT])

                score(0)
                score(1)
                for kt in range(NT):
                    if kt + 2 < NT:
                        score(kt + 2)
                    pv()
                def fin():
                    rd, t_rd = rdr.next()
                    self.recip(rd[0:64, :], OT[64:128, :], [t_OT], [t_rd])
                    ob, t_ob = obr.next()
                    self.tt("dve", ob[:], OT[0:64, :], rd[0:64, :], ALU.mult, [t_OT, t_rd], [t_ob])
                    self.dma("sp", self.obT_d[hq * 64:(hq + 1) * 64, c * 512:(c + 1) * 512], ob[:], [t_ob], [self.t_obT])
                return fin

            for t4 in range(4):
                qproj1(0, t4)
                qproj2(0, t4)
            for u in range(len(units)):
                nxt = u + 1 < len(units)
                f = attend(u, 0)
                f()
                if nxt:
                    qproj1(u + 1, 0)
                    qproj1(u + 1, 1)
                f = attend(u, 1)
                if nxt:
                    qproj2(u + 1, 0)
                    qproj2(u + 1, 1)
                f()
                if nxt:
                    qproj1(u + 1, 2)
                f = attend(u, 2)
                if nxt:
                    qproj2(u + 1, 2)
                f()
                if nxt:
                    qproj1(u + 1, 3)
                f = attend(u, 3)
                if nxt:
                    qproj2(u + 1, 3)
                f()

    def phase_window(self, l):
        with ExitStack() as st:
            sb = self.sb
            bias = sb(st, "wb", [128, 12, 384], F32)
            t_bias = Tok()
            self.dma("sp", bias[:], self.wbias, [], [t_bias])
            QT = sb(st, "wQT", [128, 2, S], BF16)
            KT = sb(st, "wKT", [128, 2, S], BF16)
            Vf = sb(st, "wVf", [128, NT, 256], BF16)
            wq = sb(st, "wwq", [128, 8, 768], BF16)
            t_QT, t_KT, t_Vf, t_wq = Tok(), Tok(), Tok(), Tok()
            sr = Ring([sb(st, "ws%d" % i, [128, 384], F32) for i in range(3)])
            pr = Ring([sb(st, "wp%d" % i, [128, 384], BF16) for i in range(4)])
            ptr = Ring([sb(st, "wpt%d" % i, [128, 3, 128], BF16) for i in range(2)])
            mr = Ring([sb(st, "wm%d" % i, [128, 2], F32) for i in range(6)])
            stg = Ring([sb(st, "wstg%d" % i, [128, 264], F32) for i in range(2)])
            Sr = Ring([None] * 3)
            Sr.items = [(self.ps[i], self.tp[i]) for i in (0, 1, 7)]
            Tr = Ring([None] * 2)
            Tr.items = [(self.ps[i][:].bitcast(BF16)[:, 0:384].rearrange("p (a b) -> p a b", a=3), self.tp[i]) for i in (2, 3)]
            Or = Ring([None] * 2)
            Or.items = [(self.ps[i], self.tp[i]) for i in (4, 5)]
            import os
            wstop = int(os.environ.get("WSTOP", "99"))
            for a, Dl in enumerate(A_DIL):
                L = S // Dl
                nj = L // 128
                if str(a) not in os.environ.get("WGRPS", "012"):
                    continue
                self.load_w(wq[:], self.w_in[l][:, a * 768:(a + 1) * 768], t_wq)
                n = 0
                for which, dst, t_dst in ((0, QT, t_QT), (1, KT, t_KT)):
                    for m in range(2):
                        for c in range(8):
                            pbk = (6, 7, 0, 1)[n % 4]
                            ps, t_ps = self.ps[pbk], self.tp[pbk]
                            n += 1
                            col = which * 256 + m * 128
                            for kc in range(8):
                                self.mm(ps[:], wq[:, kc, col:col + 128], self.hT[:, kc, c * 512:(c + 1) * 512], kc == 0, kc == 7,
                                        [t_wq, self.t_hT], [t_ps])
                            self.cp("act" if n % 2 else "dve", dst[:, m, c * 512:(c + 1) * 512], ps[:], [t_ps], [t_dst])
                if wstop <= 1:
                    continue
                for r in range(Dl):
                    for j in range(nj):
                        bi = r * nj + j
                        tok0 = j * 128 * Dl + r
                        pbk = (6, 7, 0, 1)[bi % 4]
                        ps, t_ps = self.ps[pbk], self.tp[pbk]
                        for kc in range(8):
                            self.mm(ps[:, 0:256], self.hT[:, kc, sl(tok0, 128, Dl)], wq[:, kc, 512:768], kc == 0, kc == 7,
                                    [self.t_hT, t_wq], [t_ps])
                        self.cp("act" if bi % 2 else "dve", Vf[:, bi, :], ps[:, 0:256], [t_ps], [t_Vf])
                if wstop <= 2:
                    continue
                units = []
                for r in range(Dl):
                    for j in range(nj):
                        for h in range(4):
                            units.append((r, j, h))
                ust = {}
                blk = {}

                def W1(n):
                    r, j, h = units[n]
                    tok0 = j * 128 * Dl + r
                    jt0 = max(j - 1, 0)
                    jt1 = min(j + 1, nj - 1)
                    ntl = jt1 - jt0 + 1
                    c0 = (jt0 - (j - 1)) * 128
                    w = ntl * 128
                    k0 = jt0 * 128 * Dl + r
                    if h == 0:
                        blk[(r, j)] = (Or.next(), stg.next())
                    (O4, t_O4), (sg, t_sg) = blk[(r, j)]
                    Sb, t_S = Sr.next()
                    hb_ = (h % 2) * 64
                    self.mm(Sb[:, 0:w], QT[hb_:hb_ + 64, h // 2, sl(tok0, 128, Dl)], KT[hb_:hb_ + 64, h // 2, sl(k0, w, Dl)],
                            True, True, [t_QT, t_KT], [t_S])
                    s_, t_s = sr.next()
                    self.stt(s_[:, 0:w], Sb[:, 0:w], 0.125, bias[:, a * 4 + h, c0:c0 + w], ALU.mult, ALU.add, [t_S, t_bias], [t_s])
                    m, t_m = mr.next()
                    self.P.op("dve", lambda e, m=m, s_=s_, w=w: e.reduce_max(out=m[:, 0:1], in_=s_[:, 0:w], axis=AX.X), [t_s], [t_m])
                    self.ts("dve", m[:, 1:2], m[:, 0:1], -1.0, ALU.mult, [t_m], [t_m])
                    self.cp("dve", sg[:, 256 + h:257 + h], m[:, 0:1], [t_m], [t_sg])
                    ust[n] = (s_, t_s, m, t_m, sg, t_sg, w, h, ntl, jt0)

                def W1b(n):
                    s_, t_s, m, t_m, sg, t_sg, w, h, ntl, jt0 = ust[n]
                    p, t_p = pr.next()
                    self.act(p[:, 0:w], s_[:, 0:w], AF.Exp, [t_s, t_m], [t_p, t_sg], bias=m[:, 1:2], scale=1.0,
                             accum=sg[:, 260 + h:261 + h])
                    ust[n] = (p, t_p, ntl, jt0)

                def W2(n):
                    r, j, h = units[n]
                    tok0 = j * 128 * Dl + r
                    p, t_p, ntl, jt0 = ust.pop(n)
                    (O4, t_O4), (sg, t_sg) = blk[(r, j)]
                    pT, t_pT = Tr.next()
                    for ti in range(ntl):
                        self.tr(pT[:, ti, :], p[:, ti * 128:(ti + 1) * 128], self.identb[:], [t_p, self.t_const], [t_pT])
                    pts, t_pts = ptr.next()
                    self.cp("act", pts[:, 0:ntl, :], pT[:, 0:ntl, :], [t_pT], [t_pts])
                    for ti in range(ntl):
                        self.mm(O4[:, h * 64:(h + 1) * 64], pts[:, ti, :], Vf[:, r * nj + jt0 + ti, h * 64:(h + 1) * 64],
                                ti == 0, ti == ntl - 1, [t_pts, t_Vf], [t_O4])
                    if h == 3:
                        self.cp("act", sg[:, 0:256], O4[:, 0:256], [t_O4], [t_sg])
                        self.dma("sp", self.oaw_d[a, sl(tok0, 128, Dl), :], sg[:], [t_sg], [self.t_oaw])
                        del blk[(r, j)]

                NU = len(units)
                W1(0)
                W1(1)
                W1b(0)
                for n in range(NU):
                    if n + 2 < NU:
                        W1(n + 2)
                    W2(n)
                    if n + 1 < NU:
                        W1b(n + 1)
            if wstop <= 4:
                return
            self.P.barrier()
            cr = Ring([sb(st, "wc%d" % i, [128, 3, 264], F32) for i in range(2)])
            ms = sb(st, "wms", [128, 4], F32)
            wgt = sb(st, "wwgt", [128, 3, 4], F32)
            dt_ = sb(st, "wdt", [128, 4], F32)
            acc = sb(st, "wacc", [128, 256], F32)
            tmp = sb(st, "wtmp", [128, 256], F32)
            obr = Ring([sb(st, "wob%d" % i, [128, 256], BF16) for i in range(2)])
            t_k = Tok()
            for t in range(NT):
                ct, t_ct = cr.next()
                self.dma("sp", ct[:], self.oaw_d[:, t * 128:(t + 1) * 128, :].rearrange("a p c -> p a c"), [self.t_oaw], [t_ct])
                mv = ct[:, :, 256:260]
                dv = ct[:, :, 260:264]
                self.tt("dve", ms[:], mv[:, 0, :], mv[:, 1, :], ALU.max, [t_ct], [t_k])
                self.tt("dve", ms[:], ms[:], mv[:, 2, :], ALU.max, [t_ct, t_k], [t_k])
                self.tt("dve", wgt[:], mv, ms[:].unsqueeze(1).to_broadcast([128, 3, 4]), ALU.subtract, [t_ct, t_k], [t_k])
                self.act(wgt[:], wgt[:], AF.Exp, [t_k], [t_k])
                self.tt("dve", dv, dv, wgt[:], ALU.mult, [t_ct, t_k], [t_ct])
                self.tt("dve", dt_[:], dv[:, 0, :], dv[:, 1, :], ALU.add, [t_ct], [t_k])
                self.tt("dve", dt_[:], dt_[:], dv[:, 2, :], ALU.add, [t_ct, t_k], [t_k])
                self.recip(dt_[:], dt_[:], [t_k], [t_k])
                self.tt("dve", wgt[:], wgt[:], dt_[:].unsqueeze(1).to_broadcast([128, 3, 4]), ALU.mult, [t_k], [t_k])
                ob, t_ob = obr.next()
                v3 = lambda ap: ap.rearrange("p (h d) -> p h d", h=4)
                for a in range(3):
                    cb = wgt[:, a, :].unsqueeze(2).to_broadcast([128, 4, 64])
                    if a == 0:
                        self.tt("dve", v3(acc[:]), v3(ct[:, 0, 0:256]), cb, ALU.mult, [t_ct, t_k], [t_k])
                    else:
                        self.tt("dve", v3(tmp[:]), v3(ct[:, a, 0:256]), cb, ALU.mult, [t_ct, t_k], [t_k])
                        if a == 1:
                            self.tt("dve", acc[:], acc[:], tmp[:], ALU.add, [t_k], [t_k])
                        else:
                            self.tt("dve", ob[:], acc[:], tmp[:], ALU.add, [t_k], [t_ob])
                pT, t_pT = Tr.next()
                for kc in range(2):
                    self.tr(pT[:, kc, :], ob[:, kc * 128:(kc + 1) * 128], self.identb[:], [t_ob, self.t_const], [t_pT])
                self.cp("act", self.oaT[:, :, t * 128:(t + 1) * 128], pT[:, 0:2, :], [t_pT], [self.t_oaT])

    def phase_merge(self, l, xsrc):
        with ExitStack() as st:
            sb = self.sb
            self.load_mod(st, [2, 3, 4])
            wa = sb(st, "m_wa", [128, 2, D], BF16)
            wb = sb(st, "m_wb", [128, 8, D], BF16)
            wo = sb(st, "m_wo", [128, 8, D], BF16)
            wr = sb(st, "m_wr", [128, 8, NE], F32)
            brr = sb(st, "m_brr", [128, NE], F32)
            t_w = Tok()
            self.load_w(wa[:], self.w_br_a[l], t_w)
            self.load_w(wb[:], self.w_br_b[l], t_w)
            self.load_w(wo[:], self.w_out[l], t_w)
            self.dma("sp", wr[:], self.w_router[l].rearrange("(kc p) n -> p kc n", p=128), [], [t_w])
            self.dma("sp", brr[:], self.b_router[l:l + 1, :].partition_broadcast(128), [], [t_w])
            gtr = Ring([sb(st, "m_gt%d" % i, [128, 16, 512], BF16) for i in range(2)])
            obr = Ring([sb(st, "m_ob%d" % i, [128, 8, 512], BF16) for i in range(2)])
            mgr = Ring([sb(st, "m_mg%d" % i, [128, 8, 512], BF16) for i in range(2)])
            tA = sb(st, "m_tA", [128, 512], F32)
            tB = sb(st, "m_tB", [128, 512], F32)
            t_tA, t_tB = Tok(), Tok()
            xr = Ring([sb(st, "m_x%d" % i, [128, D], F32) for i in range(2)])
            x1r = Ring([sb(st, "m_x1%d" % i, [128, D], F32) for i in range(2)])
            h2fr = Ring([sb(st, "m_h2f%d" % i, [128, D], F32) for i in range(2)])
            h2br = Ring([sb(st, "m_h2b%d" % i, [128, D], BF16) for i in range(2)])
            h2Tr = Ring([sb(st, "m_h2T%d" % i, [128, 8, 128], F32) for i in range(1)])
            work = (sb(st, "m_junk", [128, D], F32), sb(st, "m_ss", [128, 1], F32), sb(st, "m_rstd", [128, 1], F32),
                    sb(st, "m_tmp", [128, D], F32), Tok())
            e4 = sb(st, "m_e4", [128, 4], F32)
            ssum = sb(st, "m_ssum", [128, 2], F32)
            t_e4 = Tok()
            Ar = Ring([None] * 2)
            Ar.items = [(self.ps[i], self.tp[i]) for i in (0, 1)]
            Br = Ring([None] * 2)
            Br.items = [(self.ps[i], self.tp[i]) for i in (2, 3)]
            Or = Ring([None] * 2)
            Or.items = [(self.ps[i], self.tp[i]) for i in (4, 5)]
            psT2 = [self.ps[i][:].rearrange("p (a b) -> p a b", a=4) for i in (6, 7)]
            def router(t, h2f, t_h2f):
                h2T, t_h2T = h2Tr.next()
                for hh in range(2):
                    for k4 in range(4):
                        kc = hh * 4 + k4
                        self.tr(psT2[hh][:, k4, :], h2f[:, kc * 128:(kc + 1) * 128], self.identf[:], [t_h2f, self.t_const], [self.tp[6 + hh]])
                    self.cp("act", h2T[:, hh * 4:(hh + 1) * 4, :], psT2[hh], [self.tp[6 + hh]], [t_h2T])
                pL, t_pL = Or.next()
                for kc in range(8):
                    self.mm(pL[:, 0:NE], h2T[:, kc, :], wr[:, kc, :], kc == 0, kc == 7, [t_h2T, t_w], [t_pL])
                lg = self.logits_all[:, t, :]
                m8 = self.max8_all[:, t, :]
                self.tt("dve", lg, pL[:, 0:NE], brr[:], ALU.add, [t_pL, t_w], [self.t_route])
                self.P.op("dve", lambda e, m8=m8, lg=lg: e.max(out=m8, in_=lg), [self.t_route], [self.t_route])
                self.ts("dve", self.M_all[:, t, :], lg, m8[:, 3:4], ALU.is_ge, [self.t_route], [self.t_route])
                self.ts("dve", ssum[:, 1:2], m8[:, 0:1], -1.0, ALU.mult, [self.t_route], [t_e4])
                self.act(e4[:], m8[:, 0:4], AF.Exp, [self.t_route, t_e4], [t_e4], bias=ssum[:, 1:2], scale=1.0, accum=ssum[:, 0:1])
                self.recip(ssum[:, 0:1], ssum[:, 0:1], [t_e4], [t_e4])
                self.ts("dve", self.g4_all[:, t, :], e4[:], ssum[:, 0:1], ALU.mult, [t_e4], [self.t_route])

            pend_router = [None]
            for c in range(8):
                gt, t_gt = gtr.next()
                ob, t_ob = obr.next()
                mg, t_mg = mgr.next()
                self.dma("sp", gt[:], self.gT_d[:, c * 512:(c + 1) * 512].rearrange("(m p) n -> p m n", p=128), [self.t_gT], [t_gt])
                self.dma("sp", ob[:], self.obT_d[:, c * 512:(c + 1) * 512].rearrange("(m p) n -> p m n", p=128), [self.t_obT], [t_ob])
                for m in range(8):
                    pA, t_pA = Ar.next()
                    pB, t_pB = Br.next()
                    for kc in range(2):
                        self.mm(pA[:], wa[:, kc, m * 128:(m + 1) * 128], self.oaT[:, kc, c * 512:(c + 1) * 512], kc == 0, kc == 1,
                                [t_w, self.t_oaT], [t_pA])
                    for kc in range(8):
                        self.mm(pB[:], wb[:, kc, m * 128:(m + 1) * 128], ob[:, kc, :], kc == 0, kc == 7, [t_w, t_ob], [t_pB])
                    self.tt("dve", tA[:], pA[:], gt[:, m, :], ALU.mult, [t_pA, t_gt], [t_tA])
                    self.tt("dve", tB[:], pB[:], gt[:, 8 + m, :], ALU.mult, [t_pB, t_gt], [t_tB])
                    self.tt("pool", mg[:, m, :], tA[:], tB[:], ALU.add, [t_tA, t_tB], [t_mg])
                for t4 in range(4):
                    t = c * 4 + t4
                    xt, t_x = xr.next()
                    x1, t_x1 = x1r.next()
                    self.dma("sp", xt[:], xsrc[t * 128:(t + 1) * 128, :], [], [t_x])
                    for hf in range(2):
                        pO, t_pO = Or.next()
                        for kc in range(8):
                            self.mm(pO[:], mg[:, kc, t4 * 128:(t4 + 1) * 128], wo[:, kc, hf * 512:(hf + 1) * 512], kc == 0, kc == 7,
                                    [t_mg, t_w], [t_pO])
                        self.tt("dve", x1[:, hf * 512:(hf + 1) * 512], pO[:], self.modr[2][:, hf * 512:(hf + 1) * 512], ALU.mult,
                                [t_pO, self.t_modr], [t_x1])
                    self.tt("pool", x1[:], x1[:], xt[:], ALU.add, [t_x1, t_x], [t_x1])
                    self.dma("sp", self.xs_d[t * 128:(t + 1) * 128, :], x1[:], [t_x1, self.t_xs], [])
                    h2f, t_h2f = h2fr.next()
                    h2b, t_h2b = h2br.next()
                    self.norm_mod(x1, t_x1, 4, 3, work, h2b, t_h2f, hf=h2f)
                    self.dma("sp", self.h2_d[t * 128:(t + 1) * 128, :], h2b[:], [t_h2f, self.t_h2d], [])
                    if pend_router[0] is not None:
                        pend_router[0]()
                    pend_router[0] = (lambda t=t, h2f=h2f, t_h2f=t_h2f: router(t, h2f, t_h2f))
            pend_router[0]()

    def phase_slots(self, l):
        sb = self.sb
        with ExitStack() as s2:
            tris = sb(s2, "s_tris", [128, 128], BF16)
            tri32s = sb(s2, "s_t32s", [32, 32], BF16)
            tri32i = sb(s2, "s_t32i", [32, 32], BF16)
            iota160 = sb(s2, "s_iota", [32, NBLK], F32)
            iotap = sb(s2, "s_iotap", [128, 1], F32)
            iotap32 = sb(s2, "s_iotap32", [128, 1], F32)
            t_c = Tok()
            for dst, src in ((tris, self.c_tris), (tri32s, self.c_tri32s), (tri32i, self.c_tri32i), (iota160, self.c_iota160),
                             (iotap, self.c_iotap), (iotap32, self.c_iotap32)):
                self.dma("sp", dst[:], src, [], [t_c])
            cnt = sb(s2, "s_cnt", [32, 1], F32)
            cnti = sb(s2, "s_cnti", [32, 1], I32)
            nblk = sb(s2, "s_nblk", [32, 1], F32)
            nblkb = sb(s2, "s_nblkb", [32, 128], BF16)
            nblkc = sb(s2, "s_nblkc", [32, 1], BF16)
            bstart = sb(s2, "s_bstart", [128, NE], F32)
            bend = sb(s2, "s_bend", [32, 1], F32)
            cmp = sb(s2, "s_cmp", [32, NBLK], BF16)
            erep = sb(s2, "s_erep", [128, NBLK], F32)
            chg = sb(s2, "s_chg", [128, NBLK], F32)
            wf = sb(s2, "s_wf", [128, NBLK], F32)
            pos = sb(s2, "s_pos", [128, NE], F32)
            junk = sb(s2, "s_junk", [128, NE], F32)
            p4f = sb(s2, "s_p4f", [128, NT, 4], F32)
            t_s = Tok()
            pc, t_pc = self.ps[0], self.tp[0]
            for t in range(NT):
                self.mm(pc[0:32, 0:1], self.M_all[:, t, :], self.onesb[:, 0:1], t == 0, t == NT - 1, [self.t_route, self.t_const], [t_pc])
            self.ts("dve", cnt[:], pc[0:32, 0:1], 127.0, ALU.add, [t_pc], [t_s], s2=1.0 / 128.0, op1=ALU.mult)
            self.ts("dve", nblk[:], cnt[:], -0.49609375, ALU.add, [t_s], [t_s])
            self.cp("dve", cnti[:], nblk[:], [t_s], [t_s])
            self.cp("dve", nblk[:], cnti[:], [t_s], [t_s])
            self.tt("dve", cnt[:], cnt[:], nblk[:], ALU.subtract, [t_s], [t_s])
            self.ts("dve", cnt[:], cnt[:], 1.0, ALU.is_ge, [t_s], [t_s])
            self.tt("dve", nblk[:], nblk[:], cnt[:], ALU.add, [t_s], [t_s])
            self.ts("dve", nblk[:], nblk[:], 1.0, ALU.max, [t_s], [t_s])
            self.cp("dve", nblkb[:], nblk[:, 0:1].to_broadcast([32, 128]), [t_s], [t_s])
            self.cp("dve", nblkc[:], nblk[:], [t_s], [t_s])
            p1, t_p1 = self.ps[1], self.tp[1]
            self.mm(p1[:, 0:NE], nblkb[:], tri32s[:], True, True, [t_s, t_c], [t_p1])
            self.cp("dve", bstart[:], p1[:, 0:NE], [t_p1], [t_s])
            p2, t_p2 = self.ps[2], self.tp[2]
            self.mm(p2[0:32, 0:1], tri32i[:], nblkc[:], True, True, [t_s, t_c], [t_p2])
            self.cp("dve", bend[:], p2[0:32, 0:1], [t_p2], [t_s])
            self.ts("dve", cmp[:], iota160[:], bend[:, 0:1], ALU.is_ge, [t_s, t_c], [t_s])
            p3, t_p3 = self.ps[3], self.tp[3]
            self.mm(p3[:, 0:NBLK], self.onesb[0:32, :], cmp[:], True, True, [t_s, self.t_const], [t_p3])
            self.ts("dve", erep[:], p3[:, 0:NBLK], float(NE - 1), ALU.min, [t_p3], [t_s])
            self.memset("dve", chg[:, 0:1], 1.0, [t_s])
            self.tt("dve", chg[:, 1:NBLK], erep[:, 1:NBLK], erep[:, 0:NBLK - 1], ALU.not_equal, [t_s], [t_s])
            self.ts("dve", wf[:], erep[:], 128.0, ALU.mult, [t_s, t_c], [t_s], s2=iotap[:, 0:1], op1=ALU.add)
            self.ts("dve", chg[:], chg[:], -BIGIDX, ALU.mult, [t_s], [t_s], s2=BIGIDX, op1=ALU.add)
            self.tt("dve", wf[:], wf[:], chg[:], ALU.add, [t_s], [t_s])
            if l > 0:
                self.ts("dve", wf[:], wf[:], float(l * NE * 128), ALU.add, [t_s], [t_s])
            self.cp("dve", self.widx[:], wf[:], [t_s], [self.t_widx])
            self.ts("dve", wf[:], wf[:], 2.0, ALU.mult, [t_s], [t_s])
            self.cp("dve", self.widxA[:], wf[:], [t_s], [self.t_widx])
            self.ts("dve", wf[:], wf[:], 1.0, ALU.add, [t_s], [t_s])
            self.cp("dve", self.widxB[:], wf[:], [t_s], [self.t_widx])
            self.ts("dve", self.OHall[:], erep[0:64, :], iotap32[0:64, 0:1], ALU.is_equal, [t_s, t_c], [self.t_widx])
            for t in range(NT):
                pr_, t_pr = self.ps[4 + t % 2], self.tp[4 + t % 2]
                self.mm(pr_[:, 0:NE], tris[:], self.M_all[:, t, :], True, t == 0, [t_c, self.t_route], [t_pr])
                for t2 in range(t):
                    self.mm(pr_[:, 0:NE], self.onesb[:], self.M_all[:, t2, :], False, t2 == t - 1, [self.t_const, self.t_route], [t_pr])
                self.stt(pos[:], bstart[:], 128.0, pr_[:, 0:NE], ALU.mult, ALU.add, [t_s, t_pr], [t_s])
                for k in range(4):
                    self.stt(junk[:], self.logits_all[:, t, :], self.max8_all[:, t, k:k + 1], pos[:], ALU.is_equal, ALU.mult,
                             [self.t_route, t_s], [t_s], accum=p4f[:, t, k:k + 1])
            self.cp("dve", self.pos4_all[:], p4f[:], [t_s], [self.t_pos4])
            self.P.barrier()
        with ExitStack() as s3:
            hr = Ring([sb(s3, "s_h2%d" % i, [128, D], BF16) for i in range(3)])
            for t in range(NT):
                hb, t_hb = hr.next()
                self.dma("sp", hb[:], self.h2_d[t * 128:(t + 1) * 128, :], [self.t_h2d], [t_hb])
                for k in range(4):
                    self.scatter(self.Xs_d, hb[:], self.pos4_all[:, t, k:k + 1], [t_hb, self.t_pos4, self.t_Xs], [], NBLK * 128 - 1)
            self.P.barrier()

    def phase_experts(self, l):
        with ExitStack() as st:
            sb = self.sb
            wguA = sb(st, "e_wguA", [128, 4 * 2 * D], BF16)
            wguB = sb(st, "e_wguB", [128, 4 * 2 * D], BF16)
            t_wgA, t_wgB = Tok(), Tok()
            wdn = sb(st, "e_wdn", [128, 8 * D], BF16)
            bgu = sb(st, "e_bgu", [64, 2 * D], BF16)
            bdn = sb(st, "e_bdn", [64, D], BF16)
            bf = sb(st, "e_bf", [64, 3 * D], F32)
            bt = sb(st, "e_bt", [64, 3 * D], F32)
            t_wgu, t_wdn, t_b = Tok(), Tok(), Tok()
            for half in range(2):
                self.dma("sp", bf[half * 32:(half + 1) * 32, 0:2 * D], self.b_gu[l], [], [t_b])
                self.dma("sp", bf[half * 32:(half + 1) * 32, 2 * D:3 * D], self.b_dn[l], [], [t_b])
            self.cp("dve", bgu[0:32, :], bf[0:32, 0:2 * D], [t_b], [t_b])
            self.cp("dve", bdn[0:32, :], bf[0:32, 2 * D:3 * D], [t_b], [t_b])
            self.cp("dve", bgu[32:64, :], bf[32:64, 0:2 * D], [t_b], [t_b])
            self.cp("dve", bdn[32:64, :], bf[32:64, 2 * D:3 * D], [t_b], [t_b])
            self.cp("dve", bt[32:64, 0:2 * D], bgu[32:64, :], [t_b], [t_b])
            self.cp("dve", bt[32:64, 2 * D:3 * D], bdn[32:64, :], [t_b], [t_b])
            self.tt("dve", bt[32:64, :], bf[32:64, :], bt[32:64, :], ALU.subtract, [t_b], [t_b])
            self.cp("dve", bgu[32:64, :], bt[32:64, 0:2 * D], [t_b], [t_b])
            self.cp("dve", bdn[32:64, :], bt[32:64, 2 * D:3 * D], [t_b], [t_b])
            wgu_v = self.w_gu.rearrange("l e (p kh kl) n -> (l e p kh) (kl n)", kh=2, kl=4)
            wdn_v = self.w_dn.rearrange("l e (p kc) n -> (l e p) (kc n)", kc=8)
            R = 3
            xb_ = [(sb(st, "e_x%d" % i, [128, D], BF16), Tok()) for i in range(R)]
            xT_ = [(sb(st, "e_xT%d" % i, [128, 8, 128], BF16), Tok()) for i in range(R)]
            oh_ = [(sb(st, "e_oh%d" % i, [64, 128], BF16), Tok()) for i in range(R)]
            gc_ = [(sb(st, "e_gc%d" % i, [128, 512], F32), Tok()) for i in range(2)]
            sg_ = [(sb(st, "e_sg%d" % i, [128, 512], F32), Tok()) for i in range(2)]
            lc_ = [(sb(st, "e_lc%d" % i, [128, 512], F32), Tok()) for i in range(2)]
            ac_ = [(sb(st, "e_ac%d" % i, [128, D], BF16), Tok()) for i in range(2)]
            aT_ = [(sb(st, "e_aT%d" % i, [128, 8, 128], BF16), Tok()) for i in range(2)]
            out_ = [(sb(st, "e_out%d" % i, [128, D], F32), Tok()) for i in range(2)]
            pX = self.ps[6][:].bitcast(BF16).rearrange("p (a b) -> p a b", a=8)
            pA = self.ps[7][:].bitcast(BF16).rearrange("p (a b) -> p a b", a=8)
            N = NBLK
            bound = 2 * NE * 128 - 1

            def WguA(j):
                self.gather(wguA[:], wgu_v, self.widxA[:, j:j + 1], [self.t_widx], [t_wgA], 2 * bound + 1)

            def WguB(j):
                self.gather(wguB[:], wgu_v, self.widxB[:, j:j + 1], [self.t_widx], [t_wgB], 2 * bound + 1)

            def Wdn(j):
                self.gather(wdn[:], wdn_v, self.widx[:, j:j + 1], [self.t_widx], [t_wdn], bound)

            def Ax(j):
                xb, t_xb = xb_[j % R]
                self.dma("sp", xb[:], self.Xs_d[j * 128:(j + 1) * 128, :], [self.t_Xs], [t_xb])
                oh, t_oh = oh_[j % R]
                self.cp("dve", oh[:], self.OHall[:, j:j + 1].to_broadcast([64, 128]), [self.t_widx], [t_oh])
                xv = xb[:].rearrange("p (a kc) -> p kc a", kc=8)
                for kc in range(8):
                    self.tr(pX[:, kc, :], xv[:, kc, :], self.identb[:], [t_xb, self.t_const], [self.tp[6]])
                xT, t_xT = xT_[j % R]
                self.cp("act", xT[:], pX, [self.tp[6]], [t_xT])

            def Bk(j, half):
                xT, t_xT = xT_[j % R]
                oh, t_oh = oh_[j % R]
                wb, t_wb = (wguA, t_wgA) if half == 0 else (wguB, t_wgB)
                for k4 in range(4):
                    kc = half * 4 + k4
                    for nb in range(4):
                        self.mm(self.ps[nb][:], xT[:, kc, :], wb[:, k4 * 2048 + nb * 512:k4 * 2048 + (nb + 1) * 512], kc == 0, False,
                                [t_xT, t_wb], [self.tp[nb]])
                if half == 1:
                    for nb in range(4):
                        self.mm(self.ps[nb][:], oh[:], bgu[:, nb * 512:(nb + 1) * 512], False, True, [t_oh, t_b], [self.tp[nb]])

            def E(j, hf):
                gc, t_gc = gc_[hf]
                sg, t_sg = sg_[hf]
                lc, t_lc = lc_[hf]
                ac, t_ac = ac_[j % 2]
                self.ts("dve", gc[:], self.ps[hf][:], 7.0, ALU.min, [self.tp[hf]], [t_gc])
                self.act(sg[:], gc[:], AF.Sigmoid, [t_gc], [t_sg], scale=1.702)
                self.ts("dve", lc[:], self.ps[2 + hf][:], 7.0, ALU.min, [self.tp[2 + hf]], [t_lc], s2=-7.0, op1=ALU.max)
                self.stt(lc[:], lc[:], 1.0, gc[:], ALU.add, ALU.mult, [t_lc, t_gc], [t_lc])
                self.tt("dve", ac[:, hf * 512:(hf + 1) * 512], lc[:], sg[:], ALU.mult, [t_lc, t_sg], [t_ac])

            def Ca(j):
                ac, t_ac = ac_[j % 2]
                av = ac[:].rearrange("p (a kc) -> p kc a", kc=8)
                for kc in range(8):
                    self.tr(pA[:, kc, :], av[:, kc, :], self.identb[:], [t_ac, self.t_const], [self.tp[7]])
                aT, t_aT = aT_[j % 2]
                self.cp("act", aT[:], pA, [self.tp[7]], [t_aT])

            def Cb(j):
                oh, t_oh = oh_[j % R]
                aT, t_aT = aT_[j % 2]
                ot, t_ot = out_[j % 2]
                for hf in range(2):
                    ps, t_ps = self.ps[4 + hf], self.tp[4 + hf]
                    for kc in range(8):
                        self.mm(ps[:], aT[:, kc, :], wdn[:, kc * 1024 + hf * 512:kc * 1024 + (hf + 1) * 512], kc == 0, False,
                                [t_aT, t_wdn], [t_ps])
                    self.mm(ps[:], oh[:], bdn[:, hf * 512:(hf + 1) * 512], False, True, [t_oh, t_b], [t_ps])
                    self.cp("act" if hf else "dve", ot[:, hf * 512:(hf + 1) * 512], ps[:], [t_ps], [t_ot])
                self.dma("sp", self.Out_d[j * 128:(j + 1) * 128, :], ot[:], [t_ot, self.t_Outd], [])

            WguA(0)
            WguB(0)
            Wdn(0)
            Ax(0)
            Ax(1)
            Bk(0, 0)
            WguA(1)
            Bk(0, 1)
            WguB(1)
            E(0, 0)
            E(0, 1)
            for j in range(N):
                if j + 1 < N:
                    Bk(j + 1, 0)
                if j + 2 < N:
                    WguA(j + 2)
                if j + 1 < N:
                    Bk(j + 1, 1)
                if j + 2 < N:
                    WguB(j + 2)
                Ca(j)
                if j + 2 < N:
                    Ax(j + 2)
                if j + 1 < N:
                    E(j + 1, 0)
                Cb(j)
                if j + 1 < N:
                    Wdn(j + 1)
                    E(j + 1, 1)

    def phase_combine(self, l, last):
        with ExitStack() as st:
            sb = self.sb
            self.load_mod(st, [5])
            gr = Ring([sb(st, "c_g%d" % i, [128, D], F32) for i in range(12)])
            xr = Ring([sb(st, "c_x%d" % i, [128, D], F32) for i in range(3)])
            yr = Ring([sb(st, "c_y%d" % i, [128, D], F32) for i in range(3)])
            fg = sb(st, "c_fg", [128, D], F32)
            junk = sb(st, "c_junk", [128, D], F32)
            ss = sb(st, "c_ss", [128, 2], F32)
            t_fg, t_w = Tok(), Tok()
            if last:
                self.dma("sp", fg[:], self.fng.rearrange("(o d) -> o d", o=1).partition_broadcast(128), [], [t_fg])
            for t in range(NT):
                xt, t_x = xr.next()
                y, t_y = yr.next()
                self.dma("sp", xt[:], self.xs_d[t * 128:(t + 1) * 128, :], [], [t_x])
                for k in range(4):
                    g, t_g = gr.next()
                    self.gather(g[:], self.Out_d, self.pos4_all[:, t, k:k + 1], [self.t_Outd, self.t_pos4], [t_g], None)
                    if k == 0:
                        self.ts("dve", y[:], g[:], self.g4_all[:, t, 0:1], ALU.mult, [t_g, self.t_route], [t_y])
                    else:
                        self.stt(y[:], g[:], self.g4_all[:, t, k:k + 1], y[:], ALU.mult, ALU.add, [t_g, self.t_route, t_y], [t_y])
                self.tt("dve", y[:], y[:], self.modr[5], ALU.mult, [t_y, self.t_modr], [t_y])
                self.tt("dve", y[:], y[:], xt[:], ALU.add, [t_y, t_x], [t_y])
                if not last:
                    self.dma("sp", self.xs_d[t * 128:(t + 1) * 128, :], y[:], [t_y, self.t_xs], [])
                else:
                    self.stt(junk[:], y[:], 1.0, y[:], ALU.mult, ALU.mult, [t_y], [t_w], accum=ss[:, 0:1])
                    self.rstd_from_ss(ss[:, 1:2], ss[:, 0:1], D, t_w, t_w)
                    self.stt(y[:], y[:], ss[:, 1:2], fg[:], ALU.mult, ALU.mult, [t_y, t_w, t_fg], [t_y])
                    self.dma("sp", self.out[t * 128:(t + 1) * 128, :], y[:], [t_y], [])


def _t5_bucket(rel):
    nb = 16
    max_exact = 8
    ret = np.where(rel > 0, nb, 0)
    n = np.abs(rel)
    nf = np.maximum(n, 1).astype(np.float32)
    large = max_exact + (np.log(nf / max_exact) / math.log(1024 / max_exact) * (nb - max_exact)).astype(np.int32)
    large = np.minimum(large, nb - 1)
    return ret + np.where(n < max_exact, n, large)


def host_consts(rel_bias):
    bf = ml_dtypes.bfloat16
    c = {}
    c["identb"] = np.eye(128, dtype=np.float32).astype(bf)
    c["identf"] = np.eye(128, dtype=np.float32)
    tok = np.arange(S)
    row = (tok // 64).astype(np.float32)
    col = (tok % 64).astype(np.float32)
    inv = (10000.0 ** (-np.arange(0, 32, 2, dtype=np.float32) / 32)).astype(np.float32)
    ar = row[:, None] * inv
    ac = col[:, None] * inv
    cosT = np.concatenate([np.cos(ar), np.cos(ar), np.cos(ac), np.cos(ac)], 1).astype(np.float32)
    sinT = np.concatenate([-np.sin(ar), np.sin(ar), -np.sin(ac), np.sin(ac)], 1).astype(np.float32)
    c["cosT"] = np.ascontiguousarray(cosT.reshape(NT, 128, 64).transpose(1, 0, 2))
    c["sinT"] = np.ascontiguousarray(sinT.reshape(NT, 128, 64).transpose(1, 0, 2))
    k = np.arange(128)
    c["tri_s"] = (k[:, None] < k[None, :]).astype(np.float32).astype(bf)
    k32 = np.arange(32)
    c["tri32s"] = (k32[:, None] < k32[None, :]).astype(np.float32).astype(bf)
    c["tri32i"] = (k32[:, None] <= k32[None, :]).astype(np.float32).astype(bf)
    c["iota160"] = np.broadcast_to(np.arange(NBLK, dtype=np.float32), (32, NBLK)).copy()
    c["iotap"] = np.arange(128, dtype=np.float32).reshape(128, 1)
    c["iotap32"] = (np.arange(128) % 32).astype(np.float32).reshape(128, 1)
    q = np.arange(128)[:, None]
    cc = np.arange(384)[None, :]
    rel = cc - 128 - q
    wb = np.full((128, 12, 384), -30000.0, np.float32)
    valid = np.abs(rel) <= 64
    for a, dl in enumerate(A_DIL):
        bkt = _t5_bucket(rel * dl)
        for h in range(4):
            vals = rel_bias[bkt, a * 4 + h]
            wb[:, a * 4 + h, :] = np.where(valid, vals, np.float32(-30000.0))
    c["wbias"] = wb
    return c


_CACHE = {}


def kernel(**inputs):
    inp = {k: np.ascontiguousarray(np.asarray(v, dtype=np.float32)) for k, v in inputs.items()}
    if "nc" not in _CACHE:
        _CACHE["nc"] = Builder(nlayers=2).build()
    nc = _CACHE["nc"]
    consts = host_consts(inp["rel_bias"])
    shared = {k: inp[k] for k in ("w_ada", "b_ada", "norm1_g", "w_in", "q_norm_g", "k_norm_g", "w_br_a", "w_br_b", "w_out",
                                   "norm2_g", "w_router", "b_router", "w_gate_up", "b_gate_up", "w_down", "b_down", "final_norm_g")}
    in_maps = []
    for b in range(8):
        m = dict(shared)
        m.update(consts)
        m["x"] = inp["x"][b]
        m["ccol"] = np.ascontiguousarray(inp["c"][b].reshape(8, 128).T)
        in_maps.append(m)
    res = run_bass_kernel_spmd(nc, in_maps, core_ids=list(range(8)))
    return np.stack([np.asarray(res.results[b]["out"], dtype=np.float32) for b in range(8)], 0)
```
